# Optimizing a Trainium2 kernel written in Bass

```python
import math
import jax, jax.numpy as jnp
from jax import lax
import numpy as np

D_MODEL = 1024
BATCH = 8
SEQ = 4096
DEPTH = 1

N_HEADS_A = 8
HEAD_DIM_A = 64
N_IDX_HEADS = 16
IDX_DIM = 64
TOPK_MAX = 256
N_HEADS_B = 8
QK_NOPE_DIM = 64
QK_ROPE_DIM = 32
V_DIM_B = 64
Q_LORA = 384
KV_LORA = 256
D_MIX = N_HEADS_A * HEAD_DIM_A + N_HEADS_B * V_DIM_B
D_FF = 2816
ROPE_THETA = 10000.0
EPS = 1e-6
Q_BLOCK = 128

PROJ_SIZES = (
    N_HEADS_A * HEAD_DIM_A,
    HEAD_DIM_A,
    HEAD_DIM_A,
    N_IDX_HEADS * IDX_DIM,
    IDX_DIM,
    N_IDX_HEADS,
    Q_LORA,
    KV_LORA,
    QK_ROPE_DIM,
)
D_PROJ = sum(PROJ_SIZES)

kernel_name = "hybrid_dsa_mla_macaron"


def rmsnorm(x, g):
    xf = x.astype(jnp.float32)
    y = xf * lax.rsqrt(jnp.mean(xf * xf, axis=-1, keepdims=True) + EPS)
    return (y * g.astype(jnp.float32)).astype(x.dtype)


def swiglu(x, w_gate, w_up, w_down):
    return (jax.nn.silu(x @ w_gate) * (x @ w_up)) @ w_down


def rope_tables(seq_len, dim):
    pos = jnp.arange(seq_len, dtype=jnp.float32)
    inv_freq = ROPE_THETA ** (-jnp.arange(0, dim, 2, dtype=jnp.float32) / dim)
    ang = pos[:, None] * inv_freq[None, :]
    return jnp.cos(ang), jnp.sin(ang)


def apply_rope(x, cos, sin):
    shape = (cos.shape[0],) + (1,) * (x.ndim - 3) + (cos.shape[1],)
    c = cos.reshape(shape)
    s = sin.reshape(shape)
    xf = x.astype(jnp.float32)
    x1, x2 = jnp.split(xf, 2, axis=-1)
    return jnp.concatenate([x1 * c - x2 * s, x1 * s + x2 * c], axis=-1).astype(x.dtype)


def block_slice(a, start):
    return lax.dynamic_slice_in_dim(a, start, Q_BLOCK, axis=1)


def dsa_sparse_attention(q, k, v, q_idx, k_idx, w_idx):
    B, T = q.shape[0], q.shape[1]
    top_k = min(TOPK_MAX, T // 4)
    n_blocks = T // Q_BLOCK
    key_pos = jnp.arange(T)
    scale = HEAD_DIM_A ** -0.5
    idx_scale = (N_IDX_HEADS * IDX_DIM) ** -0.5

    def one_block(i):
        s0 = i * Q_BLOCK
        qb = block_slice(q, s0)
        qib = block_slice(q_idx, s0)
        wb = block_slice(w_idx, s0).astype(jnp.float32) * idx_scale
        qpos = s0 + jnp.arange(Q_BLOCK)
        causal = key_pos[None, :] <= qpos[:, None]
        idx_logits = jnp.einsum('bqhd,bsd->bqhs', qib, k_idx).astype(jnp.float32)
        score = jnp.einsum('bqh,bqhs->bqs', wb, jax.nn.relu(idx_logits))
        score = jnp.where(causal[None], score, -jnp.inf)
        _, sel = lax.top_k(score, top_k)
        valid = sel <= qpos[None, :, None]
        kg = jax.vmap(lambda kb, ib: kb[ib])(k, sel)
        vg = jax.vmap(lambda vb, ib: vb[ib])(v, sel)
        att = jnp.einsum('bqhd,bqkd->bqhk', qb, kg).astype(jnp.float32) * scale
        att = jnp.where(valid[:, :, None, :], att, -jnp.inf)
        p = jax.nn.softmax(att, axis=-1)
        return jnp.einsum('bqhk,bqkd->bqhd', p.astype(vg.dtype), vg)

    out = lax.map(one_block, jnp.arange(n_blocks))
    return jnp.moveaxis(out, 0, 1).reshape(B, T, N_HEADS_A * HEAD_DIM_A)


def mla_attention(c_q, c_kv, k_rope, g_q_lat, g_kv_lat, w_uq, w_ukv, cos_b, sin_b):
    B, T = c_q.shape[0], c_q.shape[1]
    n_blocks = T // Q_BLOCK
    key_pos = jnp.arange(T)
    scale = (QK_NOPE_DIM + QK_ROPE_DIM) ** -0.5
    q = (rmsnorm(c_q, g_q_lat) @ w_uq).reshape(B, T, N_HEADS_B, QK_NOPE_DIM + QK_ROPE_DIM)
    q_nope, q_pe = q[..., :QK_NOPE_DIM], q[..., QK_NOPE_DIM:]
    q_pe = apply_rope(q_pe, cos_b, sin_b)
    kv = (rmsnorm(c_kv, g_kv_lat) @ w_ukv).reshape(B, T, N_HEADS_B, QK_NOPE_DIM + V_DIM_B)
    k_nope, v = kv[..., :QK_NOPE_DIM], kv[..., QK_NOPE_DIM:]
    k_pe = apply_rope(k_rope, cos_b, sin_b)

    def one_block(i):
        s0 = i * Q_BLOCK
        qn = block_slice(q_nope, s0)
        qp = block_slice(q_pe, s0)
        qpos = s0 + jnp.arange(Q_BLOCK)
        causal = key_pos[None, :] <= qpos[:, None]
        s = (jnp.einsum('bqhd,bshd->bhqs', qn, k_nope)
             + jnp.einsum('bqhr,bsr->bhqs', qp, k_pe)).astype(jnp.float32) * scale
        s = jnp.where(causal[None, None], s, -jnp.inf)
        p = jax.nn.softmax(s, axis=-1)
        return jnp.einsum('bhqs,bshd->bqhd', p.astype(v.dtype), v)

    out = lax.map(one_block, jnp.arange(n_blocks))
    return jnp.moveaxis(out, 0, 1).reshape(B, T, N_HEADS_B * V_DIM_B)


def setup_inputs(seed: int = 0) -> dict:
    key = jax.random.key(seed)
    ks = jax.random.split(key, 20)

    def w(k, shape, fan_in):
        return jax.random.normal(k, shape, jnp.float32) * (fan_in ** -0.5)

    def gain(k, shape):
        return 1.0 + 0.1 * jax.random.normal(k, shape, jnp.float32)

    L = DEPTH
    return {
        "x": jax.random.normal(ks[0], (BATCH, SEQ, D_MODEL), jnp.float32),
        "g_ffn1": gain(ks[1], (L, D_MODEL)),
        "w1_gate": w(ks[2], (L, D_MODEL, D_FF), D_MODEL),
        "w1_up": w(ks[3], (L, D_MODEL, D_FF), D_MODEL),
        "w1_down": w(ks[4], (L, D_FF, D_MODEL), D_FF),
        "g_mix": gain(ks[5], (L, D_MODEL)),
        "w_in": w(ks[6], (L, D_MODEL, D_PROJ), D_MODEL),
        "g_q_lat": gain(ks[7], (L, Q_LORA)),
        "g_kv_lat": gain(ks[8], (L, KV_LORA)),
        "w_uq": w(ks[9], (L, Q_LORA, N_HEADS_B * (QK_NOPE_DIM + QK_ROPE_DIM)), Q_LORA),
        "w_ukv": w(ks[10], (L, KV_LORA, N_HEADS_B * (QK_NOPE_DIM + V_DIM_B)), KV_LORA),
        "w_out": w(ks[11], (L, D_MIX, D_MODEL), D_MIX),
        "g_ffn2": gain(ks[12], (L, D_MODEL)),
        "w2_gate": w(ks[13], (L, D_MODEL, D_FF), D_MODEL),
        "w2_up": w(ks[14], (L, D_MODEL, D_FF), D_MODEL),
        "w2_down": w(ks[15], (L, D_FF, D_MODEL), D_FF),
        "g_final": gain(ks[16], (D_MODEL,)),
    }


def reference(x, g_ffn1, w1_gate, w1_up, w1_down, g_mix, w_in, g_q_lat, g_kv_lat,
              w_uq, w_ukv, w_out, g_ffn2, w2_gate, w2_up, w2_down, g_final):
    B, T, _ = x.shape
    cos_a, sin_a = rope_tables(T, HEAD_DIM_A)
    cos_i, sin_i = rope_tables(T, IDX_DIM)
    cos_b, sin_b = rope_tables(T, QK_ROPE_DIM)
    split_points = [int(v) for v in np.cumsum(PROJ_SIZES)[:-1]]
    for l in range(DEPTH):
        x = x + 0.5 * swiglu(rmsnorm(x, g_ffn1[l]), w1_gate[l], w1_up[l], w1_down[l])
        h = rmsnorm(x, g_mix[l])
        p = h @ w_in[l]
        qa, ka, va, qi, ki, wi, cq, ckv, kr = jnp.split(p, split_points, axis=-1)
        qa = apply_rope(qa.reshape(B, T, N_HEADS_A, HEAD_DIM_A), cos_a, sin_a)
        ka = apply_rope(ka, cos_a, sin_a)
        qi = apply_rope(qi.reshape(B, T, N_IDX_HEADS, IDX_DIM), cos_i, sin_i)
        ki = apply_rope(ki, cos_i, sin_i)
        out_a = dsa_sparse_attention(qa, ka, va, qi, ki, wi)
        out_b = mla_attention(cq, ckv, kr, g_q_lat[l], g_kv_lat[l], w_uq[l], w_ukv[l], cos_b, sin_b)
        x = x + jnp.concatenate([out_a, out_b], axis=-1) @ w_out[l]
        x = x + 0.5 * swiglu(rmsnorm(x, g_ffn2[l]), w2_gate[l], w2_up[l], w2_down[l])
    return rmsnorm(x, g_final)
```

```python
import math
from contextlib import ExitStack

import numpy as np
import concourse.bass as bass
import concourse.mybir as mybir
from concourse.bass_utils import run_bass_kernel_spmd

F32 = mybir.dt.float32
BF16 = mybir.dt.bfloat16
AF = mybir.ActivationFunctionType
ALU = mybir.AluOpType
AX = mybir.AxisListType

D = 1024
DFF = 2816
NFC = DFF // 128
EPS = 1e-6
ENGS = ("pe", "act", "dve", "pool", "sp")


class Res:
    __slots__ = ("name", "last_w", "readers")

    def __init__(self, name):
        self.name = name
        self.last_w = None
        self.readers = []


class Op:
    __slots__ = ("eng", "fn", "deps", "idx", "signal", "sem", "val", "is_dma", "waits", "key", "emitted")

    def __init__(self, eng, fn, is_dma=False):
        self.eng = eng
        self.fn = fn
        self.deps = []
        self.signal = False
        self.sem = None
        self.val = None
        self.is_dma = is_dma
        self.waits = []
        self.key = None
        self.emitted = False


class Prog:
    def __init__(self, nc, stack):
        self.nc = nc
        self.stack = stack
        self.ops = []
        self.n_total = 0
        self.eng_sem = {e: stack.enter_context(nc.semaphore("S_" + e)) for e in ENGS}
        self.cnt = {e: 0 for e in ENGS}
        self.dma_sem = {}
        self.dcnt = {}
        self.keymap = {}
        for e_, n_ in (("pool", 40), ("sp", 44)):
            for i_ in range(n_):
                self.dma_sem[(e_, i_)] = stack.enter_context(nc.semaphore("D_%s_%d" % (e_, i_)))
                self.dcnt[(e_, i_)] = 0
        self.block = stack.enter_context(nc.Block())
        self.waited = {e: {} for e in ENGS}
        self.phase_dmas = []
        self.last_op = {e: None for e in ENGS}

    def res(self, name="r"):
        return Res(name)

    def add(self, eng, fn, reads=(), writes=(), dma_key=None):
        op = Op(eng, fn, is_dma=dma_key is not None)
        op.idx = self.n_total
        self.n_total += 1
        seen = set()

        def dep(d):
            if d is None or d.idx in seen or d.emitted:
                return
            seen.add(d.idx)
            if d.eng == op.eng and not d.is_dma and not op.is_dma and d.eng == "pe":
                return
            op.deps.append(d)
            d.signal = True

        for r in reads:
            dep(r.last_w)
        for w in writes:
            dep(w.last_w)
            for rd in w.readers:
                dep(rd)
        for r in reads:
            r.readers.append(op)
        for w in writes:
            w.last_w = op
            w.readers = []
        if dma_key is not None:
            op.key = dma_key
            op.signal = True
            self.phase_dmas.append(op)
        self.ops.append(op)
        return op

    def dma(self, eng, out, in_, reads=(), writes=(), key=None):
        return self.add(eng, lambda e: e.dma_start(out=out, in_=in_), reads=reads, writes=writes, dma_key=key)

    def end_phase(self, pstack):
        nc = self.nc
        last = {}
        for op in self.ops:
            if op.fn is not None and not op.is_dma:
                last[op.eng] = op
        for e in ENGS:
            fin = self.add(e, None)
            fin.deps = [o for e2, o in last.items() if e2 != e] + list(self.phase_dmas)
            for o in fin.deps:
                o.signal = True
        self.phase_dmas = []
        for op in self.ops:
            if op.is_dma:
                km = self.keymap.setdefault(op.eng, {})
                if op.key not in km:
                    km[op.key] = len(km)
                k = (op.eng, km[op.key])
                assert k in self.dma_sem, ("out of preallocated DMA semaphores", k)
                self.dcnt[k] += 16
                op.sem = self.dma_sem[k]
                op.val = self.dcnt[k]
            elif op.signal:
                self.cnt[op.eng] += 1
                op.sem = self.eng_sem[op.eng]
                op.val = self.cnt[op.eng]
        for op in self.ops:
            need = {}
            w = self.waited[op.eng]
            for d in op.deps:
                key = id(d.sem)
                if w.get(key, 0) >= d.val:
                    continue
                if key not in need or need[key][1] < d.val:
                    need[key] = (d.sem, d.val)
            for key, (s, v) in need.items():
                w[key] = v
                op.waits.append((s, v))
        per_eng = {e: [op for op in self.ops if op.eng == e] for e in ENGS}
        block = self.block
        handles = {"pe": block.tensor, "act": block.scalar, "dve": block.vector,
                   "pool": block.gpsimd, "sp": block.sync}

        def make(e):
            def body(eng):
                for op in per_eng[e]:
                    for (s, v) in op.waits:
                        eng.wait_ge(s, v)
                    if op.fn is None:
                        continue
                    ins = op.fn(eng)
                    if op.signal:
                        ins.then_inc(op.sem, 16 if op.is_dma else 1)
            return body

        for e in ENGS:
            if per_eng[e]:
                handles[e](make(e))
        n = len(self.ops)
        for op in self.ops:
            op.emitted = True
            op.fn = None
        self.ops = []
        self.keymap = {}
        return n


def bcast_rows(vec_ap, n):
    return bass.AP(tensor=vec_ap.tensor, offset=vec_ap.offset, ap=[[0, 128], [1, n]])


class Ctx:
    pass


def load_weight_cast(P, c, dst_tile, dst_res_list, src_ap, nk, ncols, name, col_perm=None):
    j = 0
    for k in range(nk):
        for c0 in range(0, ncols, 512):
            cw = min(512, ncols - c0)
            P.dma("pool", dst_tile[:, k, c0:c0 + cw], src_ap[k * 128:(k + 1) * 128, c0:c0 + cw],
                  writes=[dst_res_list[k]], key=f"w{name}_{k}")
            j += 1


def rmsnorm_to_bf16(P, c, x_ap, x_res, n, g_bc, g_res, junk, junk_res, ss, ss_res, rstd, rstd_res, out_ap, out_res,
                    x_in_psum=False):
    P.add("act", lambda e: e.activation(out=junk, in_=x_ap, func=AF.Square, accum_out=ss),
          reads=[x_res], writes=[junk_res, ss_res])
    P.add("act", lambda e: e.activation(out=rstd, in_=ss, func=AF.Sqrt, scale=1.0 / n, bias=c.eps_t[:, 0:1]),
          reads=[ss_res, c.rconst], writes=[rstd_res])
    P.add("dve", lambda e: e.reciprocal(out=rstd, in_=rstd), reads=[rstd_res], writes=[rstd_res])
    P.add("dve", lambda e: e.scalar_tensor_tensor(out=out_ap, in0=x_ap, scalar=rstd, in1=g_bc,
                                                  op0=ALU.mult, op1=ALU.mult),
          reads=[x_res, rstd_res, g_res], writes=[out_res])


def ffn_phase(nc, P, c, T, src, src_res, dst, dst_res, g_vec, wg, wu, wd, final_g=None, tag="f1"):
    NT = T // 128
    with ExitStack() as ps:
        def sb(name, shape, dt):
            return ps.enter_context(nc.sbuf_tensor(tag + name, shape, dt))

        def pm(name, shape, dt):
            return ps.enter_context(nc.psum_tensor(tag + name, shape, dt))

        Wg = sb("Wg", [128, 8, DFF], BF16)
        Wu = sb("Wu", [128, 8, DFF], BF16)
        Wd = sb("Wd", [128, NFC, D], BF16)
        gbc = sb("gbc", [128, D], F32)
        gfin = sb("gfin", [128, D], F32) if final_g is not None else None
        xt = [sb(f"xt{i}", [128, D], F32) for i in range(3)]
        junk = [sb(f"junk{i}", [128, D], BF16) for i in range(2)]
        ss = [sb(f"ss{i}", [128, 4], F32) for i in range(2)]
        hb = [sb(f"hb{i}", [128, D], BF16) for i in range(2)]
        hT = [sb(f"hT{i}", [128, 8, 128], BF16) for i in range(2)]
        sg = [sb(f"sg{i}", [128, 512], F32) for i in range(2)]
        act = [sb(f"act{i}", [128, DFF], BF16) for i in range(2)]
        actT = [sb(f"actT{i}", [128, NFC, 128], BF16) for i in range(2)]
        yo = [sb(f"yo{i}", [128, D], F32) for i in range(2)] if final_g is not None else None
        tp = [pm(f"tp{i}", [128, 1024], BF16) for i in range(2)]
        mm = [pm(f"mm{i}", [128, 512], F32) for i in range(6)]

        R = P.res
        rWg = [R() for _ in range(8)]
        rWu = [R() for _ in range(8)]
        rWd = [R() for _ in range(NFC)]
        rg, rgf = R(), R()
        rxt = [R() for _ in range(3)]
        rjunk = [R() for _ in range(2)]
        rss = [R() for _ in range(2)]
        rrs = [R() for _ in range(2)]
        rhb = [R() for _ in range(2)]
        rhT = [R() for _ in range(2)]
        rsg = [R() for _ in range(2)]
        ract = [R() for _ in range(2)]
        ractT = [R() for _ in range(2)]
        ryo = [R() for _ in range(2)]
        rtp = [R() for _ in range(2)]
        rmm = [R() for _ in range(6)]

        P.dma("sp", gbc[:], bcast_rows(g_vec, D), writes=[rg], key="gbc")
        if final_g is not None:
            P.dma("sp", gfin[:], bcast_rows(final_g, D), writes=[rgf], key="gfin")
        load_weight_cast(P, c, Wg, rWg, wg, 8, DFF, "g")
        load_weight_cast(P, c, Wu, rWu, wu, 8, DFF, "u")
        load_weight_cast(P, c, Wd, rWd, wd, NFC, D, "d")

        slabs = [(s0, min(512, DFF - s0)) for s0 in range(0, DFF, 512)]
        cn = {"mmi": 0, "tpi": 0}

        def stage1(i):
            b = i % 2
            b3 = i % 3
            rows = slice(i * 128, (i + 1) * 128)
            P.dma("sp", xt[b3][:], src[rows, :], reads=[src_res[i]], writes=[rxt[b3]], key=f"xt{b3}")
            rmsnorm_to_bf16(P, c, xt[b3][:], rxt[b3], D, gbc[:], rg, junk[b][:], rjunk[b], ss[b][:, 0:1], rss[b],
                            ss[b][:, 1:2], rrs[b], hb[b][:], rhb[b])
            t = cn['tpi'] % 2
            cn['tpi'] += 1
            for k in range(8):
                P.add("pe", lambda e, k=k, t=t, b=b, b3=b3: e.transpose(out=tp[t][:, k * 128:(k + 1) * 128],
                                                                in_=hb[b][:, k * 128:(k + 1) * 128],
                                                                identity=c.ident[:]),
                      reads=[rhb[b], c.rident], writes=[rtp[t]])
            P.add("act", lambda e, t=t, b=b, b3=b3: e.activation(out=hT[b][:].rearrange("p k t -> p (k t)"),
                                                          in_=tp[t][:], func=AF.Copy),
                  reads=[rtp[t]], writes=[rhT[b]])
            for si, (s0, sw) in enumerate(slabs):
                ga = cn['mmi'] % 6
                ua = (cn['mmi'] + 1) % 6
                cn['mmi'] += 2
                for k in range(8):
                    P.add("pe", lambda e, k=k, ga=ga, b=b, b3=b3, s0=s0, sw=sw: e.matmul(
                        mm[ga][:, 0:sw], lhsT=hT[b][:, k, :], rhs=Wg[:, k, s0:s0 + sw],
                        start=(k == 0), stop=(k == 7)),
                          reads=[rhT[b], rWg[k]], writes=[rmm[ga]])
                for k in range(8):
                    P.add("pe", lambda e, k=k, ua=ua, b=b, b3=b3, s0=s0, sw=sw: e.matmul(
                        mm[ua][:, 0:sw], lhsT=hT[b][:, k, :], rhs=Wu[:, k, s0:s0 + sw],
                        start=(k == 0), stop=(k == 7)),
                          reads=[rhT[b], rWu[k]], writes=[rmm[ua]])
                s2 = si % 2
                P.add("act", lambda e, ga=ga, s2=s2, sw=sw: e.activation(out=sg[s2][:, 0:sw], in_=mm[ga][:, 0:sw],
                                                                        func=AF.Silu),
                      reads=[rmm[ga]], writes=[rsg[s2]])
                P.add("dve", lambda e, ua=ua, s2=s2, b=b, b3=b3, s0=s0, sw=sw: e.tensor_tensor(
                    out=act[b][:, s0:s0 + sw], in0=sg[s2][:, 0:sw], in1=mm[ua][:, 0:sw], op=ALU.mult),
                      reads=[rsg[s2], rmm[ua]], writes=[ract[b]])

        def stage2(i):
            b = i % 2
            b3 = i % 3
            rows = slice(i * 128, (i + 1) * 128)
            for f0 in range(0, NFC, 8):
                nf = min(8, NFC - f0)
                t = cn['tpi'] % 2
                cn['tpi'] += 1
                for f in range(nf):
                    P.add("pe", lambda e, f=f, f0=f0, t=t, b=b, b3=b3: e.transpose(
                        out=tp[t][:, f * 128:(f + 1) * 128],
                        in_=act[b][:, (f0 + f) * 128:(f0 + f + 1) * 128], identity=c.ident[:]),
                          reads=[ract[b], c.rident], writes=[rtp[t]])
                P.add("act" if (f0 // 8) % 2 == 0 else "dve",
                      (lambda e, t=t, b=b, b3=b3, f0=f0, nf=nf: e.activation(
                          out=actT[b][:, f0:f0 + nf, :].rearrange("p k t -> p (k t)"),
                          in_=tp[t][:, 0:nf * 128], func=AF.Copy)) if (f0 // 8) % 2 == 0 else
                      (lambda e, t=t, b=b, b3=b3, f0=f0, nf=nf: e.tensor_copy(
                          out=actT[b][:, f0:f0 + nf, :].rearrange("p k t -> p (k t)"),
                          in_=tp[t][:, 0:nf * 128])),
                      reads=[rtp[t]], writes=[ractT[b]])
            for half in range(2):
                da = cn['mmi'] % 6
                cn['mmi'] += 1
                for f in range(NFC):
                    P.add("pe", lambda e, f=f, da=da, b=b, b3=b3, half=half: e.matmul(
                        mm[da][:, :], lhsT=actT[b][:, f, :], rhs=Wd[:, f, half * 512:(half + 1) * 512],
                        start=(f == 0), stop=(f == NFC - 1)),
                          reads=[ractT[b], rWd[f]], writes=[rmm[da]])
                P.add("dve", lambda e, da=da, b=b, b3=b3, half=half: e.scalar_tensor_tensor(
                    out=xt[b3][:, half * 512:(half + 1) * 512], in0=mm[da][:, :], scalar=0.5,
                    in1=xt[b3][:, half * 512:(half + 1) * 512], op0=ALU.mult, op1=ALU.add),
                      reads=[rmm[da], rxt[b3]], writes=[rxt[b3]])
            if final_g is None:
                P.dma("sp", dst[rows, :], xt[b3][:], reads=[rxt[b3]], writes=[dst_res[i]], key=f"xo{b3}")
            else:
                P.add("act", lambda e, b=b, b3=b3: e.activation(out=junk[b][:], in_=xt[b3][:], func=AF.Square,
                                                         accum_out=ss[b][:, 2:3]),
                      reads=[rxt[b3]], writes=[rjunk[b], rss[b]])
                P.add("act", lambda e, b=b, b3=b3: e.activation(out=ss[b][:, 3:4], in_=ss[b][:, 2:3], func=AF.Sqrt,
                                                         scale=1.0 / D, bias=c.eps_t[:, 0:1]),
                      reads=[rss[b], c.rconst], writes=[rrs[b]])
                P.add("dve", lambda e, b=b, b3=b3: e.reciprocal(out=ss[b][:, 3:4], in_=ss[b][:, 3:4]),
                      reads=[rrs[b]], writes=[rrs[b]])
                P.add("dve", lambda e, b=b, b3=b3: e.scalar_tensor_tensor(out=yo[b][:], in0=xt[b3][:], scalar=ss[b][:, 3:4],
                                                                   in1=gfin[:], op0=ALU.mult, op1=ALU.mult),
                      reads=[rxt[b3], rrs[b], rgf], writes=[ryo[b]])
                P.dma("sp", dst[rows, :], yo[b][:], reads=[ryo[b]], writes=[dst_res[i]], key=f"yo{b}")

        stage1(0)
        for i in range(NT):
            if i + 1 < NT:
                stage1(i + 1)
            stage2(i)
        return P.end_phase(ps)


def const_phase(nc, P, c, stack):
    c.ident = stack.enter_context(nc.sbuf_tensor("ident", [128, 128], BF16))
    c.identf = stack.enter_context(nc.sbuf_tensor("identf", [128, 128], F32))
    c.eps_t = stack.enter_context(nc.sbuf_tensor("eps_t", [128, 1], F32))
    c.rident = P.res()
    c.rconst = P.res()
    P.add("pool", lambda e: e.memset(c.identf[:], 0.0), writes=[c.rident])
    P.add("pool", lambda e: e.affine_select(out=c.identf[:], in_=c.identf[:], pattern=[[-1, 128]],
                                            compare_op=ALU.not_equal, fill=1.0, base=0, channel_multiplier=1),
          reads=[c.rident], writes=[c.rident])
    P.add("pool", lambda e: e.tensor_copy(out=c.ident[:], in_=c.identf[:]), reads=[c.rident], writes=[c.rident])
    P.add("pool", lambda e: e.memset(c.eps_t[:], EPS), writes=[c.rconst])


def bc_mid(ap2d, H):
    a = [list(x) for x in ap2d.ap]
    return bass.AP(tensor=ap2d.tensor, offset=ap2d.offset, ap=[a[0], [0, H], a[1]])


def bc_last(ap2d, w):
    a = [list(x) for x in ap2d.ap]
    return bass.AP(tensor=ap2d.tensor, offset=ap2d.offset, ap=[a[0], a[1], [0, w]])


class BankPool:
    def __init__(self, tiles, res):
        self.tiles = tiles
        self.res = res
        self.i = 0

    def next(self):
        k = self.i % len(self.tiles)
        self.i += 1
        return self.tiles[k], self.res[k]


def transposes_to(P, c, tpp, srcs, src_res, dst_ap, dst_res, np_out=128, copy_eng="act"):
    tp, rtp = tpp.next()
    n = len(srcs)
    for j, s_ap in enumerate(srcs):
        P.add("pe", lambda e, j=j, s_ap=s_ap: e.transpose(out=tp[0:np_out, j * 128:(j + 1) * 128], in_=s_ap,
                                                         identity=c.ident[:]),
              reads=list(src_res) + [c.rident], writes=[rtp])
    if copy_eng == "act":
        P.add("act", lambda e: e.activation(out=dst_ap, in_=tp[0:np_out, 0:n * 128], func=AF.Copy),
              reads=[rtp], writes=[dst_res])
    else:
        P.add("dve", lambda e: e.tensor_copy(out=dst_ap, in_=tp[0:np_out, 0:n * 128]),
              reads=[rtp], writes=[dst_res])


def rope_ops(P, c, src3, src_res, H, d, tab, tab_res, ta, rta, tb, rtb, out3, out_res, out3b=None):
    h2 = d // 2
    cc = bc_mid(tab[:, 0:d], H)
    s0 = bc_mid(tab[:, d:d + h2], H)
    s1 = bc_mid(tab[:, d + h2:2 * d], H)
    P.add("dve", lambda e: e.tensor_tensor(out=ta, in0=src3, in1=cc, op=ALU.mult),
          reads=[src_res, tab_res], writes=[rta])
    P.add("dve", lambda e: e.tensor_tensor(out=tb[:, :, 0:h2], in0=src3[:, :, h2:d], in1=s0, op=ALU.mult),
          reads=[src_res, tab_res], writes=[rtb])
    P.add("dve", lambda e: e.tensor_tensor(out=tb[:, :, h2:d], in0=src3[:, :, 0:h2], in1=s1, op=ALU.mult),
          reads=[src_res, tab_res], writes=[rtb])
    P.add("pool", lambda e: e.tensor_tensor(out=out3, in0=ta, in1=tb, op=ALU.add),
          reads=[rta, rtb], writes=[out_res])
    if out3b is not None:
        P.add("pool", lambda e: e.tensor_tensor(out=out3b, in0=ta, in1=tb, op=ALU.add),
              reads=[rta, rtb], writes=[out_res])


WIN_SEGS = [
    (0, 0, 512),
    (512, 640, 512),
    (1024, 1152, 512),
    (1536, 1744, 384),
    (1920, 512, 64),
    (1984, 1664, 64),
    (2048, 2128, 256),
    (2304, 2384, 32),
    (2336, 576, 64),
    (2400, 1728, 16),
]
WIN_GROUPS = [(0, 512), (512, 512), (1024, 512), (1536, 512), (2048, 368)]
DPROJ = 2416


def proj_phase(nc, P, c, T, S):
    import os
    LIM = float(os.environ.get('PROJ_STOP', '99'))
    NT = T // 128
    with ExitStack() as ps:
        def sb(name, shape, dt):
            return ps.enter_context(nc.sbuf_tensor(name, shape, dt))

        def pm(name, shape, dt):
            return ps.enter_context(nc.psum_tensor(name, shape, dt))

        R = P.res
        Win = sb("Win", [128, 8, DPROJ], BF16)
        Wuq = sb("Wuq", [128, 3, 768], BF16)
        Wukv = sb("Wukv", [128, 2, 1024], BF16)
        gbc = sb("gbcm", [128, D], F32)
        gq = sb("gq", [128, 384], F32)
        gkv = sb("gkv", [128, 256], F32)
        rWin = [R() for _ in range(8)]
        rWuq = [R() for _ in range(3)]
        rWukv = [R() for _ in range(2)]
        rg, rgq, rgkv = R(), R(), R()

        def dbl(name, shape, dt):
            return [sb(f"{name}{i}", shape, dt) for i in range(2)], [R() for _ in range(2)]

        xt, rxt = dbl("pxt", [128, D], F32)
        junk, rjunk = dbl("pjunk", [128, D], BF16)
        ss, rss = dbl("pss", [128, 8], F32)
        rrs = [[R() for _ in range(3)] for _ in range(2)]
        hb, rhb = dbl("phb", [128, D], BF16)
        hT, rhT = dbl("phT", [128, 8, 128], BF16)
        r64, rr64 = dbl("r64", [128, 128], F32)
        r32, rr32 = dbl("r32", [128, 64], F32)
        ta, rta = dbl("ta", [128, 512], F32)
        tb, rtb = dbl("tb", [128, 512], F32)
        qar, rqar = dbl("qar", [128, 512], BF16)
        qir, rqir = dbl("qir", [128, 1024], BF16)
        qif, rqif = dbl("qif", [128, 512], F32)
        qaT, rqaT = dbl("qaT", [128, 512], BF16)
        qiT, rqiT = dbl("qiT", [128, 1024], BF16)
        aw, raw = dbl("aw", [128, 16], F32)
        sgt, rsgt = dbl("sgt", [128, 16], F32)
        kdup, rkdup = dbl("kdup", [128, 256], BF16)
        kT, rkT = dbl("kT", [128, 256], BF16)
        kpe, rkpe = dbl("kpe", [128, 32], BF16)
        va1, rva1 = dbl("va1", [128, 192], BF16)
        cqn, rcqn = dbl("cqn", [128, 384], BF16)
        cqT, rcqT = dbl("cqT", [128, 384], BF16)
        ckn, rckn = dbl("ckn", [128, 256], BF16)
        ckT, rckT = dbl("ckT", [128, 256], BF16)
        qbr, rqbr = dbl("qbr", [128, 8, 128], BF16)
        kbr, rkbr = dbl("kbr", [128, 8, 128], BF16)
        qbT, rqbT = dbl("qbT", [128, 1024], BF16)
        kbT, rkbT = dbl("kbT", [128, 1024], BF16)
        vb1, rvb1 = dbl("vb1", [128, 8, 192], BF16)
        tpt = [pm(f"ptp{i}", [128, 1024], BF16) for i in range(2)]
        tpp = BankPool(tpt, [R() for _ in range(2)])
        mmt = [pm(f"pmm{i}", [128, 512], F32) for i in range(6)]
        mmp = BankPool(mmt, [R() for _ in range(6)])

        P.dma("sp", gbc[:], bcast_rows(S.g_mix, D), writes=[rg], key="gbc")
        P.dma("sp", gq[:], bcast_rows(S.g_q_lat, 384), writes=[rgq], key="gq")
        P.dma("sp", gkv[:], bcast_rows(S.g_kv_lat, 256), writes=[rgkv], key="gkv")
        for k in range(8):
            for (dc, sc, w) in WIN_SEGS:
                P.dma("pool", Win[:, k, dc:dc + w], S.w_in[k * 128:(k + 1) * 128, sc:sc + w],
                      writes=[rWin[k]], key=f"wg_{k}")
        for k in range(3):
            for c0 in (0, 384):
                P.dma("pool", Wuq[:, k, c0:c0 + 384], S.w_uq[k * 128:(k + 1) * 128, c0:c0 + 384],
                      writes=[rWuq[k]], key=f"wu_{k}")
        for k in range(2):
            for c0 in (0, 512):
                P.dma("pool", Wukv[:, k, c0:c0 + 512], S.w_ukv[k * 128:(k + 1) * 128, c0:c0 + 512],
                      writes=[rWukv[k]], key=f"wd_{k}")
        for b in range(2):
            P.add("pool", lambda e, b=b: e.memset(qbr[b][:], 0.0), writes=[rqbr[b]])
            P.add("pool", lambda e, b=b: e.memset(kbr[b][:], 0.0), writes=[rkbr[b]])
            P.add("pool", lambda e, b=b: e.memset(va1[b][:], 1.0), writes=[rva1[b]])
            P.add("pool", lambda e, b=b: e.memset(vb1[b][:], 1.0), writes=[rvb1[b]])

        for i in range(NT):
            b = i % 2
            rows = slice(i * 128, (i + 1) * 128)
            cols = slice(i * 128, (i + 1) * 128)
            P.dma("sp", xt[b][:], S.X1[rows, :], reads=[S.rX1[i]], writes=[rxt[b]], key=f"xt{b}")
            P.dma("sp", r64[b][:], S.rope64[rows, :], writes=[rr64[b]], key=f"r64{b}")
            P.dma("sp", r32[b][:], S.rope32[rows, :], writes=[rr32[b]], key=f"r32{b}")
            rmsnorm_to_bf16(P, c, xt[b][:], rxt[b], D, gbc[:], rg, junk[b][:], rjunk[b], ss[b][:, 0:1], rss[b],
                            ss[b][:, 1:2], rrs[b][0], hb[b][:], rhb[b])
            transposes_to(P, c, tpp, [hb[b][:, k * 128:(k + 1) * 128] for k in range(8)], [rhb[b]],
                          hT[b][:].rearrange("p k t -> p (k t)"), rhT[b])
            if LIM < 2:
                continue
            banks = []
            for (g0, gw) in WIN_GROUPS:
                bk, rbk = mmp.next()
                for k in range(8):
                    P.add("pe", lambda e, k=k, bk=bk, g0=g0, gw=gw, b=b: e.matmul(
                        bk[:, 0:gw], lhsT=hT[b][:, k, :], rhs=Win[:, k, g0:g0 + gw], start=(k == 0), stop=(k == 7)),
                          reads=[rhT[b], rWin[k]], writes=[rbk])
                banks.append((bk, rbk))
            (B0, rB0), (B1, rB1), (B2, rB2), (B3, rB3), (B4, rB4) = banks
            v3 = lambda ap, H: ap.rearrange("p (h d) -> p h d", h=H)
            if LIM < 3:
                continue
            P.add("act", lambda e, b=b, B4=B4: e.activation(out=aw[b][:], in_=B4[:, 352:368], func=AF.Abs,
                                                           scale=1.0 / 32.0),
                  reads=[rB4], writes=[raw[b]])
            P.add("act", lambda e, b=b, B4=B4: e.activation(out=sgt[b][:], in_=B4[:, 352:368], func=AF.Sign),
                  reads=[rB4], writes=[rsgt[b]])
            P.dma("sp", S.SG[rows, :], sgt[b][:], reads=[rsgt[b]], writes=[S.rSG[i]], key=f"sgt{b}")
            if LIM < 4:
                continue
            rope_ops(P, c, v3(B0[:, 0:512], 8), rB0, 8, 64, r64[b], rr64[b], v3(ta[b][:], 8), rta[b],
                     v3(tb[b][:], 8), rtb[b], v3(qar[b][:], 8), rqar[b])
            transposes_to(P, c, tpp, [qar[b][:, j * 128:(j + 1) * 128] for j in range(4)], [rqar[b]],
                          qaT[b][:], rqaT[b], copy_eng="dve")
            P.dma("sp", S.QA_T[i], qaT[b][:], reads=[rqaT[b]], writes=[S.rQA[i]], key=f"qaT{b}")
            if LIM < 5:
                continue
            for hh, (Bq, rBq) in enumerate(((B1, rB1), (B2, rB2))):
                rope_ops(P, c, v3(Bq[:, 0:512], 8), rBq, 8, 64, r64[b], rr64[b], v3(ta[b][:], 8), rta[b],
                         v3(tb[b][:], 8), rtb[b], v3(qif[b][:], 8), rqif[b])
                P.add("dve", lambda e, b=b, hh=hh: e.tensor_tensor(
                    out=v3(qir[b][:, hh * 512:(hh + 1) * 512], 8), in0=v3(qif[b][:], 8),
                    in1=bc_last(aw[b][:, hh * 8:(hh + 1) * 8], 64), op=ALU.mult),
                      reads=[rqif[b], raw[b]], writes=[rqir[b]])
            transposes_to(P, c, tpp, [qir[b][:, j * 128:(j + 1) * 128] for j in range(8)], [rqir[b]],
                          qiT[b][:], rqiT[b])
            P.dma("sp", S.QI_T[i], qiT[b][:], reads=[rqiT[b]], writes=[S.rQI[i]], key=f"qiT{b}")
            if LIM < 6:
                continue
            kd4 = kdup[b][:].rearrange("p (a r d) -> p a r d", a=2, r=2)
            rope_ops(P, c, v3(B3[:, 384:512], 2), rB3, 2, 64, r64[b], rr64[b], v3(ta[b][:, 0:128], 2), rta[b],
                     v3(tb[b][:, 0:128], 2), rtb[b], kd4[:, :, 0, :], rkdup[b], out3b=kd4[:, :, 1, :])
            transposes_to(P, c, tpp, [kdup[b][:, 0:128], kdup[b][:, 128:256]], [rkdup[b]], kT[b][:], rkT[b],
                          copy_eng="dve")
            P.dma("sp", S.KA_T2[:, cols], kT[b][:, 0:128], reads=[rkT[b]], writes=[S.rKA[i]], key=f"kTa{b}")
            P.dma("sp", S.KI_T2[:, cols], kT[b][:, 128:256], reads=[rkT[b]], writes=[S.rKI[i]], key=f"kTi{b}")
            if LIM < 7:
                continue
            rope_ops(P, c, v3(B4[:, 256:288], 1), rB4, 1, 32, r32[b], rr32[b], v3(ta[b][:, 0:32], 1), rta[b],
                     v3(tb[b][:, 0:32], 1), rtb[b], v3(kpe[b][:], 1), rkpe[b])
            if LIM < 8:
                continue
            P.add("act", lambda e, b=b, B4=B4: e.activation(out=va1[b][:, 64:128], in_=B4[:, 288:352], func=AF.Copy),
                  reads=[rB4], writes=[rva1[b]])
            P.dma("sp", S.VA1[rows, :], va1[b][:], reads=[rva1[b]], writes=[S.rVA[i]], key=f"va1{b}")
            if LIM < 9:
                continue
            rmsnorm_to_bf16(P, c, B3[:, 0:384], rB3, 384, gq[:], rgq, junk[b][:, 0:384], rjunk[b], ss[b][:, 2:3],
                            rss[b], ss[b][:, 3:4], rrs[b][1], cqn[b][:], rcqn[b])
            if LIM < 9.1:
                continue
            transposes_to(P, c, tpp, [cqn[b][:, k * 128:(k + 1) * 128] for k in range(3)], [rcqn[b]],
                          cqT[b][:], rcqT[b], copy_eng="dve")
            if LIM < 9.2:
                continue
            for (q0, qw, h0, nh) in ((0, 480, 0, 5), (480, 288, 5, 3)):
                bk, rbk = mmp.next()
                for k in range(3):
                    P.add("pe", lambda e, k=k, bk=bk, q0=q0, qw=qw, b=b: e.matmul(
                        bk[:, 0:qw], lhsT=cqT[b][:, k * 128:(k + 1) * 128], rhs=Wuq[:, k, q0:q0 + qw],
                        start=(k == 0), stop=(k == 2)),
                          reads=[rcqT[b], rWuq[k]], writes=[rbk])
                if LIM < 9.3:
                    continue
                bv = bk[:, 0:qw].rearrange("p (h d) -> p h d", h=nh)
                P.add("dve", lambda e, bv=bv, b=b, h0=h0, nh=nh: e.tensor_copy(
                    out=qbr[b][:, h0:h0 + nh, 0:64], in_=bv[:, :, 0:64]),
                      reads=[rbk], writes=[rqbr[b]])
                if LIM < 9.4:
                    continue
                rope_ops(P, c, bv[:, :, 64:96], rbk, nh, 32, r32[b], rr32[b],
                         ta[b][:, 0:nh * 32].rearrange("p (h d) -> p h d", h=nh), rta[b],
                         tb[b][:, 0:nh * 32].rearrange("p (h d) -> p h d", h=nh), rtb[b],
                         qbr[b][:, h0:h0 + nh, 64:96], rqbr[b])
            if LIM < 9.5:
                continue
            transposes_to(P, c, tpp, [qbr[b][:, h, :] for h in range(8)], [rqbr[b]], qbT[b][:], rqbT[b])
            if LIM < 9.6:
                continue
            P.dma("sp", S.QB_T[:, :, cols].rearrange("h p t -> p h t"),
                  qbT[b][:].rearrange("p (h t) -> p h t", h=8), reads=[rqbT[b]], writes=[S.rQB[i]], key=f"qbT{b}")
            if LIM < 10:
                continue
            rmsnorm_to_bf16(P, c, B4[:, 0:256], rB4, 256, gkv[:], rgkv, junk[b][:, 0:256], rjunk[b], ss[b][:, 4:5],
                            rss[b], ss[b][:, 5:6], rrs[b][2], ckn[b][:], rckn[b])
            transposes_to(P, c, tpp, [ckn[b][:, k * 128:(k + 1) * 128] for k in range(2)], [rckn[b]],
                          ckT[b][:], rckT[b], copy_eng="dve")
            P.add("pool", lambda e, b=b: e.tensor_copy(out=kbr[b][:, :, 64:96], in_=bc_mid(kpe[b][:], 8)),
                  reads=[rkpe[b]], writes=[rkbr[b]])
            for hf in range(2):
                bk, rbk = mmp.next()
                for k in range(2):
                    P.add("pe", lambda e, k=k, bk=bk, hf=hf, b=b: e.matmul(
                        bk[:, :], lhsT=ckT[b][:, k * 128:(k + 1) * 128], rhs=Wukv[:, k, hf * 512:(hf + 1) * 512],
                        start=(k == 0), stop=(k == 1)),
                          reads=[rckT[b], rWukv[k]], writes=[rbk])
                bv = bk[:, :].rearrange("p (h d) -> p h d", h=4)
                P.add("dve", lambda e, bv=bv, b=b, hf=hf: e.tensor_copy(
                    out=kbr[b][:, hf * 4:(hf + 1) * 4, 0:64], in_=bv[:, :, 0:64]),
                      reads=[rbk], writes=[rkbr[b]])
                P.add("dve", lambda e, bv=bv, b=b, hf=hf: e.tensor_copy(
                    out=vb1[b][:, hf * 4:(hf + 1) * 4, 64:128], in_=bv[:, :, 64:128]),
                      reads=[rbk], writes=[rvb1[b]])
            transposes_to(P, c, tpp, [kbr[b][:, h, :] for h in range(8)], [rkbr[b]], kbT[b][:], rkbT[b])
            P.dma("sp", S.KB_T[:, :, cols].rearrange("h p t -> p h t"),
                  kbT[b][:].rearrange("p (h t) -> p h t", h=8), reads=[rkbT[b]], writes=[S.rKB[i]], key=f"kbT{b}")
            P.dma("sp", S.VB1[rows, :, :], vb1[b][:], reads=[rvb1[b]], writes=[S.rVB[i]], key=f"vb1{b}")
        return P.end_phase(ps)


NEG = -1.0e30
NBIS = 16


def normalize_out(P, c, O, rO, num_lo, rc, rrc, out_ap, out_res):
    den_lo = 64 - num_lo
    P.add("dve", lambda e: e.reciprocal(out=rc[den_lo:den_lo + 64, :], in_=O[den_lo:den_lo + 64, :]),
          reads=[rO], writes=[rrc])
    P.add("dve", lambda e: e.tensor_tensor(out=out_ap, in0=O[num_lo:num_lo + 64, :],
                                           in1=rc[den_lo:den_lo + 64, :], op=ALU.mult),
          reads=[rO, rrc], writes=[out_res])


def dsa_phase(nc, P, c, T, S):
    NT = T // 128
    TOPK = min(256, T // 4)
    QT0 = TOPK // 128
    with ExitStack() as ps:
        def sb(name, shape, dt):
            return ps.enter_context(nc.sbuf_tensor(name, shape, dt))

        def pm(name, shape, dt):
            return ps.enter_context(nc.psum_tensor(name, shape, dt))

        R = P.res

        def dbl(name, shape, dt):
            return [sb(f"{name}{i}", shape, dt) for i in range(2)], [R() for _ in range(2)]

        KA2 = sb("KA2", [128, T], BF16)
        KI2 = sb("KI2", [128, T], BF16)
        VAs = sb("VAs", [128, NT, 192], BF16)
        rKA2 = [R() for _ in range(NT)]
        rKI2 = [R() for _ in range(NT)]
        rVAs = [R() for _ in range(NT)]
        cneg = sb("cneg", [128, 128], F32)
        pow2 = sb("pow2", [128, NBIS], F32)
        rcn = R()
        qiT, rqiT = dbl("dqiT", [128, 1024], BF16)
        qaT, rqaT = dbl("dqaT", [128, 512], BF16)
        sg, rsg = dbl("dsg", [128, 16], F32)
        Rt = [sb(f"Rt{i}", [128, 512], F32) for i in range(4)]
        rRt = [R() for _ in range(4)]
        Isb, rIsb = dbl("Isb", [128, T], F32)
        cjunk = sb("cjunk", [128, T], BF16)
        rcj = R()
        st_, rst = dbl("dst", [128, 8 + NBIS], F32)
        maskq, rmq = dbl("maskq", [128, T], BF16)
        maskT, rmT = dbl("maskT", [128, NT, 128], BF16)
        PT = [sb(f"PT{i}", [128, 1024], BF16) for i in range(4)]
        rPT = [R() for _ in range(4)]
        rc, rrc = dbl("drc", [128, 512], F32)
        aT, raT = dbl("daT", [128, 512], BF16)
        Lt = [pm(f"dL{i}", [128, 512], F32) for i in range(4)]
        Lp = BankPool(Lt, [R() for _ in range(4)])
        At = [pm(f"dA{i}", [128, 512], F32) for i in range(2)]
        rAt = [R() for _ in range(2)]
        OE = pm("dOE", [128, 512], F32)
        OO = pm("dOO", [128, 512], F32)
        rOE, rOO = R(), R()

        P.add("pool", lambda e: e.memset(cneg[:], 0.0), writes=[rcn])
        P.add("pool", lambda e: e.affine_select(out=cneg[:], in_=cneg[:], pattern=[[-1, 128]],
                                                compare_op=ALU.is_ge, fill=NEG, base=0, channel_multiplier=1),
              reads=[rcn], writes=[rcn])
        for k in range(NBIS):
            P.add("pool", lambda e, k=k: e.memset(pow2[:, k:k + 1], 2.0 ** (-(k + 1))), writes=[rcn])
        CH = min(T, 1024)
        for ci in range(T // CH):
            cols = slice(ci * CH, (ci + 1) * CH)
            tl = range(ci * (CH // 128), (ci + 1) * (CH // 128))
            rk, rki, rv = R(), R(), R()
            P.dma("sp", KA2[:, cols], S.KA_T2[:, cols], reads=[S.rKA[i] for i in tl], writes=[rk], key=f"KA2_{ci}")
            P.dma("sp", KI2[:, cols], S.KI_T2[:, cols], reads=[S.rKI[i] for i in tl], writes=[rki], key=f"KI2_{ci}")
            P.dma("sp", VAs[:, ci * (CH // 128):(ci + 1) * (CH // 128), :],
                  S.VA1[cols, :].rearrange("(n p) c -> p n c", p=128), reads=[S.rVA[i] for i in tl], writes=[rv],
                  key=f"VAs_{ci}")
            for i in tl:
                rKA2[i], rKI2[i], rVAs[i] = rk, rki, rv

        cnt = {"ri": 0, "pti": 0}

        def stage_A(qt):
            b = qt % 2
            SL = (qt + 1) * 128
            P.dma("sp", qiT[b][:], S.QI_T[qt], reads=[S.rQI[qt]], writes=[rqiT[b]], key=f"dqiT{b}")
            P.dma("sp", sg[b][:], S.SG[qt * 128:(qt + 1) * 128, :], reads=[S.rSG[qt]], writes=[rsg[b]], key=f"dsg{b}")
            for sbk, s0 in enumerate(range(0, SL, 512)):
                sw = min(512, SL - s0)
                kres = [rKI2[j] for j in range(s0 // 128, (s0 + sw) // 128)]
                A, rA = At[sbk % 2], rAt[sbk % 2]
                for hp in range(8):
                    for par in range(2):
                        L, rL = Lp.next()
                        pl = par * 64
                        P.add("pe", lambda e, L=L, hp=hp, pl=pl, b=b, s0=s0, sw=sw: e.matmul(
                            L[:, 0:sw], lhsT=qiT[b][pl:pl + 64, hp * 128:(hp + 1) * 128],
                            rhs=KI2[pl:pl + 64, s0:s0 + sw], start=True, stop=True),
                              reads=[rqiT[b]] + kres, writes=[rL])
                        r = cnt['ri'] % 4
                        cnt['ri'] += 1
                        P.add("act", lambda e, L=L, r=r, sw=sw: e.activation(out=Rt[r][:, 0:sw], in_=L[:, 0:sw],
                                                                            func=AF.Relu),
                              reads=[rL], writes=[rRt[r]])
                        h = 2 * hp + par
                        if hp == 0 and par == 0:
                            P.add("dve", lambda e, A=A, r=r, sw=sw, b=b, h=h: e.tensor_scalar(
                                out=A[:, 0:sw], in0=Rt[r][:, 0:sw], scalar1=sg[b][:, h:h + 1], scalar2=None,
                                op0=ALU.mult),
                                  reads=[rRt[r], rsg[b]], writes=[rA])
                        else:
                            P.add("dve", lambda e, A=A, r=r, sw=sw, b=b, h=h: e.scalar_tensor_tensor(
                                out=A[:, 0:sw], in0=Rt[r][:, 0:sw], scalar=sg[b][:, h:h + 1], in1=A[:, 0:sw],
                                op0=ALU.mult, op1=ALU.add),
                                  reads=[rRt[r], rsg[b], rA], writes=[rA])
                P.add("act", lambda e, A=A, b=b, s0=s0, sw=sw: e.activation(out=Isb[b][:, s0:s0 + sw], in_=A[:, 0:sw],
                                                                          func=AF.Copy),
                      reads=[rA], writes=[rIsb[b]])

        def stage_B(qt):
            b = qt % 2
            SL = (qt + 1) * 128
            P.dma("sp", qaT[b][:], S.QA_T[qt], reads=[S.rQA[qt]], writes=[rqaT[b]], key=f"dqaT{b}")
            S_ = st_[b]
            if qt >= QT0:
                P.add("dve", lambda e, b=b, SL=SL, S_=S_: e.tensor_reduce(out=S_[:, 0:1], in_=Isb[b][:, 0:SL], axis=AX.X,
                                                                        op=ALU.min),
                      reads=[rIsb[b]], writes=[rst[b]])
                P.add("dve", lambda e, b=b, SL=SL, S_=S_: e.tensor_reduce(out=S_[:, 1:2], in_=Isb[b][:, 0:SL], axis=AX.X,
                                                                        op=ALU.max),
                      reads=[rIsb[b]], writes=[rst[b]])
            P.add("pool", lambda e, b=b, qt=qt: e.tensor_tensor(out=Isb[b][:, qt * 128:(qt + 1) * 128],
                                                              in0=Isb[b][:, qt * 128:(qt + 1) * 128], in1=cneg[:],
                                                              op=ALU.add),
                  reads=[rIsb[b], rcn], writes=[rIsb[b]])
            if qt >= QT0:
                P.add("dve", lambda e, S_=S_: e.tensor_tensor(out=S_[:, 2:3], in0=S_[:, 1:2], in1=S_[:, 0:1],
                                                             op=ALU.subtract),
                      reads=[rst[b]], writes=[rst[b]])
                P.add("dve", lambda e, S_=S_: e.tensor_scalar(out=S_[:, 8:8 + NBIS], in0=pow2[:], scalar1=S_[:, 2:3],
                                                             scalar2=None, op0=ALU.mult),
                      reads=[rst[b], rcn], writes=[rst[b]])
                P.add("dve", lambda e, S_=S_: e.tensor_copy(out=S_[:, 3:4], in_=S_[:, 0:1]),
                      reads=[rst[b]], writes=[rst[b]])
                for k in range(NBIS):
                    P.add("dve", lambda e, S_=S_, k=k: e.tensor_tensor(out=S_[:, 4:5], in0=S_[:, 3:4],
                                                                      in1=S_[:, 8 + k:9 + k], op=ALU.add),
                          reads=[rst[b]], writes=[rst[b]])
                    P.add("dve", lambda e, S_=S_, b=b, SL=SL: e.tensor_scalar(
                        out=cjunk[:, 0:SL], in0=Isb[b][:, 0:SL], scalar1=S_[:, 4:5], scalar2=None,
                        op0=ALU.is_ge, op1=ALU.add, accum_out=S_[:, 5:6]),
                          reads=[rst[b], rIsb[b]], writes=[rst[b], rcj])
                    P.add("dve", lambda e, S_=S_, k=k: e.tensor_scalar(
                        out=S_[:, 6:7], in0=S_[:, 5:6], scalar1=float(TOPK), scalar2=S_[:, 8 + k:9 + k],
                        op0=ALU.is_ge, op1=ALU.mult),
                          reads=[rst[b]], writes=[rst[b]])
                    P.add("dve", lambda e, S_=S_: e.tensor_tensor(out=S_[:, 3:4], in0=S_[:, 3:4], in1=S_[:, 6:7],
                                                                 op=ALU.add),
                          reads=[rst[b]], writes=[rst[b]])
            else:
                P.add("dve", lambda e, S_=S_: e.memset(S_[:, 3:4], -1.0e29), writes=[rst[b]])
            P.add("dve", lambda e, S_=S_, b=b, SL=SL: e.tensor_scalar(
                out=maskq[b][:, 0:SL], in0=Isb[b][:, 0:SL], scalar1=S_[:, 3:4], scalar2=None, op0=ALU.is_ge),
                  reads=[rst[b], rIsb[b]], writes=[rmq[b]])

        def stage_C(qt):
            b = qt % 2
            SL = (qt + 1) * 128
            for s8 in range(0, qt + 1, 8):
                n8 = min(8, qt + 1 - s8)
                L, rL = Lp.next()
                Lb = L[:, :].bitcast(BF16)
                for j in range(n8):
                    P.add("pe", lambda e, Lb=Lb, j=j, s8=s8, b=b: e.transpose(
                        out=Lb[:, j * 128:(j + 1) * 128], in_=maskq[b][:, (s8 + j) * 128:(s8 + j + 1) * 128],
                        identity=c.ident[:]),
                          reads=[rmq[b], c.rident], writes=[rL])
                P.add("act", lambda e, Lb=Lb, s8=s8, n8=n8, b=b: e.activation(
                    out=maskT[b][:, s8:s8 + n8, :].rearrange("p n q -> p (n q)"), in_=Lb[:, 0:n8 * 128],
                    func=AF.Copy),
                      reads=[rL], writes=[rmT[b]])
            def qk(st):
                sc = slice(st * 128, (st + 1) * 128)
                LE, rLE = Lp.next()
                LO, rLO = Lp.next()
                P.add("pe", lambda e: e.matmul(LE[:, :], lhsT=KA2[0:64, sc], rhs=qaT[b][0:64, :], start=True, stop=True),
                      reads=[rKA2[st], rqaT[b]], writes=[rLE])
                P.add("pe", lambda e: e.matmul(LO[:, :], lhsT=KA2[64:128, sc], rhs=qaT[b][64:128, :], start=True,
                                               stop=True),
                      reads=[rKA2[st], rqaT[b]], writes=[rLO])
                p = cnt["pti"] % len(PT)
                cnt["pti"] += 1
                P.add("act", lambda e: e.activation(out=PT[p][:, 0:512], in_=LE[:, :], func=AF.Exp, scale=0.125),
                      reads=[rLE], writes=[rPT[p]])
                P.add("act", lambda e: e.activation(out=PT[p][:, 512:1024], in_=LO[:, :], func=AF.Exp, scale=0.125),
                      reads=[rLO], writes=[rPT[p]])
                P.add("pool", lambda e: e.tensor_tensor(
                    out=PT[p][:].rearrange("p (a q) -> p a q", a=8), in0=PT[p][:].rearrange("p (a q) -> p a q", a=8),
                    in1=bc_mid(maskT[b][:, st, :], 8), op=ALU.mult),
                      reads=[rPT[p], rmT[b]], writes=[rPT[p]])
                return p

            def pv(st, p):
                P.add("pe", lambda e: e.matmul(OE[:, :], lhsT=VAs[:, st, 64:192], rhs=PT[p][:, 0:512],
                                               start=(st == 0), stop=(st == qt)),
                      reads=[rVAs[st], rPT[p]], writes=[rOE])
                P.add("pe", lambda e: e.matmul(OO[:, :], lhsT=VAs[:, st, 0:128], rhs=PT[p][:, 512:1024],
                                               start=(st == 0), stop=(st == qt)),
                      reads=[rVAs[st], rPT[p]], writes=[rOO])

            pend = {0: qk(0)}
            for st in range(qt + 1):
                if st + 1 <= qt:
                    pend[st + 1] = qk(st + 1)
                pv(st, pend.pop(st))
            normalize_out(P, c, OE, rOE, 0, rc[b], rrc[b], aT[b][0:64, :], raT[b])
            normalize_out(P, c, OO, rOO, 64, rc[b], rrc[b], aT[b][64:128, :], raT[b])
            P.dma("sp", S.ATT_T[0:4, :, qt * 128:(qt + 1) * 128].rearrange("j p t -> p j t"),
                  aT[b][:].rearrange("p (j t) -> p j t", j=4), reads=[raT[b]], writes=[S.rATa[qt]], key=f"daT{b}")

        for step in range(NT + 2):
            if step < NT:
                stage_A(step)
            if 0 <= step - 1 < NT:
                stage_B(step - 1)
            if 0 <= step - 2 < NT:
                stage_C(step - 2)
        return P.end_phase(ps)


SCALE_B = 96.0 ** -0.5


def mla_phase(nc, P, c, T, S):
    NT = T // 128
    NQB = T // 512
    with ExitStack() as ps:
        def sb(name, shape, dt):
            return ps.enter_context(nc.sbuf_tensor(name, shape, dt))

        def pm(name, shape, dt):
            return ps.enter_context(nc.psum_tensor(name, shape, dt))

        R = P.res

        def dbl(name, shape, dt):
            return [sb(f"{name}{i}", shape, dt) for i in range(2)], [R() for _ in range(2)]

        KB = [sb(f"mKB{i}", [128, T], BF16) for i in range(2)]
        VB = [sb(f"mVB{i}", [128, NT, 192], BF16) for i in range(2)]
        rKB = [[R() for _ in range(NQB)] for _ in range(2)]
        rVB = [[R() for _ in range(NQB)] for _ in range(2)]
        Cm = sb("Cm", [128, 4, 512], BF16)
        rCm = R()
        QT, rQT = dbl("mQT", [128, 512], BF16)
        PT = [sb(f"mPT{i}", [128, 512], BF16) for i in range(4)]
        rPT = [R() for _ in range(4)]
        rc, rrc = dbl("mrc", [128, 512], F32)
        aT, raT = dbl("maT", [128, 512], BF16)
        Lt = [pm(f"mL{i}", [128, 512], F32) for i in range(4)]
        Lp = BankPool(Lt, [R() for _ in range(4)])
        Ot = [pm(f"mO{i}", [128, 512], F32) for i in range(2)]
        rOt = [R() for _ in range(2)]

        P.add("pool", lambda e: e.memset(Cm[:], 1.0), writes=[rCm])
        P.add("pool", lambda e: e.affine_select(out=Cm[:], in_=Cm[:], pattern=[[-128, 4], [1, 512]],
                                                compare_op=ALU.is_ge, fill=0.0, base=0, channel_multiplier=-1),
              reads=[rCm], writes=[rCm])
        CH = min(T, 1024)
        NCH = T // CH

        def load_head(h):
            hb_ = h % 2
            for ci in range(NCH):
                cs = slice(ci * CH, (ci + 1) * CH)
                tl = range(ci * (CH // 128), (ci + 1) * (CH // 128))
                P.dma("sp", KB[hb_][:, cs], S.KB_T[h, :, cs], reads=[S.rKB[i] for i in tl],
                      writes=[rKB[hb_][ci]], key=f"mKB{hb_}_{ci}")
                P.dma("sp", VB[hb_][:, ci * (CH // 128):(ci + 1) * (CH // 128), :],
                      S.VB1[cs, h, :].rearrange("(n p) c -> p n c", p=128),
                      reads=[S.rVB[i] for i in tl], writes=[rVB[hb_][ci]], key=f"mVB{hb_}_{ci}")

        groups = [(h, qb) for h in range(8) for qb in range(NQB)]

        def load_q(g):
            h, qb = groups[g]
            bq = g % 2
            cs = slice(qb * 512, (qb + 1) * 512)
            P.dma("sp", QT[bq][:], S.QB_T[h, :, cs], reads=[S.rQB[i] for i in range(qb * 4, qb * 4 + 4)],
                  writes=[rQT[bq]], key=f"mQT{bq}")

        steps = [(g, st) for g, (h, qb) in enumerate(groups) for st in range(4 * (qb + 1))]
        cnt = {"pti": 0}

        def qk(g, st):
            h, qb = groups[g]
            hb_, bq = h % 2, g % 2
            L, rL = Lp.next()
            P.add("pe", lambda e: e.matmul(L[:, :], lhsT=KB[hb_][:, st * 128:(st + 1) * 128], rhs=QT[bq][:, :],
                                           start=True, stop=True),
                  reads=[rKB[hb_][(st * 128) // CH], rQT[bq]], writes=[rL])
            p = cnt["pti"] % len(PT)
            cnt["pti"] += 1
            P.add("act", lambda e: e.activation(out=PT[p][:], in_=L[:, :], func=AF.Exp, scale=SCALE_B),
                  reads=[rL], writes=[rPT[p]])
            j = st - 4 * qb
            if j >= 0:
                P.add("pool", lambda e: e.tensor_tensor(out=PT[p][:], in0=PT[p][:], in1=Cm[:, j, :], op=ALU.mult),
                      reads=[rPT[p], rCm], writes=[rPT[p]])
            return p

        def pv(g, st, p):
            h, qb = groups[g]
            hb_, bq = h % 2, g % 2
            nst = 4 * (qb + 1)
            O, rO = Ot[g % 2], rOt[g % 2]
            vsl = slice(64, 192) if h % 2 == 0 else slice(0, 128)
            num_lo = 0 if h % 2 == 0 else 64
            P.add("pe", lambda e: e.matmul(O[:, :], lhsT=VB[hb_][:, st, vsl], rhs=PT[p][:], start=(st == 0),
                                           stop=(st == nst - 1)),
                  reads=[rVB[hb_][(st * 128) // CH], rPT[p]], writes=[rO])
            if st == nst - 1:
                cs = slice(qb * 512, (qb + 1) * 512)
                normalize_out(P, c, O, rO, num_lo, rc[bq], rrc[bq], aT[bq][num_lo:num_lo + 64, :], raT[bq])
                P.dma("sp", S.ATT_T[4 + h // 2, num_lo:num_lo + 64, cs], aT[bq][num_lo:num_lo + 64, :],
                      reads=[raT[bq]], writes=[S.rATb[h][qb]], key=f"maT{bq}")

        LOOK = 2
        load_head(0)
        load_q(0)
        issued = {}
        loaded_q = {0}
        loaded_h = {0}

        def ensure_loads(g):
            if g >= len(groups):
                return
            h = groups[g][0]
            if h not in loaded_h:
                loaded_h.add(h)
                load_head(h)
            if g not in loaded_q:
                loaded_q.add(g)
                load_q(g)

        for i in range(min(LOOK, len(steps))):
            ensure_loads(steps[i][0])
            issued[i] = qk(*steps[i])
        for i, (g, st) in enumerate(steps):
            if i + LOOK < len(steps):
                ensure_loads(steps[i + LOOK][0])
                issued[i + LOOK] = qk(*steps[i + LOOK])
            pv(g, st, issued.pop(i))
            hh = groups[g][0]
            if st == 0 and groups[g][1] == 0 and hh + 1 < 8 and (hh + 1) not in loaded_h:
                loaded_h.add(hh + 1)
                load_head(hh + 1)
        return P.end_phase(ps)


def wout_phase(nc, P, c, T, S):
    NT = T // 128
    with ExitStack() as ps:
        def sb(name, shape, dt):
            return ps.enter_context(nc.sbuf_tensor(name, shape, dt))

        def pm(name, shape, dt):
            return ps.enter_context(nc.psum_tensor(name, shape, dt))

        R = P.res

        def dbl(name, shape, dt):
            return [sb(f"{name}{i}", shape, dt) for i in range(2)], [R() for _ in range(2)]

        Wo = sb("Wo", [128, 8, D], BF16)
        rWo = [R() for _ in range(8)]
        xt, rxt = dbl("wxt", [128, D], F32)
        at, rat = dbl("wat", [128, 8, 128], BF16)
        mmt = [pm(f"wmm{i}", [128, 512], F32) for i in range(4)]
        mmp = BankPool(mmt, [R() for _ in range(4)])
        load_weight_cast(P, c, Wo, rWo, S.w_out, 8, D, "o")
        for i in range(NT):
            b = i % 2
            rows = slice(i * 128, (i + 1) * 128)
            P.dma("sp", xt[b][:], S.X1[rows, :], reads=[S.rX1[i]], writes=[rxt[b]], key=f"wxt{b}")
            P.dma("sp", at[b][:], S.ATT_T[:, :, rows].rearrange("c p t -> p c t"),
                  reads=[S.rATa[i]] + [S.rATb[h][i // 4] for h in range(8)], writes=[rat[b]], key=f"wat{b}")
            for half in range(2):
                bk, rbk = mmp.next()
                for cc in range(8):
                    P.add("pe", lambda e, bk=bk, cc=cc, b=b, half=half: e.matmul(
                        bk[:, :], lhsT=at[b][:, cc, :], rhs=Wo[:, cc, half * 512:(half + 1) * 512],
                        start=(cc == 0), stop=(cc == 7)),
                          reads=[rat[b], rWo[cc]], writes=[rbk])
                P.add("dve", lambda e, bk=bk, b=b, half=half: e.tensor_tensor(
                    out=xt[b][:, half * 512:(half + 1) * 512], in0=bk[:, :], in1=xt[b][:, half * 512:(half + 1) * 512],
                    op=ALU.add),
                      reads=[rbk, rxt[b]], writes=[rxt[b]])
            P.dma("sp", S.X2[rows, :], xt[b][:], reads=[rxt[b]], writes=[S.rX2[i]], key=f"wxo{b}")
        return P.end_phase(ps)


IN_NAMES = ["x", "g_ffn1", "w1_gate", "w1_up", "w1_down", "g_mix", "w_in", "g_q_lat", "g_kv_lat", "w_uq", "w_ukv",
            "w_out", "g_ffn2", "w2_gate", "w2_up", "w2_down", "g_final"]
IN_SHAPES = {"g_ffn1": [D], "w1_gate": [D, DFF], "w1_up": [D, DFF], "w1_down": [DFF, D], "g_mix": [D],
             "w_in": [D, DPROJ], "g_q_lat": [384], "g_kv_lat": [256], "w_uq": [384, 768], "w_ukv": [256, 1024],
             "w_out": [D, D], "g_ffn2": [D], "w2_gate": [D, DFF], "w2_up": [D, DFF], "w2_down": [DFF, D],
             "g_final": [D]}


def build(T, debug=False, phases=("ffn1", "proj", "dsa", "mla", "wout", "ffn2")):
    NT = T // 128
    NQB = T // 512
    nc = bass.Bass("TRN2", target_bir_lowering=False)
    S = Ctx()
    x = nc.dram_tensor("x", [T, D], F32, kind="ExternalInput").ap()
    for n, shp in IN_SHAPES.items():
        setattr(S, n, nc.dram_tensor(n, list(shp), F32, kind="ExternalInput").ap())
    S.rope64 = nc.dram_tensor("rope64", [T, 128], F32, kind="ExternalInput").ap()
    S.rope32 = nc.dram_tensor("rope32", [T, 64], F32, kind="ExternalInput").ap()
    out = nc.dram_tensor("out", [T, D], F32, kind="ExternalOutput").ap()
    kind = "ExternalOutput" if debug else "Internal"

    def scr(name, shape, dt):
        return nc.dram_tensor(name, list(shape), dt, kind=kind).ap()

    S.X1 = scr("X1", [T, D], F32)
    S.X2 = scr("X2", [T, D], F32)
    S.QA_T = scr("QA_T", [NT, 128, 512], BF16)
    S.QI_T = scr("QI_T", [NT, 128, 1024], BF16)
    S.SG = scr("SG", [T, 16], F32)
    S.KA_T2 = scr("KA_T2", [128, T], BF16)
    S.KI_T2 = scr("KI_T2", [128, T], BF16)
    S.VA1 = scr("VA1", [T, 192], BF16)
    S.QB_T = scr("QB_T", [8, 128, T], BF16)
    S.KB_T = scr("KB_T", [8, 128, T], BF16)
    S.VB1 = scr("VB1", [T, 8, 192], BF16)
    S.ATT_T = scr("ATT_T", [8, 128, T], BF16)
    with ExitStack() as stack:
        P = Prog(nc, stack)
        c = Ctx()
        const_phase(nc, P, c, stack)
        rl = lambda: [P.res() for _ in range(NT)]
        rx = rl()
        rout = rl()
        S.rX1, S.rX2, S.rQA, S.rQI, S.rSG, S.rKA, S.rKI, S.rVA, S.rQB, S.rKB, S.rVB, S.rATa = [rl() for _ in range(12)]
        S.rATb = [[P.res() for _ in range(NQB)] for _ in range(8)]
        info = {}
        if "ffn1" in phases:
            info["ffn1"] = ffn_phase(nc, P, c, T, x, rx, S.X1, S.rX1, S.g_ffn1, S.w1_gate, S.w1_up, S.w1_down)
        if "proj" in phases:
            info["proj"] = proj_phase(nc, P, c, T, S)
        if "dsa" in phases:
            info["dsa"] = dsa_phase(nc, P, c, T, S)
        if "mla" in phases:
            info["mla"] = mla_phase(nc, P, c, T, S)
        if "wout" in phases:
            info["wout"] = wout_phase(nc, P, c, T, S)
        if "ffn2" in phases:
            info["ffn2"] = ffn_phase(nc, P, c, T, S.X2, S.rX2, out, rout, S.g_ffn2, S.w2_gate, S.w2_up, S.w2_down,
                                     final_g=S.g_final, tag="f2")
        nc._mk_info = (info, dict(P.cnt), max(P.dcnt.values()) if P.dcnt else 0)
    return nc


def rope_table(T, dim):
    pos = np.arange(T, dtype=np.float32)
    inv_freq = (np.float32(10000.0) ** (-np.arange(0, dim, 2, dtype=np.float32) / np.float32(dim))).astype(np.float32)
    ang = pos[:, None] * inv_freq[None, :]
    cs, sn = np.cos(ang).astype(np.float32), np.sin(ang).astype(np.float32)
    return np.concatenate([cs, cs, -sn, sn], axis=1).astype(np.float32)


_NC_CACHE = {}


def kernel(**inputs):
    x = np.ascontiguousarray(np.asarray(inputs["x"], dtype=np.float32))
    B, T, _ = x.shape
    if T not in _NC_CACHE:
        _NC_CACHE[T] = build(T)
    nc = _NC_CACHE[T]
    shared = {}
    for n, shp in IN_SHAPES.items():
        shared[n] = np.ascontiguousarray(np.asarray(inputs[n], dtype=np.float32).reshape(shp))
    shared["rope64"] = rope_table(T, 64)
    shared["rope32"] = rope_table(T, 32)
    in_maps = []
    for bi in range(B):
        m = dict(shared)
        m["x"] = x[bi]
        in_maps.append(m)
    res = run_bass_kernel_spmd(nc, in_maps, core_ids=list(range(B)))
    return np.stack([np.asarray(r["out"]) for r in res.results], axis=0).astype(np.float32)
```

```python
import math
from contextlib import ExitStack

import numpy as np
import concourse.bass as bass
import concourse.mybir as mybir
from concourse.bass_utils import run_bass_kernel_spmd

F32 = mybir.dt.float32
BF16 = mybir.dt.bfloat16
AF = mybir.ActivationFunctionType
ALU = mybir.AluOpType
AX = mybir.AxisListType

D = 1024
DFF = 2816
NFC = DFF // 128
EPS = 1e-6
ENGS = ("pe", "act", "dve", "pool", "sp")


class Res:
    __slots__ = ("name", "last_w", "readers")

    def __init__(self, name):
        self.name = name
        self.last_w = None
        self.readers = []


class Op:
    __slots__ = ("eng", "fn", "deps", "idx", "signal", "sem", "val", "is_dma", "waits", "key", "emitted")

    def __init__(self, eng, fn, is_dma=False):
        self.eng = eng
        self.fn = fn
        self.deps = []
        self.signal = False
        self.sem = None
        self.val = None
        self.is_dma = is_dma
        self.waits = []
        self.key = None
        self.emitted = False


class Prog:
    def __init__(self, nc, stack):
        self.nc = nc
        self.stack = stack
        self.ops = []
        self.n_total = 0
        self.eng_sem = {e: stack.enter_context(nc.semaphore("S_" + e)) for e in ENGS}
        self.cnt = {e: 0 for e in ENGS}
        self.dma_sem = {}
        self.dcnt = {}
        self.keymap = {}
        for e_, n_ in (("pool", 40), ("sp", 44)):
            for i_ in range(n_):
                self.dma_sem[(e_, i_)] = stack.enter_context(nc.semaphore("D_%s_%d" % (e_, i_)))
                self.dcnt[(e_, i_)] = 0
        self.block = stack.enter_context(nc.Block())
        self.waited = {e: {} for e in ENGS}
        self.phase_dmas = []
        self.last_op = {e: None for e in ENGS}

    def res(self, name="r"):
        return Res(name)

    def add(self, eng, fn, reads=(), writes=(), dma_key=None):
        op = Op(eng, fn, is_dma=dma_key is not None)
        op.idx = self.n_total
        self.n_total += 1
        seen = set()

        def dep(d):
            if d is None or d.idx in seen or d.emitted:
                return
            seen.add(d.idx)
            if d.eng == op.eng and not d.is_dma and not op.is_dma and d.eng == "pe":
                return
            op.deps.append(d)
            d.signal = True

        for r in reads:
            dep(r.last_w)
        for w in writes:
            dep(w.last_w)
            for rd in w.readers:
                dep(rd)
        for r in reads:
            r.readers.append(op)
        for w in writes:
            w.last_w = op
            w.readers = []
        if dma_key is not None:
            op.key = dma_key
            op.signal = True
            self.phase_dmas.append(op)
        self.ops.append(op)
        return op

    def dma(self, eng, out, in_, reads=(), writes=(), key=None):
        return self.add(eng, lambda e: e.dma_start(out=out, in_=in_), reads=reads, writes=writes, dma_key=key)

    def end_phase(self, pstack):
        nc = self.nc
        last = {}
        for op in self.ops:
            if op.fn is not None and not op.is_dma:
                last[op.eng] = op
        for e in ENGS:
            fin = self.add(e, None)
            fin.deps = [o for e2, o in last.items() if e2 != e] + list(self.phase_dmas)
            for o in fin.deps:
                o.signal = True
        self.phase_dmas = []
        for op in self.ops:
            if op.is_dma:
                km = self.keymap.setdefault(op.eng, {})
                if op.key not in km:
                    km[op.key] = len(km)
                k = (op.eng, km[op.key])
                assert k in self.dma_sem, ("out of preallocated DMA semaphores", k)
                self.dcnt[k] += 16
                op.sem = self.dma_sem[k]
                op.val = self.dcnt[k]
            elif op.signal:
                self.cnt[op.eng] += 1
                op.sem = self.eng_sem[op.eng]
                op.val = self.cnt[op.eng]
        for op in self.ops:
            need = {}
            w = self.waited[op.eng]
            for d in op.deps:
                key = id(d.sem)
                if w.get(key, 0) >= d.val:
                    continue
                if key not in need or need[key][1] < d.val:
                    need[key] = (d.sem, d.val)
            for key, (s, v) in need.items():
                w[key] = v
                op.waits.append((s, v))
        per_eng = {e: [op for op in self.ops if op.eng == e] for e in ENGS}
        block = self.block
        handles = {"pe": block.tensor, "act": block.scalar, "dve": block.vector,
                   "pool": block.gpsimd, "sp": block.sync}

        def make(e):
            def body(eng):
                for op in per_eng[e]:
                    for (s, v) in op.waits:
                        eng.wait_ge(s, v)
                    if op.fn is None:
                        continue
                    ins = op.fn(eng)
                    if op.signal:
                        ins.then_inc(op.sem, 16 if op.is_dma else 1)
            return body

        for e in ENGS:
            if per_eng[e]:
                handles[e](make(e))
        n = len(self.ops)
        for op in self.ops:
            op.emitted = True
            op.fn = None
        self.ops = []
        self.keymap = {}
        return n


def bcast_rows(vec_ap, n):
    return bass.AP(tensor=vec_ap.tensor, offset=vec_ap.offset, ap=[[0, 128], [1, n]])


class Ctx:
    pass


def load_weight_cast(P, c, dst_tile, dst_res_list, src_ap, nk, ncols, name, col_perm=None):
    j = 0
    for k in range(nk):
        for c0 in range(0, ncols, 512):
            cw = min(512, ncols - c0)
            P.dma("pool", dst_tile[:, k, c0:c0 + cw], src_ap[k * 128:(k + 1) * 128, c0:c0 + cw],
                  writes=[dst_res_list[k]], key=f"w{name}_{k}")
            j += 1


def rmsnorm_to_bf16(P, c, x_ap, x_res, n, g_bc, g_res, junk, junk_res, ss, ss_res, rstd, rstd_res, out_ap, out_res,
                    x_in_psum=False):
    P.add("act", lambda e: e.activation(out=junk, in_=x_ap, func=AF.Square, accum_out=ss),
          reads=[x_res], writes=[junk_res, ss_res])
    P.add("act", lambda e: e.activation(out=rstd, in_=ss, func=AF.Sqrt, scale=1.0 / n, bias=c.eps_t[:, 0:1]),
          reads=[ss_res, c.rconst], writes=[rstd_res])
    P.add("dve", lambda e: e.reciprocal(out=rstd, in_=rstd), reads=[rstd_res], writes=[rstd_res])
    P.add("dve", lambda e: e.scalar_tensor_tensor(out=out_ap, in0=x_ap, scalar=rstd, in1=g_bc,
                                                  op0=ALU.mult, op1=ALU.mult),
          reads=[x_res, rstd_res, g_res], writes=[out_res])


def ffn_phase(nc, P, c, T, src, src_res, dst, dst_res, g_vec, wg, wu, wd, final_g=None, tag="f1"):
    NT = T // 128
    with ExitStack() as ps:
        def sb(name, shape, dt):
            return ps.enter_context(nc.sbuf_tensor(tag + name, shape, dt))

        def pm(name, shape, dt):
            return ps.enter_context(nc.psum_tensor(tag + name, shape, dt))

        Wg = sb("Wg", [128, 8, DFF], BF16)
        Wu = sb("Wu", [128, 8, DFF], BF16)
        Wd = sb("Wd", [128, NFC, D], BF16)
        gbc = sb("gbc", [128, D], F32)
        gfin = sb("gfin", [128, D], F32) if final_g is not None else None
        xt = [sb(f"xt{i}", [128, D], F32) for i in range(3)]
        junk = [sb(f"junk{i}", [128, D], BF16) for i in range(2)]
        ss = [sb(f"ss{i}", [128, 4], F32) for i in range(2)]
        hb = [sb(f"hb{i}", [128, D], BF16) for i in range(2)]
        hT = [sb(f"hT{i}", [128, 8, 128], BF16) for i in range(2)]
        sg = [sb(f"sg{i}", [128, 512], F32) for i in range(2)]
        act = [sb(f"act{i}", [128, DFF], BF16) for i in range(2)]
        actT = [sb(f"actT{i}", [128, NFC, 128], BF16) for i in range(2)]
        yo = [sb(f"yo{i}", [128, D], F32) for i in range(2)] if final_g is not None else None
        tp = [pm(f"tp{i}", [128, 1024], BF16) for i in range(2)]
        mm = [pm(f"mm{i}", [128, 512], F32) for i in range(6)]

        R = P.res
        rWg = [R() for _ in range(8)]
        rWu = [R() for _ in range(8)]
        rWd = [R() for _ in range(NFC)]
        rg, rgf = R(), R()
        rxt = [R() for _ in range(3)]
        rjunk = [R() for _ in range(2)]
        rss = [R() for _ in range(2)]
        rrs = [R() for _ in range(2)]
        rhb = [R() for _ in range(2)]
        rhT = [R() for _ in range(2)]
        rsg = [R() for _ in range(2)]
        ract = [R() for _ in range(2)]
        ractT = [R() for _ in range(2)]
        ryo = [R() for _ in range(2)]
        rtp = [R() for _ in range(2)]
        rmm = [R() for _ in range(6)]

        P.dma("sp", gbc[:], bcast_rows(g_vec, D), writes=[rg], key="gbc")
        if final_g is not None:
            P.dma("sp", gfin[:], bcast_rows(final_g, D), writes=[rgf], key="gfin")
        load_weight_cast(P, c, Wg, rWg, wg, 8, DFF, "g")
        load_weight_cast(P, c, Wu, rWu, wu, 8, DFF, "u")
        load_weight_cast(P, c, Wd, rWd, wd, NFC, D, "d")

        slabs = [(s0, min(512, DFF - s0)) for s0 in range(0, DFF, 512)]
        cn = {"mmi": 0, "tpi": 0}

        def stage1(i):
            b = i % 2
            b3 = i % 3
            rows = slice(i * 128, (i + 1) * 128)
            P.dma("sp", xt[b3][:], src[rows, :], reads=[src_res[i]], writes=[rxt[b3]], key=f"xt{b3}")
            rmsnorm_to_bf16(P, c, xt[b3][:], rxt[b3], D, gbc[:], rg, junk[b][:], rjunk[b], ss[b][:, 0:1], rss[b],
                            ss[b][:, 1:2], rrs[b], hb[b][:], rhb[b])
            t = cn['tpi'] % 2
            cn['tpi'] += 1
            for k in range(8):
                P.add("pe", lambda e, k=k, t=t, b=b, b3=b3: e.transpose(out=tp[t][:, k * 128:(k + 1) * 128],
                                                                in_=hb[b][:, k * 128:(k + 1) * 128],
                                                                identity=c.ident[:]),
                      reads=[rhb[b], c.rident], writes=[rtp[t]])
            P.add("act", lambda e, t=t, b=b, b3=b3: e.activation(out=hT[b][:].rearrange("p k t -> p (k t)"),
                                                          in_=tp[t][:], func=AF.Copy),
                  reads=[rtp[t]], writes=[rhT[b]])
            for si, (s0, sw) in enumerate(slabs):
                ga = cn['mmi'] % 6
                ua = (cn['mmi'] + 1) % 6
                cn['mmi'] += 2
                for k in range(8):
                    P.add("pe", lambda e, k=k, ga=ga, b=b, b3=b3, s0=s0, sw=sw: e.matmul(
                        mm[ga][:, 0:sw], lhsT=hT[b][:, k, :], rhs=Wg[:, k, s0:s0 + sw],
                        start=(k == 0), stop=(k == 7)),
                          reads=[rhT[b], rWg[k]], writes=[rmm[ga]])
                for k in range(8):
                    P.add("pe", lambda e, k=k, ua=ua, b=b, b3=b3, s0=s0, sw=sw: e.matmul(
                        mm[ua][:, 0:sw], lhsT=hT[b][:, k, :], rhs=Wu[:, k, s0:s0 + sw],
                        start=(k == 0), stop=(k == 7)),
                          reads=[rhT[b], rWu[k]], writes=[rmm[ua]])
                s2 = si % 2
                P.add("act", lambda e, ga=ga, s2=s2, sw=sw: e.activation(out=sg[s2][:, 0:sw], in_=mm[ga][:, 0:sw],
                                                                        func=AF.Silu),
                      reads=[rmm[ga]], writes=[rsg[s2]])
                P.add("dve", lambda e, ua=ua, s2=s2, b=b, b3=b3, s0=s0, sw=sw: e.tensor_tensor(
                    out=act[b][:, s0:s0 + sw], in0=sg[s2][:, 0:sw], in1=mm[ua][:, 0:sw], op=ALU.mult),
                      reads=[rsg[s2], rmm[ua]], writes=[ract[b]])

        def stage2(i):
            b = i % 2
            b3 = i % 3
            rows = slice(i * 128, (i + 1) * 128)
            for f0 in range(0, NFC, 8):
                nf = min(8, NFC - f0)
                t = cn['tpi'] % 2
                cn['tpi'] += 1
                for f in range(nf):
                    P.add("pe", lambda e, f=f, f0=f0, t=t, b=b, b3=b3: e.transpose(
                        out=tp[t][:, f * 128:(f + 1) * 128],
                        in_=act[b][:, (f0 + f) * 128:(f0 + f + 1) * 128], identity=c.ident[:]),
                          reads=[ract[b], c.rident], writes=[rtp[t]])
                P.add("act" if (f0 // 8) % 2 == 0 else "dve",
                      (lambda e, t=t, b=b, b3=b3, f0=f0, nf=nf: e.activation(
                          out=actT[b][:, f0:f0 + nf, :].rearrange("p k t -> p (k t)"),
                          in_=tp[t][:, 0:nf * 128], func=AF.Copy)) if (f0 // 8) % 2 == 0 else
                      (lambda e, t=t, b=b, b3=b3, f0=f0, nf=nf: e.tensor_copy(
                          out=actT[b][:, f0:f0 + nf, :].rearrange("p k t -> p (k t)"),
                          in_=tp[t][:, 0:nf * 128])),
                      reads=[rtp[t]], writes=[ractT[b]])
            for half in range(2):
                da = cn['mmi'] % 6
                cn['mmi'] += 1
                for f in range(NFC):
                    P.add("pe", lambda e, f=f, da=da, b=b, b3=b3, half=half: e.matmul(
                        mm[da][:, :], lhsT=actT[b][:, f, :], rhs=Wd[:, f, half * 512:(half + 1) * 512],
                        start=(f == 0), stop=(f == NFC - 1)),
                          reads=[ractT[b], rWd[f]], writes=[rmm[da]])
                P.add("dve", lambda e, da=da, b=b, b3=b3, half=half: e.scalar_tensor_tensor(
                    out=xt[b3][:, half * 512:(half + 1) * 512], in0=mm[da][:, :], scalar=0.5,
                    in1=xt[b3][:, half * 512:(half + 1) * 512], op0=ALU.mult, op1=ALU.add),
                      reads=[rmm[da], rxt[b3]], writes=[rxt[b3]])
            if final_g is None:
                P.dma("sp", dst[rows, :], xt[b3][:], reads=[rxt[b3]], writes=[dst_res[i]], key=f"xo{b3}")
            else:
                P.add("act", lambda e, b=b, b3=b3: e.activation(out=junk[b][:], in_=xt[b3][:], func=AF.Square,
                                                         accum_out=ss[b][:, 2:3]),
                      reads=[rxt[b3]], writes=[rjunk[b], rss[b]])
                P.add("act", lambda e, b=b, b3=b3: e.activation(out=ss[b][:, 3:4], in_=ss[b][:, 2:3], func=AF.Sqrt,
                                                         scale=1.0 / D, bias=c.eps_t[:, 0:1]),
                      reads=[rss[b], c.rconst], writes=[rrs[b]])
                P.add("dve", lambda e, b=b, b3=b3: e.reciprocal(out=ss[b][:, 3:4], in_=ss[b][:, 3:4]),
                      reads=[rrs[b]], writes=[rrs[b]])
                P.add("dve", lambda e, b=b, b3=b3: e.scalar_tensor_tensor(out=yo[b][:], in0=xt[b3][:], scalar=ss[b][:, 3:4],
                                                                   in1=gfin[:], op0=ALU.mult, op1=ALU.mult),
                      reads=[rxt[b3], rrs[b], rgf], writes=[ryo[b]])
                P.dma("sp", dst[rows, :], yo[b][:], reads=[ryo[b]], writes=[dst_res[i]], key=f"yo{b}")

        stage1(0)
        for i in range(NT):
            if i + 1 < NT:
                stage1(i + 1)
            stage2(i)
        return P.end_phase(ps)


def const_phase(nc, P, c, stack):
    c.ident = stack.enter_context(nc.sbuf_tensor("ident", [128, 128], BF16))
    c.identf = stack.enter_context(nc.sbuf_tensor("identf", [128, 128], F32))
    c.eps_t = stack.enter_context(nc.sbuf_tensor("eps_t", [128, 1], F32))
    c.rident = P.res()
    c.rconst = P.res()
    P.add("pool", lambda e: e.memset(c.identf[:], 0.0), writes=[c.rident])
    P.add("pool", lambda e: e.affine_select(out=c.identf[:], in_=c.identf[:], pattern=[[-1, 128]],
                                            compare_op=ALU.not_equal, fill=1.0, base=0, channel_multiplier=1),
          reads=[c.rident], writes=[c.rident])
    P.add("pool", lambda e: e.tensor_copy(out=c.ident[:], in_=c.identf[:]), reads=[c.rident], writes=[c.rident])
    P.add("pool", lambda e: e.memset(c.eps_t[:], EPS), writes=[c.rconst])


def bc_mid(ap2d, H):
    a = [list(x) for x in ap2d.ap]
    return bass.AP(tensor=ap2d.tensor, offset=ap2d.offset, ap=[a[0], [0, H], a[1]])


def bc_last(ap2d, w):
    a = [list(x) for x in ap2d.ap]
    return bass.AP(tensor=ap2d.tensor, offset=ap2d.offset, ap=[a[0], a[1], [0, w]])


class BankPool:
    def __init__(self, tiles, res):
        self.tiles = tiles
        self.res = res
        self.i = 0

    def next(self):
        k = self.i % len(self.tiles)
        self.i += 1
        return self.tiles[k], self.res[k]


def transposes_to(P, c, tpp, srcs, src_res, dst_ap, dst_res, np_out=128, copy_eng="act"):
    tp, rtp = tpp.next()
    n = len(srcs)
    for j, s_ap in enumerate(srcs):
        P.add("pe", lambda e, j=j, s_ap=s_ap: e.transpose(out=tp[0:np_out, j * 128:(j + 1) * 128], in_=s_ap,
                                                         identity=c.ident[:]),
              reads=list(src_res) + [c.rident], writes=[rtp])
    if copy_eng == "act":
        P.add("act", lambda e: e.activation(out=dst_ap, in_=tp[0:np_out, 0:n * 128], func=AF.Copy),
              reads=[rtp], writes=[dst_res])
    else:
        P.add("dve", lambda e: e.tensor_copy(out=dst_ap, in_=tp[0:np_out, 0:n * 128]),
              reads=[rtp], writes=[dst_res])


def rope_ops(P, c, src3, src_res, H, d, tab, tab_res, ta, rta, tb, rtb, out3, out_res, out3b=None):
    h2 = d // 2
    cc = bc_mid(tab[:, 0:d], H)
    s0 = bc_mid(tab[:, d:d + h2], H)
    s1 = bc_mid(tab[:, d + h2:2 * d], H)
    P.add("dve", lambda e: e.tensor_tensor(out=ta, in0=src3, in1=cc, op=ALU.mult),
          reads=[src_res, tab_res], writes=[rta])
    P.add("dve", lambda e: e.tensor_tensor(out=tb[:, :, 0:h2], in0=src3[:, :, h2:d], in1=s0, op=ALU.mult),
          reads=[src_res, tab_res], writes=[rtb])
    P.add("dve", lambda e: e.tensor_tensor(out=tb[:, :, h2:d], in0=src3[:, :, 0:h2], in1=s1, op=ALU.mult),
          reads=[src_res, tab_res], writes=[rtb])
    P.add("pool", lambda e: e.tensor_tensor(out=out3, in0=ta, in1=tb, op=ALU.add),
          reads=[rta, rtb], writes=[out_res])
    if out3b is not None:
        P.add("pool", lambda e: e.tensor_tensor(out=out3b, in0=ta, in1=tb, op=ALU.add),
              reads=[rta, rtb], writes=[out_res])


WIN_SEGS = [
    (0, 0, 512),
    (512, 640, 512),
    (1024, 1152, 512),
    (1536, 1744, 384),
    (1920, 512, 64),
    (1984, 1664, 64),
    (2048, 2128, 256),
    (2304, 2384, 32),
    (2336, 576, 64),
    (2400, 1728, 16),
]
WIN_GROUPS = [(0, 512), (512, 512), (1024, 512), (1536, 512), (2048, 368)]
DPROJ = 2416


def proj_phase(nc, P, c, T, S):
    import os
    LIM = float(os.environ.get('PROJ_STOP', '99'))
    NT = T // 128
    with ExitStack() as ps:
        def sb(name, shape, dt):
            return ps.enter_context(nc.sbuf_tensor(name, shape, dt))

        def pm(name, shape, dt):
            return ps.enter_context(nc.psum_tensor(name, shape, dt))

        R = P.res
        Win = sb("Win", [128, 8, DPROJ], BF16)
        Wuq = sb("Wuq", [128, 3, 768], BF16)
        Wukv = sb("Wukv", [128, 2, 1024], BF16)
        gbc = sb("gbcm", [128, D], F32)
        gq = sb("gq", [128, 384], F32)
        gkv = sb("gkv", [128, 256], F32)
        rWin = [R() for _ in range(8)]
        rWuq = [R() for _ in range(3)]
        rWukv = [R() for _ in range(2)]
        rg, rgq, rgkv = R(), R(), R()

        def dbl(name, shape, dt):
            return [sb(f"{name}{i}", shape, dt) for i in range(2)], [R() for _ in range(2)]

        xt, rxt = dbl("pxt", [128, D], F32)
        junk, rjunk = dbl("pjunk", [128, D], BF16)
        ss, rss = dbl("pss", [128, 8], F32)
        rrs = [[R() for _ in range(3)] for _ in range(2)]
        hb, rhb = dbl("phb", [128, D], BF16)
        hT, rhT = dbl("phT", [128, 8, 128], BF16)
        r64, rr64 = dbl("r64", [128, 128], F32)
        r32, rr32 = dbl("r32", [128, 64], F32)
        ta, rta = dbl("ta", [128, 512], F32)
        tb, rtb = dbl("tb", [128, 512], F32)
        qar, rqar = dbl("qar", [128, 512], BF16)
        qir, rqir = dbl("qir", [128, 1024], BF16)
        qif, rqif = dbl("qif", [128, 512], F32)
        qaT, rqaT = dbl("qaT", [128, 512], BF16)
        qiT, rqiT = dbl("qiT", [128, 1024], BF16)
        aw, raw = dbl("aw", [128, 16], F32)
        sgt, rsgt = dbl("sgt", [128, 16], F32)
        kdup, rkdup = dbl("kdup", [128, 256], BF16)
        kT, rkT = dbl("kT", [128, 256], BF16)
        kpe, rkpe = dbl("kpe", [128, 32], BF16)
        va1, rva1 = dbl("va1", [128, 192], BF16)
        cqn, rcqn = dbl("cqn", [128, 384], BF16)
        cqT, rcqT = dbl("cqT", [128, 384], BF16)
        ckn, rckn = dbl("ckn", [128, 256], BF16)
        ckT, rckT = dbl("ckT", [128, 256], BF16)
        qbr, rqbr = dbl("qbr", [128, 8, 128], BF16)
        kbr, rkbr = dbl("kbr", [128, 8, 128], BF16)
        qbT, rqbT = dbl("qbT", [128, 1024], BF16)
        kbT, rkbT = dbl("kbT", [128, 1024], BF16)
        vb1, rvb1 = dbl("vb1", [128, 8, 192], BF16)
        tpt = [pm(f"ptp{i}", [128, 1024], BF16) for i in range(2)]
        tpp = BankPool(tpt, [R() for _ in range(2)])
        mmt = [pm(f"pmm{i}", [128, 512], F32) for i in range(6)]
        mmp = BankPool(mmt, [R() for _ in range(6)])

        P.dma("sp", gbc[:], bcast_rows(S.g_mix, D), writes=[rg], key="gbc")
        P.dma("sp", gq[:], bcast_rows(S.g_q_lat, 384), writes=[rgq], key="gq")
        P.dma("sp", gkv[:], bcast_rows(S.g_kv_lat, 256), writes=[rgkv], key="gkv")
        for k in range(8):
            for (dc, sc, w) in WIN_SEGS:
                P.dma("pool", Win[:, k, dc:dc + w], S.w_in[k * 128:(k + 1) * 128, sc:sc + w],
                      writes=[rWin[k]], key=f"wg_{k}")
        for k in range(3):
            for c0 in (0, 384):
                P.dma("pool", Wuq[:, k, c0:c0 + 384], S.w_uq[k * 128:(k + 1) * 128, c0:c0 + 384],
                      writes=[rWuq[k]], key=f"wu_{k}")
        for k in range(2):
            for c0 in (0, 512):
                P.dma("pool", Wukv[:, k, c0:c0 + 512], S.w_ukv[k * 128:(k + 1) * 128, c0:c0 + 512],
                      writes=[rWukv[k]], key=f"wd_{k}")
        for b in range(2):
            P.add("pool", lambda e, b=b: e.memset(qbr[b][:], 0.0), writes=[rqbr[b]])
            P.add("pool", lambda e, b=b: e.memset(kbr[b][:], 0.0), writes=[rkbr[b]])
            P.add("pool", lambda e, b=b: e.memset(va1[b][:], 1.0), writes=[rva1[b]])
            P.add("pool", lambda e, b=b: e.memset(vb1[b][:], 1.0), writes=[rvb1[b]])

        for i in range(NT):
            b = i % 2
            rows = slice(i * 128, (i + 1) * 128)
            cols = slice(i * 128, (i + 1) * 128)
            P.dma("sp", xt[b][:], S.X1[rows, :], reads=[S.rX1[i]], writes=[rxt[b]], key=f"xt{b}")
            P.dma("sp", r64[b][:], S.rope64[rows, :], writes=[rr64[b]], key=f"r64{b}")
            P.dma("sp", r32[b][:], S.rope32[rows, :], writes=[rr32[b]], key=f"r32{b}")
            rmsnorm_to_bf16(P, c, xt[b][:], rxt[b], D, gbc[:], rg, junk[b][:], rjunk[b], ss[b][:, 0:1], rss[b],
                            ss[b][:, 1:2], rrs[b][0], hb[b][:], rhb[b])
            transposes_to(P, c, tpp, [hb[b][:, k * 128:(k + 1) * 128] for k in range(8)], [rhb[b]],
                          hT[b][:].rearrange("p k t -> p (k t)"), rhT[b])
            if LIM < 2:
                continue
            banks = []
            for (g0, gw) in WIN_GROUPS:
                bk, rbk = mmp.next()
                for k in range(8):
                    P.add("pe", lambda e, k=k, bk=bk, g0=g0, gw=gw, b=b: e.matmul(
                        bk[:, 0:gw], lhsT=hT[b][:, k, :], rhs=Win[:, k, g0:g0 + gw], start=(k == 0), stop=(k == 7)),
                          reads=[rhT[b], rWin[k]], writes=[rbk])
                banks.append((bk, rbk))
            (B0, rB0), (B1, rB1), (B2, rB2), (B3, rB3), (B4, rB4) = banks
            v3 = lambda ap, H: ap.rearrange("p (h d) -> p h d", h=H)
            if LIM < 3:
                continue
            P.add("act", lambda e, b=b, B4=B4: e.activation(out=aw[b][:], in_=B4[:, 352:368], func=AF.Abs,
                                                           scale=1.0 / 32.0),
                  reads=[rB4], writes=[raw[b]])
            P.add("act", lambda e, b=b, B4=B4: e.activation(out=sgt[b][:], in_=B4[:, 352:368], func=AF.Sign),
                  reads=[rB4], writes=[rsgt[b]])
            P.dma("sp", S.SG[rows, :], sgt[b][:], reads=[rsgt[b]], writes=[S.rSG[i]], key=f"sgt{b}")
            if LIM < 4:
                continue
            rope_ops(P, c, v3(B0[:, 0:512], 8), rB0, 8, 64, r64[b], rr64[b], v3(ta[b][:], 8), rta[b],
                     v3(tb[b][:], 8), rtb[b], v3(qar[b][:], 8), rqar[b])
            transposes_to(P, c, tpp, [qar[b][:, j * 128:(j + 1) * 128] for j in range(4)], [rqar[b]],
                          qaT[b][:], rqaT[b], copy_eng="dve")
            P.dma("sp", S.QA_T[i], qaT[b][:], reads=[rqaT[b]], writes=[S.rQA[i]], key=f"qaT{b}")
            if LIM < 5:
                continue
            for hh, (Bq, rBq) in enumerate(((B1, rB1), (B2, rB2))):
                rope_ops(P, c, v3(Bq[:, 0:512], 8), rBq, 8, 64, r64[b], rr64[b], v3(ta[b][:], 8), rta[b],
                         v3(tb[b][:], 8), rtb[b], v3(qif[b][:], 8), rqif[b])
                P.add("dve", lambda e, b=b, hh=hh: e.tensor_tensor(
                    out=v3(qir[b][:, hh * 512:(hh + 1) * 512], 8), in0=v3(qif[b][:], 8),
                    in1=bc_last(aw[b][:, hh * 8:(hh + 1) * 8], 64), op=ALU.mult),
                      reads=[rqif[b], raw[b]], writes=[rqir[b]])
            transposes_to(P, c, tpp, [qir[b][:, j * 128:(j + 1) * 128] for j in range(8)], [rqir[b]],
                          qiT[b][:], rqiT[b])
            P.dma("sp", S.QI_T[i], qiT[b][:], reads=[rqiT[b]], writes=[S.rQI[i]], key=f"qiT{b}")
            if LIM < 6:
                continue
            kd4 = kdup[b][:].rearrange("p (a r d) -> p a r d", a=2, r=2)
            rope_ops(P, c, v3(B3[:, 384:512], 2), rB3, 2, 64, r64[b], rr64[b], v3(ta[b][:, 0:128], 2), rta[b],
                     v3(tb[b][:, 0:128], 2), rtb[b], kd4[:, :, 0, :], rkdup[b], out3b=kd4[:, :, 1, :])
            transposes_to(P, c, tpp, [kdup[b][:, 0:128], kdup[b][:, 128:256]], [rkdup[b]], kT[b][:], rkT[b],
                          copy_eng="dve")
            P.dma("sp", S.KA_T2[:, cols], kT[b][:, 0:128], reads=[rkT[b]], writes=[S.rKA[i]], key=f"kTa{b}")
            P.dma("sp", S.KI_T2[:, cols], kT[b][:, 128:256], reads=[rkT[b]], writes=[S.rKI[i]], key=f"kTi{b}")
            if LIM < 7:
                continue
            rope_ops(P, c, v3(B4[:, 256:288], 1), rB4, 1, 32, r32[b], rr32[b], v3(ta[b][:, 0:32], 1), rta[b],
                     v3(tb[b][:, 0:32], 1), rtb[b], v3(kpe[b][:], 1), rkpe[b])
            if LIM < 8:
                continue
            P.add("act", lambda e, b=b, B4=B4: e.activation(out=va1[b][:, 64:128], in_=B4[:, 288:352], func=AF.Copy),
                  reads=[rB4], writes=[rva1[b]])
            P.dma("sp", S.VA1[rows, :], va1[b][:], reads=[rva1[b]], writes=[S.rVA[i]], key=f"va1{b}")
            if LIM < 9:
                continue
            rmsnorm_to_bf16(P, c, B3[:, 0:384], rB3, 384, gq[:], rgq, junk[b][:, 0:384], rjunk[b], ss[b][:, 2:3],
                            rss[b], ss[b][:, 3:4], rrs[b][1], cqn[b][:], rcqn[b])
            if LIM < 9.1:
                continue
            transposes_to(P, c, tpp, [cqn[b][:, k * 128:(k + 1) * 128] for k in range(3)], [rcqn[b]],
                          cqT[b][:], rcqT[b], copy_eng="dve")
            if LIM < 9.2:
                continue
            for (q0, qw, h0, nh) in ((0, 480, 0, 5), (480, 288, 5, 3)):
                bk, rbk = mmp.next()
                for k in range(3):
                    P.add("pe", lambda e, k=k, bk=bk, q0=q0, qw=qw, b=b: e.matmul(
                        bk[:, 0:qw], lhsT=cqT[b][:, k * 128:(k + 1) * 128], rhs=Wuq[:, k, q0:q0 + qw],
                        start=(k == 0), stop=(k == 2)),
                          reads=[rcqT[b], rWuq[k]], writes=[rbk])
                if LIM < 9.3:
                    continue
                bv = bk[:, 0:qw].rearrange("p (h d) -> p h d", h=nh)
                P.add("dve", lambda e, bv=bv, b=b, h0=h0, nh=nh: e.tensor_copy(
                    out=qbr[b][:, h0:h0 + nh, 0:64], in_=bv[:, :, 0:64]),
                      reads=[rbk], writes=[rqbr[b]])
                if LIM < 9.4:
                    continue
                rope_ops(P, c, bv[:, :, 64:96], rbk, nh, 32, r32[b], rr32[b],
                         ta[b][:, 0:nh * 32].rearrange("p (h d) -> p h d", h=nh), rta[b],
                         tb[b][:, 0:nh * 32].rearrange("p (h d) -> p h d", h=nh), rtb[b],
                         qbr[b][:, h0:h0 + nh, 64:96], rqbr[b])
            if LIM < 9.5:
                continue
            transposes_to(P, c, tpp, [qbr[b][:, h, :] for h in range(8)], [rqbr[b]], qbT[b][:], rqbT[b])
            if LIM < 9.6:
                continue
            P.dma("sp", S.QB_T[:, :, cols].rearrange("h p t -> p h t"),
                  qbT[b][:].rearrange("p (h t) -> p h t", h=8), reads=[rqbT[b]], writes=[S.rQB[i]], key=f"qbT{b}")
            if LIM < 10:
                continue
            rmsnorm_to_bf16(P, c, B4[:, 0:256], rB4, 256, gkv[:], rgkv, junk[b][:, 0:256], rjunk[b], ss[b][:, 4:5],
                            rss[b], ss[b][:, 5:6], rrs[b][2], ckn[b][:], rckn[b])
            transposes_to(P, c, tpp, [ckn[b][:, k * 128:(k + 1) * 128] for k in range(2)], [rckn[b]],
                          ckT[b][:], rckT[b], copy_eng="dve")
            P.add("pool", lambda e, b=b: e.tensor_copy(out=kbr[b][:, :, 64:96], in_=bc_mid(kpe[b][:], 8)),
                  reads=[rkpe[b]], writes=[rkbr[b]])
            for hf in range(2):
                bk, rbk = mmp.next()
                for k in range(2):
                    P.add("pe", lambda e, k=k, bk=bk, hf=hf, b=b: e.matmul(
                        bk[:, :], lhsT=ckT[b][:, k * 128:(k + 1) * 128], rhs=Wukv[:, k, hf * 512:(hf + 1) * 512],
                        start=(k == 0), stop=(k == 1)),
                          reads=[rckT[b], rWukv[k]], writes=[rbk])
                bv = bk[:, :].rearrange("p (h d) -> p h d", h=4)
                P.add("dve", lambda e, bv=bv, b=b, hf=hf: e.tensor_copy(
                    out=kbr[b][:, hf * 4:(hf + 1) * 4, 0:64], in_=bv[:, :, 0:64]),
                      reads=[rbk], writes=[rkbr[b]])
                P.add("dve", lambda e, bv=bv, b=b, hf=hf: e.tensor_copy(
                    out=vb1[b][:, hf * 4:(hf + 1) * 4, 64:128], in_=bv[:, :, 64:128]),
                      reads=[rbk], writes=[rvb1[b]])
            transposes_to(P, c, tpp, [kbr[b][:, h, :] for h in range(8)], [rkbr[b]], kbT[b][:], rkbT[b])
            P.dma("sp", S.KB_T[:, :, cols].rearrange("h p t -> p h t"),
                  kbT[b][:].rearrange("p (h t) -> p h t", h=8), reads=[rkbT[b]], writes=[S.rKB[i]], key=f"kbT{b}")
            P.dma("sp", S.VB1[rows, :, :], vb1[b][:], reads=[rvb1[b]], writes=[S.rVB[i]], key=f"vb1{b}")
        return P.end_phase(ps)


NEG = -1.0e30
NBIS = 16


def normalize_out(P, c, O, rO, num_lo, rc, rrc, out_ap, out_res):
    den_lo = 64 - num_lo
    P.add("dve", lambda e: e.reciprocal(out=rc[den_lo:den_lo + 64, :], in_=O[den_lo:den_lo + 64, :]),
          reads=[rO], writes=[rrc])
    P.add("dve", lambda e: e.tensor_tensor(out=out_ap, in0=O[num_lo:num_lo + 64, :],
                                           in1=rc[den_lo:den_lo + 64, :], op=ALU.mult),
          reads=[rO, rrc], writes=[out_res])


def dsa_phase(nc, P, c, T, S):
    NT = T // 128
    TOPK = min(256, T // 4)
    QT0 = TOPK // 128
    with ExitStack() as ps:
        def sb(name, shape, dt):
            return ps.enter_context(nc.sbuf_tensor(name, shape, dt))

        def pm(name, shape, dt):
            return ps.enter_context(nc.psum_tensor(name, shape, dt))

        R = P.res

        def dbl(name, shape, dt):
            return [sb(f"{name}{i}", shape, dt) for i in range(2)], [R() for _ in range(2)]

        KA2 = sb("KA2", [128, T], BF16)
        KI2 = sb("KI2", [128, T], BF16)
        VAs = sb("VAs", [128, NT, 192], BF16)
        rKA2 = [R() for _ in range(NT)]
        rKI2 = [R() for _ in range(NT)]
        rVAs = [R() for _ in range(NT)]
        cneg = sb("cneg", [128, 128], F32)
        pow2 = sb("pow2", [128, NBIS], F32)
        rcn = R()
        qiT, rqiT = dbl("dqiT", [128, 1024], BF16)
        qaT, rqaT = dbl("dqaT", [128, 512], BF16)
        sg, rsg = dbl("dsg", [128, 16], F32)
        Rt = [sb(f"Rt{i}", [128, 512], BF16) for i in range(4)]
        Dg, rDg = dbl("Dg", [128, 16, 128], BF16)
        rRt = [R() for _ in range(4)]
        Isb, rIsb = dbl("Isb", [128, T], F32)
        cjunk = sb("cjunk", [128, T], BF16)
        rcj = R()
        st_, rst = dbl("dst", [128, 8 + NBIS], F32)
        maskq, rmq = dbl("maskq", [128, T], BF16)
        maskT, rmT = dbl("maskT", [128, NT, 128], BF16)
        PT = [sb(f"PT{i}", [128, 1024], BF16) for i in range(4)]
        rPT = [R() for _ in range(4)]
        rc, rrc = dbl("drc", [128, 512], F32)
        aT, raT = dbl("daT", [128, 512], BF16)
        Lt = [pm(f"dL{i}", [128, 512], F32) for i in range(4)]
        Lp = BankPool(Lt, [R() for _ in range(4)])
        At = [pm(f"dA{i}", [128, 512], F32) for i in range(2)]
        rAt = [R() for _ in range(2)]
        OE = pm("dOE", [128, 512], F32)
        OO = pm("dOO", [128, 512], F32)
        rOE, rOO = R(), R()

        P.add("pool", lambda e: e.memset(cneg[:], 0.0), writes=[rcn])
        P.add("pool", lambda e: e.affine_select(out=cneg[:], in_=cneg[:], pattern=[[-1, 128]],
                                                compare_op=ALU.is_ge, fill=NEG, base=0, channel_multiplier=1),
              reads=[rcn], writes=[rcn])
        for k in range(NBIS):
            P.add("pool", lambda e, k=k: e.memset(pow2[:, k:k + 1], 2.0 ** (-(k + 1))), writes=[rcn])
        CH = min(T, 1024)
        for ci in range(T // CH):
            cols = slice(ci * CH, (ci + 1) * CH)
            tl = range(ci * (CH // 128), (ci + 1) * (CH // 128))
            rk, rki, rv = R(), R(), R()
            P.dma("sp", KA2[:, cols], S.KA_T2[:, cols], reads=[S.rKA[i] for i in tl], writes=[rk], key=f"KA2_{ci}")
            P.dma("sp", KI2[:, cols], S.KI_T2[:, cols], reads=[S.rKI[i] for i in tl], writes=[rki], key=f"KI2_{ci}")
            P.dma("sp", VAs[:, ci * (CH // 128):(ci + 1) * (CH // 128), :],
                  S.VA1[cols, :].rearrange("(n p) c -> p n c", p=128), reads=[S.rVA[i] for i in tl], writes=[rv],
                  key=f"VAs_{ci}")
            for i in tl:
                rKA2[i], rKI2[i], rVAs[i] = rk, rki, rv

        cnt = {"ri": 0, "pti": 0, "ai": 0}

        def stage_A(qt):
            b = qt % 2
            SL = (qt + 1) * 128
            P.dma("sp", qiT[b][:], S.QI_T[qt], reads=[S.rQI[qt]], writes=[rqiT[b]], key=f"dqiT{b}")
            P.dma("sp", sg[b][:], S.SG[qt * 128:(qt + 1) * 128, :], reads=[S.rSG[qt]], writes=[rsg[b]], key=f"dsg{b}")
            P.add("dve", lambda e: e.tensor_tensor(out=Dg[b][:], in0=bc_mid(c.ident[:], 16), in1=bc_last(sg[b][:], 128),
                                                   op=ALU.mult),
                  reads=[c.rident, rsg[b]], writes=[rDg[b]])
            steps = []
            for sbk, s0 in enumerate(range(0, SL, 512)):
                for h in range(16):
                    steps.append((sbk, s0, h))

            def lmm(sbk, s0, h):
                sw = min(512, SL - s0)
                kres = [rKI2[j] for j in range(s0 // 128, (s0 + sw) // 128)]
                hp, par = h // 2, h % 2
                pl = par * 64
                L, rL = Lp.next()
                P.add("pe", lambda e: e.matmul(L[:, 0:sw], lhsT=qiT[b][pl:pl + 64, hp * 128:(hp + 1) * 128],
                                               rhs=KI2[pl:pl + 64, s0:s0 + sw], start=True, stop=True),
                      reads=[rqiT[b]] + kres, writes=[rL])
                r = cnt['ri'] % 4
                cnt['ri'] += 1
                P.add("act", lambda e: e.activation(out=Rt[r][:, 0:sw], in_=L[:, 0:sw], func=AF.Relu),
                      reads=[rL], writes=[rRt[r]])
                return r

            def acc(sbk, s0, h, r):
                sw = min(512, SL - s0)
                A, rA = At[(cnt['ai'] + sbk) % 2], rAt[(cnt['ai'] + sbk) % 2]
                P.add("pe", lambda e: e.matmul(A[:, 0:sw], lhsT=Dg[b][:, h, :], rhs=Rt[r][:, 0:sw], start=(h == 0),
                                               stop=(h == 15)),
                      reads=[rDg[b], rRt[r]], writes=[rA])
                if h == 15:
                    P.add("act", lambda e: e.activation(out=Isb[b][:, s0:s0 + sw], in_=A[:, 0:sw], func=AF.Copy),
                          reads=[rA], writes=[rIsb[b]])

            LOOKA = 2
            pend = {}
            for i in range(min(LOOKA, len(steps))):
                pend[i] = lmm(*steps[i])
            for i, stp in enumerate(steps):
                if i + LOOKA < len(steps):
                    pend[i + LOOKA] = lmm(*steps[i + LOOKA])
                acc(*stp, pend.pop(i))
            cnt['ai'] += len(range(0, SL, 512))

        def stage_B(qt):
            b = qt % 2
            SL = (qt + 1) * 128
            P.dma("sp", qaT[b][:], S.QA_T[qt], reads=[S.rQA[qt]], writes=[rqaT[b]], key=f"dqaT{b}")
            S_ = st_[b]
            if qt >= QT0:
                P.add("dve", lambda e, b=b, SL=SL, S_=S_: e.tensor_reduce(out=S_[:, 0:1], in_=Isb[b][:, 0:SL], axis=AX.X,
                                                                        op=ALU.min),
                      reads=[rIsb[b]], writes=[rst[b]])
                P.add("dve", lambda e, b=b, SL=SL, S_=S_: e.tensor_reduce(out=S_[:, 1:2], in_=Isb[b][:, 0:SL], axis=AX.X,
                                                                        op=ALU.max),
                      reads=[rIsb[b]], writes=[rst[b]])
            P.add("pool", lambda e, b=b, qt=qt: e.tensor_tensor(out=Isb[b][:, qt * 128:(qt + 1) * 128],
                                                              in0=Isb[b][:, qt * 128:(qt + 1) * 128], in1=cneg[:],
                                                              op=ALU.add),
                  reads=[rIsb[b], rcn], writes=[rIsb[b]])
            if qt >= QT0:
                P.add("dve", lambda e, S_=S_: e.tensor_tensor(out=S_[:, 2:3], in0=S_[:, 1:2], in1=S_[:, 0:1],
                                                             op=ALU.subtract),
                      reads=[rst[b]], writes=[rst[b]])
                P.add("dve", lambda e, S_=S_: e.tensor_scalar(out=S_[:, 8:8 + NBIS], in0=pow2[:], scalar1=S_[:, 2:3],
                                                             scalar2=None, op0=ALU.mult),
                      reads=[rst[b], rcn], writes=[rst[b]])
                P.add("dve", lambda e, S_=S_: e.tensor_copy(out=S_[:, 3:4], in_=S_[:, 0:1]),
                      reads=[rst[b]], writes=[rst[b]])
                for k in range(NBIS):
                    P.add("dve", lambda e, S_=S_, k=k: e.tensor_tensor(out=S_[:, 4:5], in0=S_[:, 3:4],
                                                                      in1=S_[:, 8 + k:9 + k], op=ALU.add),
                          reads=[rst[b]], writes=[rst[b]])
                    P.add("dve", lambda e, S_=S_, b=b, SL=SL: e.tensor_scalar(
                        out=cjunk[:, 0:SL], in0=Isb[b][:, 0:SL], scalar1=S_[:, 4:5], scalar2=None,
                        op0=ALU.is_ge, op1=ALU.add, accum_out=S_[:, 5:6]),
                          reads=[rst[b], rIsb[b]], writes=[rst[b], rcj])
                    P.add("dve", lambda e, S_=S_, k=k: e.tensor_scalar(
                        out=S_[:, 6:7], in0=S_[:, 5:6], scalar1=float(TOPK), scalar2=S_[:, 8 + k:9 + k],
                        op0=ALU.is_ge, op1=ALU.mult),
                          reads=[rst[b]], writes=[rst[b]])
                    P.add("dve", lambda e, S_=S_: e.tensor_tensor(out=S_[:, 3:4], in0=S_[:, 3:4], in1=S_[:, 6:7],
                                                                 op=ALU.add),
                          reads=[rst[b]], writes=[rst[b]])
            else:
                P.add("dve", lambda e, S_=S_: e.memset(S_[:, 3:4], -1.0e29), writes=[rst[b]])
            P.add("dve", lambda e, S_=S_, b=b, SL=SL: e.tensor_scalar(
                out=maskq[b][:, 0:SL], in0=Isb[b][:, 0:SL], scalar1=S_[:, 3:4], scalar2=None, op0=ALU.is_ge),
                  reads=[rst[b], rIsb[b]], writes=[rmq[b]])

        def stage_C(qt):
            b = qt % 2
            SL = (qt + 1) * 128
            for s8 in range(0, qt + 1, 8):
                n8 = min(8, qt + 1 - s8)
                L, rL = Lp.next()
                Lb = L[:, :].bitcast(BF16)
                for j in range(n8):
                    P.add("pe", lambda e, Lb=Lb, j=j, s8=s8, b=b: e.transpose(
                        out=Lb[:, j * 128:(j + 1) * 128], in_=maskq[b][:, (s8 + j) * 128:(s8 + j + 1) * 128],
                        identity=c.ident[:]),
                          reads=[rmq[b], c.rident], writes=[rL])
                P.add("act", lambda e, Lb=Lb, s8=s8, n8=n8, b=b: e.activation(
                    out=maskT[b][:, s8:s8 + n8, :].rearrange("p n q -> p (n q)"), in_=Lb[:, 0:n8 * 128],
                    func=AF.Copy),
                      reads=[rL], writes=[rmT[b]])
            def qk(st):
                sc = slice(st * 128, (st + 1) * 128)
                LE, rLE = Lp.next()
                LO, rLO = Lp.next()
                P.add("pe", lambda e: e.matmul(LE[:, :], lhsT=KA2[0:64, sc], rhs=qaT[b][0:64, :], start=True, stop=True),
                      reads=[rKA2[st], rqaT[b]], writes=[rLE])
                P.add("pe", lambda e: e.matmul(LO[:, :], lhsT=KA2[64:128, sc], rhs=qaT[b][64:128, :], start=True,
                                               stop=True),
                      reads=[rKA2[st], rqaT[b]], writes=[rLO])
                p = cnt["pti"] % len(PT)
                cnt["pti"] += 1
                P.add("act", lambda e: e.activation(out=PT[p][:, 0:512], in_=LE[:, :], func=AF.Exp, scale=0.125),
                      reads=[rLE], writes=[rPT[p]])
                P.add("act", lambda e: e.activation(out=PT[p][:, 512:1024], in_=LO[:, :], func=AF.Exp, scale=0.125),
                      reads=[rLO], writes=[rPT[p]])
                P.add("pool", lambda e: e.tensor_tensor(
                    out=PT[p][:].rearrange("p (a q) -> p a q", a=8), in0=PT[p][:].rearrange("p (a q) -> p a q", a=8),
                    in1=bc_mid(maskT[b][:, st, :], 8), op=ALU.mult),
                      reads=[rPT[p], rmT[b]], writes=[rPT[p]])
                return p

            def pv(st, p):
                P.add("pe", lambda e: e.matmul(OE[:, :], lhsT=VAs[:, st, 64:192], rhs=PT[p][:, 0:512],
                                               start=(st == 0), stop=(st == qt)),
                      reads=[rVAs[st], rPT[p]], writes=[rOE])
                P.add("pe", lambda e: e.matmul(OO[:, :], lhsT=VAs[:, st, 0:128], rhs=PT[p][:, 512:1024],
                                               start=(st == 0), stop=(st == qt)),
                      reads=[rVAs[st], rPT[p]], writes=[rOO])

            pend = {0: qk(0)}
            for st in range(qt + 1):
                if st + 1 <= qt:
                    pend[st + 1] = qk(st + 1)
                pv(st, pend.pop(st))
            normalize_out(P, c, OE, rOE, 0, rc[b], rrc[b], aT[b][0:64, :], raT[b])
            normalize_out(P, c, OO, rOO, 64, rc[b], rrc[b], aT[b][64:128, :], raT[b])
            P.dma("sp", S.ATT_T[0:4, :, qt * 128:(qt + 1) * 128].rearrange("j p t -> p j t"),
                  aT[b][:].rearrange("p (j t) -> p j t", j=4), reads=[raT[b]], writes=[S.rATa[qt]], key=f"daT{b}")

        for step in range(NT + 2):
            if step < NT:
                stage_A(step)
            if 0 <= step - 1 < NT:
                stage_B(step - 1)
            if 0 <= step - 2 < NT:
                stage_C(step - 2)
        return P.end_phase(ps)


SCALE_B = 96.0 ** -0.5


def mla_phase(nc, P, c, T, S):
    NT = T // 128
    NQB = T // 512
    with ExitStack() as ps:
        def sb(name, shape, dt):
            return ps.enter_context(nc.sbuf_tensor(name, shape, dt))

        def pm(name, shape, dt):
            return ps.enter_context(nc.psum_tensor(name, shape, dt))

        R = P.res

        def dbl(name, shape, dt):
            return [sb(f"{name}{i}", shape, dt) for i in range(2)], [R() for _ in range(2)]

        KB = [sb(f"mKB{i}", [128, T], BF16) for i in range(2)]
        VB = [sb(f"mVB{i}", [128, NT, 192], BF16) for i in range(2)]
        rKB = [[R() for _ in range(NQB)] for _ in range(2)]
        rVB = [[R() for _ in range(NQB)] for _ in range(2)]
        Cm = sb("Cm", [128, 4, 512], BF16)
        rCm = R()
        QT, rQT = dbl("mQT", [128, 512], BF16)
        PT = [sb(f"mPT{i}", [128, 512], BF16) for i in range(4)]
        rPT = [R() for _ in range(4)]
        rc, rrc = dbl("mrc", [128, 512], F32)
        aT, raT = dbl("maT", [128, 512], BF16)
        Lt = [pm(f"mL{i}", [128, 512], F32) for i in range(4)]
        Lp = BankPool(Lt, [R() for _ in range(4)])
        Ot = [pm(f"mO{i}", [128, 512], F32) for i in range(2)]
        rOt = [R() for _ in range(2)]

        P.add("pool", lambda e: e.memset(Cm[:], 1.0), writes=[rCm])
        P.add("pool", lambda e: e.affine_select(out=Cm[:], in_=Cm[:], pattern=[[-128, 4], [1, 512]],
                                                compare_op=ALU.is_ge, fill=0.0, base=0, channel_multiplier=-1),
              reads=[rCm], writes=[rCm])
        CH = min(T, 1024)
        NCH = T // CH

        def load_head(h):
            hb_ = h % 2
            for ci in range(NCH):
                cs = slice(ci * CH, (ci + 1) * CH)
                tl = range(ci * (CH // 128), (ci + 1) * (CH // 128))
                P.dma("sp", KB[hb_][:, cs], S.KB_T[h, :, cs], reads=[S.rKB[i] for i in tl],
                      writes=[rKB[hb_][ci]], key=f"mKB{hb_}_{ci}")
                P.dma("sp", VB[hb_][:, ci * (CH // 128):(ci + 1) * (CH // 128), :],
                      S.VB1[cs, h, :].rearrange("(n p) c -> p n c", p=128),
                      reads=[S.rVB[i] for i in tl], writes=[rVB[hb_][ci]], key=f"mVB{hb_}_{ci}")

        groups = [(h, qb) for h in range(8) for qb in range(NQB)]

        def load_q(g):
            h, qb = groups[g]
            bq = g % 2
            cs = slice(qb * 512, (qb + 1) * 512)
            P.dma("sp", QT[bq][:], S.QB_T[h, :, cs], reads=[S.rQB[i] for i in range(qb * 4, qb * 4 + 4)],
                  writes=[rQT[bq]], key=f"mQT{bq}")

        steps = [(g, st) for g, (h, qb) in enumerate(groups) for st in range(4 * (qb + 1))]
        cnt = {"pti": 0}

        def qk(g, st):
            h, qb = groups[g]
            hb_, bq = h % 2, g % 2
            L, rL = Lp.next()
            P.add("pe", lambda e: e.matmul(L[:, :], lhsT=KB[hb_][:, st * 128:(st + 1) * 128], rhs=QT[bq][:, :],
                                           start=True, stop=True),
                  reads=[rKB[hb_][(st * 128) // CH], rQT[bq]], writes=[rL])
            p = cnt["pti"] % len(PT)
            cnt["pti"] += 1
            P.add("act", lambda e: e.activation(out=PT[p][:], in_=L[:, :], func=AF.Exp, scale=SCALE_B),
                  reads=[rL], writes=[rPT[p]])
            j = st - 4 * qb
            if j >= 0:
                P.add("pool", lambda e: e.tensor_tensor(out=PT[p][:], in0=PT[p][:], in1=Cm[:, j, :], op=ALU.mult),
                      reads=[rPT[p], rCm], writes=[rPT[p]])
            return p

        def pv(g, st, p):
            h, qb = groups[g]
            hb_, bq = h % 2, g % 2
            nst = 4 * (qb + 1)
            O, rO = Ot[g % 2], rOt[g % 2]
            vsl = slice(64, 192) if h % 2 == 0 else slice(0, 128)
            num_lo = 0 if h % 2 == 0 else 64
            P.add("pe", lambda e: e.matmul(O[:, :], lhsT=VB[hb_][:, st, vsl], rhs=PT[p][:], start=(st == 0),
                                           stop=(st == nst - 1)),
                  reads=[rVB[hb_][(st * 128) // CH], rPT[p]], writes=[rO])
            if st == nst - 1:
                cs = slice(qb * 512, (qb + 1) * 512)
                normalize_out(P, c, O, rO, num_lo, rc[bq], rrc[bq], aT[bq][num_lo:num_lo + 64, :], raT[bq])
                P.dma("sp", S.ATT_T[4 + h // 2, num_lo:num_lo + 64, cs], aT[bq][num_lo:num_lo + 64, :],
                      reads=[raT[bq]], writes=[S.rATb[h][qb]], key=f"maT{bq}")

        LOOK = 2
        load_head(0)
        load_q(0)
        issued = {}
        loaded_q = {0}
        loaded_h = {0}

        def ensure_loads(g):
            if g >= len(groups):
                return
            h = groups[g][0]
            if h not in loaded_h:
                loaded_h.add(h)
                load_head(h)
            if g not in loaded_q:
                loaded_q.add(g)
                load_q(g)

        for i in range(min(LOOK, len(steps))):
            ensure_loads(steps[i][0])
            issued[i] = qk(*steps[i])
        for i, (g, st) in enumerate(steps):
            if i + LOOK < len(steps):
                ensure_loads(steps[i + LOOK][0])
                issued[i + LOOK] = qk(*steps[i + LOOK])
            pv(g, st, issued.pop(i))
            hh = groups[g][0]
            if st == 0 and groups[g][1] == 0 and hh + 1 < 8 and (hh + 1) not in loaded_h:
                loaded_h.add(hh + 1)
                load_head(hh + 1)
        return P.end_phase(ps)


def wout_phase(nc, P, c, T, S):
    NT = T // 128
    with ExitStack() as ps:
        def sb(name, shape, dt):
            return ps.enter_context(nc.sbuf_tensor(name, shape, dt))

        def pm(name, shape, dt):
            return ps.enter_context(nc.psum_tensor(name, shape, dt))

        R = P.res

        def dbl(name, shape, dt):
            return [sb(f"{name}{i}", shape, dt) for i in range(2)], [R() for _ in range(2)]

        Wo = sb("Wo", [128, 8, D], BF16)
        rWo = [R() for _ in range(8)]
        xt, rxt = dbl("wxt", [128, D], F32)
        at, rat = dbl("wat", [128, 8, 128], BF16)
        mmt = [pm(f"wmm{i}", [128, 512], F32) for i in range(4)]
        mmp = BankPool(mmt, [R() for _ in range(4)])
        load_weight_cast(P, c, Wo, rWo, S.w_out, 8, D, "o")
        for i in range(NT):
            b = i % 2
            rows = slice(i * 128, (i + 1) * 128)
            P.dma("sp", xt[b][:], S.X1[rows, :], reads=[S.rX1[i]], writes=[rxt[b]], key=f"wxt{b}")
            P.dma("sp", at[b][:], S.ATT_T[:, :, rows].rearrange("c p t -> p c t"),
                  reads=[S.rATa[i]] + [S.rATb[h][i // 4] for h in range(8)], writes=[rat[b]], key=f"wat{b}")
            for half in range(2):
                bk, rbk = mmp.next()
                for cc in range(8):
                    P.add("pe", lambda e, bk=bk, cc=cc, b=b, half=half: e.matmul(
                        bk[:, :], lhsT=at[b][:, cc, :], rhs=Wo[:, cc, half * 512:(half + 1) * 512],
                        start=(cc == 0), stop=(cc == 7)),
                          reads=[rat[b], rWo[cc]], writes=[rbk])
                P.add("dve", lambda e, bk=bk, b=b, half=half: e.tensor_tensor(
                    out=xt[b][:, half * 512:(half + 1) * 512], in0=bk[:, :], in1=xt[b][:, half * 512:(half + 1) * 512],
                    op=ALU.add),
                      reads=[rbk, rxt[b]], writes=[rxt[b]])
            P.dma("sp", S.X2[rows, :], xt[b][:], reads=[rxt[b]], writes=[S.rX2[i]], key=f"wxo{b}")
        return P.end_phase(ps)


IN_NAMES = ["x", "g_ffn1", "w1_gate", "w1_up", "w1_down", "g_mix", "w_in", "g_q_lat", "g_kv_lat", "w_uq", "w_ukv",
            "w_out", "g_ffn2", "w2_gate", "w2_up", "w2_down", "g_final"]
IN_SHAPES = {"g_ffn1": [D], "w1_gate": [D, DFF], "w1_up": [D, DFF], "w1_down": [DFF, D], "g_mix": [D],
             "w_in": [D, DPROJ], "g_q_lat": [384], "g_kv_lat": [256], "w_uq": [384, 768], "w_ukv": [256, 1024],
             "w_out": [D, D], "g_ffn2": [D], "w2_gate": [D, DFF], "w2_up": [D, DFF], "w2_down": [DFF, D],
             "g_final": [D]}


def build(T, debug=False, phases=("ffn1", "proj", "dsa", "mla", "wout", "ffn2")):
    NT = T // 128
    NQB = T // 512
    nc = bass.Bass("TRN2", target_bir_lowering=False)
    S = Ctx()
    x = nc.dram_tensor("x", [T, D], F32, kind="ExternalInput").ap()
    for n, shp in IN_SHAPES.items():
        setattr(S, n, nc.dram_tensor(n, list(shp), F32, kind="ExternalInput").ap())
    S.rope64 = nc.dram_tensor("rope64", [T, 128], F32, kind="ExternalInput").ap()
    S.rope32 = nc.dram_tensor("rope32", [T, 64], F32, kind="ExternalInput").ap()
    out = nc.dram_tensor("out", [T, D], F32, kind="ExternalOutput").ap()
    kind = "ExternalOutput" if debug else "Internal"

    def scr(name, shape, dt):
        return nc.dram_tensor(name, list(shape), dt, kind=kind).ap()

    S.X1 = scr("X1", [T, D], F32)
    S.X2 = scr("X2", [T, D], F32)
    S.QA_T = scr("QA_T", [NT, 128, 512], BF16)
    S.QI_T = scr("QI_T", [NT, 128, 1024], BF16)
    S.SG = scr("SG", [T, 16], F32)
    S.KA_T2 = scr("KA_T2", [128, T], BF16)
    S.KI_T2 = scr("KI_T2", [128, T], BF16)
    S.VA1 = scr("VA1", [T, 192], BF16)
    S.QB_T = scr("QB_T", [8, 128, T], BF16)
    S.KB_T = scr("KB_T", [8, 128, T], BF16)
    S.VB1 = scr("VB1", [T, 8, 192], BF16)
    S.ATT_T = scr("ATT_T", [8, 128, T], BF16)
    with ExitStack() as stack:
        P = Prog(nc, stack)
        c = Ctx()
        const_phase(nc, P, c, stack)
        rl = lambda: [P.res() for _ in range(NT)]
        rx = rl()
        rout = rl()
        S.rX1, S.rX2, S.rQA, S.rQI, S.rSG, S.rKA, S.rKI, S.rVA, S.rQB, S.rKB, S.rVB, S.rATa = [rl() for _ in range(12)]
        S.rATb = [[P.res() for _ in range(NQB)] for _ in range(8)]
        info = {}
        if "ffn1" in phases:
            info["ffn1"] = ffn_phase(nc, P, c, T, x, rx, S.X1, S.rX1, S.g_ffn1, S.w1_gate, S.w1_up, S.w1_down)
        if "proj" in phases:
            info["proj"] = proj_phase(nc, P, c, T, S)
        if "dsa" in phases:
            info["dsa"] = dsa_phase(nc, P, c, T, S)
        if "mla" in phases:
            info["mla"] = mla_phase(nc, P, c, T, S)
        if "wout" in phases:
            info["wout"] = wout_phase(nc, P, c, T, S)
        if "ffn2" in phases:
            info["ffn2"] = ffn_phase(nc, P, c, T, S.X2, S.rX2, out, rout, S.g_ffn2, S.w2_gate, S.w2_up, S.w2_down,
                                     final_g=S.g_final, tag="f2")
        nc._mk_info = (info, dict(P.cnt), max(P.dcnt.values()) if P.dcnt else 0)
    return nc


def rope_table(T, dim):
    pos = np.arange(T, dtype=np.float32)
    inv_freq = (np.float32(10000.0) ** (-np.arange(0, dim, 2, dtype=np.float32) / np.float32(dim))).astype(np.float32)
    ang = pos[:, None] * inv_freq[None, :]
    cs, sn = np.cos(ang).astype(np.float32), np.sin(ang).astype(np.float32)
    return np.concatenate([cs, cs, -sn, sn], axis=1).astype(np.float32)


_NC_CACHE = {}


def kernel(**inputs):
    x = np.ascontiguousarray(np.asarray(inputs["x"], dtype=np.float32))
    B, T, _ = x.shape
    if T not in _NC_CACHE:
        _NC_CACHE[T] = build(T)
    nc = _NC_CACHE[T]
    shared = {}
    for n, shp in IN_SHAPES.items():
        shared[n] = np.ascontiguousarray(np.asarray(inputs[n], dtype=np.float32).reshape(shp))
    shared["rope64"] = rope_table(T, 64)
    shared["rope32"] = rope_table(T, 32)
    in_maps = []
    for bi in range(B):
        m = dict(shared)
        m["x"] = x[bi]
        in_maps.append(m)
    res = run_bass_kernel_spmd(nc, in_maps, core_ids=list(range(B)))
    return np.stack([np.asarray(r["out"]) for r in res.results], axis=0).astype(np.float32)
```

```python
import math
from contextlib import ExitStack

import numpy as np
import concourse.bass as bass
import concourse.mybir as mybir
from concourse.bass_utils import run_bass_kernel_spmd

F32 = mybir.dt.float32
BF16 = mybir.dt.bfloat16
AF = mybir.ActivationFunctionType
ALU = mybir.AluOpType
AX = mybir.AxisListType

D = 1024
DFF = 2816
NFC = DFF // 128
EPS = 1e-6
ENGS = ("pe", "act", "dve", "pool", "sp")


class Res:
    __slots__ = ("name", "last_w", "readers")

    def __init__(self, name):
        self.name = name
        self.last_w = None
        self.readers = []


class Op:
    __slots__ = ("eng", "fn", "deps", "idx", "signal", "sem", "val", "is_dma", "waits", "key", "emitted")

    def __init__(self, eng, fn, is_dma=False):
        self.eng = eng
        self.fn = fn
        self.deps = []
        self.signal = False
        self.sem = None
        self.val = None
        self.is_dma = is_dma
        self.waits = []
        self.key = None
        self.emitted = False


class Prog:
    def __init__(self, nc, stack):
        self.nc = nc
        self.stack = stack
        self.ops = []
        self.n_total = 0
        self.eng_sem = {e: stack.enter_context(nc.semaphore("S_" + e)) for e in ENGS}
        self.cnt = {e: 0 for e in ENGS}
        self.dma_sem = {}
        self.dcnt = {}
        self.keymap = {}
        for e_, n_ in (("pool", 40), ("sp", 44)):
            for i_ in range(n_):
                self.dma_sem[(e_, i_)] = stack.enter_context(nc.semaphore("D_%s_%d" % (e_, i_)))
                self.dcnt[(e_, i_)] = 0
        self.block = stack.enter_context(nc.Block())
        self.waited = {e: {} for e in ENGS}
        self.phase_dmas = []
        self.last_op = {e: None for e in ENGS}

    def res(self, name="r"):
        return Res(name)

    def add(self, eng, fn, reads=(), writes=(), dma_key=None):
        op = Op(eng, fn, is_dma=dma_key is not None)
        op.idx = self.n_total
        self.n_total += 1
        seen = set()

        def dep(d):
            if d is None or d.idx in seen or d.emitted:
                return
            seen.add(d.idx)
            if d.eng == op.eng and not d.is_dma and not op.is_dma and d.eng == "pe":
                return
            op.deps.append(d)
            d.signal = True

        for r in reads:
            dep(r.last_w)
        for w in writes:
            dep(w.last_w)
            for rd in w.readers:
                dep(rd)
        for r in reads:
            r.readers.append(op)
        for w in writes:
            w.last_w = op
            w.readers = []
        if dma_key is not None:
            op.key = dma_key
            op.signal = True
            self.phase_dmas.append(op)
        self.ops.append(op)
        return op

    def dma(self, eng, out, in_, reads=(), writes=(), key=None):
        return self.add(eng, lambda e: e.dma_start(out=out, in_=in_), reads=reads, writes=writes, dma_key=key)

    def end_phase(self, pstack):
        nc = self.nc
        last = {}
        for op in self.ops:
            if op.fn is not None and not op.is_dma:
                last[op.eng] = op
        for e in ENGS:
            fin = self.add(e, None)
            fin.deps = [o for e2, o in last.items() if e2 != e] + list(self.phase_dmas)
            for o in fin.deps:
                o.signal = True
        self.phase_dmas = []
        for op in self.ops:
            if op.is_dma:
                km = self.keymap.setdefault(op.eng, {})
                if op.key not in km:
                    km[op.key] = len(km)
                k = (op.eng, km[op.key])
                assert k in self.dma_sem, ("out of preallocated DMA semaphores", k)
                self.dcnt[k] += 16
                op.sem = self.dma_sem[k]
                op.val = self.dcnt[k]
            elif op.signal:
                self.cnt[op.eng] += 1
                op.sem = self.eng_sem[op.eng]
                op.val = self.cnt[op.eng]
        for op in self.ops:
            need = {}
            w = self.waited[op.eng]
            for d in op.deps:
                key = id(d.sem)
                if w.get(key, 0) >= d.val:
                    continue
                if key not in need or need[key][1] < d.val:
                    need[key] = (d.sem, d.val)
            for key, (s, v) in need.items():
                w[key] = v
                op.waits.append((s, v))
        per_eng = {e: [op for op in self.ops if op.eng == e] for e in ENGS}
        block = self.block
        handles = {"pe": block.tensor, "act": block.scalar, "dve": block.vector,
                   "pool": block.gpsimd, "sp": block.sync}

        def make(e):
            def body(eng):
                for op in per_eng[e]:
                    for (s, v) in op.waits:
                        eng.wait_ge(s, v)
                    if op.fn is None:
                        continue
                    ins = op.fn(eng)
                    if op.signal:
                        ins.then_inc(op.sem, 16 if op.is_dma else 1)
            return body

        for e in ENGS:
            if per_eng[e]:
                handles[e](make(e))
        n = len(self.ops)
        for op in self.ops:
            op.emitted = True
            op.fn = None
        self.ops = []
        self.keymap = {}
        return n


def bcast_rows(vec_ap, n):
    return bass.AP(tensor=vec_ap.tensor, offset=vec_ap.offset, ap=[[0, 128], [1, n]])


class Ctx:
    pass


def load_weight_cast(P, c, dst_tile, res2d, src_ap, nk, pieces, stage, rstage):
    for (dc, sc, w, ri) in pieces:
        for k in range(nk):
            s = c.sj % len(stage)
            c.sj += 1
            P.dma("sp", stage[s][:, 0:w], src_ap[k * 128:(k + 1) * 128, sc:sc + w], writes=[rstage[s]],
                  key=f"stg{s}")
            eng = ("act", "dve", "pool")[c.sj % 3]
            if eng == "act":
                P.add("act", lambda e, s=s, k=k, dc=dc, w=w: e.activation(out=dst_tile[:, k, dc:dc + w],
                                                                          in_=stage[s][:, 0:w], func=AF.Copy),
                      reads=[rstage[s]], writes=[res2d[k][ri]])
            else:
                P.add(eng, lambda e, s=s, k=k, dc=dc, w=w: e.tensor_copy(out=dst_tile[:, k, dc:dc + w],
                                                                        in_=stage[s][:, 0:w]),
                      reads=[rstage[s]], writes=[res2d[k][ri]])


def make_stage(nc, P, c, ps, tag, n=5):
    st = [ps.enter_context(nc.sbuf_tensor(f"{tag}stg{i}", [128, 512], F32)) for i in range(n)]
    return st, [P.res() for _ in range(n)]


def rmsnorm_to_bf16(P, c, x_ap, x_res, n, g_bc, g_res, junk, junk_res, ss, ss_res, rstd, rstd_res, out_ap, out_res,
                    x_in_psum=False):
    P.add("act", lambda e: e.activation(out=junk, in_=x_ap, func=AF.Square, accum_out=ss),
          reads=[x_res], writes=[junk_res, ss_res])
    P.add("act", lambda e: e.activation(out=rstd, in_=ss, func=AF.Sqrt, scale=1.0 / n, bias=c.eps_t[:, 0:1]),
          reads=[ss_res, c.rconst], writes=[rstd_res])
    P.add("dve", lambda e: e.reciprocal(out=rstd, in_=rstd), reads=[rstd_res], writes=[rstd_res])
    P.add("dve", lambda e: e.scalar_tensor_tensor(out=out_ap, in0=x_ap, scalar=rstd, in1=g_bc,
                                                  op0=ALU.mult, op1=ALU.mult),
          reads=[x_res, rstd_res, g_res], writes=[out_res])


def ffn_phase(nc, P, c, T, src, src_res, dst, dst_res, g_vec, wg, wu, wd, final_g=None, tag="f1"):
    NT = T // 128
    with ExitStack() as ps:
        def sb(name, shape, dt):
            return ps.enter_context(nc.sbuf_tensor(tag + name, shape, dt))

        def pm(name, shape, dt):
            return ps.enter_context(nc.psum_tensor(tag + name, shape, dt))

        Wg = sb("Wg", [128, 8, DFF], BF16)
        Wu = sb("Wu", [128, 8, DFF], BF16)
        Wd = sb("Wd", [128, NFC, D], BF16)
        gbc = sb("gbc", [128, D], F32)
        gfin = sb("gfin", [128, D], F32) if final_g is not None else None
        xt = [sb(f"xt{i}", [128, D], F32) for i in range(3)]
        junk = [sb(f"junk{i}", [128, D], BF16) for i in range(2)]
        ss = [sb(f"ss{i}", [128, 4], F32) for i in range(2)]
        hb = [sb(f"hb{i}", [128, D], BF16) for i in range(2)]
        hT = [sb(f"hT{i}", [128, 8, 128], BF16) for i in range(2)]
        sg = [sb(f"sg{i}", [128, 512], F32) for i in range(2)]
        act = [sb(f"act{i}", [128, DFF], BF16) for i in range(2)]
        actT = [sb(f"actT{i}", [128, NFC, 128], BF16) for i in range(2)]
        tp = [pm(f"tp{i}", [128, 1024], BF16) for i in range(2)]
        mm = [pm(f"mm{i}", [128, 512], F32) for i in range(6)]

        R = P.res
        rWg = [[R() for _ in range(6)] for _ in range(8)]
        rWu = [[R() for _ in range(6)] for _ in range(8)]
        rWd = [[R(), R()] for _ in range(NFC)]
        stg, rstg = make_stage(nc, P, c, ps, tag)
        rg, rgf = R(), R()
        rxt = [R() for _ in range(3)]
        rjunk = [R() for _ in range(2)]
        rss = [R() for _ in range(2)]
        rrs = [R() for _ in range(2)]
        rhb = [R() for _ in range(2)]
        rhT = [R() for _ in range(2)]
        rsg = [R() for _ in range(2)]
        ract = [R() for _ in range(2)]
        ractT = [R() for _ in range(2)]
        ryo = [R() for _ in range(2)]
        rtp = [R() for _ in range(2)]
        rmm = [R() for _ in range(6)]

        P.dma("sp", gbc[:], bcast_rows(g_vec, D), writes=[rg], key="gbc")
        if final_g is not None:
            P.dma("sp", gfin[:], bcast_rows(final_g, D), writes=[rgf], key="gfin")
        for i0 in range(min(2, NT)):
            P.dma("sp", xt[i0][:], src[i0 * 128:(i0 + 1) * 128, :], reads=[src_res[i0]], writes=[rxt[i0]],
                  key=f"xt{i0}")
        for si0, s00 in enumerate(range(0, DFF, 512)):
            pc = [(s00, s00, min(512, DFF - s00), si0)]
            load_weight_cast(P, c, Wg, rWg, wg, 8, pc, stg, rstg)
            load_weight_cast(P, c, Wu, rWu, wu, 8, pc, stg, rstg)
        load_weight_cast(P, c, Wd, rWd, wd, NFC, [(0, 0, 512, 0), (512, 512, 512, 1)], stg, rstg)

        slabs = [(s0, min(512, DFF - s0)) for s0 in range(0, DFF, 512)]
        cn = {"mmi": 0, "tpi": 0}

        def stage1(i):
            b = i % 2
            b3 = i % 3
            rows = slice(i * 128, (i + 1) * 128)
            if i >= 2:
                P.dma("sp", xt[b3][:], src[rows, :], reads=[src_res[i]], writes=[rxt[b3]], key=f"xt{b3}")
            rmsnorm_to_bf16(P, c, xt[b3][:], rxt[b3], D, gbc[:], rg, junk[b][:], rjunk[b], ss[b][:, 0:1], rss[b],
                            ss[b][:, 1:2], rrs[b], hb[b][:], rhb[b])
            t = cn['tpi'] % 2
            cn['tpi'] += 1
            for k in range(8):
                P.add("pe", lambda e, k=k, t=t, b=b, b3=b3: e.transpose(out=tp[t][:, k * 128:(k + 1) * 128],
                                                                in_=hb[b][:, k * 128:(k + 1) * 128],
                                                                identity=c.ident[:]),
                      reads=[rhb[b], c.rident], writes=[rtp[t]])
            P.add("act", lambda e, t=t, b=b, b3=b3: e.activation(out=hT[b][:].rearrange("p k t -> p (k t)"),
                                                          in_=tp[t][:], func=AF.Copy),
                  reads=[rtp[t]], writes=[rhT[b]])
            for si, (s0, sw) in enumerate(slabs):
                ga = cn['mmi'] % 6
                ua = (cn['mmi'] + 1) % 6
                cn['mmi'] += 2
                for k in range(8):
                    P.add("pe", lambda e, k=k, ga=ga, b=b, b3=b3, s0=s0, sw=sw: e.matmul(
                        mm[ga][:, 0:sw], lhsT=hT[b][:, k, :], rhs=Wg[:, k, s0:s0 + sw],
                        start=(k == 0), stop=(k == 7)),
                          reads=[rhT[b], rWg[k][si]], writes=[rmm[ga]])
                for k in range(8):
                    P.add("pe", lambda e, k=k, ua=ua, b=b, b3=b3, s0=s0, sw=sw: e.matmul(
                        mm[ua][:, 0:sw], lhsT=hT[b][:, k, :], rhs=Wu[:, k, s0:s0 + sw],
                        start=(k == 0), stop=(k == 7)),
                          reads=[rhT[b], rWu[k][si]], writes=[rmm[ua]])
                s2 = si % 2
                P.add("act", lambda e, ga=ga, s2=s2, sw=sw: e.activation(out=sg[s2][:, 0:sw], in_=mm[ga][:, 0:sw],
                                                                        func=AF.Silu),
                      reads=[rmm[ga]], writes=[rsg[s2]])
                P.add("dve", lambda e, ua=ua, s2=s2, b=b, b3=b3, s0=s0, sw=sw: e.tensor_tensor(
                    out=act[b][:, s0:s0 + sw], in0=sg[s2][:, 0:sw], in1=mm[ua][:, 0:sw], op=ALU.mult),
                      reads=[rsg[s2], rmm[ua]], writes=[ract[b]])

        def stage2(i):
            b = i % 2
            b3 = i % 3
            rows = slice(i * 128, (i + 1) * 128)
            for f0 in range(0, NFC, 8):
                nf = min(8, NFC - f0)
                t = cn['tpi'] % 2
                cn['tpi'] += 1
                for f in range(nf):
                    P.add("pe", lambda e, f=f, f0=f0, t=t, b=b, b3=b3: e.transpose(
                        out=tp[t][:, f * 128:(f + 1) * 128],
                        in_=act[b][:, (f0 + f) * 128:(f0 + f + 1) * 128], identity=c.ident[:]),
                          reads=[ract[b], c.rident], writes=[rtp[t]])
                P.add("act" if (f0 // 8) % 2 == 0 else "dve",
                      (lambda e, t=t, b=b, b3=b3, f0=f0, nf=nf: e.activation(
                          out=actT[b][:, f0:f0 + nf, :].rearrange("p k t -> p (k t)"),
                          in_=tp[t][:, 0:nf * 128], func=AF.Copy)) if (f0 // 8) % 2 == 0 else
                      (lambda e, t=t, b=b, b3=b3, f0=f0, nf=nf: e.tensor_copy(
                          out=actT[b][:, f0:f0 + nf, :].rearrange("p k t -> p (k t)"),
                          in_=tp[t][:, 0:nf * 128])),
                      reads=[rtp[t]], writes=[ractT[b]])
            for half in range(2):
                da = cn['mmi'] % 6
                cn['mmi'] += 1
                for f in range(NFC):
                    P.add("pe", lambda e, f=f, da=da, b=b, b3=b3, half=half: e.matmul(
                        mm[da][:, :], lhsT=actT[b][:, f, :], rhs=Wd[:, f, half * 512:(half + 1) * 512],
                        start=(f == 0), stop=(f == NFC - 1)),
                          reads=[ractT[b], rWd[f][half]], writes=[rmm[da]])
                P.add("dve", lambda e, da=da, b=b, b3=b3, half=half: e.scalar_tensor_tensor(
                    out=xt[b3][:, half * 512:(half + 1) * 512], in0=mm[da][:, :], scalar=0.5,
                    in1=xt[b3][:, half * 512:(half + 1) * 512], op0=ALU.mult, op1=ALU.add),
                      reads=[rmm[da], rxt[b3]], writes=[rxt[b3]])
            if final_g is None:
                P.dma("sp", dst[rows, :], xt[b3][:], reads=[rxt[b3]], writes=[dst_res[i]], key=f"xo{b3}")
            else:
                P.add("act", lambda e, b=b, b3=b3: e.activation(out=junk[b][:], in_=xt[b3][:], func=AF.Square,
                                                         accum_out=ss[b][:, 2:3]),
                      reads=[rxt[b3]], writes=[rjunk[b], rss[b]])
                P.add("act", lambda e, b=b, b3=b3: e.activation(out=ss[b][:, 3:4], in_=ss[b][:, 2:3], func=AF.Sqrt,
                                                         scale=1.0 / D, bias=c.eps_t[:, 0:1]),
                      reads=[rss[b], c.rconst], writes=[rrs[b]])
                P.add("dve", lambda e, b=b, b3=b3: e.reciprocal(out=ss[b][:, 3:4], in_=ss[b][:, 3:4]),
                      reads=[rrs[b]], writes=[rrs[b]])
                P.add("dve", lambda e, b=b, b3=b3: e.scalar_tensor_tensor(out=xt[b3][:], in0=xt[b3][:], scalar=ss[b][:, 3:4],
                                                                   in1=gfin[:], op0=ALU.mult, op1=ALU.mult),
                      reads=[rxt[b3], rrs[b], rgf], writes=[rxt[b3]])
                P.dma("sp", dst[rows, :], xt[b3][:], reads=[rxt[b3]], writes=[dst_res[i]], key=f"xo{b3}")

        stage1(0)
        for i in range(NT):
            if i + 1 < NT:
                stage1(i + 1)
            stage2(i)
        return P.end_phase(ps)


def const_phase(nc, P, c, stack):
    c.ident = stack.enter_context(nc.sbuf_tensor("ident", [128, 128], BF16))
    c.identf = stack.enter_context(nc.sbuf_tensor("identf", [128, 128], F32))
    c.eps_t = stack.enter_context(nc.sbuf_tensor("eps_t", [128, 1], F32))
    c.rident = P.res()
    c.rconst = P.res()
    c.sj = 0
    P.add("pool", lambda e: e.memset(c.identf[:], 0.0), writes=[c.rident])
    P.add("pool", lambda e: e.affine_select(out=c.identf[:], in_=c.identf[:], pattern=[[-1, 128]],
                                            compare_op=ALU.not_equal, fill=1.0, base=0, channel_multiplier=1),
          reads=[c.rident], writes=[c.rident])
    P.add("pool", lambda e: e.tensor_copy(out=c.ident[:], in_=c.identf[:]), reads=[c.rident], writes=[c.rident])
    P.add("pool", lambda e: e.memset(c.eps_t[:], EPS), writes=[c.rconst])


def bc_mid(ap2d, H):
    a = [list(x) for x in ap2d.ap]
    return bass.AP(tensor=ap2d.tensor, offset=ap2d.offset, ap=[a[0], [0, H], a[1]])


def bc_last(ap2d, w):
    a = [list(x) for x in ap2d.ap]
    return bass.AP(tensor=ap2d.tensor, offset=ap2d.offset, ap=[a[0], a[1], [0, w]])


class BankPool:
    def __init__(self, tiles, res):
        self.tiles = tiles
        self.res = res
        self.i = 0

    def next(self):
        k = self.i % len(self.tiles)
        self.i += 1
        return self.tiles[k], self.res[k]


def transposes_to(P, c, tpp, srcs, src_res, dst_ap, dst_res, np_out=128, copy_eng="act"):
    tp, rtp = tpp.next()
    n = len(srcs)
    for j, s_ap in enumerate(srcs):
        P.add("pe", lambda e, j=j, s_ap=s_ap: e.transpose(out=tp[0:np_out, j * 128:(j + 1) * 128], in_=s_ap,
                                                         identity=c.ident[:]),
              reads=list(src_res) + [c.rident], writes=[rtp])
    if copy_eng == "act":
        P.add("act", lambda e: e.activation(out=dst_ap, in_=tp[0:np_out, 0:n * 128], func=AF.Copy),
              reads=[rtp], writes=[dst_res])
    else:
        P.add("dve", lambda e: e.tensor_copy(out=dst_ap, in_=tp[0:np_out, 0:n * 128]),
              reads=[rtp], writes=[dst_res])


def rope_ops(P, c, src3, src_res, H, d, tab, tab_res, ta, rta, tb, rtb, out3, out_res, out3b=None):
    h2 = d // 2
    cc = bc_mid(tab[:, 0:d], H)
    s0 = bc_mid(tab[:, d:d + h2], H)
    s1 = bc_mid(tab[:, d + h2:2 * d], H)
    P.add("dve", lambda e: e.tensor_tensor(out=ta, in0=src3, in1=cc, op=ALU.mult),
          reads=[src_res, tab_res], writes=[rta])
    P.add("dve", lambda e: e.tensor_tensor(out=tb[:, :, 0:h2], in0=src3[:, :, h2:d], in1=s0, op=ALU.mult),
          reads=[src_res, tab_res], writes=[rtb])
    P.add("dve", lambda e: e.tensor_tensor(out=tb[:, :, h2:d], in0=src3[:, :, 0:h2], in1=s1, op=ALU.mult),
          reads=[src_res, tab_res], writes=[rtb])
    P.add("pool", lambda e: e.tensor_tensor(out=out3, in0=ta, in1=tb, op=ALU.add),
          reads=[rta, rtb], writes=[out_res])
    if out3b is not None:
        P.add("pool", lambda e: e.tensor_tensor(out=out3b, in0=ta, in1=tb, op=ALU.add),
              reads=[rta, rtb], writes=[out_res])


WIN_SEGS = [
    (0, 0, 512),
    (512, 640, 512),
    (1024, 1152, 512),
    (1536, 1744, 384),
    (1920, 512, 64),
    (1984, 1664, 64),
    (2048, 2128, 256),
    (2304, 2384, 32),
    (2336, 576, 64),
    (2400, 1728, 16),
]
WIN_GROUPS = [(0, 512), (512, 512), (1024, 512), (1536, 512), (2048, 368)]
DPROJ = 2416


def proj_phase(nc, P, c, T, S):
    import os
    LIM = float(os.environ.get('PROJ_STOP', '99'))
    NT = T // 128
    with ExitStack() as ps:
        def sb(name, shape, dt):
            return ps.enter_context(nc.sbuf_tensor(name, shape, dt))

        def pm(name, shape, dt):
            return ps.enter_context(nc.psum_tensor(name, shape, dt))

        R = P.res
        Win = sb("Win", [128, 8, DPROJ], BF16)
        Wuq = sb("Wuq", [128, 3, 768], BF16)
        Wukv = sb("Wukv", [128, 2, 1024], BF16)
        gbc = sb("gbcm", [128, D], F32)
        gq = sb("gq", [128, 384], F32)
        gkv = sb("gkv", [128, 256], F32)
        rWin = [R() for _ in range(8)]
        rWuq = [R() for _ in range(3)]
        rWukv = [R() for _ in range(2)]
        rg, rgq, rgkv = R(), R(), R()

        def dbl(name, shape, dt):
            return [sb(f"{name}{i}", shape, dt) for i in range(2)], [R() for _ in range(2)]

        xt, rxt = dbl("pxt", [128, D], F32)
        junk, rjunk = dbl("pjunk", [128, D], BF16)
        ss, rss = dbl("pss", [128, 8], F32)
        rrs = [[R() for _ in range(3)] for _ in range(2)]
        hb, rhb = dbl("phb", [128, D], BF16)
        hT, rhT = dbl("phT", [128, 8, 128], BF16)
        r64, rr64 = dbl("r64", [128, 128], F32)
        r32, rr32 = dbl("r32", [128, 64], F32)
        ta, rta = dbl("ta", [128, 512], F32)
        tb, rtb = dbl("tb", [128, 512], F32)
        qar, rqar = dbl("qar", [128, 512], BF16)
        qir, rqir = dbl("qir", [128, 1024], BF16)
        qif, rqif = dbl("qif", [128, 512], F32)
        qaT, rqaT = dbl("qaT", [128, 512], BF16)
        qiT, rqiT = dbl("qiT", [128, 1024], BF16)
        aw, raw = dbl("aw", [128, 16], F32)
        sgt, rsgt = dbl("sgt", [128, 16], F32)
        kdup, rkdup = dbl("kdup", [128, 256], BF16)
        kT, rkT = dbl("kT", [128, 256], BF16)
        kpe, rkpe = dbl("kpe", [128, 32], BF16)
        va1, rva1 = dbl("va1", [128, 192], BF16)
        cqn, rcqn = dbl("cqn", [128, 384], BF16)
        cqT, rcqT = dbl("cqT", [128, 384], BF16)
        ckn, rckn = dbl("ckn", [128, 256], BF16)
        ckT, rckT = dbl("ckT", [128, 256], BF16)
        qbr, rqbr = dbl("qbr", [128, 8, 128], BF16)
        kbr, rkbr = dbl("kbr", [128, 8, 128], BF16)
        qbT, rqbT = dbl("qbT", [128, 1024], BF16)
        kbT, rkbT = dbl("kbT", [128, 1024], BF16)
        vb1, rvb1 = dbl("vb1", [128, 8, 192], BF16)
        tpt = [pm(f"ptp{i}", [128, 1024], BF16) for i in range(2)]
        tpp = BankPool(tpt, [R() for _ in range(2)])
        mmt = [pm(f"pmm{i}", [128, 512], F32) for i in range(6)]
        mmp = BankPool(mmt, [R() for _ in range(6)])

        P.dma("sp", gbc[:], bcast_rows(S.g_mix, D), writes=[rg], key="gbc")
        P.dma("sp", gq[:], bcast_rows(S.g_q_lat, 384), writes=[rgq], key="gq")
        P.dma("sp", gkv[:], bcast_rows(S.g_kv_lat, 256), writes=[rgkv], key="gkv")
        stg, rstg = make_stage(nc, P, c, ps, "pj")
        load_weight_cast(P, c, Win, [[r] for r in rWin], S.w_in, 8, [(dc, sc, w, 0) for (dc, sc, w) in WIN_SEGS],
                         stg, rstg)
        load_weight_cast(P, c, Wuq, [[r] for r in rWuq], S.w_uq, 3, [(0, 0, 384, 0), (384, 384, 384, 0)], stg, rstg)
        load_weight_cast(P, c, Wukv, [[r] for r in rWukv], S.w_ukv, 2, [(0, 0, 512, 0), (512, 512, 512, 0)], stg, rstg)
        for b in range(2):
            P.add("pool", lambda e, b=b: e.memset(qbr[b][:], 0.0), writes=[rqbr[b]])
            P.add("pool", lambda e, b=b: e.memset(kbr[b][:], 0.0), writes=[rkbr[b]])
            P.add("pool", lambda e, b=b: e.memset(va1[b][:], 1.0), writes=[rva1[b]])
            P.add("pool", lambda e, b=b: e.memset(vb1[b][:], 1.0), writes=[rvb1[b]])

        for i in range(NT):
            b = i % 2
            rows = slice(i * 128, (i + 1) * 128)
            cols = slice(i * 128, (i + 1) * 128)
            P.dma("sp", xt[b][:], S.X1[rows, :], reads=[S.rX1[i]], writes=[rxt[b]], key=f"xt{b}")
            P.dma("sp", r64[b][:], S.rope64[rows, :], writes=[rr64[b]], key=f"r64{b}")
            P.dma("sp", r32[b][:], S.rope32[rows, :], writes=[rr32[b]], key=f"r32{b}")
            rmsnorm_to_bf16(P, c, xt[b][:], rxt[b], D, gbc[:], rg, junk[b][:], rjunk[b], ss[b][:, 0:1], rss[b],
                            ss[b][:, 1:2], rrs[b][0], hb[b][:], rhb[b])
            transposes_to(P, c, tpp, [hb[b][:, k * 128:(k + 1) * 128] for k in range(8)], [rhb[b]],
                          hT[b][:].rearrange("p k t -> p (k t)"), rhT[b])
            if LIM < 2:
                continue
            banks = []
            for (g0, gw) in WIN_GROUPS:
                bk, rbk = mmp.next()
                for k in range(8):
                    P.add("pe", lambda e, k=k, bk=bk, g0=g0, gw=gw, b=b: e.matmul(
                        bk[:, 0:gw], lhsT=hT[b][:, k, :], rhs=Win[:, k, g0:g0 + gw], start=(k == 0), stop=(k == 7)),
                          reads=[rhT[b], rWin[k]], writes=[rbk])
                banks.append((bk, rbk))
            (B0, rB0), (B1, rB1), (B2, rB2), (B3, rB3), (B4, rB4) = banks
            v3 = lambda ap, H: ap.rearrange("p (h d) -> p h d", h=H)
            if LIM < 3:
                continue
            P.add("act", lambda e, b=b, B4=B4: e.activation(out=aw[b][:], in_=B4[:, 352:368], func=AF.Abs,
                                                           scale=1.0 / 32.0),
                  reads=[rB4], writes=[raw[b]])
            P.add("act", lambda e, b=b, B4=B4: e.activation(out=sgt[b][:], in_=B4[:, 352:368], func=AF.Sign),
                  reads=[rB4], writes=[rsgt[b]])
            P.dma("sp", S.SG[rows, :], sgt[b][:], reads=[rsgt[b]], writes=[S.rSG[i]], key=f"sgt{b}")
            if LIM < 4:
                continue
            rope_ops(P, c, v3(B0[:, 0:512], 8), rB0, 8, 64, r64[b], rr64[b], v3(ta[b][:], 8), rta[b],
                     v3(tb[b][:], 8), rtb[b], v3(qar[b][:], 8), rqar[b])
            transposes_to(P, c, tpp, [qar[b][:, j * 128:(j + 1) * 128] for j in range(4)], [rqar[b]],
                          qaT[b][:], rqaT[b], copy_eng="dve")
            P.dma("sp", S.QA_T[i], qaT[b][:], reads=[rqaT[b]], writes=[S.rQA[i]], key=f"qaT{b}")
            if LIM < 5:
                continue
            for hh, (Bq, rBq) in enumerate(((B1, rB1), (B2, rB2))):
                rope_ops(P, c, v3(Bq[:, 0:512], 8), rBq, 8, 64, r64[b], rr64[b], v3(ta[b][:], 8), rta[b],
                         v3(tb[b][:], 8), rtb[b], v3(qif[b][:], 8), rqif[b])
                P.add("dve", lambda e, b=b, hh=hh: e.tensor_tensor(
                    out=v3(qir[b][:, hh * 512:(hh + 1) * 512], 8), in0=v3(qif[b][:], 8),
                    in1=bc_last(aw[b][:, hh * 8:(hh + 1) * 8], 64), op=ALU.mult),
                      reads=[rqif[b], raw[b]], writes=[rqir[b]])
            transposes_to(P, c, tpp, [qir[b][:, j * 128:(j + 1) * 128] for j in range(8)], [rqir[b]],
                          qiT[b][:], rqiT[b])
            P.dma("sp", S.QI_T[i], qiT[b][:], reads=[rqiT[b]], writes=[S.rQI[i]], key=f"qiT{b}")
            if LIM < 6:
                continue
            kd4 = kdup[b][:].rearrange("p (a r d) -> p a r d", a=2, r=2)
            rope_ops(P, c, v3(B3[:, 384:512], 2), rB3, 2, 64, r64[b], rr64[b], v3(ta[b][:, 0:128], 2), rta[b],
                     v3(tb[b][:, 0:128], 2), rtb[b], kd4[:, :, 0, :], rkdup[b], out3b=kd4[:, :, 1, :])
            transposes_to(P, c, tpp, [kdup[b][:, 0:128], kdup[b][:, 128:256]], [rkdup[b]], kT[b][:], rkT[b],
                          copy_eng="dve")
            P.dma("sp", S.KA_T2[:, cols], kT[b][:, 0:128], reads=[rkT[b]], writes=[S.rKA[i]], key=f"kTa{b}")
            P.dma("sp", S.KI_T2[:, cols], kT[b][:, 128:256], reads=[rkT[b]], writes=[S.rKI[i]], key=f"kTi{b}")
            if LIM < 7:
                continue
            rope_ops(P, c, v3(B4[:, 256:288], 1), rB4, 1, 32, r32[b], rr32[b], v3(ta[b][:, 0:32], 1), rta[b],
                     v3(tb[b][:, 0:32], 1), rtb[b], v3(kpe[b][:], 1), rkpe[b])
            if LIM < 8:
                continue
            P.add("act", lambda e, b=b, B4=B4: e.activation(out=va1[b][:, 64:128], in_=B4[:, 288:352], func=AF.Copy),
                  reads=[rB4], writes=[rva1[b]])
            P.dma("sp", S.VA1[rows, :], va1[b][:], reads=[rva1[b]], writes=[S.rVA[i]], key=f"va1{b}")
            if LIM < 9:
                continue
            rmsnorm_to_bf16(P, c, B3[:, 0:384], rB3, 384, gq[:], rgq, junk[b][:, 0:384], rjunk[b], ss[b][:, 2:3],
                            rss[b], ss[b][:, 3:4], rrs[b][1], cqn[b][:], rcqn[b])
            if LIM < 9.1:
                continue
            transposes_to(P, c, tpp, [cqn[b][:, k * 128:(k + 1) * 128] for k in range(3)], [rcqn[b]],
                          cqT[b][:], rcqT[b], copy_eng="dve")
            if LIM < 9.2:
                continue
            for (q0, qw, h0, nh) in ((0, 480, 0, 5), (480, 288, 5, 3)):
                bk, rbk = mmp.next()
                for k in range(3):
                    P.add("pe", lambda e, k=k, bk=bk, q0=q0, qw=qw, b=b: e.matmul(
                        bk[:, 0:qw], lhsT=cqT[b][:, k * 128:(k + 1) * 128], rhs=Wuq[:, k, q0:q0 + qw],
                        start=(k == 0), stop=(k == 2)),
                          reads=[rcqT[b], rWuq[k]], writes=[rbk])
                if LIM < 9.3:
                    continue
                bv = bk[:, 0:qw].rearrange("p (h d) -> p h d", h=nh)
                P.add("dve", lambda e, bv=bv, b=b, h0=h0, nh=nh: e.tensor_copy(
                    out=qbr[b][:, h0:h0 + nh, 0:64], in_=bv[:, :, 0:64]),
                      reads=[rbk], writes=[rqbr[b]])
                if LIM < 9.4:
                    continue
                rope_ops(P, c, bv[:, :, 64:96], rbk, nh, 32, r32[b], rr32[b],
                         ta[b][:, 0:nh * 32].rearrange("p (h d) -> p h d", h=nh), rta[b],
                         tb[b][:, 0:nh * 32].rearrange("p (h d) -> p h d", h=nh), rtb[b],
                         qbr[b][:, h0:h0 + nh, 64:96], rqbr[b])
            if LIM < 9.5:
                continue
            transposes_to(P, c, tpp, [qbr[b][:, h, :] for h in range(8)], [rqbr[b]], qbT[b][:], rqbT[b])
            if LIM < 9.6:
                continue
            P.dma("sp", S.QB_T[:, :, cols].rearrange("h p t -> p h t"),
                  qbT[b][:].rearrange("p (h t) -> p h t", h=8), reads=[rqbT[b]], writes=[S.rQB[i]], key=f"qbT{b}")
            if LIM < 10:
                continue
            rmsnorm_to_bf16(P, c, B4[:, 0:256], rB4, 256, gkv[:], rgkv, junk[b][:, 0:256], rjunk[b], ss[b][:, 4:5],
                            rss[b], ss[b][:, 5:6], rrs[b][2], ckn[b][:], rckn[b])
            transposes_to(P, c, tpp, [ckn[b][:, k * 128:(k + 1) * 128] for k in range(2)], [rckn[b]],
                          ckT[b][:], rckT[b], copy_eng="dve")
            P.add("pool", lambda e, b=b: e.tensor_copy(out=kbr[b][:, :, 64:96], in_=bc_mid(kpe[b][:], 8)),
                  reads=[rkpe[b]], writes=[rkbr[b]])
            for hf in range(2):
                bk, rbk = mmp.next()
                for k in range(2):
                    P.add("pe", lambda e, k=k, bk=bk, hf=hf, b=b: e.matmul(
                        bk[:, :], lhsT=ckT[b][:, k * 128:(k + 1) * 128], rhs=Wukv[:, k, hf * 512:(hf + 1) * 512],
                        start=(k == 0), stop=(k == 1)),
                          reads=[rckT[b], rWukv[k]], writes=[rbk])
                bv = bk[:, :].rearrange("p (h d) -> p h d", h=4)
                P.add("dve", lambda e, bv=bv, b=b, hf=hf: e.tensor_copy(
                    out=kbr[b][:, hf * 4:(hf + 1) * 4, 0:64], in_=bv[:, :, 0:64]),
                      reads=[rbk], writes=[rkbr[b]])
                P.add("dve", lambda e, bv=bv, b=b, hf=hf: e.tensor_copy(
                    out=vb1[b][:, hf * 4:(hf + 1) * 4, 64:128], in_=bv[:, :, 64:128]),
                      reads=[rbk], writes=[rvb1[b]])
            transposes_to(P, c, tpp, [kbr[b][:, h, :] for h in range(8)], [rkbr[b]], kbT[b][:], rkbT[b])
            P.dma("sp", S.KB_T[:, :, cols].rearrange("h p t -> p h t"),
                  kbT[b][:].rearrange("p (h t) -> p h t", h=8), reads=[rkbT[b]], writes=[S.rKB[i]], key=f"kbT{b}")
            P.dma("sp", S.VB1[rows, :, :], vb1[b][:], reads=[rvb1[b]], writes=[S.rVB[i]], key=f"vb1{b}")
        return P.end_phase(ps)


NEG = -1.0e30
NBIS = 16


def normalize_out(P, c, O, rO, num_lo, rc, rrc, out_ap, out_res):
    den_lo = 64 - num_lo
    P.add("dve", lambda e: e.reciprocal(out=rc[den_lo:den_lo + 64, :], in_=O[den_lo:den_lo + 64, :]),
          reads=[rO], writes=[rrc])
    P.add("dve", lambda e: e.tensor_tensor(out=out_ap, in0=O[num_lo:num_lo + 64, :],
                                           in1=rc[den_lo:den_lo + 64, :], op=ALU.mult),
          reads=[rO, rrc], writes=[out_res])


def dsa_phase(nc, P, c, T, S):
    NT = T // 128
    TOPK = min(256, T // 4)
    QT0 = TOPK // 128
    with ExitStack() as ps:
        def sb(name, shape, dt):
            return ps.enter_context(nc.sbuf_tensor(name, shape, dt))

        def pm(name, shape, dt):
            return ps.enter_context(nc.psum_tensor(name, shape, dt))

        R = P.res

        def dbl(name, shape, dt):
            return [sb(f"{name}{i}", shape, dt) for i in range(2)], [R() for _ in range(2)]

        KA2 = sb("KA2", [128, T], BF16)
        KI2 = sb("KI2", [128, T], BF16)
        VAs = sb("VAs", [128, NT, 192], BF16)
        rKA2 = [R() for _ in range(NT)]
        rKI2 = [R() for _ in range(NT)]
        rVAs = [R() for _ in range(NT)]
        cneg = sb("cneg", [128, 128], F32)
        pow2 = sb("pow2", [128, NBIS], F32)
        rcn = R()
        qiT, rqiT = dbl("dqiT", [128, 1024], BF16)
        qaT, rqaT = dbl("dqaT", [128, 512], BF16)
        sg, rsg = dbl("dsg", [128, 16], F32)
        Rt = [sb(f"Rt{i}", [128, 512], BF16) for i in range(4)]
        Dg, rDg = dbl("Dg", [128, 16, 128], BF16)
        rRt = [R() for _ in range(4)]
        Isb, rIsb = dbl("Isb", [128, T], F32)
        cjunk = sb("cjunk", [128, T], BF16)
        rcj = R()
        st_, rst = dbl("dst", [128, 8 + NBIS], F32)
        maskq, rmq = dbl("maskq", [128, T], BF16)
        maskT, rmT = dbl("maskT", [128, NT, 128], BF16)
        PT = [sb(f"PT{i}", [128, 1024], BF16) for i in range(4)]
        rPT = [R() for _ in range(4)]
        rc, rrc = dbl("drc", [128, 512], F32)
        aT, raT = dbl("daT", [128, 512], BF16)
        Lt = [pm(f"dL{i}", [128, 512], F32) for i in range(4)]
        Lp = BankPool(Lt, [R() for _ in range(4)])
        At = [pm(f"dA{i}", [128, 512], F32) for i in range(2)]
        rAt = [R() for _ in range(2)]
        OE = pm("dOE", [128, 512], F32)
        OO = pm("dOO", [128, 512], F32)
        rOE, rOO = R(), R()

        P.add("pool", lambda e: e.memset(cneg[:], 0.0), writes=[rcn])
        P.add("pool", lambda e: e.affine_select(out=cneg[:], in_=cneg[:], pattern=[[-1, 128]],
                                                compare_op=ALU.is_ge, fill=NEG, base=0, channel_multiplier=1),
              reads=[rcn], writes=[rcn])
        for k in range(NBIS):
            P.add("pool", lambda e, k=k: e.memset(pow2[:, k:k + 1], 2.0 ** (-(k + 1))), writes=[rcn])
        CH = min(T, 1024)
        for ci in range(T // CH):
            cols = slice(ci * CH, (ci + 1) * CH)
            tl = range(ci * (CH // 128), (ci + 1) * (CH // 128))
            rk, rki, rv = R(), R(), R()
            P.dma("sp", KA2[:, cols], S.KA_T2[:, cols], reads=[S.rKA[i] for i in tl], writes=[rk], key=f"KA2_{ci}")
            P.dma("sp", KI2[:, cols], S.KI_T2[:, cols], reads=[S.rKI[i] for i in tl], writes=[rki], key=f"KI2_{ci}")
            P.dma("sp", VAs[:, ci * (CH // 128):(ci + 1) * (CH // 128), :],
                  S.VA1[cols, :].rearrange("(n p) c -> p n c", p=128), reads=[S.rVA[i] for i in tl], writes=[rv],
                  key=f"VAs_{ci}")
            for i in tl:
                rKA2[i], rKI2[i], rVAs[i] = rk, rki, rv

        cnt = {"ri": 0, "pti": 0, "ai": 0}

        def stage_A(qt):
            b = qt % 2
            SL = (qt + 1) * 128
            P.dma("sp", qiT[b][:], S.QI_T[qt], reads=[S.rQI[qt]], writes=[rqiT[b]], key=f"dqiT{b}")
            P.dma("sp", sg[b][:], S.SG[qt * 128:(qt + 1) * 128, :], reads=[S.rSG[qt]], writes=[rsg[b]], key=f"dsg{b}")
            P.add("dve", lambda e: e.tensor_tensor(out=Dg[b][:], in0=bc_mid(c.ident[:], 16), in1=bc_last(sg[b][:], 128),
                                                   op=ALU.mult),
                  reads=[c.rident, rsg[b]], writes=[rDg[b]])
            steps = []
            for sbk, s0 in enumerate(range(0, SL, 512)):
                for h in range(16):
                    steps.append((sbk, s0, h))

            def lmm(sbk, s0, h):
                sw = min(512, SL - s0)
                kres = [rKI2[j] for j in range(s0 // 128, (s0 + sw) // 128)]
                hp, par = h // 2, h % 2
                pl = par * 64
                L, rL = Lp.next()
                P.add("pe", lambda e: e.matmul(L[:, 0:sw], lhsT=qiT[b][pl:pl + 64, hp * 128:(hp + 1) * 128],
                                               rhs=KI2[pl:pl + 64, s0:s0 + sw], start=True, stop=True),
                      reads=[rqiT[b]] + kres, writes=[rL])
                r = cnt['ri'] % 4
                cnt['ri'] += 1
                P.add("act", lambda e: e.activation(out=Rt[r][:, 0:sw], in_=L[:, 0:sw], func=AF.Relu),
                      reads=[rL], writes=[rRt[r]])
                return r

            def acc(sbk, s0, h, r):
                sw = min(512, SL - s0)
                A, rA = At[(cnt['ai'] + sbk) % 2], rAt[(cnt['ai'] + sbk) % 2]
                P.add("pe", lambda e: e.matmul(A[:, 0:sw], lhsT=Dg[b][:, h, :], rhs=Rt[r][:, 0:sw], start=(h == 0),
                                               stop=(h == 15)),
                      reads=[rDg[b], rRt[r]], writes=[rA])
                if h == 15:
                    P.add("act", lambda e: e.activation(out=Isb[b][:, s0:s0 + sw], in_=A[:, 0:sw], func=AF.Copy),
                          reads=[rA], writes=[rIsb[b]])

            LOOKA = 2
            pend = {}
            for i in range(min(LOOKA, len(steps))):
                pend[i] = lmm(*steps[i])
            for i, stp in enumerate(steps):
                if i + LOOKA < len(steps):
                    pend[i + LOOKA] = lmm(*steps[i + LOOKA])
                acc(*stp, pend.pop(i))
            cnt['ai'] += len(range(0, SL, 512))

        def stage_B(qt):
            b = qt % 2
            SL = (qt + 1) * 128
            P.dma("sp", qaT[b][:], S.QA_T[qt], reads=[S.rQA[qt]], writes=[rqaT[b]], key=f"dqaT{b}")
            S_ = st_[b]
            if qt >= QT0:
                P.add("dve", lambda e, b=b, SL=SL, S_=S_: e.tensor_reduce(out=S_[:, 0:1], in_=Isb[b][:, 0:SL], axis=AX.X,
                                                                        op=ALU.min),
                      reads=[rIsb[b]], writes=[rst[b]])
                P.add("dve", lambda e, b=b, SL=SL, S_=S_: e.tensor_reduce(out=S_[:, 1:2], in_=Isb[b][:, 0:SL], axis=AX.X,
                                                                        op=ALU.max),
                      reads=[rIsb[b]], writes=[rst[b]])
            P.add("pool", lambda e, b=b, qt=qt: e.tensor_tensor(out=Isb[b][:, qt * 128:(qt + 1) * 128],
                                                              in0=Isb[b][:, qt * 128:(qt + 1) * 128], in1=cneg[:],
                                                              op=ALU.add),
                  reads=[rIsb[b], rcn], writes=[rIsb[b]])
            if qt >= QT0:
                P.add("dve", lambda e, S_=S_: e.tensor_tensor(out=S_[:, 2:3], in0=S_[:, 1:2], in1=S_[:, 0:1],
                                                             op=ALU.subtract),
                      reads=[rst[b]], writes=[rst[b]])
                P.add("dve", lambda e, S_=S_: e.tensor_scalar(out=S_[:, 8:8 + NBIS], in0=pow2[:], scalar1=S_[:, 2:3],
                                                             scalar2=None, op0=ALU.mult),
                      reads=[rst[b], rcn], writes=[rst[b]])
                P.add("dve", lambda e, S_=S_: e.tensor_copy(out=S_[:, 3:4], in_=S_[:, 0:1]),
                      reads=[rst[b]], writes=[rst[b]])
                for k in range(NBIS):
                    P.add("dve", lambda e, S_=S_, k=k: e.tensor_tensor(out=S_[:, 4:5], in0=S_[:, 3:4],
                                                                      in1=S_[:, 8 + k:9 + k], op=ALU.add),
                          reads=[rst[b]], writes=[rst[b]])
                    P.add("dve", lambda e, S_=S_, b=b, SL=SL: e.tensor_scalar(
                        out=cjunk[:, 0:SL], in0=Isb[b][:, 0:SL], scalar1=S_[:, 4:5], scalar2=None,
                        op0=ALU.is_ge, op1=ALU.add, accum_out=S_[:, 5:6]),
                          reads=[rst[b], rIsb[b]], writes=[rst[b], rcj])
                    P.add("dve", lambda e, S_=S_, k=k: e.tensor_scalar(
                        out=S_[:, 6:7], in0=S_[:, 5:6], scalar1=float(TOPK), scalar2=S_[:, 8 + k:9 + k],
                        op0=ALU.is_ge, op1=ALU.mult),
                          reads=[rst[b]], writes=[rst[b]])
                    P.add("dve", lambda e, S_=S_: e.tensor_tensor(out=S_[:, 3:4], in0=S_[:, 3:4], in1=S_[:, 6:7],
                                                                 op=ALU.add),
                          reads=[rst[b]], writes=[rst[b]])
            else:
                P.add("dve", lambda e, S_=S_: e.memset(S_[:, 3:4], -1.0e29), writes=[rst[b]])
            P.add("dve", lambda e, S_=S_, b=b, SL=SL: e.tensor_scalar(
                out=maskq[b][:, 0:SL], in0=Isb[b][:, 0:SL], scalar1=S_[:, 3:4], scalar2=None, op0=ALU.is_ge),
                  reads=[rst[b], rIsb[b]], writes=[rmq[b]])

        def stage_C(qt):
            b = qt % 2
            SL = (qt + 1) * 128
            for s8 in range(0, qt + 1, 8):
                n8 = min(8, qt + 1 - s8)
                L, rL = Lp.next()
                Lb = L[:, :].bitcast(BF16)
                for j in range(n8):
                    P.add("pe", lambda e, Lb=Lb, j=j, s8=s8, b=b: e.transpose(
                        out=Lb[:, j * 128:(j + 1) * 128], in_=maskq[b][:, (s8 + j) * 128:(s8 + j + 1) * 128],
                        identity=c.ident[:]),
                          reads=[rmq[b], c.rident], writes=[rL])
                P.add("act", lambda e, Lb=Lb, s8=s8, n8=n8, b=b: e.activation(
                    out=maskT[b][:, s8:s8 + n8, :].rearrange("p n q -> p (n q)"), in_=Lb[:, 0:n8 * 128],
                    func=AF.Copy),
                      reads=[rL], writes=[rmT[b]])
            def qk(st):
                sc = slice(st * 128, (st + 1) * 128)
                LE, rLE = Lp.next()
                LO, rLO = Lp.next()
                P.add("pe", lambda e: e.matmul(LE[:, :], lhsT=KA2[0:64, sc], rhs=qaT[b][0:64, :], start=True, stop=True),
                      reads=[rKA2[st], rqaT[b]], writes=[rLE])
                P.add("pe", lambda e: e.matmul(LO[:, :], lhsT=KA2[64:128, sc], rhs=qaT[b][64:128, :], start=True,
                                               stop=True),
                      reads=[rKA2[st], rqaT[b]], writes=[rLO])
                p = cnt["pti"] % len(PT)
                cnt["pti"] += 1
                P.add("act", lambda e: e.activation(out=PT[p][:, 0:512], in_=LE[:, :], func=AF.Exp, scale=0.125),
                      reads=[rLE], writes=[rPT[p]])
                P.add("act", lambda e: e.activation(out=PT[p][:, 512:1024], in_=LO[:, :], func=AF.Exp, scale=0.125),
                      reads=[rLO], writes=[rPT[p]])
                P.add("pool", lambda e: e.tensor_tensor(
                    out=PT[p][:].rearrange("p (a q) -> p a q", a=8), in0=PT[p][:].rearrange("p (a q) -> p a q", a=8),
                    in1=bc_mid(maskT[b][:, st, :], 8), op=ALU.mult),
                      reads=[rPT[p], rmT[b]], writes=[rPT[p]])
                return p

            def pv(st, p):
                P.add("pe", lambda e: e.matmul(OE[:, :], lhsT=VAs[:, st, 64:192], rhs=PT[p][:, 0:512],
                                               start=(st == 0), stop=(st == qt)),
                      reads=[rVAs[st], rPT[p]], writes=[rOE])
                P.add("pe", lambda e: e.matmul(OO[:, :], lhsT=VAs[:, st, 0:128], rhs=PT[p][:, 512:1024],
                                               start=(st == 0), stop=(st == qt)),
                      reads=[rVAs[st], rPT[p]], writes=[rOO])

            pend = {0: qk(0)}
            for st in range(qt + 1):
                if st + 1 <= qt:
                    pend[st + 1] = qk(st + 1)
                pv(st, pend.pop(st))
            normalize_out(P, c, OE, rOE, 0, rc[b], rrc[b], aT[b][0:64, :], raT[b])
            normalize_out(P, c, OO, rOO, 64, rc[b], rrc[b], aT[b][64:128, :], raT[b])
            P.dma("sp", S.ATT_T[0:4, :, qt * 128:(qt + 1) * 128].rearrange("j p t -> p j t"),
                  aT[b][:].rearrange("p (j t) -> p j t", j=4), reads=[raT[b]], writes=[S.rATa[qt]], key=f"daT{b}")

        for step in range(NT + 2):
            if step < NT:
                stage_A(step)
            if 0 <= step - 1 < NT:
                stage_B(step - 1)
            if 0 <= step - 2 < NT:
                stage_C(step - 2)
        return P.end_phase(ps)


SCALE_B = 96.0 ** -0.5


def mla_phase(nc, P, c, T, S):
    NT = T // 128
    NQB = T // 512
    with ExitStack() as ps:
        def sb(name, shape, dt):
            return ps.enter_context(nc.sbuf_tensor(name, shape, dt))

        def pm(name, shape, dt):
            return ps.enter_context(nc.psum_tensor(name, shape, dt))

        R = P.res

        def dbl(name, shape, dt):
            return [sb(f"{name}{i}", shape, dt) for i in range(2)], [R() for _ in range(2)]

        KB = [sb(f"mKB{i}", [128, T], BF16) for i in range(2)]
        VB = [sb(f"mVB{i}", [128, NT, 192], BF16) for i in range(2)]
        rKB = [[R() for _ in range(NQB)] for _ in range(2)]
        rVB = [[R() for _ in range(NQB)] for _ in range(2)]
        Cm = sb("Cm", [128, 4, 512], BF16)
        rCm = R()
        QT, rQT = dbl("mQT", [128, 512], BF16)
        PT = [sb(f"mPT{i}", [128, 512], BF16) for i in range(4)]
        rPT = [R() for _ in range(4)]
        rc, rrc = dbl("mrc", [128, 512], F32)
        aT, raT = dbl("maT", [128, 512], BF16)
        Lt = [pm(f"mL{i}", [128, 512], F32) for i in range(4)]
        Lp = BankPool(Lt, [R() for _ in range(4)])
        Ot = [pm(f"mO{i}", [128, 512], F32) for i in range(2)]
        rOt = [R() for _ in range(2)]

        P.add("pool", lambda e: e.memset(Cm[:], 1.0), writes=[rCm])
        P.add("pool", lambda e: e.affine_select(out=Cm[:], in_=Cm[:], pattern=[[-128, 4], [1, 512]],
                                                compare_op=ALU.is_ge, fill=0.0, base=0, channel_multiplier=-1),
              reads=[rCm], writes=[rCm])
        CH = min(T, 1024)
        NCH = T // CH

        def load_head(h):
            hb_ = h % 2
            for ci in range(NCH):
                cs = slice(ci * CH, (ci + 1) * CH)
                tl = range(ci * (CH // 128), (ci + 1) * (CH // 128))
                P.dma("sp", KB[hb_][:, cs], S.KB_T[h, :, cs], reads=[S.rKB[i] for i in tl],
                      writes=[rKB[hb_][ci]], key=f"mKB{hb_}_{ci}")
                P.dma("sp", VB[hb_][:, ci * (CH // 128):(ci + 1) * (CH // 128), :],
                      S.VB1[cs, h, :].rearrange("(n p) c -> p n c", p=128),
                      reads=[S.rVB[i] for i in tl], writes=[rVB[hb_][ci]], key=f"mVB{hb_}_{ci}")

        groups = [(h, qb) for h in range(8) for qb in range(NQB)]

        def load_q(g):
            h, qb = groups[g]
            bq = g % 2
            cs = slice(qb * 512, (qb + 1) * 512)
            P.dma("sp", QT[bq][:], S.QB_T[h, :, cs], reads=[S.rQB[i] for i in range(qb * 4, qb * 4 + 4)],
                  writes=[rQT[bq]], key=f"mQT{bq}")

        steps = [(g, st) for g, (h, qb) in enumerate(groups) for st in range(4 * (qb + 1))]
        cnt = {"pti": 0}

        def qk(g, st):
            h, qb = groups[g]
            hb_, bq = h % 2, g % 2
            L, rL = Lp.next()
            P.add("pe", lambda e: e.matmul(L[:, :], lhsT=KB[hb_][:, st * 128:(st + 1) * 128], rhs=QT[bq][:, :],
                                           start=True, stop=True),
                  reads=[rKB[hb_][(st * 128) // CH], rQT[bq]], writes=[rL])
            p = cnt["pti"] % len(PT)
            cnt["pti"] += 1
            P.add("act", lambda e: e.activation(out=PT[p][:], in_=L[:, :], func=AF.Exp, scale=SCALE_B),
                  reads=[rL], writes=[rPT[p]])
            j = st - 4 * qb
            if j >= 0:
                P.add("pool", lambda e: e.tensor_tensor(out=PT[p][:], in0=PT[p][:], in1=Cm[:, j, :], op=ALU.mult),
                      reads=[rPT[p], rCm], writes=[rPT[p]])
            return p

        def pv(g, st, p):
            h, qb = groups[g]
            hb_, bq = h % 2, g % 2
            nst = 4 * (qb + 1)
            O, rO = Ot[g % 2], rOt[g % 2]
            vsl = slice(64, 192) if h % 2 == 0 else slice(0, 128)
            num_lo = 0 if h % 2 == 0 else 64
            P.add("pe", lambda e: e.matmul(O[:, :], lhsT=VB[hb_][:, st, vsl], rhs=PT[p][:], start=(st == 0),
                                           stop=(st == nst - 1)),
                  reads=[rVB[hb_][(st * 128) // CH], rPT[p]], writes=[rO])
            if st == nst - 1:
                cs = slice(qb * 512, (qb + 1) * 512)
                normalize_out(P, c, O, rO, num_lo, rc[bq], rrc[bq], aT[bq][num_lo:num_lo + 64, :], raT[bq])
                P.dma("sp", S.ATT_T[4 + h // 2, num_lo:num_lo + 64, cs], aT[bq][num_lo:num_lo + 64, :],
                      reads=[raT[bq]], writes=[S.rATb[h][qb]], key=f"maT{bq}")

        LOOK = 2
        load_head(0)
        load_q(0)
        issued = {}
        loaded_q = {0}
        loaded_h = {0}

        def ensure_loads(g):
            if g >= len(groups):
                return
            h = groups[g][0]
            if h not in loaded_h:
                loaded_h.add(h)
                load_head(h)
            if g not in loaded_q:
                loaded_q.add(g)
                load_q(g)

        for i in range(min(LOOK, len(steps))):
            ensure_loads(steps[i][0])
            issued[i] = qk(*steps[i])
        for i, (g, st) in enumerate(steps):
            if i + LOOK < len(steps):
                ensure_loads(steps[i + LOOK][0])
                issued[i + LOOK] = qk(*steps[i + LOOK])
            pv(g, st, issued.pop(i))
            hh = groups[g][0]
            if st == 0 and groups[g][1] == 0 and hh + 1 < 8 and (hh + 1) not in loaded_h:
                loaded_h.add(hh + 1)
                load_head(hh + 1)
        return P.end_phase(ps)


def wout_phase(nc, P, c, T, S):
    NT = T // 128
    with ExitStack() as ps:
        def sb(name, shape, dt):
            return ps.enter_context(nc.sbuf_tensor(name, shape, dt))

        def pm(name, shape, dt):
            return ps.enter_context(nc.psum_tensor(name, shape, dt))

        R = P.res

        def dbl(name, shape, dt):
            return [sb(f"{name}{i}", shape, dt) for i in range(2)], [R() for _ in range(2)]

        Wo = sb("Wo", [128, 8, D], BF16)
        rWo = [R() for _ in range(8)]
        xt, rxt = dbl("wxt", [128, D], F32)
        at, rat = dbl("wat", [128, 8, 128], BF16)
        mmt = [pm(f"wmm{i}", [128, 512], F32) for i in range(4)]
        mmp = BankPool(mmt, [R() for _ in range(4)])
        stg, rstg = make_stage(nc, P, c, ps, "wo")
        load_weight_cast(P, c, Wo, [[r] for r in rWo], S.w_out, 8, [(0, 0, 512, 0), (512, 512, 512, 0)], stg, rstg)
        for i in range(NT):
            b = i % 2
            rows = slice(i * 128, (i + 1) * 128)
            P.dma("sp", xt[b][:], S.X1[rows, :], reads=[S.rX1[i]], writes=[rxt[b]], key=f"wxt{b}")
            P.dma("sp", at[b][:], S.ATT_T[:, :, rows].rearrange("c p t -> p c t"),
                  reads=[S.rATa[i]] + [S.rATb[h][i // 4] for h in range(8)], writes=[rat[b]], key=f"wat{b}")
            for half in range(2):
                bk, rbk = mmp.next()
                for cc in range(8):
                    P.add("pe", lambda e, bk=bk, cc=cc, b=b, half=half: e.matmul(
                        bk[:, :], lhsT=at[b][:, cc, :], rhs=Wo[:, cc, half * 512:(half + 1) * 512],
                        start=(cc == 0), stop=(cc == 7)),
                          reads=[rat[b], rWo[cc]], writes=[rbk])
                P.add("dve", lambda e, bk=bk, b=b, half=half: e.tensor_tensor(
                    out=xt[b][:, half * 512:(half + 1) * 512], in0=bk[:, :], in1=xt[b][:, half * 512:(half + 1) * 512],
                    op=ALU.add),
                      reads=[rbk, rxt[b]], writes=[rxt[b]])
            P.dma("sp", S.X2[rows, :], xt[b][:], reads=[rxt[b]], writes=[S.rX2[i]], key=f"wxo{b}")
        return P.end_phase(ps)


IN_NAMES = ["x", "g_ffn1", "w1_gate", "w1_up", "w1_down", "g_mix", "w_in", "g_q_lat", "g_kv_lat", "w_uq", "w_ukv",
            "w_out", "g_ffn2", "w2_gate", "w2_up", "w2_down", "g_final"]
IN_SHAPES = {"g_ffn1": [D], "w1_gate": [D, DFF], "w1_up": [D, DFF], "w1_down": [DFF, D], "g_mix": [D],
             "w_in": [D, DPROJ], "g_q_lat": [384], "g_kv_lat": [256], "w_uq": [384, 768], "w_ukv": [256, 1024],
             "w_out": [D, D], "g_ffn2": [D], "w2_gate": [D, DFF], "w2_up": [D, DFF], "w2_down": [DFF, D],
             "g_final": [D]}


def build(T, debug=False, phases=("ffn1", "proj", "dsa", "mla", "wout", "ffn2")):
    NT = T // 128
    NQB = T // 512
    nc = bass.Bass("TRN2", target_bir_lowering=False)
    S = Ctx()
    x = nc.dram_tensor("x", [T, D], F32, kind="ExternalInput").ap()
    for n, shp in IN_SHAPES.items():
        setattr(S, n, nc.dram_tensor(n, list(shp), F32, kind="ExternalInput").ap())
    S.rope64 = nc.dram_tensor("rope64", [T, 128], F32, kind="ExternalInput").ap()
    S.rope32 = nc.dram_tensor("rope32", [T, 64], F32, kind="ExternalInput").ap()
    out = nc.dram_tensor("out", [T, D], F32, kind="ExternalOutput").ap()
    kind = "ExternalOutput" if debug else "Internal"

    def scr(name, shape, dt):
        return nc.dram_tensor(name, list(shape), dt, kind=kind).ap()

    S.X1 = scr("X1", [T, D], F32)
    S.X2 = scr("X2", [T, D], F32)
    S.QA_T = scr("QA_T", [NT, 128, 512], BF16)
    S.QI_T = scr("QI_T", [NT, 128, 1024], BF16)
    S.SG = scr("SG", [T, 16], F32)
    S.KA_T2 = scr("KA_T2", [128, T], BF16)
    S.KI_T2 = scr("KI_T2", [128, T], BF16)
    S.VA1 = scr("VA1", [T, 192], BF16)
    S.QB_T = scr("QB_T", [8, 128, T], BF16)
    S.KB_T = scr("KB_T", [8, 128, T], BF16)
    S.VB1 = scr("VB1", [T, 8, 192], BF16)
    S.ATT_T = scr("ATT_T", [8, 128, T], BF16)
    with ExitStack() as stack:
        P = Prog(nc, stack)
        c = Ctx()
        const_phase(nc, P, c, stack)
        rl = lambda: [P.res() for _ in range(NT)]
        rx = rl()
        rout = rl()
        S.rX1, S.rX2, S.rQA, S.rQI, S.rSG, S.rKA, S.rKI, S.rVA, S.rQB, S.rKB, S.rVB, S.rATa = [rl() for _ in range(12)]
        S.rATb = [[P.res() for _ in range(NQB)] for _ in range(8)]
        info = {}
        if "ffn1" in phases:
            info["ffn1"] = ffn_phase(nc, P, c, T, x, rx, S.X1, S.rX1, S.g_ffn1, S.w1_gate, S.w1_up, S.w1_down)
        if "proj" in phases:
            info["proj"] = proj_phase(nc, P, c, T, S)
        if "dsa" in phases:
            info["dsa"] = dsa_phase(nc, P, c, T, S)
        if "mla" in phases:
            info["mla"] = mla_phase(nc, P, c, T, S)
        if "wout" in phases:
            info["wout"] = wout_phase(nc, P, c, T, S)
        if "ffn2" in phases:
            info["ffn2"] = ffn_phase(nc, P, c, T, S.X2, S.rX2, out, rout, S.g_ffn2, S.w2_gate, S.w2_up, S.w2_down,
                                     final_g=S.g_final, tag="f2")
        nc._mk_info = (info, dict(P.cnt), max(P.dcnt.values()) if P.dcnt else 0)
    return nc


def rope_table(T, dim):
    pos = np.arange(T, dtype=np.float32)
    inv_freq = (np.float32(10000.0) ** (-np.arange(0, dim, 2, dtype=np.float32) / np.float32(dim))).astype(np.float32)
    ang = pos[:, None] * inv_freq[None, :]
    cs, sn = np.cos(ang).astype(np.float32), np.sin(ang).astype(np.float32)
    return np.concatenate([cs, cs, -sn, sn], axis=1).astype(np.float32)


_NC_CACHE = {}


def kernel(**inputs):
    x = np.ascontiguousarray(np.asarray(inputs["x"], dtype=np.float32))
    B, T, _ = x.shape
    if T not in _NC_CACHE:
        _NC_CACHE[T] = build(T)
    nc = _NC_CACHE[T]
    shared = {}
    for n, shp in IN_SHAPES.items():
        shared[n] = np.ascontiguousarray(np.asarray(inputs[n], dtype=np.float32).reshape(shp))
    shared["rope64"] = rope_table(T, 64)
    shared["rope32"] = rope_table(T, 32)
    in_maps = []
    for bi in range(B):
        m = dict(shared)
        m["x"] = x[bi]
        in_maps.append(m)
    res = run_bass_kernel_spmd(nc, in_maps, core_ids=list(range(B)))
    return np.stack([np.asarray(r["out"]) for r in res.results], axis=0).astype(np.float32)
```

```python
import math
from contextlib import ExitStack

import numpy as np
import concourse.bass as bass
import concourse.mybir as mybir
from concourse.bass_utils import run_bass_kernel_spmd

F32 = mybir.dt.float32
BF16 = mybir.dt.bfloat16
AF = mybir.ActivationFunctionType
ALU = mybir.AluOpType
AX = mybir.AxisListType

D = 1024
DFF = 2816
NFC = DFF // 128
EPS = 1e-6
ENGS = ("pe", "act", "dve", "pool", "sp")


class Res:
    __slots__ = ("name", "last_w", "readers")

    def __init__(self, name):
        self.name = name
        self.last_w = None
        self.readers = []


class Op:
    __slots__ = ("eng", "fn", "deps", "idx", "signal", "sem", "val", "is_dma", "waits", "key", "emitted")

    def __init__(self, eng, fn, is_dma=False):
        self.eng = eng
        self.fn = fn
        self.deps = []
        self.signal = False
        self.sem = None
        self.val = None
        self.is_dma = is_dma
        self.waits = []
        self.key = None
        self.emitted = False


class Prog:
    def __init__(self, nc, stack):
        self.nc = nc
        self.stack = stack
        self.ops = []
        self.n_total = 0
        self.eng_sem = {e: stack.enter_context(nc.semaphore("S_" + e)) for e in ENGS}
        self.cnt = {e: 0 for e in ENGS}
        self.dma_sem = {}
        self.dcnt = {}
        self.keymap = {}
        for e_, n_ in (("pool", 40), ("sp", 44)):
            for i_ in range(n_):
                self.dma_sem[(e_, i_)] = stack.enter_context(nc.semaphore("D_%s_%d" % (e_, i_)))
                self.dcnt[(e_, i_)] = 0
        self.block = stack.enter_context(nc.Block())
        self.waited = {e: {} for e in ENGS}
        self.phase_dmas = []
        self.last_op = {e: None for e in ENGS}

    def res(self, name="r"):
        return Res(name)

    def add(self, eng, fn, reads=(), writes=(), dma_key=None):
        op = Op(eng, fn, is_dma=dma_key is not None)
        op.idx = self.n_total
        self.n_total += 1
        seen = set()

        def dep(d):
            if d is None or d.idx in seen or d.emitted:
                return
            seen.add(d.idx)
            if d.eng == op.eng and not d.is_dma and not op.is_dma and d.eng == "pe":
                return
            op.deps.append(d)
            d.signal = True

        for r in reads:
            dep(r.last_w)
        for w in writes:
            dep(w.last_w)
            for rd in w.readers:
                dep(rd)
        for r in reads:
            r.readers.append(op)
        for w in writes:
            w.last_w = op
            w.readers = []
        if dma_key is not None:
            op.key = dma_key
            op.signal = True
            self.phase_dmas.append(op)
        self.ops.append(op)
        return op

    def dma(self, eng, out, in_, reads=(), writes=(), key=None):
        return self.add(eng, lambda e: e.dma_start(out=out, in_=in_), reads=reads, writes=writes, dma_key=key)

    def end_phase(self, pstack):
        nc = self.nc
        last = {}
        for op in self.ops:
            if op.fn is not None and not op.is_dma:
                last[op.eng] = op
        for e in ENGS:
            fin = self.add(e, None)
            fin.deps = [o for e2, o in last.items() if e2 != e] + list(self.phase_dmas)
            for o in fin.deps:
                o.signal = True
        self.phase_dmas = []
        for op in self.ops:
            if op.is_dma:
                km = self.keymap.setdefault(op.eng, {})
                if op.key not in km:
                    km[op.key] = len(km)
                k = (op.eng, km[op.key])
                assert k in self.dma_sem, ("out of preallocated DMA semaphores", k)
                self.dcnt[k] += 16
                op.sem = self.dma_sem[k]
                op.val = self.dcnt[k]
            elif op.signal:
                self.cnt[op.eng] += 1
                op.sem = self.eng_sem[op.eng]
                op.val = self.cnt[op.eng]
        for op in self.ops:
            need = {}
            w = self.waited[op.eng]
            for d in op.deps:
                key = id(d.sem)
                if w.get(key, 0) >= d.val:
                    continue
                if key not in need or need[key][1] < d.val:
                    need[key] = (d.sem, d.val)
            for key, (s, v) in need.items():
                w[key] = v
                op.waits.append((s, v))
        per_eng = {e: [op for op in self.ops if op.eng == e] for e in ENGS}
        block = self.block
        handles = {"pe": block.tensor, "act": block.scalar, "dve": block.vector,
                   "pool": block.gpsimd, "sp": block.sync}

        def make(e):
            def body(eng):
                for op in per_eng[e]:
                    for (s, v) in op.waits:
                        eng.wait_ge(s, v)
                    if op.fn is None:
                        continue
                    ins = op.fn(eng)
                    if op.signal:
                        ins.then_inc(op.sem, 16 if op.is_dma else 1)
            return body

        for e in ENGS:
            if per_eng[e]:
                handles[e](make(e))
        n = len(self.ops)
        for op in self.ops:
            op.emitted = True
            op.fn = None
        self.ops = []
        self.keymap = {}
        return n


def bcast_rows(vec_ap, n):
    return bass.AP(tensor=vec_ap.tensor, offset=vec_ap.offset, ap=[[0, 128], [1, n]])


class Ctx:
    pass


def load_weight_cast(P, c, dst_tile, res2d, src_ap, nk, pieces, stage, rstage):
    for (dc, sc, w, ri) in pieces:
        for k in range(nk):
            s = c.sj % len(stage)
            c.sj += 1
            P.dma("sp", stage[s][:, 0:w], src_ap[k * 128:(k + 1) * 128, sc:sc + w], writes=[rstage[s]],
                  key=f"stg{s}")
            eng = ("act", "dve", "pool")[c.sj % 3]
            if eng == "act":
                P.add("act", lambda e, s=s, k=k, dc=dc, w=w: e.activation(out=dst_tile[:, k, dc:dc + w],
                                                                          in_=stage[s][:, 0:w], func=AF.Copy),
                      reads=[rstage[s]], writes=[res2d[k][ri]])
            else:
                P.add(eng, lambda e, s=s, k=k, dc=dc, w=w: e.tensor_copy(out=dst_tile[:, k, dc:dc + w],
                                                                        in_=stage[s][:, 0:w]),
                      reads=[rstage[s]], writes=[res2d[k][ri]])


def make_stage(nc, P, c, ps, tag, n=5):
    st = [ps.enter_context(nc.sbuf_tensor(f"{tag}stg{i}", [128, 512], F32)) for i in range(n)]
    return st, [P.res() for _ in range(n)]


def rmsnorm_to_bf16(P, c, x_ap, x_res, n, g_bc, g_res, junk, junk_res, ss, ss_res, rstd, rstd_res, out_ap, out_res,
                    x_in_psum=False):
    P.add("act", lambda e: e.activation(out=junk, in_=x_ap, func=AF.Square, accum_out=ss),
          reads=[x_res], writes=[junk_res, ss_res])
    P.add("act", lambda e: e.activation(out=rstd, in_=ss, func=AF.Sqrt, scale=1.0 / n, bias=c.eps_t[:, 0:1]),
          reads=[ss_res, c.rconst], writes=[rstd_res])
    P.add("dve", lambda e: e.reciprocal(out=rstd, in_=rstd), reads=[rstd_res], writes=[rstd_res])
    P.add("dve", lambda e: e.scalar_tensor_tensor(out=out_ap, in0=x_ap, scalar=rstd, in1=g_bc,
                                                  op0=ALU.mult, op1=ALU.mult),
          reads=[x_res, rstd_res, g_res], writes=[out_res])


def ffn_phase(nc, P, c, T, src, src_res, dst, dst_res, g_vec, wg, wu, wd, final_g=None, tag="f1"):
    NT = T // 128
    with ExitStack() as ps:
        def sb(name, shape, dt):
            return ps.enter_context(nc.sbuf_tensor(tag + name, shape, dt))

        def pm(name, shape, dt):
            return ps.enter_context(nc.psum_tensor(tag + name, shape, dt))

        Wg = sb("Wg", [128, 8, DFF], BF16)
        Wu = sb("Wu", [128, 8, DFF], BF16)
        Wd = sb("Wd", [128, NFC, D], BF16)
        gbc = sb("gbc", [128, D], F32)
        gfin = sb("gfin", [128, D], F32) if final_g is not None else None
        xt = [sb(f"xt{i}", [128, D], F32) for i in range(3)]
        junk = [sb(f"junk{i}", [128, D], BF16) for i in range(2)]
        ss = [sb(f"ss{i}", [128, 4], F32) for i in range(2)]
        hb = [sb(f"hb{i}", [128, D], BF16) for i in range(2)]
        hT = [sb(f"hT{i}", [128, 8, 128], BF16) for i in range(2)]
        sg = [sb(f"sg{i}", [128, 512], F32) for i in range(2)]
        act = [sb(f"act{i}", [128, DFF], BF16) for i in range(2)]
        actT = [sb(f"actT{i}", [128, NFC, 128], BF16) for i in range(2)]
        tp = [pm(f"tp{i}", [128, 1024], BF16) for i in range(2)]
        mm = [pm(f"mm{i}", [128, 512], F32) for i in range(6)]

        R = P.res
        rWg = [[R() for _ in range(6)] for _ in range(8)]
        rWu = [[R() for _ in range(6)] for _ in range(8)]
        rWd = [[R(), R()] for _ in range(NFC)]
        stg, rstg = make_stage(nc, P, c, ps, tag)
        rg, rgf = R(), R()
        rxt = [R() for _ in range(3)]
        rjunk = [R() for _ in range(2)]
        rss = [R() for _ in range(2)]
        rrs = [R() for _ in range(2)]
        rhb = [R() for _ in range(2)]
        rhT = [R() for _ in range(2)]
        rsg = [R() for _ in range(2)]
        ract = [R() for _ in range(2)]
        ractT = [R() for _ in range(2)]
        ryo = [R() for _ in range(2)]
        rtp = [R() for _ in range(2)]
        rmm = [R() for _ in range(6)]

        P.dma("sp", gbc[:], bcast_rows(g_vec, D), writes=[rg], key="gbc")
        if final_g is not None:
            P.dma("sp", gfin[:], bcast_rows(final_g, D), writes=[rgf], key="gfin")
        for i0 in range(min(2, NT)):
            P.dma("sp", xt[i0][:], src[i0 * 128:(i0 + 1) * 128, :], reads=[src_res[i0]], writes=[rxt[i0]],
                  key=f"xt{i0}")
        for si0, s00 in enumerate(range(0, DFF, 512)):
            pc = [(s00, s00, min(512, DFF - s00), si0)]
            load_weight_cast(P, c, Wg, rWg, wg, 8, pc, stg, rstg)
            load_weight_cast(P, c, Wu, rWu, wu, 8, pc, stg, rstg)
        load_weight_cast(P, c, Wd, rWd, wd, NFC, [(0, 0, 512, 0), (512, 512, 512, 1)], stg, rstg)

        slabs = [(s0, min(512, DFF - s0)) for s0 in range(0, DFF, 512)]
        cn = {"mmi": 0, "tpi": 0}

        def stage1(i):
            b = i % 2
            b3 = i % 3
            rows = slice(i * 128, (i + 1) * 128)
            if i >= 2:
                P.dma("sp", xt[b3][:], src[rows, :], reads=[src_res[i]], writes=[rxt[b3]], key=f"xt{b3}")
            rmsnorm_to_bf16(P, c, xt[b3][:], rxt[b3], D, gbc[:], rg, junk[b][:], rjunk[b], ss[b][:, 0:1], rss[b],
                            ss[b][:, 1:2], rrs[b], hb[b][:], rhb[b])
            t = cn['tpi'] % 2
            cn['tpi'] += 1
            for k in range(8):
                P.add("pe", lambda e, k=k, t=t, b=b, b3=b3: e.transpose(out=tp[t][:, k * 128:(k + 1) * 128],
                                                                in_=hb[b][:, k * 128:(k + 1) * 128],
                                                                identity=c.ident[:]),
                      reads=[rhb[b], c.rident], writes=[rtp[t]])
            P.add("act", lambda e, t=t, b=b, b3=b3: e.activation(out=hT[b][:].rearrange("p k t -> p (k t)"),
                                                          in_=tp[t][:], func=AF.Copy),
                  reads=[rtp[t]], writes=[rhT[b]])
            for si, (s0, sw) in enumerate(slabs):
                ga = cn['mmi'] % 6
                ua = (cn['mmi'] + 1) % 6
                cn['mmi'] += 2
                for k in range(8):
                    P.add("pe", lambda e, k=k, ga=ga, b=b, b3=b3, s0=s0, sw=sw: e.matmul(
                        mm[ga][:, 0:sw], lhsT=hT[b][:, k, :], rhs=Wg[:, k, s0:s0 + sw],
                        start=(k == 0), stop=(k == 7)),
                          reads=[rhT[b], rWg[k][si]], writes=[rmm[ga]])
                for k in range(8):
                    P.add("pe", lambda e, k=k, ua=ua, b=b, b3=b3, s0=s0, sw=sw: e.matmul(
                        mm[ua][:, 0:sw], lhsT=hT[b][:, k, :], rhs=Wu[:, k, s0:s0 + sw],
                        start=(k == 0), stop=(k == 7)),
                          reads=[rhT[b], rWu[k][si]], writes=[rmm[ua]])
                s2 = si % 2
                P.add("act", lambda e, ga=ga, s2=s2, sw=sw: e.activation(out=sg[s2][:, 0:sw], in_=mm[ga][:, 0:sw],
                                                                        func=AF.Silu),
                      reads=[rmm[ga]], writes=[rsg[s2]])
                P.add("dve", lambda e, ua=ua, s2=s2, b=b, b3=b3, s0=s0, sw=sw: e.tensor_tensor(
                    out=act[b][:, s0:s0 + sw], in0=sg[s2][:, 0:sw], in1=mm[ua][:, 0:sw], op=ALU.mult),
                      reads=[rsg[s2], rmm[ua]], writes=[ract[b]])

        def stage2(i):
            b = i % 2
            b3 = i % 3
            rows = slice(i * 128, (i + 1) * 128)
            for f0 in range(0, NFC, 8):
                nf = min(8, NFC - f0)
                t = cn['tpi'] % 2
                cn['tpi'] += 1
                for f in range(nf):
                    P.add("pe", lambda e, f=f, f0=f0, t=t, b=b, b3=b3: e.transpose(
                        out=tp[t][:, f * 128:(f + 1) * 128],
                        in_=act[b][:, (f0 + f) * 128:(f0 + f + 1) * 128], identity=c.ident[:]),
                          reads=[ract[b], c.rident], writes=[rtp[t]])
                P.add("act" if (f0 // 8) % 2 == 0 else "dve",
                      (lambda e, t=t, b=b, b3=b3, f0=f0, nf=nf: e.activation(
                          out=actT[b][:, f0:f0 + nf, :].rearrange("p k t -> p (k t)"),
                          in_=tp[t][:, 0:nf * 128], func=AF.Copy)) if (f0 // 8) % 2 == 0 else
                      (lambda e, t=t, b=b, b3=b3, f0=f0, nf=nf: e.tensor_copy(
                          out=actT[b][:, f0:f0 + nf, :].rearrange("p k t -> p (k t)"),
                          in_=tp[t][:, 0:nf * 128])),
                      reads=[rtp[t]], writes=[ractT[b]])
            for half in range(2):
                da = cn['mmi'] % 6
                cn['mmi'] += 1
                for f in range(NFC):
                    P.add("pe", lambda e, f=f, da=da, b=b, b3=b3, half=half: e.matmul(
                        mm[da][:, :], lhsT=actT[b][:, f, :], rhs=Wd[:, f, half * 512:(half + 1) * 512],
                        start=(f == 0), stop=(f == NFC - 1)),
                          reads=[ractT[b], rWd[f][half]], writes=[rmm[da]])
                P.add("dve", lambda e, da=da, b=b, b3=b3, half=half: e.scalar_tensor_tensor(
                    out=xt[b3][:, half * 512:(half + 1) * 512], in0=mm[da][:, :], scalar=0.5,
                    in1=xt[b3][:, half * 512:(half + 1) * 512], op0=ALU.mult, op1=ALU.add),
                      reads=[rmm[da], rxt[b3]], writes=[rxt[b3]])
            if final_g is None:
                P.dma("sp", dst[rows, :], xt[b3][:], reads=[rxt[b3]], writes=[dst_res[i]], key=f"xo{b3}")
            else:
                P.add("act", lambda e, b=b, b3=b3: e.activation(out=junk[b][:], in_=xt[b3][:], func=AF.Square,
                                                         accum_out=ss[b][:, 2:3]),
                      reads=[rxt[b3]], writes=[rjunk[b], rss[b]])
                P.add("act", lambda e, b=b, b3=b3: e.activation(out=ss[b][:, 3:4], in_=ss[b][:, 2:3], func=AF.Sqrt,
                                                         scale=1.0 / D, bias=c.eps_t[:, 0:1]),
                      reads=[rss[b], c.rconst], writes=[rrs[b]])
                P.add("dve", lambda e, b=b, b3=b3: e.reciprocal(out=ss[b][:, 3:4], in_=ss[b][:, 3:4]),
                      reads=[rrs[b]], writes=[rrs[b]])
                P.add("dve", lambda e, b=b, b3=b3: e.scalar_tensor_tensor(out=xt[b3][:], in0=xt[b3][:], scalar=ss[b][:, 3:4],
                                                                   in1=gfin[:], op0=ALU.mult, op1=ALU.mult),
                      reads=[rxt[b3], rrs[b], rgf], writes=[rxt[b3]])
                P.dma("sp", dst[rows, :], xt[b3][:], reads=[rxt[b3]], writes=[dst_res[i]], key=f"xo{b3}")

        stage1(0)
        for i in range(NT):
            if i + 1 < NT:
                stage1(i + 1)
            stage2(i)
        return P.end_phase(ps)


def const_phase(nc, P, c, stack):
    c.ident = stack.enter_context(nc.sbuf_tensor("ident", [128, 128], BF16))
    c.identf = stack.enter_context(nc.sbuf_tensor("identf", [128, 128], F32))
    c.eps_t = stack.enter_context(nc.sbuf_tensor("eps_t", [128, 1], F32))
    c.rident = P.res()
    c.rconst = P.res()
    c.sj = 0
    P.add("pool", lambda e: e.memset(c.identf[:], 0.0), writes=[c.rident])
    P.add("pool", lambda e: e.affine_select(out=c.identf[:], in_=c.identf[:], pattern=[[-1, 128]],
                                            compare_op=ALU.not_equal, fill=1.0, base=0, channel_multiplier=1),
          reads=[c.rident], writes=[c.rident])
    P.add("pool", lambda e: e.tensor_copy(out=c.ident[:], in_=c.identf[:]), reads=[c.rident], writes=[c.rident])
    P.add("pool", lambda e: e.memset(c.eps_t[:], EPS), writes=[c.rconst])


def bc_mid(ap2d, H):
    a = [list(x) for x in ap2d.ap]
    return bass.AP(tensor=ap2d.tensor, offset=ap2d.offset, ap=[a[0], [0, H], a[1]])


def bc_last(ap2d, w):
    a = [list(x) for x in ap2d.ap]
    return bass.AP(tensor=ap2d.tensor, offset=ap2d.offset, ap=[a[0], a[1], [0, w]])


class BankPool:
    def __init__(self, tiles, res):
        self.tiles = tiles
        self.res = res
        self.i = 0

    def next(self):
        k = self.i % len(self.tiles)
        self.i += 1
        return self.tiles[k], self.res[k]


def transposes_to(P, c, tpp, srcs, src_res, dst_ap, dst_res, np_out=128, copy_eng="act"):
    tp, rtp = tpp.next()
    n = len(srcs)
    for j, s_ap in enumerate(srcs):
        P.add("pe", lambda e, j=j, s_ap=s_ap: e.transpose(out=tp[0:np_out, j * 128:(j + 1) * 128], in_=s_ap,
                                                         identity=c.ident[:]),
              reads=list(src_res) + [c.rident], writes=[rtp])
    if copy_eng == "act":
        P.add("act", lambda e: e.activation(out=dst_ap, in_=tp[0:np_out, 0:n * 128], func=AF.Copy),
              reads=[rtp], writes=[dst_res])
    else:
        P.add("dve", lambda e: e.tensor_copy(out=dst_ap, in_=tp[0:np_out, 0:n * 128]),
              reads=[rtp], writes=[dst_res])


def rope_ops(P, c, src3, src_res, H, d, tab, tab_res, ta, rta, tb, rtb, out3, out_res, out3b=None):
    h2 = d // 2
    cc = bc_mid(tab[:, 0:d], H)
    s0 = bc_mid(tab[:, d:d + h2], H)
    s1 = bc_mid(tab[:, d + h2:2 * d], H)
    P.add("dve", lambda e: e.tensor_tensor(out=ta, in0=src3, in1=cc, op=ALU.mult),
          reads=[src_res, tab_res], writes=[rta])
    P.add("dve", lambda e: e.tensor_tensor(out=tb[:, :, 0:h2], in0=src3[:, :, h2:d], in1=s0, op=ALU.mult),
          reads=[src_res, tab_res], writes=[rtb])
    P.add("dve", lambda e: e.tensor_tensor(out=tb[:, :, h2:d], in0=src3[:, :, 0:h2], in1=s1, op=ALU.mult),
          reads=[src_res, tab_res], writes=[rtb])
    P.add("pool", lambda e: e.tensor_tensor(out=out3, in0=ta, in1=tb, op=ALU.add),
          reads=[rta, rtb], writes=[out_res])
    if out3b is not None:
        P.add("pool", lambda e: e.tensor_tensor(out=out3b, in0=ta, in1=tb, op=ALU.add),
              reads=[rta, rtb], writes=[out_res])


WIN_SEGS = [
    (0, 0, 512),
    (512, 640, 512),
    (1024, 1152, 512),
    (1536, 1744, 384),
    (1920, 512, 64),
    (1984, 1664, 64),
    (2048, 2128, 256),
    (2304, 2384, 32),
    (2336, 576, 64),
    (2400, 1728, 16),
]
WIN_GROUPS = [(0, 512), (512, 512), (1024, 512), (1536, 512), (2048, 368)]
DPROJ = 2416


def proj_phase(nc, P, c, T, S):
    import os
    LIM = float(os.environ.get('PROJ_STOP', '99'))
    NT = T // 128
    with ExitStack() as ps:
        def sb(name, shape, dt):
            return ps.enter_context(nc.sbuf_tensor(name, shape, dt))

        def pm(name, shape, dt):
            return ps.enter_context(nc.psum_tensor(name, shape, dt))

        R = P.res
        Win = sb("Win", [128, 8, DPROJ], BF16)
        Wuq = sb("Wuq", [128, 3, 768], BF16)
        Wukv = sb("Wukv", [128, 2, 1024], BF16)
        gbc = sb("gbcm", [128, D], F32)
        gq = sb("gq", [128, 384], F32)
        gkv = sb("gkv", [128, 256], F32)
        rWin = [R() for _ in range(8)]
        rWuq = [R() for _ in range(3)]
        rWukv = [R() for _ in range(2)]
        rg, rgq, rgkv = R(), R(), R()

        def dbl(name, shape, dt):
            return [sb(f"{name}{i}", shape, dt) for i in range(2)], [R() for _ in range(2)]

        xt, rxt = dbl("pxt", [128, D], F32)
        junk, rjunk = dbl("pjunk", [128, D], BF16)
        ss, rss = dbl("pss", [128, 8], F32)
        rrs = [[R() for _ in range(3)] for _ in range(2)]
        hb, rhb = dbl("phb", [128, D], BF16)
        hT, rhT = dbl("phT", [128, 8, 128], BF16)
        r64, rr64 = dbl("r64", [128, 128], F32)
        r32, rr32 = dbl("r32", [128, 64], F32)
        ta, rta = dbl("ta", [128, 512], F32)
        tb, rtb = dbl("tb", [128, 512], F32)
        qar, rqar = dbl("qar", [128, 512], BF16)
        qir, rqir = dbl("qir", [128, 1024], BF16)
        qif, rqif = dbl("qif", [128, 512], F32)
        qaT, rqaT = dbl("qaT", [128, 512], BF16)
        qiT, rqiT = dbl("qiT", [128, 1024], BF16)
        aw, raw = dbl("aw", [128, 16], F32)
        sgt, rsgt = dbl("sgt", [128, 16], F32)
        kdup, rkdup = dbl("kdup", [128, 256], BF16)
        kT, rkT = dbl("kT", [128, 256], BF16)
        kpe, rkpe = dbl("kpe", [128, 32], BF16)
        va1, rva1 = dbl("va1", [128, 192], BF16)
        cqn, rcqn = dbl("cqn", [128, 384], BF16)
        cqT, rcqT = dbl("cqT", [128, 384], BF16)
        ckn, rckn = dbl("ckn", [128, 256], BF16)
        ckT, rckT = dbl("ckT", [128, 256], BF16)
        qbr, rqbr = dbl("qbr", [128, 8, 128], BF16)
        kbr, rkbr = dbl("kbr", [128, 8, 128], BF16)
        qbT, rqbT = dbl("qbT", [128, 1024], BF16)
        kbT, rkbT = dbl("kbT", [128, 1024], BF16)
        vb1, rvb1 = dbl("vb1", [128, 8, 192], BF16)
        tpt = [pm(f"ptp{i}", [128, 1024], BF16) for i in range(2)]
        tpp = BankPool(tpt, [R() for _ in range(2)])
        mmt = [pm(f"pmm{i}", [128, 512], F32) for i in range(6)]
        mmp = BankPool(mmt, [R() for _ in range(6)])

        P.dma("sp", gbc[:], bcast_rows(S.g_mix, D), writes=[rg], key="gbc")
        P.dma("sp", gq[:], bcast_rows(S.g_q_lat, 384), writes=[rgq], key="gq")
        P.dma("sp", gkv[:], bcast_rows(S.g_kv_lat, 256), writes=[rgkv], key="gkv")
        stg, rstg = make_stage(nc, P, c, ps, "pj")
        load_weight_cast(P, c, Win, [[r] for r in rWin], S.w_in, 8, [(dc, sc, w, 0) for (dc, sc, w) in WIN_SEGS],
                         stg, rstg)
        load_weight_cast(P, c, Wuq, [[r] for r in rWuq], S.w_uq, 3, [(0, 0, 384, 0), (384, 384, 384, 0)], stg, rstg)
        load_weight_cast(P, c, Wukv, [[r] for r in rWukv], S.w_ukv, 2, [(0, 0, 512, 0), (512, 512, 512, 0)], stg, rstg)
        for b in range(2):
            P.add("pool", lambda e, b=b: e.memset(qbr[b][:], 0.0), writes=[rqbr[b]])
            P.add("pool", lambda e, b=b: e.memset(kbr[b][:], 0.0), writes=[rkbr[b]])
            P.add("pool", lambda e, b=b: e.memset(va1[b][:], 1.0), writes=[rva1[b]])
            P.add("pool", lambda e, b=b: e.memset(vb1[b][:], 1.0), writes=[rvb1[b]])

        def stage1a(i):
            b = i % 2
            rows = slice(i * 128, (i + 1) * 128)
            P.dma("sp", xt[b][:], S.X1[rows, :], reads=[S.rX1[i]], writes=[rxt[b]], key=f"xt{b}")
            P.dma("sp", r64[b][:], S.rope64[rows, :], writes=[rr64[b]], key=f"r64{b}")
            P.dma("sp", r32[b][:], S.rope32[rows, :], writes=[rr32[b]], key=f"r32{b}")
            rmsnorm_to_bf16(P, c, xt[b][:], rxt[b], D, gbc[:], rg, junk[b][:], rjunk[b], ss[b][:, 0:1], rss[b],
                            ss[b][:, 1:2], rrs[b][0], hb[b][:], rhb[b])
            transposes_to(P, c, tpp, [hb[b][:, k * 128:(k + 1) * 128] for k in range(8)], [rhb[b]],
                          hT[b][:].rearrange("p k t -> p (k t)"), rhT[b])

        def stage2(i):
            b = i % 2
            rows = slice(i * 128, (i + 1) * 128)
            cols = slice(i * 128, (i + 1) * 128)
            if LIM < 2:
                return
            banks = []
            for (g0, gw) in WIN_GROUPS:
                bk, rbk = mmp.next()
                for k in range(8):
                    P.add("pe", lambda e, k=k, bk=bk, g0=g0, gw=gw, b=b: e.matmul(
                        bk[:, 0:gw], lhsT=hT[b][:, k, :], rhs=Win[:, k, g0:g0 + gw], start=(k == 0), stop=(k == 7)),
                          reads=[rhT[b], rWin[k]], writes=[rbk])
                banks.append((bk, rbk))
            (B0, rB0), (B1, rB1), (B2, rB2), (B3, rB3), (B4, rB4) = banks
            v3 = lambda ap, H: ap.rearrange("p (h d) -> p h d", h=H)
            if LIM < 3:
                return
            P.add("act", lambda e, b=b, B4=B4: e.activation(out=aw[b][:], in_=B4[:, 352:368], func=AF.Abs,
                                                           scale=1.0 / 32.0),
                  reads=[rB4], writes=[raw[b]])
            P.add("act", lambda e, b=b, B4=B4: e.activation(out=sgt[b][:], in_=B4[:, 352:368], func=AF.Sign),
                  reads=[rB4], writes=[rsgt[b]])
            P.dma("sp", S.SG[rows, :], sgt[b][:], reads=[rsgt[b]], writes=[S.rSG[i]], key=f"sgt{b}")
            if LIM < 4:
                return
            rope_ops(P, c, v3(B0[:, 0:512], 8), rB0, 8, 64, r64[b], rr64[b], v3(ta[b][:], 8), rta[b],
                     v3(tb[b][:], 8), rtb[b], v3(qar[b][:], 8), rqar[b])
            transposes_to(P, c, tpp, [qar[b][:, j * 128:(j + 1) * 128] for j in range(4)], [rqar[b]],
                          qaT[b][:], rqaT[b], copy_eng="dve")
            P.dma("sp", S.QA_T[i], qaT[b][:], reads=[rqaT[b]], writes=[S.rQA[i]], key=f"qaT{b}")
            if LIM < 5:
                return
            for hh, (Bq, rBq) in enumerate(((B1, rB1), (B2, rB2))):
                rope_ops(P, c, v3(Bq[:, 0:512], 8), rBq, 8, 64, r64[b], rr64[b], v3(ta[b][:], 8), rta[b],
                         v3(tb[b][:], 8), rtb[b], v3(qif[b][:], 8), rqif[b])
                P.add("dve", lambda e, b=b, hh=hh: e.tensor_tensor(
                    out=v3(qir[b][:, hh * 512:(hh + 1) * 512], 8), in0=v3(qif[b][:], 8),
                    in1=bc_last(aw[b][:, hh * 8:(hh + 1) * 8], 64), op=ALU.mult),
                      reads=[rqif[b], raw[b]], writes=[rqir[b]])
            transposes_to(P, c, tpp, [qir[b][:, j * 128:(j + 1) * 128] for j in range(8)], [rqir[b]],
                          qiT[b][:], rqiT[b])
            P.dma("sp", S.QI_T[i], qiT[b][:], reads=[rqiT[b]], writes=[S.rQI[i]], key=f"qiT{b}")
            if LIM < 6:
                return
            kd4 = kdup[b][:].rearrange("p (a r d) -> p a r d", a=2, r=2)
            rope_ops(P, c, v3(B3[:, 384:512], 2), rB3, 2, 64, r64[b], rr64[b], v3(ta[b][:, 0:128], 2), rta[b],
                     v3(tb[b][:, 0:128], 2), rtb[b], kd4[:, :, 0, :], rkdup[b], out3b=kd4[:, :, 1, :])
            transposes_to(P, c, tpp, [kdup[b][:, 0:128], kdup[b][:, 128:256]], [rkdup[b]], kT[b][:], rkT[b],
                          copy_eng="dve")
            P.dma("sp", S.KA_T2[:, cols], kT[b][:, 0:128], reads=[rkT[b]], writes=[S.rKA[i]], key=f"kTa{b}")
            P.dma("sp", S.KI_T2[:, cols], kT[b][:, 128:256], reads=[rkT[b]], writes=[S.rKI[i]], key=f"kTi{b}")
            if LIM < 7:
                return
            rope_ops(P, c, v3(B4[:, 256:288], 1), rB4, 1, 32, r32[b], rr32[b], v3(ta[b][:, 0:32], 1), rta[b],
                     v3(tb[b][:, 0:32], 1), rtb[b], v3(kpe[b][:], 1), rkpe[b])
            if LIM < 8:
                return
            P.add("act", lambda e, b=b, B4=B4: e.activation(out=va1[b][:, 64:128], in_=B4[:, 288:352], func=AF.Copy),
                  reads=[rB4], writes=[rva1[b]])
            P.dma("sp", S.VA1[rows, :], va1[b][:], reads=[rva1[b]], writes=[S.rVA[i]], key=f"va1{b}")
            if LIM < 9:
                return
            rmsnorm_to_bf16(P, c, B3[:, 0:384], rB3, 384, gq[:], rgq, junk[b][:, 0:384], rjunk[b], ss[b][:, 2:3],
                            rss[b], ss[b][:, 3:4], rrs[b][1], cqn[b][:], rcqn[b])
            if LIM < 9.1:
                return
            transposes_to(P, c, tpp, [cqn[b][:, k * 128:(k + 1) * 128] for k in range(3)], [rcqn[b]],
                          cqT[b][:], rcqT[b], copy_eng="dve")
            if LIM < 9.2:
                return
            for (q0, qw, h0, nh) in ((0, 480, 0, 5), (480, 288, 5, 3)):
                bk, rbk = mmp.next()
                for k in range(3):
                    P.add("pe", lambda e, k=k, bk=bk, q0=q0, qw=qw, b=b: e.matmul(
                        bk[:, 0:qw], lhsT=cqT[b][:, k * 128:(k + 1) * 128], rhs=Wuq[:, k, q0:q0 + qw],
                        start=(k == 0), stop=(k == 2)),
                          reads=[rcqT[b], rWuq[k]], writes=[rbk])
                if LIM < 9.3:
                    return
                bv = bk[:, 0:qw].rearrange("p (h d) -> p h d", h=nh)
                P.add("dve", lambda e, bv=bv, b=b, h0=h0, nh=nh: e.tensor_copy(
                    out=qbr[b][:, h0:h0 + nh, 0:64], in_=bv[:, :, 0:64]),
                      reads=[rbk], writes=[rqbr[b]])
                if LIM < 9.4:
                    return
                rope_ops(P, c, bv[:, :, 64:96], rbk, nh, 32, r32[b], rr32[b],
                         ta[b][:, 0:nh * 32].rearrange("p (h d) -> p h d", h=nh), rta[b],
                         tb[b][:, 0:nh * 32].rearrange("p (h d) -> p h d", h=nh), rtb[b],
                         qbr[b][:, h0:h0 + nh, 64:96], rqbr[b])
            if LIM < 9.5:
                return
            transposes_to(P, c, tpp, [qbr[b][:, h, :] for h in range(8)], [rqbr[b]], qbT[b][:], rqbT[b])
            if LIM < 9.6:
                return
            P.dma("sp", S.QB_T[:, :, cols].rearrange("h p t -> p h t"),
                  qbT[b][:].rearrange("p (h t) -> p h t", h=8), reads=[rqbT[b]], writes=[S.rQB[i]], key=f"qbT{b}")
            if LIM < 10:
                return
            rmsnorm_to_bf16(P, c, B4[:, 0:256], rB4, 256, gkv[:], rgkv, junk[b][:, 0:256], rjunk[b], ss[b][:, 4:5],
                            rss[b], ss[b][:, 5:6], rrs[b][2], ckn[b][:], rckn[b])
            transposes_to(P, c, tpp, [ckn[b][:, k * 128:(k + 1) * 128] for k in range(2)], [rckn[b]],
                          ckT[b][:], rckT[b], copy_eng="dve")
            P.add("pool", lambda e, b=b: e.tensor_copy(out=kbr[b][:, :, 64:96], in_=bc_mid(kpe[b][:], 8)),
                  reads=[rkpe[b]], writes=[rkbr[b]])
            for hf in range(2):
                bk, rbk = mmp.next()
                for k in range(2):
                    P.add("pe", lambda e, k=k, bk=bk, hf=hf, b=b: e.matmul(
                        bk[:, :], lhsT=ckT[b][:, k * 128:(k + 1) * 128], rhs=Wukv[:, k, hf * 512:(hf + 1) * 512],
                        start=(k == 0), stop=(k == 1)),
                          reads=[rckT[b], rWukv[k]], writes=[rbk])
                bv = bk[:, :].rearrange("p (h d) -> p h d", h=4)
                P.add("dve", lambda e, bv=bv, b=b, hf=hf: e.tensor_copy(
                    out=kbr[b][:, hf * 4:(hf + 1) * 4, 0:64], in_=bv[:, :, 0:64]),
                      reads=[rbk], writes=[rkbr[b]])
                P.add("dve", lambda e, bv=bv, b=b, hf=hf: e.tensor_copy(
                    out=vb1[b][:, hf * 4:(hf + 1) * 4, 64:128], in_=bv[:, :, 64:128]),
                      reads=[rbk], writes=[rvb1[b]])
            transposes_to(P, c, tpp, [kbr[b][:, h, :] for h in range(8)], [rkbr[b]], kbT[b][:], rkbT[b])
            P.dma("sp", S.KB_T[:, :, cols].rearrange("h p t -> p h t"),
                  kbT[b][:].rearrange("p (h t) -> p h t", h=8), reads=[rkbT[b]], writes=[S.rKB[i]], key=f"kbT{b}")
            P.dma("sp", S.VB1[rows, :, :], vb1[b][:], reads=[rvb1[b]], writes=[S.rVB[i]], key=f"vb1{b}")

        stage1a(0)
        for i in range(NT):
            if i + 1 < NT:
                stage1a(i + 1)
            stage2(i)
        return P.end_phase(ps)


NEG = -1.0e30
NBIS = 16


def normalize_out(P, c, O, rO, num_lo, rc, rrc, out_ap, out_res):
    den_lo = 64 - num_lo
    P.add("dve", lambda e: e.reciprocal(out=rc[den_lo:den_lo + 64, :], in_=O[den_lo:den_lo + 64, :]),
          reads=[rO], writes=[rrc])
    P.add("dve", lambda e: e.tensor_tensor(out=out_ap, in0=O[num_lo:num_lo + 64, :],
                                           in1=rc[den_lo:den_lo + 64, :], op=ALU.mult),
          reads=[rO, rrc], writes=[out_res])


def dsa_phase(nc, P, c, T, S):
    NT = T // 128
    TOPK = min(256, T // 4)
    QT0 = TOPK // 128
    with ExitStack() as ps:
        def sb(name, shape, dt):
            return ps.enter_context(nc.sbuf_tensor(name, shape, dt))

        def pm(name, shape, dt):
            return ps.enter_context(nc.psum_tensor(name, shape, dt))

        R = P.res

        def dbl(name, shape, dt):
            return [sb(f"{name}{i}", shape, dt) for i in range(2)], [R() for _ in range(2)]

        KA2 = sb("KA2", [128, T], BF16)
        KI2 = sb("KI2", [128, T], BF16)
        VAs = sb("VAs", [128, NT, 192], BF16)
        rKA2 = [R() for _ in range(NT)]
        rKI2 = [R() for _ in range(NT)]
        rVAs = [R() for _ in range(NT)]
        cneg = sb("cneg", [128, 128], F32)
        pow2 = sb("pow2", [128, NBIS], F32)
        rcn = R()
        qiT, rqiT = dbl("dqiT", [128, 1024], BF16)
        qaT, rqaT = dbl("dqaT", [128, 512], BF16)
        sg, rsg = dbl("dsg", [128, 16], F32)
        Rt = [sb(f"Rt{i}", [128, 512], BF16) for i in range(4)]
        Dg, rDg = dbl("Dg", [128, 16, 128], BF16)
        rRt = [R() for _ in range(4)]
        Isb, rIsb = dbl("Isb", [128, T], F32)
        cjunk = sb("cjunk", [128, T], BF16)
        rcj = R()
        st_, rst = dbl("dst", [128, 8 + NBIS], F32)
        maskq, rmq = dbl("maskq", [128, T], BF16)
        maskT, rmT = dbl("maskT", [128, NT, 128], BF16)
        PT = [sb(f"PT{i}", [128, 1024], BF16) for i in range(4)]
        rPT = [R() for _ in range(4)]
        rc, rrc = dbl("drc", [128, 512], F32)
        aT, raT = dbl("daT", [128, 512], BF16)
        Lt = [pm(f"dL{i}", [128, 512], F32) for i in range(4)]
        Lp = BankPool(Lt, [R() for _ in range(4)])
        At = [pm(f"dA{i}", [128, 512], F32) for i in range(2)]
        rAt = [R() for _ in range(2)]
        OE = pm("dOE", [128, 512], F32)
        OO = pm("dOO", [128, 512], F32)
        rOE, rOO = R(), R()

        P.add("pool", lambda e: e.memset(cneg[:], 0.0), writes=[rcn])
        P.add("pool", lambda e: e.affine_select(out=cneg[:], in_=cneg[:], pattern=[[-1, 128]],
                                                compare_op=ALU.is_ge, fill=NEG, base=0, channel_multiplier=1),
              reads=[rcn], writes=[rcn])
        for k in range(NBIS):
            P.add("pool", lambda e, k=k: e.memset(pow2[:, k:k + 1], 2.0 ** (-(k + 1))), writes=[rcn])
        CH = min(T, 1024)
        for ci in range(T // CH):
            cols = slice(ci * CH, (ci + 1) * CH)
            tl = range(ci * (CH // 128), (ci + 1) * (CH // 128))
            rk, rki, rv = R(), R(), R()
            P.dma("sp", KA2[:, cols], S.KA_T2[:, cols], reads=[S.rKA[i] for i in tl], writes=[rk], key=f"KA2_{ci}")
            P.dma("sp", KI2[:, cols], S.KI_T2[:, cols], reads=[S.rKI[i] for i in tl], writes=[rki], key=f"KI2_{ci}")
            P.dma("sp", VAs[:, ci * (CH // 128):(ci + 1) * (CH // 128), :],
                  S.VA1[cols, :].rearrange("(n p) c -> p n c", p=128), reads=[S.rVA[i] for i in tl], writes=[rv],
                  key=f"VAs_{ci}")
            for i in tl:
                rKA2[i], rKI2[i], rVAs[i] = rk, rki, rv

        cnt = {"ri": 0, "pti": 0, "ai": 0}

        def stage_A(qt):
            b = qt % 2
            SL = (qt + 1) * 128
            P.dma("sp", qiT[b][:], S.QI_T[qt], reads=[S.rQI[qt]], writes=[rqiT[b]], key=f"dqiT{b}")
            P.dma("sp", sg[b][:], S.SG[qt * 128:(qt + 1) * 128, :], reads=[S.rSG[qt]], writes=[rsg[b]], key=f"dsg{b}")
            P.add("dve", lambda e: e.tensor_tensor(out=Dg[b][:], in0=bc_mid(c.ident[:], 16), in1=bc_last(sg[b][:], 128),
                                                   op=ALU.mult),
                  reads=[c.rident, rsg[b]], writes=[rDg[b]])
            steps = []
            for sbk, s0 in enumerate(range(0, SL, 512)):
                for h in range(16):
                    steps.append((sbk, s0, h))

            def lmm(sbk, s0, h):
                sw = min(512, SL - s0)
                kres = [rKI2[j] for j in range(s0 // 128, (s0 + sw) // 128)]
                hp, par = h // 2, h % 2
                pl = par * 64
                L, rL = Lp.next()
                P.add("pe", lambda e: e.matmul(L[:, 0:sw], lhsT=qiT[b][pl:pl + 64, hp * 128:(hp + 1) * 128],
                                               rhs=KI2[pl:pl + 64, s0:s0 + sw], start=True, stop=True),
                      reads=[rqiT[b]] + kres, writes=[rL])
                r = cnt['ri'] % 4
                cnt['ri'] += 1
                P.add("act", lambda e: e.activation(out=Rt[r][:, 0:sw], in_=L[:, 0:sw], func=AF.Relu),
                      reads=[rL], writes=[rRt[r]])
                return r

            def acc(sbk, s0, h, r):
                sw = min(512, SL - s0)
                A, rA = At[(cnt['ai'] + sbk) % 2], rAt[(cnt['ai'] + sbk) % 2]
                P.add("pe", lambda e: e.matmul(A[:, 0:sw], lhsT=Dg[b][:, h, :], rhs=Rt[r][:, 0:sw], start=(h == 0),
                                               stop=(h == 15)),
                      reads=[rDg[b], rRt[r]], writes=[rA])
                if h == 15:
                    P.add("act", lambda e: e.activation(out=Isb[b][:, s0:s0 + sw], in_=A[:, 0:sw], func=AF.Copy),
                          reads=[rA], writes=[rIsb[b]])

            LOOKA = 2
            pend = {}
            for i in range(min(LOOKA, len(steps))):
                pend[i] = lmm(*steps[i])
            for i, stp in enumerate(steps):
                if i + LOOKA < len(steps):
                    pend[i + LOOKA] = lmm(*steps[i + LOOKA])
                acc(*stp, pend.pop(i))
            cnt['ai'] += len(range(0, SL, 512))

        def stage_B(qt):
            b = qt % 2
            SL = (qt + 1) * 128
            P.dma("sp", qaT[b][:], S.QA_T[qt], reads=[S.rQA[qt]], writes=[rqaT[b]], key=f"dqaT{b}")
            S_ = st_[b]
            if qt >= QT0:
                P.add("dve", lambda e, b=b, SL=SL, S_=S_: e.tensor_reduce(out=S_[:, 0:1], in_=Isb[b][:, 0:SL], axis=AX.X,
                                                                        op=ALU.min),
                      reads=[rIsb[b]], writes=[rst[b]])
                P.add("dve", lambda e, b=b, SL=SL, S_=S_: e.tensor_reduce(out=S_[:, 1:2], in_=Isb[b][:, 0:SL], axis=AX.X,
                                                                        op=ALU.max),
                      reads=[rIsb[b]], writes=[rst[b]])
            P.add("pool", lambda e, b=b, qt=qt: e.tensor_tensor(out=Isb[b][:, qt * 128:(qt + 1) * 128],
                                                              in0=Isb[b][:, qt * 128:(qt + 1) * 128], in1=cneg[:],
                                                              op=ALU.add),
                  reads=[rIsb[b], rcn], writes=[rIsb[b]])
            if qt >= QT0:
                P.add("dve", lambda e, S_=S_: e.tensor_tensor(out=S_[:, 2:3], in0=S_[:, 1:2], in1=S_[:, 0:1],
                                                             op=ALU.subtract),
                      reads=[rst[b]], writes=[rst[b]])
                P.add("dve", lambda e, S_=S_: e.tensor_scalar(out=S_[:, 8:8 + NBIS], in0=pow2[:], scalar1=S_[:, 2:3],
                                                             scalar2=None, op0=ALU.mult),
                      reads=[rst[b], rcn], writes=[rst[b]])
                P.add("dve", lambda e, S_=S_: e.tensor_copy(out=S_[:, 3:4], in_=S_[:, 0:1]),
                      reads=[rst[b]], writes=[rst[b]])
                for k in range(NBIS):
                    P.add("dve", lambda e, S_=S_, k=k: e.tensor_tensor(out=S_[:, 4:5], in0=S_[:, 3:4],
                                                                      in1=S_[:, 8 + k:9 + k], op=ALU.add),
                          reads=[rst[b]], writes=[rst[b]])
                    P.add("dve", lambda e, S_=S_, b=b, SL=SL: e.tensor_scalar(
                        out=cjunk[:, 0:SL], in0=Isb[b][:, 0:SL], scalar1=S_[:, 4:5], scalar2=None,
                        op0=ALU.is_ge, op1=ALU.add, accum_out=S_[:, 5:6]),
                          reads=[rst[b], rIsb[b]], writes=[rst[b], rcj])
                    P.add("dve", lambda e, S_=S_, k=k: e.tensor_scalar(
                        out=S_[:, 6:7], in0=S_[:, 5:6], scalar1=float(TOPK), scalar2=S_[:, 8 + k:9 + k],
                        op0=ALU.is_ge, op1=ALU.mult),
                          reads=[rst[b]], writes=[rst[b]])
                    P.add("dve", lambda e, S_=S_: e.tensor_tensor(out=S_[:, 3:4], in0=S_[:, 3:4], in1=S_[:, 6:7],
                                                                 op=ALU.add),
                          reads=[rst[b]], writes=[rst[b]])
            else:
                P.add("dve", lambda e, S_=S_: e.memset(S_[:, 3:4], -1.0e29), writes=[rst[b]])
            P.add("dve", lambda e, S_=S_, b=b, SL=SL: e.tensor_scalar(
                out=maskq[b][:, 0:SL], in0=Isb[b][:, 0:SL], scalar1=S_[:, 3:4], scalar2=None, op0=ALU.is_ge),
                  reads=[rst[b], rIsb[b]], writes=[rmq[b]])

        def stage_C(qt):
            b = qt % 2
            SL = (qt + 1) * 128
            for s8 in range(0, qt + 1, 8):
                n8 = min(8, qt + 1 - s8)
                L, rL = Lp.next()
                Lb = L[:, :].bitcast(BF16)
                for j in range(n8):
                    P.add("pe", lambda e, Lb=Lb, j=j, s8=s8, b=b: e.transpose(
                        out=Lb[:, j * 128:(j + 1) * 128], in_=maskq[b][:, (s8 + j) * 128:(s8 + j + 1) * 128],
                        identity=c.ident[:]),
                          reads=[rmq[b], c.rident], writes=[rL])
                P.add("act", lambda e, Lb=Lb, s8=s8, n8=n8, b=b: e.activation(
                    out=maskT[b][:, s8:s8 + n8, :].rearrange("p n q -> p (n q)"), in_=Lb[:, 0:n8 * 128],
                    func=AF.Copy),
                      reads=[rL], writes=[rmT[b]])
            def qk(st):
                sc = slice(st * 128, (st + 1) * 128)
                LE, rLE = Lp.next()
                LO, rLO = Lp.next()
                P.add("pe", lambda e: e.matmul(LE[:, :], lhsT=KA2[0:64, sc], rhs=qaT[b][0:64, :], start=True, stop=True),
                      reads=[rKA2[st], rqaT[b]], writes=[rLE])
                P.add("pe", lambda e: e.matmul(LO[:, :], lhsT=KA2[64:128, sc], rhs=qaT[b][64:128, :], start=True,
                                               stop=True),
                      reads=[rKA2[st], rqaT[b]], writes=[rLO])
                p = cnt["pti"] % len(PT)
                cnt["pti"] += 1
                P.add("act", lambda e: e.activation(out=PT[p][:, 0:512], in_=LE[:, :], func=AF.Exp, scale=0.125),
                      reads=[rLE], writes=[rPT[p]])
                P.add("act", lambda e: e.activation(out=PT[p][:, 512:1024], in_=LO[:, :], func=AF.Exp, scale=0.125),
                      reads=[rLO], writes=[rPT[p]])
                P.add("pool", lambda e: e.tensor_tensor(
                    out=PT[p][:].rearrange("p (a q) -> p a q", a=8), in0=PT[p][:].rearrange("p (a q) -> p a q", a=8),
                    in1=bc_mid(maskT[b][:, st, :], 8), op=ALU.mult),
                      reads=[rPT[p], rmT[b]], writes=[rPT[p]])
                return p

            def pv(st, p):
                P.add("pe", lambda e: e.matmul(OE[:, :], lhsT=VAs[:, st, 64:192], rhs=PT[p][:, 0:512],
                                               start=(st == 0), stop=(st == qt)),
                      reads=[rVAs[st], rPT[p]], writes=[rOE])
                P.add("pe", lambda e: e.matmul(OO[:, :], lhsT=VAs[:, st, 0:128], rhs=PT[p][:, 512:1024],
                                               start=(st == 0), stop=(st == qt)),
                      reads=[rVAs[st], rPT[p]], writes=[rOO])

            pend = {0: qk(0)}
            for st in range(qt + 1):
                if st + 1 <= qt:
                    pend[st + 1] = qk(st + 1)
                pv(st, pend.pop(st))
            normalize_out(P, c, OE, rOE, 0, rc[b], rrc[b], aT[b][0:64, :], raT[b])
            normalize_out(P, c, OO, rOO, 64, rc[b], rrc[b], aT[b][64:128, :], raT[b])
            P.dma("sp", S.ATT_T[0:4, :, qt * 128:(qt + 1) * 128].rearrange("j p t -> p j t"),
                  aT[b][:].rearrange("p (j t) -> p j t", j=4), reads=[raT[b]], writes=[S.rATa[qt]], key=f"daT{b}")

        for step in range(NT + 2):
            if step < NT:
                stage_A(step)
            if 0 <= step - 1 < NT:
                stage_B(step - 1)
            if 0 <= step - 2 < NT:
                stage_C(step - 2)
        return P.end_phase(ps)


SCALE_B = 96.0 ** -0.5


def mla_phase(nc, P, c, T, S):
    NT = T // 128
    NQB = T // 512
    with ExitStack() as ps:
        def sb(name, shape, dt):
            return ps.enter_context(nc.sbuf_tensor(name, shape, dt))

        def pm(name, shape, dt):
            return ps.enter_context(nc.psum_tensor(name, shape, dt))

        R = P.res

        def dbl(name, shape, dt):
            return [sb(f"{name}{i}", shape, dt) for i in range(2)], [R() for _ in range(2)]

        KB = [sb(f"mKB{i}", [128, T], BF16) for i in range(2)]
        VB = [sb(f"mVB{i}", [128, NT, 192], BF16) for i in range(2)]
        rKB = [[R() for _ in range(NQB)] for _ in range(2)]
        rVB = [[R() for _ in range(NQB)] for _ in range(2)]
        Cm = sb("Cm", [128, 4, 512], BF16)
        rCm = R()
        QT, rQT = dbl("mQT", [128, 512], BF16)
        PT = [sb(f"mPT{i}", [128, 512], BF16) for i in range(4)]
        rPT = [R() for _ in range(4)]
        rc, rrc = dbl("mrc", [128, 512], F32)
        aT, raT = dbl("maT", [128, 512], BF16)
        Lt = [pm(f"mL{i}", [128, 512], F32) for i in range(4)]
        Lp = BankPool(Lt, [R() for _ in range(4)])
        Ot = [pm(f"mO{i}", [128, 512], F32) for i in range(2)]
        rOt = [R() for _ in range(2)]

        P.add("pool", lambda e: e.memset(Cm[:], 1.0), writes=[rCm])
        P.add("pool", lambda e: e.affine_select(out=Cm[:], in_=Cm[:], pattern=[[-128, 4], [1, 512]],
                                                compare_op=ALU.is_ge, fill=0.0, base=0, channel_multiplier=-1),
              reads=[rCm], writes=[rCm])
        CH = min(T, 1024)
        NCH = T // CH

        def load_head(h):
            hb_ = h % 2
            for ci in range(NCH):
                cs = slice(ci * CH, (ci + 1) * CH)
                tl = range(ci * (CH // 128), (ci + 1) * (CH // 128))
                P.dma("sp", KB[hb_][:, cs], S.KB_T[h, :, cs], reads=[S.rKB[i] for i in tl],
                      writes=[rKB[hb_][ci]], key=f"mKB{hb_}_{ci}")
                P.dma("sp", VB[hb_][:, ci * (CH // 128):(ci + 1) * (CH // 128), :],
                      S.VB1[cs, h, :].rearrange("(n p) c -> p n c", p=128),
                      reads=[S.rVB[i] for i in tl], writes=[rVB[hb_][ci]], key=f"mVB{hb_}_{ci}")

        groups = [(h, qb) for h in range(8) for qb in range(NQB)]

        def load_q(g):
            h, qb = groups[g]
            bq = g % 2
            cs = slice(qb * 512, (qb + 1) * 512)
            P.dma("sp", QT[bq][:], S.QB_T[h, :, cs], reads=[S.rQB[i] for i in range(qb * 4, qb * 4 + 4)],
                  writes=[rQT[bq]], key=f"mQT{bq}")

        steps = [(g, st) for g, (h, qb) in enumerate(groups) for st in range(4 * (qb + 1))]
        cnt = {"pti": 0}

        def qk(g, st):
            h, qb = groups[g]
            hb_, bq = h % 2, g % 2
            L, rL = Lp.next()
            P.add("pe", lambda e: e.matmul(L[:, :], lhsT=KB[hb_][:, st * 128:(st + 1) * 128], rhs=QT[bq][:, :],
                                           start=True, stop=True),
                  reads=[rKB[hb_][(st * 128) // CH], rQT[bq]], writes=[rL])
            p = cnt["pti"] % len(PT)
            cnt["pti"] += 1
            P.add("act", lambda e: e.activation(out=PT[p][:], in_=L[:, :], func=AF.Exp, scale=SCALE_B),
                  reads=[rL], writes=[rPT[p]])
            j = st - 4 * qb
            if j >= 0:
                P.add("pool", lambda e: e.tensor_tensor(out=PT[p][:], in0=PT[p][:], in1=Cm[:, j, :], op=ALU.mult),
                      reads=[rPT[p], rCm], writes=[rPT[p]])
            return p

        def pv(g, st, p):
            h, qb = groups[g]
            hb_, bq = h % 2, g % 2
            nst = 4 * (qb + 1)
            O, rO = Ot[g % 2], rOt[g % 2]
            vsl = slice(64, 192) if h % 2 == 0 else slice(0, 128)
            num_lo = 0 if h % 2 == 0 else 64
            P.add("pe", lambda e: e.matmul(O[:, :], lhsT=VB[hb_][:, st, vsl], rhs=PT[p][:], start=(st == 0),
                                           stop=(st == nst - 1)),
                  reads=[rVB[hb_][(st * 128) // CH], rPT[p]], writes=[rO])
            if st == nst - 1:
                cs = slice(qb * 512, (qb + 1) * 512)
                normalize_out(P, c, O, rO, num_lo, rc[bq], rrc[bq], aT[bq][num_lo:num_lo + 64, :], raT[bq])
                P.dma("sp", S.ATT_T[4 + h // 2, num_lo:num_lo + 64, cs], aT[bq][num_lo:num_lo + 64, :],
                      reads=[raT[bq]], writes=[S.rATb[h][qb]], key=f"maT{bq}")

        LOOK = 2
        load_head(0)
        load_q(0)
        issued = {}
        loaded_q = {0}
        loaded_h = {0}

        def ensure_loads(g):
            if g >= len(groups):
                return
            h = groups[g][0]
            if h not in loaded_h:
                loaded_h.add(h)
                load_head(h)
            if g not in loaded_q:
                loaded_q.add(g)
                load_q(g)

        for i in range(min(LOOK, len(steps))):
            ensure_loads(steps[i][0])
            issued[i] = qk(*steps[i])
        for i, (g, st) in enumerate(steps):
            if i + LOOK < len(steps):
                ensure_loads(steps[i + LOOK][0])
                issued[i + LOOK] = qk(*steps[i + LOOK])
            pv(g, st, issued.pop(i))
            hh = groups[g][0]
            if st == 0 and groups[g][1] == 0 and hh + 1 < 8 and (hh + 1) not in loaded_h:
                loaded_h.add(hh + 1)
                load_head(hh + 1)
        return P.end_phase(ps)


def wout_phase(nc, P, c, T, S):
    NT = T // 128
    with ExitStack() as ps:
        def sb(name, shape, dt):
            return ps.enter_context(nc.sbuf_tensor(name, shape, dt))

        def pm(name, shape, dt):
            return ps.enter_context(nc.psum_tensor(name, shape, dt))

        R = P.res

        def dbl(name, shape, dt):
            return [sb(f"{name}{i}", shape, dt) for i in range(2)], [R() for _ in range(2)]

        Wo = sb("Wo", [128, 8, D], BF16)
        rWo = [R() for _ in range(8)]
        xt, rxt = dbl("wxt", [128, D], F32)
        at, rat = dbl("wat", [128, 8, 128], BF16)
        mmt = [pm(f"wmm{i}", [128, 512], F32) for i in range(4)]
        mmp = BankPool(mmt, [R() for _ in range(4)])
        stg, rstg = make_stage(nc, P, c, ps, "wo")
        load_weight_cast(P, c, Wo, [[r] for r in rWo], S.w_out, 8, [(0, 0, 512, 0), (512, 512, 512, 0)], stg, rstg)
        for i in range(NT):
            b = i % 2
            rows = slice(i * 128, (i + 1) * 128)
            P.dma("sp", xt[b][:], S.X1[rows, :], reads=[S.rX1[i]], writes=[rxt[b]], key=f"wxt{b}")
            P.dma("sp", at[b][:], S.ATT_T[:, :, rows].rearrange("c p t -> p c t"),
                  reads=[S.rATa[i]] + [S.rATb[h][i // 4] for h in range(8)], writes=[rat[b]], key=f"wat{b}")
            for half in range(2):
                bk, rbk = mmp.next()
                for cc in range(8):
                    P.add("pe", lambda e, bk=bk, cc=cc, b=b, half=half: e.matmul(
                        bk[:, :], lhsT=at[b][:, cc, :], rhs=Wo[:, cc, half * 512:(half + 1) * 512],
                        start=(cc == 0), stop=(cc == 7)),
                          reads=[rat[b], rWo[cc]], writes=[rbk])
                P.add("dve", lambda e, bk=bk, b=b, half=half: e.tensor_tensor(
                    out=xt[b][:, half * 512:(half + 1) * 512], in0=bk[:, :], in1=xt[b][:, half * 512:(half + 1) * 512],
                    op=ALU.add),
                      reads=[rbk, rxt[b]], writes=[rxt[b]])
            P.dma("sp", S.X2[rows, :], xt[b][:], reads=[rxt[b]], writes=[S.rX2[i]], key=f"wxo{b}")
        return P.end_phase(ps)


IN_NAMES = ["x", "g_ffn1", "w1_gate", "w1_up", "w1_down", "g_mix", "w_in", "g_q_lat", "g_kv_lat", "w_uq", "w_ukv",
            "w_out", "g_ffn2", "w2_gate", "w2_up", "w2_down", "g_final"]
IN_SHAPES = {"g_ffn1": [D], "w1_gate": [D, DFF], "w1_up": [D, DFF], "w1_down": [DFF, D], "g_mix": [D],
             "w_in": [D, DPROJ], "g_q_lat": [384], "g_kv_lat": [256], "w_uq": [384, 768], "w_ukv": [256, 1024],
             "w_out": [D, D], "g_ffn2": [D], "w2_gate": [D, DFF], "w2_up": [D, DFF], "w2_down": [DFF, D],
             "g_final": [D]}


def build(T, debug=False, phases=("ffn1", "proj", "dsa", "mla", "wout", "ffn2")):
    NT = T // 128
    NQB = T // 512
    nc = bass.Bass("TRN2", target_bir_lowering=False)
    S = Ctx()
    x = nc.dram_tensor("x", [T, D], F32, kind="ExternalInput").ap()
    for n, shp in IN_SHAPES.items():
        setattr(S, n, nc.dram_tensor(n, list(shp), F32, kind="ExternalInput").ap())
    S.rope64 = nc.dram_tensor("rope64", [T, 128], F32, kind="ExternalInput").ap()
    S.rope32 = nc.dram_tensor("rope32", [T, 64], F32, kind="ExternalInput").ap()
    out = nc.dram_tensor("out", [T, D], F32, kind="ExternalOutput").ap()
    kind = "ExternalOutput" if debug else "Internal"

    def scr(name, shape, dt):
        return nc.dram_tensor(name, list(shape), dt, kind=kind).ap()

    S.X1 = scr("X1", [T, D], F32)
    S.X2 = scr("X2", [T, D], F32)
    S.QA_T = scr("QA_T", [NT, 128, 512], BF16)
    S.QI_T = scr("QI_T", [NT, 128, 1024], BF16)
    S.SG = scr("SG", [T, 16], F32)
    S.KA_T2 = scr("KA_T2", [128, T], BF16)
    S.KI_T2 = scr("KI_T2", [128, T], BF16)
    S.VA1 = scr("VA1", [T, 192], BF16)
    S.QB_T = scr("QB_T", [8, 128, T], BF16)
    S.KB_T = scr("KB_T", [8, 128, T], BF16)
    S.VB1 = scr("VB1", [T, 8, 192], BF16)
    S.ATT_T = scr("ATT_T", [8, 128, T], BF16)
    with ExitStack() as stack:
        P = Prog(nc, stack)
        c = Ctx()
        const_phase(nc, P, c, stack)
        rl = lambda: [P.res() for _ in range(NT)]
        rx = rl()
        rout = rl()
        S.rX1, S.rX2, S.rQA, S.rQI, S.rSG, S.rKA, S.rKI, S.rVA, S.rQB, S.rKB, S.rVB, S.rATa = [rl() for _ in range(12)]
        S.rATb = [[P.res() for _ in range(NQB)] for _ in range(8)]
        info = {}
        if "ffn1" in phases:
            info["ffn1"] = ffn_phase(nc, P, c, T, x, rx, S.X1, S.rX1, S.g_ffn1, S.w1_gate, S.w1_up, S.w1_down)
        if "proj" in phases:
            info["proj"] = proj_phase(nc, P, c, T, S)
        if "dsa" in phases:
            info["dsa"] = dsa_phase(nc, P, c, T, S)
        if "mla" in phases:
            info["mla"] = mla_phase(nc, P, c, T, S)
        if "wout" in phases:
            info["wout"] = wout_phase(nc, P, c, T, S)
        if "ffn2" in phases:
            info["ffn2"] = ffn_phase(nc, P, c, T, S.X2, S.rX2, out, rout, S.g_ffn2, S.w2_gate, S.w2_up, S.w2_down,
                                     final_g=S.g_final, tag="f2")
        nc._mk_info = (info, dict(P.cnt), max(P.dcnt.values()) if P.dcnt else 0)
    return nc


def rope_table(T, dim):
    pos = np.arange(T, dtype=np.float32)
    inv_freq = (np.float32(10000.0) ** (-np.arange(0, dim, 2, dtype=np.float32) / np.float32(dim))).astype(np.float32)
    ang = pos[:, None] * inv_freq[None, :]
    cs, sn = np.cos(ang).astype(np.float32), np.sin(ang).astype(np.float32)
    return np.concatenate([cs, cs, -sn, sn], axis=1).astype(np.float32)


_NC_CACHE = {}


def kernel(**inputs):
    x = np.ascontiguousarray(np.asarray(inputs["x"], dtype=np.float32))
    B, T, _ = x.shape
    if T not in _NC_CACHE:
        _NC_CACHE[T] = build(T)
    nc = _NC_CACHE[T]
    shared = {}
    for n, shp in IN_SHAPES.items():
        shared[n] = np.ascontiguousarray(np.asarray(inputs[n], dtype=np.float32).reshape(shp))
    shared["rope64"] = rope_table(T, 64)
    shared["rope32"] = rope_table(T, 32)
    in_maps = []
    for bi in range(B):
        m = dict(shared)
        m["x"] = x[bi]
        in_maps.append(m)
    res = run_bass_kernel_spmd(nc, in_maps, core_ids=list(range(B)))
    return np.stack([np.asarray(r["out"]) for r in res.results], axis=0).astype(np.float32)
```

```python
import math
from contextlib import ExitStack

import numpy as np
import concourse.bass as bass
import concourse.mybir as mybir
from concourse.bass_utils import run_bass_kernel_spmd

F32 = mybir.dt.float32
BF16 = mybir.dt.bfloat16
AF = mybir.ActivationFunctionType
ALU = mybir.AluOpType
AX = mybir.AxisListType

D = 1024
DFF = 2816
NFC = DFF // 128
EPS = 1e-6
ENGS = ("pe", "act", "dve", "pool", "sp")


class Res:
    __slots__ = ("name", "last_w", "readers")

    def __init__(self, name):
        self.name = name
        self.last_w = None
        self.readers = []


class Op:
    __slots__ = ("eng", "fn", "deps", "idx", "signal", "sem", "val", "is_dma", "waits", "key", "emitted")

    def __init__(self, eng, fn, is_dma=False):
        self.eng = eng
        self.fn = fn
        self.deps = []
        self.signal = False
        self.sem = None
        self.val = None
        self.is_dma = is_dma
        self.waits = []
        self.key = None
        self.emitted = False


class Prog:
    def __init__(self, nc, stack):
        self.nc = nc
        self.stack = stack
        self.ops = []
        self.n_total = 0
        self.eng_sem = {e: stack.enter_context(nc.semaphore("S_" + e)) for e in ENGS}
        self.cnt = {e: 0 for e in ENGS}
        self.dma_sem = {}
        self.dcnt = {}
        self.keymap = {}
        for e_, n_ in (("pool", 40), ("sp", 44)):
            for i_ in range(n_):
                self.dma_sem[(e_, i_)] = stack.enter_context(nc.semaphore("D_%s_%d" % (e_, i_)))
                self.dcnt[(e_, i_)] = 0
        self.block = stack.enter_context(nc.Block())
        self.waited = {e: {} for e in ENGS}
        self.phase_dmas = []
        self.last_op = {e: None for e in ENGS}

    def res(self, name="r"):
        return Res(name)

    def add(self, eng, fn, reads=(), writes=(), dma_key=None):
        op = Op(eng, fn, is_dma=dma_key is not None)
        op.idx = self.n_total
        self.n_total += 1
        seen = set()

        def dep(d):
            if d is None or d.idx in seen or d.emitted:
                return
            seen.add(d.idx)
            if d.eng == op.eng and not d.is_dma and not op.is_dma and d.eng == "pe":
                return
            op.deps.append(d)
            d.signal = True

        for r in reads:
            dep(r.last_w)
        for w in writes:
            dep(w.last_w)
            for rd in w.readers:
                dep(rd)
        for r in reads:
            r.readers.append(op)
        for w in writes:
            w.last_w = op
            w.readers = []
        if dma_key is not None:
            op.key = dma_key
            op.signal = True
            self.phase_dmas.append(op)
        self.ops.append(op)
        return op

    def dma(self, eng, out, in_, reads=(), writes=(), key=None):
        return self.add(eng, lambda e: e.dma_start(out=out, in_=in_), reads=reads, writes=writes, dma_key=key)

    def end_phase(self, pstack):
        nc = self.nc
        last = {}
        for op in self.ops:
            if op.fn is not None and not op.is_dma:
                last[op.eng] = op
        for e in ENGS:
            fin = self.add(e, None)
            fin.deps = [o for e2, o in last.items() if e2 != e] + list(self.phase_dmas)
            for o in fin.deps:
                o.signal = True
        self.phase_dmas = []
        for op in self.ops:
            if op.is_dma:
                km = self.keymap.setdefault(op.eng, {})
                if op.key not in km:
                    km[op.key] = len(km)
                k = (op.eng, km[op.key])
                assert k in self.dma_sem, ("out of preallocated DMA semaphores", k)
                self.dcnt[k] += 16
                op.sem = self.dma_sem[k]
                op.val = self.dcnt[k]
            elif op.signal:
                self.cnt[op.eng] += 1
                op.sem = self.eng_sem[op.eng]
                op.val = self.cnt[op.eng]
        for op in self.ops:
            need = {}
            w = self.waited[op.eng]
            for d in op.deps:
                key = id(d.sem)
                if w.get(key, 0) >= d.val:
                    continue
                if key not in need or need[key][1] < d.val:
                    need[key] = (d.sem, d.val)
            for key, (s, v) in need.items():
                w[key] = v
                op.waits.append((s, v))
        per_eng = {e: [op for op in self.ops if op.eng == e] for e in ENGS}
        block = self.block
        handles = {"pe": block.tensor, "act": block.scalar, "dve": block.vector,
                   "pool": block.gpsimd, "sp": block.sync}

        def make(e):
            def body(eng):
                for op in per_eng[e]:
                    for (s, v) in op.waits:
                        eng.wait_ge(s, v)
                    if op.fn is None:
                        continue
                    ins = op.fn(eng)
                    if op.signal:
                        ins.then_inc(op.sem, 16 if op.is_dma else 1)
            return body

        for e in ENGS:
            if per_eng[e]:
                handles[e](make(e))
        n = len(self.ops)
        for op in self.ops:
            op.emitted = True
            op.fn = None
        self.ops = []
        self.keymap = {}
        return n


def bcast_rows(vec_ap, n):
    return bass.AP(tensor=vec_ap.tensor, offset=vec_ap.offset, ap=[[0, 128], [1, n]])


class Ctx:
    pass


def load_weight_cast(P, c, dst_tile, res2d, src_ap, nk, pieces, stage, rstage):
    for (dc, sc, w, ri) in pieces:
        for k in range(nk):
            s = c.sj % len(stage)
            c.sj += 1
            P.dma("sp", stage[s][:, 0:w], src_ap[k * 128:(k + 1) * 128, sc:sc + w], writes=[rstage[s]],
                  key=f"stg{s}")
            eng = ("act", "dve", "pool")[c.sj % 3]
            if eng == "act":
                P.add("act", lambda e, s=s, k=k, dc=dc, w=w: e.activation(out=dst_tile[:, k, dc:dc + w],
                                                                          in_=stage[s][:, 0:w], func=AF.Copy),
                      reads=[rstage[s]], writes=[res2d[k][ri]])
            else:
                P.add(eng, lambda e, s=s, k=k, dc=dc, w=w: e.tensor_copy(out=dst_tile[:, k, dc:dc + w],
                                                                        in_=stage[s][:, 0:w]),
                      reads=[rstage[s]], writes=[res2d[k][ri]])


def make_stage(nc, P, c, ps, tag, n=5):
    st = [ps.enter_context(nc.sbuf_tensor(f"{tag}stg{i}", [128, 512], F32)) for i in range(n)]
    return st, [P.res() for _ in range(n)]


def rmsnorm_to_bf16(P, c, x_ap, x_res, n, g_bc, g_res, junk, junk_res, ss, ss_res, rstd, rstd_res, out_ap, out_res,
                    x_in_psum=False):
    P.add("act", lambda e: e.activation(out=junk, in_=x_ap, func=AF.Square, accum_out=ss),
          reads=[x_res], writes=[junk_res, ss_res])
    P.add("act", lambda e: e.activation(out=rstd, in_=ss, func=AF.Sqrt, scale=1.0 / n, bias=c.eps_t[:, 0:1]),
          reads=[ss_res, c.rconst], writes=[rstd_res])
    P.add("dve", lambda e: e.reciprocal(out=rstd, in_=rstd), reads=[rstd_res], writes=[rstd_res])
    P.add("dve", lambda e: e.scalar_tensor_tensor(out=out_ap, in0=x_ap, scalar=rstd, in1=g_bc,
                                                  op0=ALU.mult, op1=ALU.mult),
          reads=[x_res, rstd_res, g_res], writes=[out_res])


def ffn_phase(nc, P, c, T, src, src_res, dst, dst_res, g_vec, wg, wu, wd, final_g=None, tag="f1"):
    NT = T // 128
    with ExitStack() as ps:
        def sb(name, shape, dt):
            return ps.enter_context(nc.sbuf_tensor(tag + name, shape, dt))

        def pm(name, shape, dt):
            return ps.enter_context(nc.psum_tensor(tag + name, shape, dt))

        Wg = sb("Wg", [128, 8, DFF], BF16)
        Wu = sb("Wu", [128, 8, DFF], BF16)
        Wd = sb("Wd", [128, NFC, D], BF16)
        gbc = sb("gbc", [128, D], F32)
        gfin = sb("gfin", [128, D], F32) if final_g is not None else None
        xt = [sb(f"xt{i}", [128, D], F32) for i in range(3)]
        junk = [sb(f"junk{i}", [128, D], BF16) for i in range(2)]
        ss = [sb(f"ss{i}", [128, 4], F32) for i in range(2)]
        hb = [sb(f"hb{i}", [128, D], BF16) for i in range(2)]
        hT = [sb(f"hT{i}", [128, 8, 128], BF16) for i in range(2)]
        sg = [sb(f"sg{i}", [128, 512], F32) for i in range(2)]
        act = [sb(f"act{i}", [128, DFF], BF16) for i in range(2)]
        actT = [sb(f"actT{i}", [128, NFC, 128], BF16) for i in range(2)]
        tp = [pm(f"tp{i}", [128, 1024], BF16) for i in range(2)]
        mm = [pm(f"mm{i}", [128, 512], F32) for i in range(6)]

        R = P.res
        rWg = [[R() for _ in range(6)] for _ in range(8)]
        rWu = [[R() for _ in range(6)] for _ in range(8)]
        rWd = [[R(), R()] for _ in range(NFC)]
        stg, rstg = make_stage(nc, P, c, ps, tag)
        rg, rgf = R(), R()
        rxt = [R() for _ in range(3)]
        rjunk = [R() for _ in range(2)]
        rss = [R() for _ in range(2)]
        rrs = [R() for _ in range(2)]
        rhb = [R() for _ in range(2)]
        rhT = [R() for _ in range(2)]
        rsg = [R() for _ in range(2)]
        ract = [R() for _ in range(2)]
        ractT = [R() for _ in range(2)]
        ryo = [R() for _ in range(2)]
        rtp = [R() for _ in range(2)]
        rmm = [R() for _ in range(6)]

        P.dma("sp", gbc[:], bcast_rows(g_vec, D), writes=[rg], key="gbc")
        if final_g is not None:
            P.dma("sp", gfin[:], bcast_rows(final_g, D), writes=[rgf], key="gfin")
        for i0 in range(min(2, NT)):
            P.dma("sp", xt[i0][:], src[i0 * 128:(i0 + 1) * 128, :], reads=[src_res[i0]], writes=[rxt[i0]],
                  key=f"xt{i0}")
        for si0, s00 in enumerate(range(0, DFF, 512)):
            pc = [(s00, s00, min(512, DFF - s00), si0)]
            load_weight_cast(P, c, Wg, rWg, wg, 8, pc, stg, rstg)
            load_weight_cast(P, c, Wu, rWu, wu, 8, pc, stg, rstg)
        load_weight_cast(P, c, Wd, rWd, wd, NFC, [(0, 0, 512, 0), (512, 512, 512, 1)], stg, rstg)

        slabs = [(s0, min(512, DFF - s0)) for s0 in range(0, DFF, 512)]
        cn = {"mmi": 0, "tpi": 0}

        def stage1(i):
            b = i % 2
            b3 = i % 3
            rows = slice(i * 128, (i + 1) * 128)
            if i >= 2:
                P.dma("sp", xt[b3][:], src[rows, :], reads=[src_res[i]], writes=[rxt[b3]], key=f"xt{b3}")
            rmsnorm_to_bf16(P, c, xt[b3][:], rxt[b3], D, gbc[:], rg, junk[b][:], rjunk[b], ss[b][:, 0:1], rss[b],
                            ss[b][:, 1:2], rrs[b], hb[b][:], rhb[b])
            t = cn['tpi'] % 2
            cn['tpi'] += 1
            for k in range(8):
                P.add("pe", lambda e, k=k, t=t, b=b, b3=b3: e.transpose(out=tp[t][:, k * 128:(k + 1) * 128],
                                                                in_=hb[b][:, k * 128:(k + 1) * 128],
                                                                identity=c.ident[:]),
                      reads=[rhb[b], c.rident], writes=[rtp[t]])
            P.add("act", lambda e, t=t, b=b, b3=b3: e.activation(out=hT[b][:].rearrange("p k t -> p (k t)"),
                                                          in_=tp[t][:], func=AF.Copy),
                  reads=[rtp[t]], writes=[rhT[b]])
            for si, (s0, sw) in enumerate(slabs):
                ga = cn['mmi'] % 6
                ua = (cn['mmi'] + 1) % 6
                cn['mmi'] += 2
                for k in range(8):
                    P.add("pe", lambda e, k=k, ga=ga, b=b, b3=b3, s0=s0, sw=sw: e.matmul(
                        mm[ga][:, 0:sw], lhsT=hT[b][:, k, :], rhs=Wg[:, k, s0:s0 + sw],
                        start=(k == 0), stop=(k == 7)),
                          reads=[rhT[b], rWg[k][si]], writes=[rmm[ga]])
                for k in range(8):
                    P.add("pe", lambda e, k=k, ua=ua, b=b, b3=b3, s0=s0, sw=sw: e.matmul(
                        mm[ua][:, 0:sw], lhsT=hT[b][:, k, :], rhs=Wu[:, k, s0:s0 + sw],
                        start=(k == 0), stop=(k == 7)),
                          reads=[rhT[b], rWu[k][si]], writes=[rmm[ua]])
                s2 = si % 2
                P.add("act", lambda e, ga=ga, s2=s2, sw=sw: e.activation(out=sg[s2][:, 0:sw], in_=mm[ga][:, 0:sw],
                                                                        func=AF.Silu),
                      reads=[rmm[ga]], writes=[rsg[s2]])
                P.add("dve", lambda e, ua=ua, s2=s2, b=b, b3=b3, s0=s0, sw=sw: e.tensor_tensor(
                    out=act[b][:, s0:s0 + sw], in0=sg[s2][:, 0:sw], in1=mm[ua][:, 0:sw], op=ALU.mult),
                      reads=[rsg[s2], rmm[ua]], writes=[ract[b]])

        def stage2(i):
            b = i % 2
            b3 = i % 3
            rows = slice(i * 128, (i + 1) * 128)
            for f0 in range(0, NFC, 8):
                nf = min(8, NFC - f0)
                t = cn['tpi'] % 2
                cn['tpi'] += 1
                for f in range(nf):
                    P.add("pe", lambda e, f=f, f0=f0, t=t, b=b, b3=b3: e.transpose(
                        out=tp[t][:, f * 128:(f + 1) * 128],
                        in_=act[b][:, (f0 + f) * 128:(f0 + f + 1) * 128], identity=c.ident[:]),
                          reads=[ract[b], c.rident], writes=[rtp[t]])
                P.add("act" if (f0 // 8) % 2 == 0 else "dve",
                      (lambda e, t=t, b=b, b3=b3, f0=f0, nf=nf: e.activation(
                          out=actT[b][:, f0:f0 + nf, :].rearrange("p k t -> p (k t)"),
                          in_=tp[t][:, 0:nf * 128], func=AF.Copy)) if (f0 // 8) % 2 == 0 else
                      (lambda e, t=t, b=b, b3=b3, f0=f0, nf=nf: e.tensor_copy(
                          out=actT[b][:, f0:f0 + nf, :].rearrange("p k t -> p (k t)"),
                          in_=tp[t][:, 0:nf * 128])),
                      reads=[rtp[t]], writes=[ractT[b]])
            for half in range(2):
                da = cn['mmi'] % 6
                cn['mmi'] += 1
                for f in range(NFC):
                    P.add("pe", lambda e, f=f, da=da, b=b, b3=b3, half=half: e.matmul(
                        mm[da][:, :], lhsT=actT[b][:, f, :], rhs=Wd[:, f, half * 512:(half + 1) * 512],
                        start=(f == 0), stop=(f == NFC - 1)),
                          reads=[ractT[b], rWd[f][half]], writes=[rmm[da]])
                P.add("dve", lambda e, da=da, b=b, b3=b3, half=half: e.scalar_tensor_tensor(
                    out=xt[b3][:, half * 512:(half + 1) * 512], in0=mm[da][:, :], scalar=0.5,
                    in1=xt[b3][:, half * 512:(half + 1) * 512], op0=ALU.mult, op1=ALU.add),
                      reads=[rmm[da], rxt[b3]], writes=[rxt[b3]])
            if final_g is None:
                P.dma("sp", dst[rows, :], xt[b3][:], reads=[rxt[b3]], writes=[dst_res[i]], key=f"xo{b3}")
            else:
                P.add("act", lambda e, b=b, b3=b3: e.activation(out=junk[b][:], in_=xt[b3][:], func=AF.Square,
                                                         accum_out=ss[b][:, 2:3]),
                      reads=[rxt[b3]], writes=[rjunk[b], rss[b]])
                P.add("act", lambda e, b=b, b3=b3: e.activation(out=ss[b][:, 3:4], in_=ss[b][:, 2:3], func=AF.Sqrt,
                                                         scale=1.0 / D, bias=c.eps_t[:, 0:1]),
                      reads=[rss[b], c.rconst], writes=[rrs[b]])
                P.add("dve", lambda e, b=b, b3=b3: e.reciprocal(out=ss[b][:, 3:4], in_=ss[b][:, 3:4]),
                      reads=[rrs[b]], writes=[rrs[b]])
                P.add("dve", lambda e, b=b, b3=b3: e.scalar_tensor_tensor(out=xt[b3][:], in0=xt[b3][:], scalar=ss[b][:, 3:4],
                                                                   in1=gfin[:], op0=ALU.mult, op1=ALU.mult),
                      reads=[rxt[b3], rrs[b], rgf], writes=[rxt[b3]])
                P.dma("sp", dst[rows, :], xt[b3][:], reads=[rxt[b3]], writes=[dst_res[i]], key=f"xo{b3}")

        stage1(0)
        for i in range(NT):
            if i + 1 < NT:
                stage1(i + 1)
            stage2(i)
        return P.end_phase(ps)


def const_phase(nc, P, c, stack):
    c.ident = stack.enter_context(nc.sbuf_tensor("ident", [128, 128], BF16))
    c.identf = stack.enter_context(nc.sbuf_tensor("identf", [128, 128], F32))
    c.eps_t = stack.enter_context(nc.sbuf_tensor("eps_t", [128, 1], F32))
    c.rident = P.res()
    c.rconst = P.res()
    c.sj = 0
    P.add("pool", lambda e: e.memset(c.identf[:], 0.0), writes=[c.rident])
    P.add("pool", lambda e: e.affine_select(out=c.identf[:], in_=c.identf[:], pattern=[[-1, 128]],
                                            compare_op=ALU.not_equal, fill=1.0, base=0, channel_multiplier=1),
          reads=[c.rident], writes=[c.rident])
    P.add("pool", lambda e: e.tensor_copy(out=c.ident[:], in_=c.identf[:]), reads=[c.rident], writes=[c.rident])
    P.add("pool", lambda e: e.memset(c.eps_t[:], EPS), writes=[c.rconst])


def bc_mid(ap2d, H):
    a = [list(x) for x in ap2d.ap]
    return bass.AP(tensor=ap2d.tensor, offset=ap2d.offset, ap=[a[0], [0, H], a[1]])


def bc_last(ap2d, w):
    a = [list(x) for x in ap2d.ap]
    return bass.AP(tensor=ap2d.tensor, offset=ap2d.offset, ap=[a[0], a[1], [0, w]])


class BankPool:
    def __init__(self, tiles, res):
        self.tiles = tiles
        self.res = res
        self.i = 0

    def next(self):
        k = self.i % len(self.tiles)
        self.i += 1
        return self.tiles[k], self.res[k]


def transposes_to(P, c, tpp, srcs, src_res, dst_ap, dst_res, np_out=128, copy_eng="act"):
    tp, rtp = tpp.next()
    n = len(srcs)
    for j, s_ap in enumerate(srcs):
        P.add("pe", lambda e, j=j, s_ap=s_ap: e.transpose(out=tp[0:np_out, j * 128:(j + 1) * 128], in_=s_ap,
                                                         identity=c.ident[:]),
              reads=list(src_res) + [c.rident], writes=[rtp])
    if copy_eng == "act":
        P.add("act", lambda e: e.activation(out=dst_ap, in_=tp[0:np_out, 0:n * 128], func=AF.Copy),
              reads=[rtp], writes=[dst_res])
    else:
        P.add("dve", lambda e: e.tensor_copy(out=dst_ap, in_=tp[0:np_out, 0:n * 128]),
              reads=[rtp], writes=[dst_res])


def rope_ops(P, c, src3, src_res, H, d, tab, tab_res, ta, rta, tb, rtb, out3, out_res, out3b=None):
    h2 = d // 2
    cc = bc_mid(tab[:, 0:d], H)
    s0 = bc_mid(tab[:, d:d + h2], H)
    s1 = bc_mid(tab[:, d + h2:2 * d], H)
    P.add("dve", lambda e: e.tensor_tensor(out=ta, in0=src3, in1=cc, op=ALU.mult),
          reads=[src_res, tab_res], writes=[rta])
    P.add("dve", lambda e: e.tensor_tensor(out=tb[:, :, 0:h2], in0=src3[:, :, h2:d], in1=s0, op=ALU.mult),
          reads=[src_res, tab_res], writes=[rtb])
    P.add("dve", lambda e: e.tensor_tensor(out=tb[:, :, h2:d], in0=src3[:, :, 0:h2], in1=s1, op=ALU.mult),
          reads=[src_res, tab_res], writes=[rtb])
    P.add("pool", lambda e: e.tensor_tensor(out=out3, in0=ta, in1=tb, op=ALU.add),
          reads=[rta, rtb], writes=[out_res])
    if out3b is not None:
        P.add("pool", lambda e: e.tensor_tensor(out=out3b, in0=ta, in1=tb, op=ALU.add),
              reads=[rta, rtb], writes=[out_res])


WIN_SEGS = [
    (0, 0, 512),
    (512, 640, 512),
    (1024, 1152, 512),
    (1536, 1744, 384),
    (1920, 512, 64),
    (1984, 1664, 64),
    (2048, 2128, 256),
    (2304, 2384, 32),
    (2336, 576, 64),
    (2400, 1728, 16),
]
WIN_GROUPS = [(0, 512), (512, 512), (1024, 512), (1536, 512), (2048, 368)]
DPROJ = 2416


def proj_phase(nc, P, c, T, S):
    LIM = 99.0
    NT = T // 128
    with ExitStack() as ps:
        def sb(name, shape, dt):
            return ps.enter_context(nc.sbuf_tensor(name, shape, dt))

        def pm(name, shape, dt):
            return ps.enter_context(nc.psum_tensor(name, shape, dt))

        R = P.res
        Win = sb("Win", [128, 8, DPROJ], BF16)
        Wuq = sb("Wuq", [128, 3, 768], BF16)
        Wukv = sb("Wukv", [128, 2, 1024], BF16)
        gbc = sb("gbcm", [128, D], F32)
        gq = sb("gq", [128, 384], F32)
        gkv = sb("gkv", [128, 256], F32)
        rWin = [R() for _ in range(8)]
        rWuq = [R() for _ in range(3)]
        rWukv = [R() for _ in range(2)]
        rg, rgq, rgkv = R(), R(), R()

        def dbl(name, shape, dt):
            return [sb(f"{name}{i}", shape, dt) for i in range(2)], [R() for _ in range(2)]

        xt, rxt = dbl("pxt", [128, D], F32)
        junk, rjunk = dbl("pjunk", [128, D], BF16)
        ss, rss = dbl("pss", [128, 8], F32)
        rrs = [[R() for _ in range(3)] for _ in range(2)]
        hb, rhb = dbl("phb", [128, D], BF16)
        hT, rhT = dbl("phT", [128, 8, 128], BF16)
        r64, rr64 = dbl("r64", [128, 128], F32)
        r32, rr32 = dbl("r32", [128, 64], F32)
        ta, rta = dbl("ta", [128, 512], F32)
        tb, rtb = dbl("tb", [128, 512], F32)
        qar, rqar = dbl("qar", [128, 512], BF16)
        qir, rqir = dbl("qir", [128, 1024], BF16)
        qif, rqif = dbl("qif", [128, 512], F32)
        qaT, rqaT = dbl("qaT", [128, 512], BF16)
        qiT, rqiT = dbl("qiT", [128, 1024], BF16)
        aw, raw = dbl("aw", [128, 16], F32)
        sgt, rsgt = dbl("sgt", [128, 16], F32)
        kdup, rkdup = dbl("kdup", [128, 256], BF16)
        kT, rkT = dbl("kT", [128, 256], BF16)
        kpe, rkpe = dbl("kpe", [128, 32], BF16)
        va1, rva1 = dbl("va1", [128, 192], BF16)
        cqn, rcqn = dbl("cqn", [128, 384], BF16)
        cqT, rcqT = dbl("cqT", [128, 384], BF16)
        ckn, rckn = dbl("ckn", [128, 256], BF16)
        ckT, rckT = dbl("ckT", [128, 256], BF16)
        qbr, rqbr = dbl("qbr", [128, 8, 128], BF16)
        kbr, rkbr = dbl("kbr", [128, 8, 128], BF16)
        qbT, rqbT = dbl("qbT", [128, 1024], BF16)
        kbT, rkbT = dbl("kbT", [128, 1024], BF16)
        vb1, rvb1 = dbl("vb1", [128, 8, 192], BF16)
        tpt = [pm(f"ptp{i}", [128, 1024], BF16) for i in range(2)]
        tpp = BankPool(tpt, [R() for _ in range(2)])
        mmt = [pm(f"pmm{i}", [128, 512], F32) for i in range(6)]
        mmp = BankPool(mmt, [R() for _ in range(6)])

        P.dma("sp", gbc[:], bcast_rows(S.g_mix, D), writes=[rg], key="gbc")
        P.dma("sp", gq[:], bcast_rows(S.g_q_lat, 384), writes=[rgq], key="gq")
        P.dma("sp", gkv[:], bcast_rows(S.g_kv_lat, 256), writes=[rgkv], key="gkv")
        stg, rstg = make_stage(nc, P, c, ps, "pj")
        load_weight_cast(P, c, Win, [[r] for r in rWin], S.w_in, 8, [(dc, sc, w, 0) for (dc, sc, w) in WIN_SEGS],
                         stg, rstg)
        load_weight_cast(P, c, Wuq, [[r] for r in rWuq], S.w_uq, 3, [(0, 0, 384, 0), (384, 384, 384, 0)], stg, rstg)
        load_weight_cast(P, c, Wukv, [[r] for r in rWukv], S.w_ukv, 2, [(0, 0, 512, 0), (512, 512, 512, 0)], stg, rstg)
        for b in range(2):
            P.add("pool", lambda e, b=b: e.memset(qbr[b][:], 0.0), writes=[rqbr[b]])
            P.add("pool", lambda e, b=b: e.memset(kbr[b][:], 0.0), writes=[rkbr[b]])
            P.add("pool", lambda e, b=b: e.memset(va1[b][:], 1.0), writes=[rva1[b]])
            P.add("pool", lambda e, b=b: e.memset(vb1[b][:], 1.0), writes=[rvb1[b]])

        def stage1a(i):
            b = i % 2
            rows = slice(i * 128, (i + 1) * 128)
            P.dma("sp", xt[b][:], S.X1[rows, :], reads=[S.rX1[i]], writes=[rxt[b]], key=f"xt{b}")
            P.dma("sp", r64[b][:], S.rope64[rows, :], writes=[rr64[b]], key=f"r64{b}")
            P.dma("sp", r32[b][:], S.rope32[rows, :], writes=[rr32[b]], key=f"r32{b}")
            rmsnorm_to_bf16(P, c, xt[b][:], rxt[b], D, gbc[:], rg, junk[b][:], rjunk[b], ss[b][:, 0:1], rss[b],
                            ss[b][:, 1:2], rrs[b][0], hb[b][:], rhb[b])
            transposes_to(P, c, tpp, [hb[b][:, k * 128:(k + 1) * 128] for k in range(8)], [rhb[b]],
                          hT[b][:].rearrange("p k t -> p (k t)"), rhT[b])

        def stage2(i):
            b = i % 2
            rows = slice(i * 128, (i + 1) * 128)
            cols = slice(i * 128, (i + 1) * 128)
            if LIM < 2:
                return
            banks = []
            for (g0, gw) in WIN_GROUPS:
                bk, rbk = mmp.next()
                for k in range(8):
                    P.add("pe", lambda e, k=k, bk=bk, g0=g0, gw=gw, b=b: e.matmul(
                        bk[:, 0:gw], lhsT=hT[b][:, k, :], rhs=Win[:, k, g0:g0 + gw], start=(k == 0), stop=(k == 7)),
                          reads=[rhT[b], rWin[k]], writes=[rbk])
                banks.append((bk, rbk))
            (B0, rB0), (B1, rB1), (B2, rB2), (B3, rB3), (B4, rB4) = banks
            v3 = lambda ap, H: ap.rearrange("p (h d) -> p h d", h=H)
            if LIM < 3:
                return
            P.add("act", lambda e, b=b, B4=B4: e.activation(out=aw[b][:], in_=B4[:, 352:368], func=AF.Abs,
                                                           scale=1.0 / 32.0),
                  reads=[rB4], writes=[raw[b]])
            P.add("act", lambda e, b=b, B4=B4: e.activation(out=sgt[b][:], in_=B4[:, 352:368], func=AF.Sign),
                  reads=[rB4], writes=[rsgt[b]])
            P.dma("sp", S.SG[rows, :], sgt[b][:], reads=[rsgt[b]], writes=[S.rSG[i]], key=f"sgt{b}")
            if LIM < 4:
                return
            rope_ops(P, c, v3(B0[:, 0:512], 8), rB0, 8, 64, r64[b], rr64[b], v3(ta[b][:], 8), rta[b],
                     v3(tb[b][:], 8), rtb[b], v3(qar[b][:], 8), rqar[b])
            transposes_to(P, c, tpp, [qar[b][:, j * 128:(j + 1) * 128] for j in range(4)], [rqar[b]],
                          qaT[b][:], rqaT[b], copy_eng="dve")
            P.dma("sp", S.QA_T[i], qaT[b][:], reads=[rqaT[b]], writes=[S.rQA[i]], key=f"qaT{b}")
            if LIM < 5:
                return
            for hh, (Bq, rBq) in enumerate(((B1, rB1), (B2, rB2))):
                rope_ops(P, c, v3(Bq[:, 0:512], 8), rBq, 8, 64, r64[b], rr64[b], v3(ta[b][:], 8), rta[b],
                         v3(tb[b][:], 8), rtb[b], v3(qif[b][:], 8), rqif[b])
                P.add("dve", lambda e, b=b, hh=hh: e.tensor_tensor(
                    out=v3(qir[b][:, hh * 512:(hh + 1) * 512], 8), in0=v3(qif[b][:], 8),
                    in1=bc_last(aw[b][:, hh * 8:(hh + 1) * 8], 64), op=ALU.mult),
                      reads=[rqif[b], raw[b]], writes=[rqir[b]])
            transposes_to(P, c, tpp, [qir[b][:, j * 128:(j + 1) * 128] for j in range(8)], [rqir[b]],
                          qiT[b][:], rqiT[b])
            P.dma("sp", S.QI_T[i], qiT[b][:], reads=[rqiT[b]], writes=[S.rQI[i]], key=f"qiT{b}")
            if LIM < 6:
                return
            kd4 = kdup[b][:].rearrange("p (a r d) -> p a r d", a=2, r=2)
            rope_ops(P, c, v3(B3[:, 384:512], 2), rB3, 2, 64, r64[b], rr64[b], v3(ta[b][:, 0:128], 2), rta[b],
                     v3(tb[b][:, 0:128], 2), rtb[b], kd4[:, :, 0, :], rkdup[b], out3b=kd4[:, :, 1, :])
            transposes_to(P, c, tpp, [kdup[b][:, 0:128], kdup[b][:, 128:256]], [rkdup[b]], kT[b][:], rkT[b],
                          copy_eng="dve")
            P.dma("sp", S.KA_T2[:, cols], kT[b][:, 0:128], reads=[rkT[b]], writes=[S.rKA[i]], key=f"kTa{b}")
            P.dma("sp", S.KI_T2[:, cols], kT[b][:, 128:256], reads=[rkT[b]], writes=[S.rKI[i]], key=f"kTi{b}")
            if LIM < 7:
                return
            rope_ops(P, c, v3(B4[:, 256:288], 1), rB4, 1, 32, r32[b], rr32[b], v3(ta[b][:, 0:32], 1), rta[b],
                     v3(tb[b][:, 0:32], 1), rtb[b], v3(kpe[b][:], 1), rkpe[b])
            if LIM < 8:
                return
            P.add("act", lambda e, b=b, B4=B4: e.activation(out=va1[b][:, 64:128], in_=B4[:, 288:352], func=AF.Copy),
                  reads=[rB4], writes=[rva1[b]])
            P.dma("sp", S.VA1[rows, :], va1[b][:], reads=[rva1[b]], writes=[S.rVA[i]], key=f"va1{b}")
            if LIM < 9:
                return
            rmsnorm_to_bf16(P, c, B3[:, 0:384], rB3, 384, gq[:], rgq, junk[b][:, 0:384], rjunk[b], ss[b][:, 2:3],
                            rss[b], ss[b][:, 3:4], rrs[b][1], cqn[b][:], rcqn[b])
            if LIM < 9.1:
                return
            transposes_to(P, c, tpp, [cqn[b][:, k * 128:(k + 1) * 128] for k in range(3)], [rcqn[b]],
                          cqT[b][:], rcqT[b], copy_eng="dve")
            if LIM < 9.2:
                return
            for (q0, qw, h0, nh) in ((0, 480, 0, 5), (480, 288, 5, 3)):
                bk, rbk = mmp.next()
                for k in range(3):
                    P.add("pe", lambda e, k=k, bk=bk, q0=q0, qw=qw, b=b: e.matmul(
                        bk[:, 0:qw], lhsT=cqT[b][:, k * 128:(k + 1) * 128], rhs=Wuq[:, k, q0:q0 + qw],
                        start=(k == 0), stop=(k == 2)),
                          reads=[rcqT[b], rWuq[k]], writes=[rbk])
                if LIM < 9.3:
                    return
                bv = bk[:, 0:qw].rearrange("p (h d) -> p h d", h=nh)
                P.add("dve", lambda e, bv=bv, b=b, h0=h0, nh=nh: e.tensor_copy(
                    out=qbr[b][:, h0:h0 + nh, 0:64], in_=bv[:, :, 0:64]),
                      reads=[rbk], writes=[rqbr[b]])
                if LIM < 9.4:
                    return
                rope_ops(P, c, bv[:, :, 64:96], rbk, nh, 32, r32[b], rr32[b],
                         ta[b][:, 0:nh * 32].rearrange("p (h d) -> p h d", h=nh), rta[b],
                         tb[b][:, 0:nh * 32].rearrange("p (h d) -> p h d", h=nh), rtb[b],
                         qbr[b][:, h0:h0 + nh, 64:96], rqbr[b])
            if LIM < 9.5:
                return
            transposes_to(P, c, tpp, [qbr[b][:, h, :] for h in range(8)], [rqbr[b]], qbT[b][:], rqbT[b])
            if LIM < 9.6:
                return
            P.dma("sp", S.QB_T[:, :, cols].rearrange("h p t -> p h t"),
                  qbT[b][:].rearrange("p (h t) -> p h t", h=8), reads=[rqbT[b]], writes=[S.rQB[i]], key=f"qbT{b}")
            if LIM < 10:
                return
            rmsnorm_to_bf16(P, c, B4[:, 0:256], rB4, 256, gkv[:], rgkv, junk[b][:, 0:256], rjunk[b], ss[b][:, 4:5],
                            rss[b], ss[b][:, 5:6], rrs[b][2], ckn[b][:], rckn[b])
            transposes_to(P, c, tpp, [ckn[b][:, k * 128:(k + 1) * 128] for k in range(2)], [rckn[b]],
                          ckT[b][:], rckT[b], copy_eng="dve")
            P.add("pool", lambda e, b=b: e.tensor_copy(out=kbr[b][:, :, 64:96], in_=bc_mid(kpe[b][:], 8)),
                  reads=[rkpe[b]], writes=[rkbr[b]])
            for hf in range(2):
                bk, rbk = mmp.next()
                for k in range(2):
                    P.add("pe", lambda e, k=k, bk=bk, hf=hf, b=b: e.matmul(
                        bk[:, :], lhsT=ckT[b][:, k * 128:(k + 1) * 128], rhs=Wukv[:, k, hf * 512:(hf + 1) * 512],
                        start=(k == 0), stop=(k == 1)),
                          reads=[rckT[b], rWukv[k]], writes=[rbk])
                bv = bk[:, :].rearrange("p (h d) -> p h d", h=4)
                P.add("dve", lambda e, bv=bv, b=b, hf=hf: e.tensor_copy(
                    out=kbr[b][:, hf * 4:(hf + 1) * 4, 0:64], in_=bv[:, :, 0:64]),
                      reads=[rbk], writes=[rkbr[b]])
                P.add("dve", lambda e, bv=bv, b=b, hf=hf: e.tensor_copy(
                    out=vb1[b][:, hf * 4:(hf + 1) * 4, 64:128], in_=bv[:, :, 64:128]),
                      reads=[rbk], writes=[rvb1[b]])
            transposes_to(P, c, tpp, [kbr[b][:, h, :] for h in range(8)], [rkbr[b]], kbT[b][:], rkbT[b])
            P.dma("sp", S.KB_T[:, :, cols].rearrange("h p t -> p h t"),
                  kbT[b][:].rearrange("p (h t) -> p h t", h=8), reads=[rkbT[b]], writes=[S.rKB[i]], key=f"kbT{b}")
            P.dma("sp", S.VB1[rows, :, :], vb1[b][:], reads=[rvb1[b]], writes=[S.rVB[i]], key=f"vb1{b}")

        stage1a(0)
        for i in range(NT):
            if i + 1 < NT:
                stage1a(i + 1)
            stage2(i)
        return P.end_phase(ps)


NEG = -1.0e30
NBIS = 16


def normalize_out(P, c, O, rO, num_lo, rc, rrc, out_ap, out_res):
    den_lo = 64 - num_lo
    P.add("dve", lambda e: e.reciprocal(out=rc[den_lo:den_lo + 64, :], in_=O[den_lo:den_lo + 64, :]),
          reads=[rO], writes=[rrc])
    P.add("dve", lambda e: e.tensor_tensor(out=out_ap, in0=O[num_lo:num_lo + 64, :],
                                           in1=rc[den_lo:den_lo + 64, :], op=ALU.mult),
          reads=[rO, rrc], writes=[out_res])


def dsa_phase(nc, P, c, T, S):
    NT = T // 128
    TOPK = min(256, T // 4)
    QT0 = TOPK // 128
    with ExitStack() as ps:
        def sb(name, shape, dt):
            return ps.enter_context(nc.sbuf_tensor(name, shape, dt))

        def pm(name, shape, dt):
            return ps.enter_context(nc.psum_tensor(name, shape, dt))

        R = P.res

        def dbl(name, shape, dt):
            return [sb(f"{name}{i}", shape, dt) for i in range(2)], [R() for _ in range(2)]

        KA2 = sb("KA2", [128, T], BF16)
        KI2 = sb("KI2", [128, T], BF16)
        VAs = sb("VAs", [128, NT, 192], BF16)
        rKA2 = [R() for _ in range(NT)]
        rKI2 = [R() for _ in range(NT)]
        rVAs = [R() for _ in range(NT)]
        cneg = sb("cneg", [128, 128], F32)
        pow2 = sb("pow2", [128, NBIS], F32)
        rcn = R()
        qiT, rqiT = dbl("dqiT", [128, 1024], BF16)
        qaT, rqaT = dbl("dqaT", [128, 512], BF16)
        sg, rsg = dbl("dsg", [128, 16], F32)
        Rt = [sb(f"Rt{i}", [128, 512], BF16) for i in range(4)]
        Dg, rDg = dbl("Dg", [128, 16, 128], BF16)
        rRt = [R() for _ in range(4)]
        Isb, rIsb = dbl("Isb", [128, T], F32)
        cjunk = sb("cjunk", [128, T], BF16)
        rcj = R()
        st_, rst = dbl("dst", [128, 8 + NBIS], F32)
        maskq, rmq = dbl("maskq", [128, T], BF16)
        maskT, rmT = dbl("maskT", [128, NT, 128], BF16)
        PT = [sb(f"PT{i}", [128, 1024], BF16) for i in range(4)]
        rPT = [R() for _ in range(4)]
        rc, rrc = dbl("drc", [128, 512], F32)
        aT, raT = dbl("daT", [128, 512], BF16)
        Lt = [pm(f"dL{i}", [128, 512], F32) for i in range(4)]
        Lp = BankPool(Lt, [R() for _ in range(4)])
        At = [pm(f"dA{i}", [128, 512], F32) for i in range(2)]
        rAt = [R() for _ in range(2)]
        OE = pm("dOE", [128, 512], F32)
        OO = pm("dOO", [128, 512], F32)
        rOE, rOO = R(), R()

        P.add("pool", lambda e: e.memset(cneg[:], 0.0), writes=[rcn])
        P.add("pool", lambda e: e.affine_select(out=cneg[:], in_=cneg[:], pattern=[[-1, 128]],
                                                compare_op=ALU.is_ge, fill=NEG, base=0, channel_multiplier=1),
              reads=[rcn], writes=[rcn])
        for k in range(NBIS):
            P.add("pool", lambda e, k=k: e.memset(pow2[:, k:k + 1], 2.0 ** (-(k + 1))), writes=[rcn])
        CH = min(T, 1024)
        for ci in range(T // CH):
            cols = slice(ci * CH, (ci + 1) * CH)
            tl = range(ci * (CH // 128), (ci + 1) * (CH // 128))
            rk, rki, rv = R(), R(), R()
            P.dma("sp", KA2[:, cols], S.KA_T2[:, cols], reads=[S.rKA[i] for i in tl], writes=[rk], key=f"KA2_{ci}")
            P.dma("sp", KI2[:, cols], S.KI_T2[:, cols], reads=[S.rKI[i] for i in tl], writes=[rki], key=f"KI2_{ci}")
            P.dma("sp", VAs[:, ci * (CH // 128):(ci + 1) * (CH // 128), :],
                  S.VA1[cols, :].rearrange("(n p) c -> p n c", p=128), reads=[S.rVA[i] for i in tl], writes=[rv],
                  key=f"VAs_{ci}")
            for i in tl:
                rKA2[i], rKI2[i], rVAs[i] = rk, rki, rv

        cnt = {"ri": 0, "pti": 0, "ai": 0}

        def stage_A(qt):
            b = qt % 2
            SL = (qt + 1) * 128
            P.dma("sp", qiT[b][:], S.QI_T[qt], reads=[S.rQI[qt]], writes=[rqiT[b]], key=f"dqiT{b}")
            P.dma("sp", sg[b][:], S.SG[qt * 128:(qt + 1) * 128, :], reads=[S.rSG[qt]], writes=[rsg[b]], key=f"dsg{b}")
            P.add("dve", lambda e: e.tensor_tensor(out=Dg[b][:], in0=bc_mid(c.ident[:], 16), in1=bc_last(sg[b][:], 128),
                                                   op=ALU.mult),
                  reads=[c.rident, rsg[b]], writes=[rDg[b]])
            steps = []
            for sbk, s0 in enumerate(range(0, SL, 512)):
                for h in range(16):
                    steps.append((sbk, s0, h))

            def lmm(sbk, s0, h):
                sw = min(512, SL - s0)
                kres = [rKI2[j] for j in range(s0 // 128, (s0 + sw) // 128)]
                hp, par = h // 2, h % 2
                pl = par * 64
                L, rL = Lp.next()
                P.add("pe", lambda e: e.matmul(L[:, 0:sw], lhsT=qiT[b][pl:pl + 64, hp * 128:(hp + 1) * 128],
                                               rhs=KI2[pl:pl + 64, s0:s0 + sw], start=True, stop=True),
                      reads=[rqiT[b]] + kres, writes=[rL])
                r = cnt['ri'] % 4
                cnt['ri'] += 1
                P.add("act", lambda e: e.activation(out=Rt[r][:, 0:sw], in_=L[:, 0:sw], func=AF.Relu),
                      reads=[rL], writes=[rRt[r]])
                return r

            def acc(sbk, s0, h, r):
                sw = min(512, SL - s0)
                A, rA = At[(cnt['ai'] + sbk) % 2], rAt[(cnt['ai'] + sbk) % 2]
                P.add("pe", lambda e: e.matmul(A[:, 0:sw], lhsT=Dg[b][:, h, :], rhs=Rt[r][:, 0:sw], start=(h == 0),
                                               stop=(h == 15)),
                      reads=[rDg[b], rRt[r]], writes=[rA])
                if h == 15:
                    P.add("act", lambda e: e.activation(out=Isb[b][:, s0:s0 + sw], in_=A[:, 0:sw], func=AF.Copy),
                          reads=[rA], writes=[rIsb[b]])

            npair = len(steps) // 2
            pend = {0: (lmm(*steps[0]), lmm(*steps[1]))}
            for pi in range(npair):
                if pi + 1 < npair:
                    pend[pi + 1] = (lmm(*steps[2 * pi + 2]), lmm(*steps[2 * pi + 3]))
                r0, r1 = pend.pop(pi)
                acc(*steps[2 * pi], r0)
                acc(*steps[2 * pi + 1], r1)
            cnt['ai'] += len(range(0, SL, 512))

        def stage_B(qt):
            b = qt % 2
            SL = (qt + 1) * 128
            P.dma("sp", qaT[b][:], S.QA_T[qt], reads=[S.rQA[qt]], writes=[rqaT[b]], key=f"dqaT{b}")
            S_ = st_[b]
            if qt >= QT0:
                P.add("dve", lambda e, b=b, SL=SL, S_=S_: e.tensor_reduce(out=S_[:, 0:1], in_=Isb[b][:, 0:SL], axis=AX.X,
                                                                        op=ALU.min),
                      reads=[rIsb[b]], writes=[rst[b]])
                P.add("dve", lambda e, b=b, SL=SL, S_=S_: e.tensor_reduce(out=S_[:, 1:2], in_=Isb[b][:, 0:SL], axis=AX.X,
                                                                        op=ALU.max),
                      reads=[rIsb[b]], writes=[rst[b]])
            P.add("pool", lambda e, b=b, qt=qt: e.tensor_tensor(out=Isb[b][:, qt * 128:(qt + 1) * 128],
                                                              in0=Isb[b][:, qt * 128:(qt + 1) * 128], in1=cneg[:],
                                                              op=ALU.add),
                  reads=[rIsb[b], rcn], writes=[rIsb[b]])
            if qt >= QT0:
                P.add("dve", lambda e, S_=S_: e.tensor_tensor(out=S_[:, 2:3], in0=S_[:, 1:2], in1=S_[:, 0:1],
                                                             op=ALU.subtract),
                      reads=[rst[b]], writes=[rst[b]])
                P.add("dve", lambda e, S_=S_: e.tensor_scalar(out=S_[:, 8:8 + NBIS], in0=pow2[:], scalar1=S_[:, 2:3],
                                                             scalar2=None, op0=ALU.mult),
                      reads=[rst[b], rcn], writes=[rst[b]])
                P.add("dve", lambda e, S_=S_: e.tensor_copy(out=S_[:, 3:4], in_=S_[:, 0:1]),
                      reads=[rst[b]], writes=[rst[b]])
                for k in range(NBIS):
                    P.add("dve", lambda e, S_=S_, k=k: e.tensor_tensor(out=S_[:, 4:5], in0=S_[:, 3:4],
                                                                      in1=S_[:, 8 + k:9 + k], op=ALU.add),
                          reads=[rst[b]], writes=[rst[b]])
                    P.add("dve", lambda e, S_=S_, b=b, SL=SL: e.tensor_scalar(
                        out=cjunk[:, 0:SL], in0=Isb[b][:, 0:SL], scalar1=S_[:, 4:5], scalar2=None,
                        op0=ALU.is_ge, op1=ALU.add, accum_out=S_[:, 5:6]),
                          reads=[rst[b], rIsb[b]], writes=[rst[b], rcj])
                    P.add("dve", lambda e, S_=S_, k=k: e.tensor_scalar(
                        out=S_[:, 6:7], in0=S_[:, 5:6], scalar1=float(TOPK), scalar2=S_[:, 8 + k:9 + k],
                        op0=ALU.is_ge, op1=ALU.mult),
                          reads=[rst[b]], writes=[rst[b]])
                    P.add("dve", lambda e, S_=S_: e.tensor_tensor(out=S_[:, 3:4], in0=S_[:, 3:4], in1=S_[:, 6:7],
                                                                 op=ALU.add),
                          reads=[rst[b]], writes=[rst[b]])
            else:
                P.add("dve", lambda e, S_=S_: e.memset(S_[:, 3:4], -1.0e29), writes=[rst[b]])
            P.add("dve", lambda e, S_=S_, b=b, SL=SL: e.tensor_scalar(
                out=maskq[b][:, 0:SL], in0=Isb[b][:, 0:SL], scalar1=S_[:, 3:4], scalar2=None, op0=ALU.is_ge),
                  reads=[rst[b], rIsb[b]], writes=[rmq[b]])

        def stage_C(qt):
            b = qt % 2
            SL = (qt + 1) * 128
            for s8 in range(0, qt + 1, 8):
                n8 = min(8, qt + 1 - s8)
                L, rL = Lp.next()
                Lb = L[:, :].bitcast(BF16)
                for j in range(n8):
                    P.add("pe", lambda e, Lb=Lb, j=j, s8=s8, b=b: e.transpose(
                        out=Lb[:, j * 128:(j + 1) * 128], in_=maskq[b][:, (s8 + j) * 128:(s8 + j + 1) * 128],
                        identity=c.ident[:]),
                          reads=[rmq[b], c.rident], writes=[rL])
                P.add("act", lambda e, Lb=Lb, s8=s8, n8=n8, b=b: e.activation(
                    out=maskT[b][:, s8:s8 + n8, :].rearrange("p n q -> p (n q)"), in_=Lb[:, 0:n8 * 128],
                    func=AF.Copy),
                      reads=[rL], writes=[rmT[b]])
            def qk(st):
                sc = slice(st * 128, (st + 1) * 128)
                LE, rLE = Lp.next()
                LO, rLO = Lp.next()
                P.add("pe", lambda e: e.matmul(LE[:, :], lhsT=KA2[0:64, sc], rhs=qaT[b][0:64, :], start=True, stop=True),
                      reads=[rKA2[st], rqaT[b]], writes=[rLE])
                P.add("pe", lambda e: e.matmul(LO[:, :], lhsT=KA2[64:128, sc], rhs=qaT[b][64:128, :], start=True,
                                               stop=True),
                      reads=[rKA2[st], rqaT[b]], writes=[rLO])
                p = cnt["pti"] % len(PT)
                cnt["pti"] += 1
                P.add("act", lambda e: e.activation(out=PT[p][:, 0:512], in_=LE[:, :], func=AF.Exp, scale=0.125),
                      reads=[rLE], writes=[rPT[p]])
                P.add("act", lambda e: e.activation(out=PT[p][:, 512:1024], in_=LO[:, :], func=AF.Exp, scale=0.125),
                      reads=[rLO], writes=[rPT[p]])
                P.add("pool", lambda e: e.tensor_tensor(
                    out=PT[p][:].rearrange("p (a q) -> p a q", a=8), in0=PT[p][:].rearrange("p (a q) -> p a q", a=8),
                    in1=bc_mid(maskT[b][:, st, :], 8), op=ALU.mult),
                      reads=[rPT[p], rmT[b]], writes=[rPT[p]])
                return p

            def pv(st, p):
                P.add("pe", lambda e: e.matmul(OE[:, :], lhsT=VAs[:, st, 64:192], rhs=PT[p][:, 0:512],
                                               start=(st == 0), stop=(st == qt)),
                      reads=[rVAs[st], rPT[p]], writes=[rOE])
                P.add("pe", lambda e: e.matmul(OO[:, :], lhsT=VAs[:, st, 0:128], rhs=PT[p][:, 512:1024],
                                               start=(st == 0), stop=(st == qt)),
                      reads=[rVAs[st], rPT[p]], writes=[rOO])

            pend = {0: qk(0)}
            for st in range(qt + 1):
                if st + 1 <= qt:
                    pend[st + 1] = qk(st + 1)
                pv(st, pend.pop(st))
            normalize_out(P, c, OE, rOE, 0, rc[b], rrc[b], aT[b][0:64, :], raT[b])
            normalize_out(P, c, OO, rOO, 64, rc[b], rrc[b], aT[b][64:128, :], raT[b])
            P.dma("sp", S.ATT_T[0:4, :, qt * 128:(qt + 1) * 128].rearrange("j p t -> p j t"),
                  aT[b][:].rearrange("p (j t) -> p j t", j=4), reads=[raT[b]], writes=[S.rATa[qt]], key=f"daT{b}")

        for step in range(NT + 2):
            if step < NT:
                stage_A(step)
            if 0 <= step - 1 < NT:
                stage_B(step - 1)
            if 0 <= step - 2 < NT:
                stage_C(step - 2)
        return P.end_phase(ps)


SCALE_B = 96.0 ** -0.5


def mla_phase(nc, P, c, T, S):
    NT = T // 128
    NQB = T // 512
    with ExitStack() as ps:
        def sb(name, shape, dt):
            return ps.enter_context(nc.sbuf_tensor(name, shape, dt))

        def pm(name, shape, dt):
            return ps.enter_context(nc.psum_tensor(name, shape, dt))

        R = P.res

        def dbl(name, shape, dt):
            return [sb(f"{name}{i}", shape, dt) for i in range(2)], [R() for _ in range(2)]

        KB = [sb(f"mKB{i}", [128, T], BF16) for i in range(2)]
        VB = [sb(f"mVB{i}", [128, NT, 192], BF16) for i in range(2)]
        rKB = [[R() for _ in range(NQB)] for _ in range(2)]
        rVB = [[R() for _ in range(NQB)] for _ in range(2)]
        Cm = sb("Cm", [128, 4, 512], BF16)
        rCm = R()
        QT, rQT = dbl("mQT", [128, 512], BF16)
        PT = [sb(f"mPT{i}", [128, 512], BF16) for i in range(4)]
        rPT = [R() for _ in range(4)]
        rc, rrc = dbl("mrc", [128, 512], F32)
        aT, raT = dbl("maT", [128, 512], BF16)
        Lt = [pm(f"mL{i}", [128, 512], F32) for i in range(4)]
        Lp = BankPool(Lt, [R() for _ in range(4)])
        Ot = [pm(f"mO{i}", [128, 512], F32) for i in range(2)]
        rOt = [R() for _ in range(2)]

        P.add("pool", lambda e: e.memset(Cm[:], 1.0), writes=[rCm])
        P.add("pool", lambda e: e.affine_select(out=Cm[:], in_=Cm[:], pattern=[[-128, 4], [1, 512]],
                                                compare_op=ALU.is_ge, fill=0.0, base=0, channel_multiplier=-1),
              reads=[rCm], writes=[rCm])
        CH = min(T, 1024)
        NCH = T // CH

        def load_head(h):
            hb_ = h % 2
            for ci in range(NCH):
                cs = slice(ci * CH, (ci + 1) * CH)
                tl = range(ci * (CH // 128), (ci + 1) * (CH // 128))
                P.dma("sp", KB[hb_][:, cs], S.KB_T[h, :, cs], reads=[S.rKB[i] for i in tl],
                      writes=[rKB[hb_][ci]], key=f"mKB{hb_}_{ci}")
                P.dma("sp", VB[hb_][:, ci * (CH // 128):(ci + 1) * (CH // 128), :],
                      S.VB1[cs, h, :].rearrange("(n p) c -> p n c", p=128),
                      reads=[S.rVB[i] for i in tl], writes=[rVB[hb_][ci]], key=f"mVB{hb_}_{ci}")

        groups = [(h, qb) for h in range(8) for qb in range(NQB)]

        def load_q(g):
            h, qb = groups[g]
            bq = g % 2
            cs = slice(qb * 512, (qb + 1) * 512)
            P.dma("sp", QT[bq][:], S.QB_T[h, :, cs], reads=[S.rQB[i] for i in range(qb * 4, qb * 4 + 4)],
                  writes=[rQT[bq]], key=f"mQT{bq}")

        steps = [(g, st) for g, (h, qb) in enumerate(groups) for st in range(4 * (qb + 1))]
        cnt = {"pti": 0}

        def qk(g, st):
            h, qb = groups[g]
            hb_, bq = h % 2, g % 2
            L, rL = Lp.next()
            P.add("pe", lambda e: e.matmul(L[:, :], lhsT=KB[hb_][:, st * 128:(st + 1) * 128], rhs=QT[bq][:, :],
                                           start=True, stop=True),
                  reads=[rKB[hb_][(st * 128) // CH], rQT[bq]], writes=[rL])
            p = cnt["pti"] % len(PT)
            cnt["pti"] += 1
            P.add("act", lambda e: e.activation(out=PT[p][:], in_=L[:, :], func=AF.Exp, scale=SCALE_B),
                  reads=[rL], writes=[rPT[p]])
            j = st - 4 * qb
            if j >= 0:
                P.add("pool", lambda e: e.tensor_tensor(out=PT[p][:], in0=PT[p][:], in1=Cm[:, j, :], op=ALU.mult),
                      reads=[rPT[p], rCm], writes=[rPT[p]])
            return p

        def pv(g, st, p):
            h, qb = groups[g]
            hb_, bq = h % 2, g % 2
            nst = 4 * (qb + 1)
            O, rO = Ot[g % 2], rOt[g % 2]
            vsl = slice(64, 192) if h % 2 == 0 else slice(0, 128)
            num_lo = 0 if h % 2 == 0 else 64
            P.add("pe", lambda e: e.matmul(O[:, :], lhsT=VB[hb_][:, st, vsl], rhs=PT[p][:], start=(st == 0),
                                           stop=(st == nst - 1)),
                  reads=[rVB[hb_][(st * 128) // CH], rPT[p]], writes=[rO])
            if st == nst - 1:
                cs = slice(qb * 512, (qb + 1) * 512)
                normalize_out(P, c, O, rO, num_lo, rc[bq], rrc[bq], aT[bq][num_lo:num_lo + 64, :], raT[bq])
                P.dma("sp", S.ATT_T[4 + h // 2, num_lo:num_lo + 64, cs], aT[bq][num_lo:num_lo + 64, :],
                      reads=[raT[bq]], writes=[S.rATb[h][qb]], key=f"maT{bq}")

        LOOK = 2
        load_head(0)
        load_q(0)
        issued = {}
        loaded_q = {0}
        loaded_h = {0}

        def ensure_loads(g):
            if g >= len(groups):
                return
            h = groups[g][0]
            if h not in loaded_h:
                loaded_h.add(h)
                load_head(h)
            if g not in loaded_q:
                loaded_q.add(g)
                load_q(g)

        for i in range(min(LOOK, len(steps))):
            ensure_loads(steps[i][0])
            issued[i] = qk(*steps[i])
        for i, (g, st) in enumerate(steps):
            if i + LOOK < len(steps):
                ensure_loads(steps[i + LOOK][0])
                issued[i + LOOK] = qk(*steps[i + LOOK])
            pv(g, st, issued.pop(i))
            hh = groups[g][0]
            if st == 0 and groups[g][1] == 0 and hh + 1 < 8 and (hh + 1) not in loaded_h:
                loaded_h.add(hh + 1)
                load_head(hh + 1)
        return P.end_phase(ps)


def wout_phase(nc, P, c, T, S):
    NT = T // 128
    with ExitStack() as ps:
        def sb(name, shape, dt):
            return ps.enter_context(nc.sbuf_tensor(name, shape, dt))

        def pm(name, shape, dt):
            return ps.enter_context(nc.psum_tensor(name, shape, dt))

        R = P.res

        def dbl(name, shape, dt):
            return [sb(f"{name}{i}", shape, dt) for i in range(2)], [R() for _ in range(2)]

        Wo = sb("Wo", [128, 8, D], BF16)
        rWo = [R() for _ in range(8)]
        xt, rxt = dbl("wxt", [128, D], F32)
        at, rat = dbl("wat", [128, 8, 128], BF16)
        mmt = [pm(f"wmm{i}", [128, 512], F32) for i in range(4)]
        mmp = BankPool(mmt, [R() for _ in range(4)])
        stg, rstg = make_stage(nc, P, c, ps, "wo")
        load_weight_cast(P, c, Wo, [[r] for r in rWo], S.w_out, 8, [(0, 0, 512, 0), (512, 512, 512, 0)], stg, rstg)
        for i in range(NT):
            b = i % 2
            rows = slice(i * 128, (i + 1) * 128)
            P.dma("sp", xt[b][:], S.X1[rows, :], reads=[S.rX1[i]], writes=[rxt[b]], key=f"wxt{b}")
            P.dma("sp", at[b][:], S.ATT_T[:, :, rows].rearrange("c p t -> p c t"),
                  reads=[S.rATa[i]] + [S.rATb[h][i // 4] for h in range(8)], writes=[rat[b]], key=f"wat{b}")
            for half in range(2):
                bk, rbk = mmp.next()
                for cc in range(8):
                    P.add("pe", lambda e, bk=bk, cc=cc, b=b, half=half: e.matmul(
                        bk[:, :], lhsT=at[b][:, cc, :], rhs=Wo[:, cc, half * 512:(half + 1) * 512],
                        start=(cc == 0), stop=(cc == 7)),
                          reads=[rat[b], rWo[cc]], writes=[rbk])
                P.add("dve", lambda e, bk=bk, b=b, half=half: e.tensor_tensor(
                    out=xt[b][:, half * 512:(half + 1) * 512], in0=bk[:, :], in1=xt[b][:, half * 512:(half + 1) * 512],
                    op=ALU.add),
                      reads=[rbk, rxt[b]], writes=[rxt[b]])
            P.dma("sp", S.X2[rows, :], xt[b][:], reads=[rxt[b]], writes=[S.rX2[i]], key=f"wxo{b}")
        return P.end_phase(ps)


IN_NAMES = ["x", "g_ffn1", "w1_gate", "w1_up", "w1_down", "g_mix", "w_in", "g_q_lat", "g_kv_lat", "w_uq", "w_ukv",
            "w_out", "g_ffn2", "w2_gate", "w2_up", "w2_down", "g_final"]
IN_SHAPES = {"g_ffn1": [D], "w1_gate": [D, DFF], "w1_up": [D, DFF], "w1_down": [DFF, D], "g_mix": [D],
             "w_in": [D, DPROJ], "g_q_lat": [384], "g_kv_lat": [256], "w_uq": [384, 768], "w_ukv": [256, 1024],
             "w_out": [D, D], "g_ffn2": [D], "w2_gate": [D, DFF], "w2_up": [D, DFF], "w2_down": [DFF, D],
             "g_final": [D]}


def build(T, debug=False, phases=("ffn1", "proj", "dsa", "mla", "wout", "ffn2")):
    NT = T // 128
    NQB = T // 512
    nc = bass.Bass("TRN2", target_bir_lowering=False)
    S = Ctx()
    x = nc.dram_tensor("x", [T, D], F32, kind="ExternalInput").ap()
    for n, shp in IN_SHAPES.items():
        setattr(S, n, nc.dram_tensor(n, list(shp), F32, kind="ExternalInput").ap())
    S.rope64 = nc.dram_tensor("rope64", [T, 128], F32, kind="ExternalInput").ap()
    S.rope32 = nc.dram_tensor("rope32", [T, 64], F32, kind="ExternalInput").ap()
    out = nc.dram_tensor("out", [T, D], F32, kind="ExternalOutput").ap()
    kind = "ExternalOutput" if debug else "Internal"

    def scr(name, shape, dt):
        return nc.dram_tensor(name, list(shape), dt, kind=kind).ap()

    S.X1 = scr("X1", [T, D], F32)
    S.X2 = scr("X2", [T, D], F32)
    S.QA_T = scr("QA_T", [NT, 128, 512], BF16)
    S.QI_T = scr("QI_T", [NT, 128, 1024], BF16)
    S.SG = scr("SG", [T, 16], F32)
    S.KA_T2 = scr("KA_T2", [128, T], BF16)
    S.KI_T2 = scr("KI_T2", [128, T], BF16)
    S.VA1 = scr("VA1", [T, 192], BF16)
    S.QB_T = scr("QB_T", [8, 128, T], BF16)
    S.KB_T = scr("KB_T", [8, 128, T], BF16)
    S.VB1 = scr("VB1", [T, 8, 192], BF16)
    S.ATT_T = scr("ATT_T", [8, 128, T], BF16)
    with ExitStack() as stack:
        P = Prog(nc, stack)
        c = Ctx()
        const_phase(nc, P, c, stack)
        rl = lambda: [P.res() for _ in range(NT)]
        rx = rl()
        rout = rl()
        S.rX1, S.rX2, S.rQA, S.rQI, S.rSG, S.rKA, S.rKI, S.rVA, S.rQB, S.rKB, S.rVB, S.rATa = [rl() for _ in range(12)]
        S.rATb = [[P.res() for _ in range(NQB)] for _ in range(8)]
        info = {}
        if "ffn1" in phases:
            info["ffn1"] = ffn_phase(nc, P, c, T, x, rx, S.X1, S.rX1, S.g_ffn1, S.w1_gate, S.w1_up, S.w1_down)
        if "proj" in phases:
            info["proj"] = proj_phase(nc, P, c, T, S)
        if "dsa" in phases:
            info["dsa"] = dsa_phase(nc, P, c, T, S)
        if "mla" in phases:
            info["mla"] = mla_phase(nc, P, c, T, S)
        if "wout" in phases:
            info["wout"] = wout_phase(nc, P, c, T, S)
        if "ffn2" in phases:
            info["ffn2"] = ffn_phase(nc, P, c, T, S.X2, S.rX2, out, rout, S.g_ffn2, S.w2_gate, S.w2_up, S.w2_down,
                                     final_g=S.g_final, tag="f2")
        nc._mk_info = (info, dict(P.cnt), max(P.dcnt.values()) if P.dcnt else 0)
    return nc


def rope_table(T, dim):
    pos = np.arange(T, dtype=np.float32)
    inv_freq = (np.float32(10000.0) ** (-np.arange(0, dim, 2, dtype=np.float32) / np.float32(dim))).astype(np.float32)
    ang = pos[:, None] * inv_freq[None, :]
    cs, sn = np.cos(ang).astype(np.float32), np.sin(ang).astype(np.float32)
    return np.concatenate([cs, cs, -sn, sn], axis=1).astype(np.float32)


_NC_CACHE = {}


def kernel(**inputs):
    x = np.ascontiguousarray(np.asarray(inputs["x"], dtype=np.float32))
    B, T, _ = x.shape
    if T not in _NC_CACHE:
        _NC_CACHE[T] = build(T)
    nc = _NC_CACHE[T]
    shared = {}
    for n, shp in IN_SHAPES.items():
        shared[n] = np.ascontiguousarray(np.asarray(inputs[n], dtype=np.float32).reshape(shp))
    shared["rope64"] = rope_table(T, 64)
    shared["rope32"] = rope_table(T, 32)
    in_maps = []
    for bi in range(B):
        m = dict(shared)
        m["x"] = x[bi]
        in_maps.append(m)
    res = run_bass_kernel_spmd(nc, in_maps, core_ids=list(range(B)))
    return np.stack([np.asarray(r["out"]) for r in res.results], axis=0).astype(np.float32)
```

```python
import math
from contextlib import ExitStack

import numpy as np
import concourse.bass as bass
import concourse.mybir as mybir
from concourse.bass_utils import run_bass_kernel_spmd

F32 = mybir.dt.float32
BF16 = mybir.dt.bfloat16
AF = mybir.ActivationFunctionType
ALU = mybir.AluOpType
AX = mybir.AxisListType

D = 1024
DFF = 2816
NFC = DFF // 128
EPS = 1e-6
ENGS = ("pe", "act", "dve", "pool", "sp")


class Res:
    __slots__ = ("name", "last_w", "readers")

    def __init__(self, name):
        self.name = name
        self.last_w = None
        self.readers = []


class Op:
    __slots__ = ("eng", "fn", "deps", "idx", "signal", "sem", "val", "is_dma", "waits", "key", "emitted")

    def __init__(self, eng, fn, is_dma=False):
        self.eng = eng
        self.fn = fn
        self.deps = []
        self.signal = False
        self.sem = None
        self.val = None
        self.is_dma = is_dma
        self.waits = []
        self.key = None
        self.emitted = False


class Prog:
    def __init__(self, nc, stack):
        self.nc = nc
        self.stack = stack
        self.ops = []
        self.n_total = 0
        self.eng_sem = {e: stack.enter_context(nc.semaphore("S_" + e)) for e in ENGS}
        self.cnt = {e: 0 for e in ENGS}
        self.dma_sem = {}
        self.dcnt = {}
        self.keymap = {}
        for e_, n_ in (("pool", 40), ("sp", 44)):
            for i_ in range(n_):
                self.dma_sem[(e_, i_)] = stack.enter_context(nc.semaphore("D_%s_%d" % (e_, i_)))
                self.dcnt[(e_, i_)] = 0
        self.block = stack.enter_context(nc.Block())
        self.waited = {e: {} for e in ENGS}
        self.phase_dmas = []
        self.last_op = {e: None for e in ENGS}

    def res(self, name="r"):
        return Res(name)

    def add(self, eng, fn, reads=(), writes=(), dma_key=None):
        op = Op(eng, fn, is_dma=dma_key is not None)
        op.idx = self.n_total
        self.n_total += 1
        seen = set()

        def dep(d):
            if d is None or d.idx in seen or d.emitted:
                return
            seen.add(d.idx)
            if d.eng == op.eng and not d.is_dma and not op.is_dma and d.eng == "pe":
                return
            op.deps.append(d)
            d.signal = True

        for r in reads:
            dep(r.last_w)
        for w in writes:
            dep(w.last_w)
            for rd in w.readers:
                dep(rd)
        for r in reads:
            r.readers.append(op)
        for w in writes:
            w.last_w = op
            w.readers = []
        if dma_key is not None:
            op.key = dma_key
            op.signal = True
            self.phase_dmas.append(op)
        self.ops.append(op)
        return op

    def dma(self, eng, out, in_, reads=(), writes=(), key=None):
        return self.add(eng, lambda e: e.dma_start(out=out, in_=in_), reads=reads, writes=writes, dma_key=key)

    def end_phase(self, pstack):
        nc = self.nc
        last = {}
        for op in self.ops:
            if op.fn is not None and not op.is_dma:
                last[op.eng] = op
        for e in ENGS:
            fin = self.add(e, None)
            fin.deps = [o for e2, o in last.items() if e2 != e] + list(self.phase_dmas)
            for o in fin.deps:
                o.signal = True
        self.phase_dmas = []
        for op in self.ops:
            if op.is_dma:
                km = self.keymap.setdefault(op.eng, {})
                if op.key not in km:
                    km[op.key] = len(km)
                k = (op.eng, km[op.key])
                assert k in self.dma_sem, ("out of preallocated DMA semaphores", k)
                self.dcnt[k] += 16
                op.sem = self.dma_sem[k]
                op.val = self.dcnt[k]
            elif op.signal:
                self.cnt[op.eng] += 1
                op.sem = self.eng_sem[op.eng]
                op.val = self.cnt[op.eng]
        for op in self.ops:
            need = {}
            w = self.waited[op.eng]
            for d in op.deps:
                key = id(d.sem)
                if w.get(key, 0) >= d.val:
                    continue
                if key not in need or need[key][1] < d.val:
                    need[key] = (d.sem, d.val)
            for key, (s, v) in need.items():
                w[key] = v
                op.waits.append((s, v))
        per_eng = {e: [op for op in self.ops if op.eng == e] for e in ENGS}
        block = self.block
        handles = {"pe": block.tensor, "act": block.scalar, "dve": block.vector,
                   "pool": block.gpsimd, "sp": block.sync}

        def make(e):
            def body(eng):
                for op in per_eng[e]:
                    for (s, v) in op.waits:
                        eng.wait_ge(s, v)
                    if op.fn is None:
                        continue
                    ins = op.fn(eng)
                    if op.signal:
                        ins.then_inc(op.sem, 16 if op.is_dma else 1)
            return body

        for e in ENGS:
            if per_eng[e]:
                handles[e](make(e))
        n = len(self.ops)
        for op in self.ops:
            op.emitted = True
            op.fn = None
        self.ops = []
        self.keymap = {}
        return n


def bcast_rows(vec_ap, n):
    return bass.AP(tensor=vec_ap.tensor, offset=vec_ap.offset, ap=[[0, 128], [1, n]])


class Ctx:
    pass


def load_weight_cast(P, c, dst_tile, res2d, src_ap, nk, pieces, stage, rstage):
    for (dc, sc, w, ri) in pieces:
        for k in range(nk):
            s = c.sj % len(stage)
            c.sj += 1
            P.dma("sp", stage[s][:, 0:w], src_ap[k * 128:(k + 1) * 128, sc:sc + w], writes=[rstage[s]],
                  key=f"stg{s}")
            eng = ("act", "dve", "pool")[c.sj % 3]
            if eng == "act":
                P.add("act", lambda e, s=s, k=k, dc=dc, w=w: e.activation(out=dst_tile[:, k, dc:dc + w],
                                                                          in_=stage[s][:, 0:w], func=AF.Copy),
                      reads=[rstage[s]], writes=[res2d[k][ri]])
            else:
                P.add(eng, lambda e, s=s, k=k, dc=dc, w=w: e.tensor_copy(out=dst_tile[:, k, dc:dc + w],
                                                                        in_=stage[s][:, 0:w]),
                      reads=[rstage[s]], writes=[res2d[k][ri]])


def make_stage(nc, P, c, ps, tag, n=5):
    st = [ps.enter_context(nc.sbuf_tensor(f"{tag}stg{i}", [128, 512], F32)) for i in range(n)]
    return st, [P.res() for _ in range(n)]


def rmsnorm_to_bf16(P, c, x_ap, x_res, n, g_bc, g_res, junk, junk_res, ss, ss_res, rstd, rstd_res, out_ap, out_res,
                    x_in_psum=False):
    P.add("act", lambda e: e.activation(out=junk, in_=x_ap, func=AF.Square, accum_out=ss),
          reads=[x_res], writes=[junk_res, ss_res])
    P.add("act", lambda e: e.activation(out=rstd, in_=ss, func=AF.Sqrt, scale=1.0 / n, bias=c.eps_t[:, 0:1]),
          reads=[ss_res, c.rconst], writes=[rstd_res])
    P.add("dve", lambda e: e.reciprocal(out=rstd, in_=rstd), reads=[rstd_res], writes=[rstd_res])
    P.add("dve", lambda e: e.scalar_tensor_tensor(out=out_ap, in0=x_ap, scalar=rstd, in1=g_bc,
                                                  op0=ALU.mult, op1=ALU.mult),
          reads=[x_res, rstd_res, g_res], writes=[out_res])


def ffn_phase(nc, P, c, T, src, src_res, dst, dst_res, g_vec, wg, wu, wd, final_g=None, tag="f1"):
    NT = T // 128
    with ExitStack() as ps:
        def sb(name, shape, dt):
            return ps.enter_context(nc.sbuf_tensor(tag + name, shape, dt))

        def pm(name, shape, dt):
            return ps.enter_context(nc.psum_tensor(tag + name, shape, dt))

        Wg = sb("Wg", [128, 8, DFF], BF16)
        Wu = sb("Wu", [128, 8, DFF], BF16)
        Wd = sb("Wd", [128, NFC, D], BF16)
        gbc = sb("gbc", [128, D], F32)
        gfin = sb("gfin", [128, D], F32) if final_g is not None else None
        xt = [sb(f"xt{i}", [128, D], F32) for i in range(3)]
        junk = [sb(f"junk{i}", [128, D], BF16) for i in range(2)]
        ss = [sb(f"ss{i}", [128, 4], F32) for i in range(2)]
        hb = [sb(f"hb{i}", [128, D], BF16) for i in range(2)]
        hT = [sb(f"hT{i}", [128, 8, 128], BF16) for i in range(2)]
        sg = [sb(f"sg{i}", [128, 512], F32) for i in range(2)]
        act = [sb(f"act{i}", [128, DFF], BF16) for i in range(2)]
        actT = [sb(f"actT{i}", [128, NFC, 128], BF16) for i in range(2)]
        tp = [pm(f"tp{i}", [128, 1024], BF16) for i in range(2)]
        mm = [pm(f"mm{i}", [128, 512], F32) for i in range(6)]

        R = P.res
        rWg = [[R() for _ in range(6)] for _ in range(8)]
        rWu = [[R() for _ in range(6)] for _ in range(8)]
        rWd = [[R(), R()] for _ in range(NFC)]
        stg, rstg = make_stage(nc, P, c, ps, tag)
        rg, rgf = R(), R()
        rxt = [R() for _ in range(3)]
        rjunk = [R() for _ in range(2)]
        rss = [R() for _ in range(2)]
        rrs = [R() for _ in range(2)]
        rhb = [R() for _ in range(2)]
        rhT = [R() for _ in range(2)]
        rsg = [R() for _ in range(2)]
        ract = [R() for _ in range(2)]
        ractT = [R() for _ in range(2)]
        ryo = [R() for _ in range(2)]
        rtp = [R() for _ in range(2)]
        rmm = [R() for _ in range(6)]

        P.dma("sp", gbc[:], bcast_rows(g_vec, D), writes=[rg], key="gbc")
        if final_g is not None:
            P.dma("sp", gfin[:], bcast_rows(final_g, D), writes=[rgf], key="gfin")
        for i0 in range(min(2, NT)):
            P.dma("sp", xt[i0][:], src[i0 * 128:(i0 + 1) * 128, :], reads=[src_res[i0]], writes=[rxt[i0]],
                  key=f"xt{i0}")
        for si0, s00 in enumerate(range(0, DFF, 512)):
            pc = [(s00, s00, min(512, DFF - s00), si0)]
            load_weight_cast(P, c, Wg, rWg, wg, 8, pc, stg, rstg)
            load_weight_cast(P, c, Wu, rWu, wu, 8, pc, stg, rstg)
        load_weight_cast(P, c, Wd, rWd, wd, NFC, [(0, 0, 512, 0), (512, 512, 512, 1)], stg, rstg)

        slabs = [(s0, min(512, DFF - s0)) for s0 in range(0, DFF, 512)]
        cn = {"mmi": 0, "tpi": 0}

        def stage1(i):
            b = i % 2
            b3 = i % 3
            rows = slice(i * 128, (i + 1) * 128)
            if i >= 2:
                P.dma("sp", xt[b3][:], src[rows, :], reads=[src_res[i]], writes=[rxt[b3]], key=f"xt{b3}")
            rmsnorm_to_bf16(P, c, xt[b3][:], rxt[b3], D, gbc[:], rg, junk[b][:], rjunk[b], ss[b][:, 0:1], rss[b],
                            ss[b][:, 1:2], rrs[b], hb[b][:], rhb[b])
            t = cn['tpi'] % 2
            cn['tpi'] += 1
            for k in range(8):
                P.add("pe", lambda e, k=k, t=t, b=b, b3=b3: e.transpose(out=tp[t][:, k * 128:(k + 1) * 128],
                                                                in_=hb[b][:, k * 128:(k + 1) * 128],
                                                                identity=c.ident[:]),
                      reads=[rhb[b], c.rident], writes=[rtp[t]])
            P.add("act", lambda e, t=t, b=b, b3=b3: e.activation(out=hT[b][:].rearrange("p k t -> p (k t)"),
                                                          in_=tp[t][:], func=AF.Copy),
                  reads=[rtp[t]], writes=[rhT[b]])
            for si, (s0, sw) in enumerate(slabs):
                ga = cn['mmi'] % 6
                ua = (cn['mmi'] + 1) % 6
                cn['mmi'] += 2
                for k in range(8):
                    P.add("pe", lambda e, k=k, ga=ga, b=b, b3=b3, s0=s0, sw=sw: e.matmul(
                        mm[ga][:, 0:sw], lhsT=hT[b][:, k, :], rhs=Wg[:, k, s0:s0 + sw],
                        start=(k == 0), stop=(k == 7)),
                          reads=[rhT[b], rWg[k][si]], writes=[rmm[ga]])
                for k in range(8):
                    P.add("pe", lambda e, k=k, ua=ua, b=b, b3=b3, s0=s0, sw=sw: e.matmul(
                        mm[ua][:, 0:sw], lhsT=hT[b][:, k, :], rhs=Wu[:, k, s0:s0 + sw],
                        start=(k == 0), stop=(k == 7)),
                          reads=[rhT[b], rWu[k][si]], writes=[rmm[ua]])
                s2 = si % 2
                P.add("act", lambda e, ga=ga, s2=s2, sw=sw: e.activation(out=sg[s2][:, 0:sw], in_=mm[ga][:, 0:sw],
                                                                        func=AF.Silu),
                      reads=[rmm[ga]], writes=[rsg[s2]])
                P.add("dve", lambda e, ua=ua, s2=s2, b=b, b3=b3, s0=s0, sw=sw: e.tensor_tensor(
                    out=act[b][:, s0:s0 + sw], in0=sg[s2][:, 0:sw], in1=mm[ua][:, 0:sw], op=ALU.mult),
                      reads=[rsg[s2], rmm[ua]], writes=[ract[b]])

        def stage2(i):
            b = i % 2
            b3 = i % 3
            rows = slice(i * 128, (i + 1) * 128)
            for f0 in range(0, NFC, 8):
                nf = min(8, NFC - f0)
                t = cn['tpi'] % 2
                cn['tpi'] += 1
                for f in range(nf):
                    P.add("pe", lambda e, f=f, f0=f0, t=t, b=b, b3=b3: e.transpose(
                        out=tp[t][:, f * 128:(f + 1) * 128],
                        in_=act[b][:, (f0 + f) * 128:(f0 + f + 1) * 128], identity=c.ident[:]),
                          reads=[ract[b], c.rident], writes=[rtp[t]])
                P.add("act" if (f0 // 8) % 2 == 0 else "dve",
                      (lambda e, t=t, b=b, b3=b3, f0=f0, nf=nf: e.activation(
                          out=actT[b][:, f0:f0 + nf, :].rearrange("p k t -> p (k t)"),
                          in_=tp[t][:, 0:nf * 128], func=AF.Copy)) if (f0 // 8) % 2 == 0 else
                      (lambda e, t=t, b=b, b3=b3, f0=f0, nf=nf: e.tensor_copy(
                          out=actT[b][:, f0:f0 + nf, :].rearrange("p k t -> p (k t)"),
                          in_=tp[t][:, 0:nf * 128])),
                      reads=[rtp[t]], writes=[ractT[b]])
            for half in range(2):
                da = cn['mmi'] % 6
                cn['mmi'] += 1
                for f in range(NFC):
                    P.add("pe", lambda e, f=f, da=da, b=b, b3=b3, half=half: e.matmul(
                        mm[da][:, :], lhsT=actT[b][:, f, :], rhs=Wd[:, f, half * 512:(half + 1) * 512],
                        start=(f == 0), stop=(f == NFC - 1)),
                          reads=[ractT[b], rWd[f][half]], writes=[rmm[da]])
                P.add("dve", lambda e, da=da, b=b, b3=b3, half=half: e.scalar_tensor_tensor(
                    out=xt[b3][:, half * 512:(half + 1) * 512], in0=mm[da][:, :], scalar=0.5,
                    in1=xt[b3][:, half * 512:(half + 1) * 512], op0=ALU.mult, op1=ALU.add),
                      reads=[rmm[da], rxt[b3]], writes=[rxt[b3]])
            if final_g is None:
                P.dma("sp", dst[rows, :], xt[b3][:], reads=[rxt[b3]], writes=[dst_res[i]], key=f"xo{b3}")
            else:
                P.add("act", lambda e, b=b, b3=b3: e.activation(out=junk[b][:], in_=xt[b3][:], func=AF.Square,
                                                         accum_out=ss[b][:, 2:3]),
                      reads=[rxt[b3]], writes=[rjunk[b], rss[b]])
                P.add("act", lambda e, b=b, b3=b3: e.activation(out=ss[b][:, 3:4], in_=ss[b][:, 2:3], func=AF.Sqrt,
                                                         scale=1.0 / D, bias=c.eps_t[:, 0:1]),
                      reads=[rss[b], c.rconst], writes=[rrs[b]])
                P.add("dve", lambda e, b=b, b3=b3: e.reciprocal(out=ss[b][:, 3:4], in_=ss[b][:, 3:4]),
                      reads=[rrs[b]], writes=[rrs[b]])
                P.add("dve", lambda e, b=b, b3=b3: e.scalar_tensor_tensor(out=xt[b3][:], in0=xt[b3][:], scalar=ss[b][:, 3:4],
                                                                   in1=gfin[:], op0=ALU.mult, op1=ALU.mult),
                      reads=[rxt[b3], rrs[b], rgf], writes=[rxt[b3]])
                P.dma("sp", dst[rows, :], xt[b3][:], reads=[rxt[b3]], writes=[dst_res[i]], key=f"xo{b3}")

        stage1(0)
        for i in range(NT):
            if i + 1 < NT:
                stage1(i + 1)
            stage2(i)
        return P.end_phase(ps)


def const_phase(nc, P, c, stack):
    c.ident = stack.enter_context(nc.sbuf_tensor("ident", [128, 128], BF16))
    c.identf = stack.enter_context(nc.sbuf_tensor("identf", [128, 128], F32))
    c.eps_t = stack.enter_context(nc.sbuf_tensor("eps_t", [128, 1], F32))
    c.rident = P.res()
    c.rconst = P.res()
    c.sj = 0
    P.add("pool", lambda e: e.memset(c.identf[:], 0.0), writes=[c.rident])
    P.add("pool", lambda e: e.affine_select(out=c.identf[:], in_=c.identf[:], pattern=[[-1, 128]],
                                            compare_op=ALU.not_equal, fill=1.0, base=0, channel_multiplier=1),
          reads=[c.rident], writes=[c.rident])
    P.add("pool", lambda e: e.tensor_copy(out=c.ident[:], in_=c.identf[:]), reads=[c.rident], writes=[c.rident])
    P.add("pool", lambda e: e.memset(c.eps_t[:], EPS), writes=[c.rconst])


def bc_mid(ap2d, H):
    a = [list(x) for x in ap2d.ap]
    return bass.AP(tensor=ap2d.tensor, offset=ap2d.offset, ap=[a[0], [0, H], a[1]])


def bc_last(ap2d, w):
    a = [list(x) for x in ap2d.ap]
    return bass.AP(tensor=ap2d.tensor, offset=ap2d.offset, ap=[a[0], a[1], [0, w]])


class BankPool:
    def __init__(self, tiles, res):
        self.tiles = tiles
        self.res = res
        self.i = 0

    def next(self):
        k = self.i % len(self.tiles)
        self.i += 1
        return self.tiles[k], self.res[k]


def transposes_to(P, c, tpp, srcs, src_res, dst_ap, dst_res, np_out=128, copy_eng="act"):
    tp, rtp = tpp.next()
    n = len(srcs)
    for j, s_ap in enumerate(srcs):
        P.add("pe", lambda e, j=j, s_ap=s_ap: e.transpose(out=tp[0:np_out, j * 128:(j + 1) * 128], in_=s_ap,
                                                         identity=c.ident[:]),
              reads=list(src_res) + [c.rident], writes=[rtp])
    if copy_eng == "act":
        P.add("act", lambda e: e.activation(out=dst_ap, in_=tp[0:np_out, 0:n * 128], func=AF.Copy),
              reads=[rtp], writes=[dst_res])
    else:
        P.add("dve", lambda e: e.tensor_copy(out=dst_ap, in_=tp[0:np_out, 0:n * 128]),
              reads=[rtp], writes=[dst_res])


def rope_ops(P, c, src3, src_res, H, d, tab, tab_res, ta, rta, tb, rtb, out3, out_res, out3b=None):
    h2 = d // 2
    cc = bc_mid(tab[:, 0:d], H)
    s0 = bc_mid(tab[:, d:d + h2], H)
    s1 = bc_mid(tab[:, d + h2:2 * d], H)
    P.add("dve", lambda e: e.tensor_tensor(out=ta, in0=src3, in1=cc, op=ALU.mult),
          reads=[src_res, tab_res], writes=[rta])
    P.add("dve", lambda e: e.tensor_tensor(out=tb[:, :, 0:h2], in0=src3[:, :, h2:d], in1=s0, op=ALU.mult),
          reads=[src_res, tab_res], writes=[rtb])
    P.add("dve", lambda e: e.tensor_tensor(out=tb[:, :, h2:d], in0=src3[:, :, 0:h2], in1=s1, op=ALU.mult),
          reads=[src_res, tab_res], writes=[rtb])
    P.add("pool", lambda e: e.tensor_tensor(out=out3, in0=ta, in1=tb, op=ALU.add),
          reads=[rta, rtb], writes=[out_res])
    if out3b is not None:
        P.add("pool", lambda e: e.tensor_tensor(out=out3b, in0=ta, in1=tb, op=ALU.add),
              reads=[rta, rtb], writes=[out_res])


WIN_SEGS = [
    (0, 0, 512),
    (512, 640, 512),
    (1024, 1152, 512),
    (1536, 1744, 384),
    (1920, 512, 64),
    (1984, 1664, 64),
    (2048, 2128, 256),
    (2304, 2384, 32),
    (2336, 576, 64),
    (2400, 1728, 16),
]
WIN_GROUPS = [(0, 512), (512, 512), (1024, 512), (1536, 512), (2048, 368)]
DPROJ = 2416


def proj_phase(nc, P, c, T, S):
    LIM = 99.0
    NT = T // 128
    with ExitStack() as ps:
        def sb(name, shape, dt):
            return ps.enter_context(nc.sbuf_tensor(name, shape, dt))

        def pm(name, shape, dt):
            return ps.enter_context(nc.psum_tensor(name, shape, dt))

        R = P.res
        Win = sb("Win", [128, 8, DPROJ], BF16)
        Wuq = sb("Wuq", [128, 3, 768], BF16)
        Wukv = sb("Wukv", [128, 2, 1024], BF16)
        gbc = sb("gbcm", [128, D], F32)
        gq = sb("gq", [128, 384], F32)
        gkv = sb("gkv", [128, 256], F32)
        rWin = [R() for _ in range(8)]
        rWuq = [R() for _ in range(3)]
        rWukv = [R() for _ in range(2)]
        rg, rgq, rgkv = R(), R(), R()

        def dbl(name, shape, dt):
            return [sb(f"{name}{i}", shape, dt) for i in range(2)], [R() for _ in range(2)]

        xt, rxt = dbl("pxt", [128, D], F32)
        junk, rjunk = dbl("pjunk", [128, D], BF16)
        ss, rss = dbl("pss", [128, 8], F32)
        rrs = [[R() for _ in range(3)] for _ in range(2)]
        hb, rhb = dbl("phb", [128, D], BF16)
        hT, rhT = dbl("phT", [128, 8, 128], BF16)
        r64, rr64 = dbl("r64", [128, 128], F32)
        r32, rr32 = dbl("r32", [128, 64], F32)
        ta, rta = dbl("ta", [128, 512], F32)
        tb, rtb = dbl("tb", [128, 512], F32)
        qar, rqar = dbl("qar", [128, 512], BF16)
        qir, rqir = dbl("qir", [128, 1024], BF16)
        qif, rqif = dbl("qif", [128, 512], F32)
        qaT, rqaT = dbl("qaT", [128, 512], BF16)
        qiT, rqiT = dbl("qiT", [128, 1024], BF16)
        aw, raw = dbl("aw", [128, 16], F32)
        sgt, rsgt = dbl("sgt", [128, 16], F32)
        kdup, rkdup = dbl("kdup", [128, 256], BF16)
        kT, rkT = dbl("kT", [128, 256], BF16)
        kpe, rkpe = dbl("kpe", [128, 32], BF16)
        va1, rva1 = dbl("va1", [128, 192], BF16)
        cqn, rcqn = dbl("cqn", [128, 384], BF16)
        cqT, rcqT = dbl("cqT", [128, 384], BF16)
        ckn, rckn = dbl("ckn", [128, 256], BF16)
        ckT, rckT = dbl("ckT", [128, 256], BF16)
        qbr, rqbr = dbl("qbr", [128, 8, 128], BF16)
        kbr, rkbr = dbl("kbr", [128, 8, 128], BF16)
        qbT, rqbT = dbl("qbT", [128, 1024], BF16)
        kbT, rkbT = dbl("kbT", [128, 1024], BF16)
        vb1, rvb1 = dbl("vb1", [128, 8, 192], BF16)
        tpt = [pm(f"ptp{i}", [128, 1024], BF16) for i in range(2)]
        tpp = BankPool(tpt, [R() for _ in range(2)])
        mmt = [pm(f"pmm{i}", [128, 512], F32) for i in range(6)]
        mmp = BankPool(mmt, [R() for _ in range(6)])

        P.dma("sp", gbc[:], bcast_rows(S.g_mix, D), writes=[rg], key="gbc")
        P.dma("sp", gq[:], bcast_rows(S.g_q_lat, 384), writes=[rgq], key="gq")
        P.dma("sp", gkv[:], bcast_rows(S.g_kv_lat, 256), writes=[rgkv], key="gkv")
        stg, rstg = make_stage(nc, P, c, ps, "pj")
        load_weight_cast(P, c, Win, [[r] for r in rWin], S.w_in, 8, [(dc, sc, w, 0) for (dc, sc, w) in WIN_SEGS],
                         stg, rstg)
        load_weight_cast(P, c, Wuq, [[r] for r in rWuq], S.w_uq, 3, [(0, 0, 384, 0), (384, 384, 384, 0)], stg, rstg)
        load_weight_cast(P, c, Wukv, [[r] for r in rWukv], S.w_ukv, 2, [(0, 0, 512, 0), (512, 512, 512, 0)], stg, rstg)
        for b in range(2):
            P.add("pool", lambda e, b=b: e.memset(qbr[b][:], 0.0), writes=[rqbr[b]])
            P.add("pool", lambda e, b=b: e.memset(kbr[b][:], 0.0), writes=[rkbr[b]])
            P.add("pool", lambda e, b=b: e.memset(va1[b][:], 1.0), writes=[rva1[b]])
            P.add("pool", lambda e, b=b: e.memset(vb1[b][:], 1.0), writes=[rvb1[b]])

        def stage1a(i):
            b = i % 2
            rows = slice(i * 128, (i + 1) * 128)
            P.dma("sp", xt[b][:], S.X1[rows, :], reads=[S.rX1[i]], writes=[rxt[b]], key=f"xt{b}")
            P.dma("sp", r64[b][:], S.rope64[rows, :], writes=[rr64[b]], key=f"r64{b}")
            P.dma("sp", r32[b][:], S.rope32[rows, :], writes=[rr32[b]], key=f"r32{b}")
            rmsnorm_to_bf16(P, c, xt[b][:], rxt[b], D, gbc[:], rg, junk[b][:], rjunk[b], ss[b][:, 0:1], rss[b],
                            ss[b][:, 1:2], rrs[b][0], hb[b][:], rhb[b])
            transposes_to(P, c, tpp, [hb[b][:, k * 128:(k + 1) * 128] for k in range(8)], [rhb[b]],
                          hT[b][:].rearrange("p k t -> p (k t)"), rhT[b])

        def stage2(i):
            b = i % 2
            rows = slice(i * 128, (i + 1) * 128)
            cols = slice(i * 128, (i + 1) * 128)
            if LIM < 2:
                return
            banks = []
            for (g0, gw) in WIN_GROUPS:
                bk, rbk = mmp.next()
                for k in range(8):
                    P.add("pe", lambda e, k=k, bk=bk, g0=g0, gw=gw, b=b: e.matmul(
                        bk[:, 0:gw], lhsT=hT[b][:, k, :], rhs=Win[:, k, g0:g0 + gw], start=(k == 0), stop=(k == 7)),
                          reads=[rhT[b], rWin[k]], writes=[rbk])
                banks.append((bk, rbk))
            (B0, rB0), (B1, rB1), (B2, rB2), (B3, rB3), (B4, rB4) = banks
            v3 = lambda ap, H: ap.rearrange("p (h d) -> p h d", h=H)
            if LIM < 3:
                return
            P.add("act", lambda e, b=b, B4=B4: e.activation(out=aw[b][:], in_=B4[:, 352:368], func=AF.Abs,
                                                           scale=1.0 / 32.0),
                  reads=[rB4], writes=[raw[b]])
            P.add("act", lambda e, b=b, B4=B4: e.activation(out=sgt[b][:], in_=B4[:, 352:368], func=AF.Sign),
                  reads=[rB4], writes=[rsgt[b]])
            P.dma("sp", S.SG[rows, :], sgt[b][:], reads=[rsgt[b]], writes=[S.rSG[i]], key=f"sgt{b}")
            if LIM < 4:
                return
            rope_ops(P, c, v3(B0[:, 0:512], 8), rB0, 8, 64, r64[b], rr64[b], v3(ta[b][:], 8), rta[b],
                     v3(tb[b][:], 8), rtb[b], v3(qar[b][:], 8), rqar[b])
            transposes_to(P, c, tpp, [qar[b][:, j * 128:(j + 1) * 128] for j in range(4)], [rqar[b]],
                          qaT[b][:], rqaT[b], copy_eng="dve")
            P.dma("sp", S.QA_T[i], qaT[b][:], reads=[rqaT[b]], writes=[S.rQA[i]], key=f"qaT{b}")
            if LIM < 5:
                return
            for hh, (Bq, rBq) in enumerate(((B1, rB1), (B2, rB2))):
                rope_ops(P, c, v3(Bq[:, 0:512], 8), rBq, 8, 64, r64[b], rr64[b], v3(ta[b][:], 8), rta[b],
                         v3(tb[b][:], 8), rtb[b], v3(qif[b][:], 8), rqif[b])
                P.add("dve", lambda e, b=b, hh=hh: e.tensor_tensor(
                    out=v3(qir[b][:, hh * 512:(hh + 1) * 512], 8), in0=v3(qif[b][:], 8),
                    in1=bc_last(aw[b][:, hh * 8:(hh + 1) * 8], 64), op=ALU.mult),
                      reads=[rqif[b], raw[b]], writes=[rqir[b]])
            transposes_to(P, c, tpp, [qir[b][:, j * 128:(j + 1) * 128] for j in range(8)], [rqir[b]],
                          qiT[b][:], rqiT[b])
            P.dma("sp", S.QI_T[i], qiT[b][:], reads=[rqiT[b]], writes=[S.rQI[i]], key=f"qiT{b}")
            if LIM < 6:
                return
            kd4 = kdup[b][:].rearrange("p (a r d) -> p a r d", a=2, r=2)
            rope_ops(P, c, v3(B3[:, 384:512], 2), rB3, 2, 64, r64[b], rr64[b], v3(ta[b][:, 0:128], 2), rta[b],
                     v3(tb[b][:, 0:128], 2), rtb[b], kd4[:, :, 0, :], rkdup[b], out3b=kd4[:, :, 1, :])
            transposes_to(P, c, tpp, [kdup[b][:, 0:128], kdup[b][:, 128:256]], [rkdup[b]], kT[b][:], rkT[b],
                          copy_eng="dve")
            P.dma("sp", S.KA_T2[:, cols], kT[b][:, 0:128], reads=[rkT[b]], writes=[S.rKA[i]], key=f"kTa{b}")
            P.dma("sp", S.KI_T2[:, cols], kT[b][:, 128:256], reads=[rkT[b]], writes=[S.rKI[i]], key=f"kTi{b}")
            if LIM < 7:
                return
            rope_ops(P, c, v3(B4[:, 256:288], 1), rB4, 1, 32, r32[b], rr32[b], v3(ta[b][:, 0:32], 1), rta[b],
                     v3(tb[b][:, 0:32], 1), rtb[b], v3(kpe[b][:], 1), rkpe[b])
            if LIM < 8:
                return
            P.add("act", lambda e, b=b, B4=B4: e.activation(out=va1[b][:, 64:128], in_=B4[:, 288:352], func=AF.Copy),
                  reads=[rB4], writes=[rva1[b]])
            P.dma("sp", S.VA1[rows, :], va1[b][:], reads=[rva1[b]], writes=[S.rVA[i]], key=f"va1{b}")
            if LIM < 9:
                return
            rmsnorm_to_bf16(P, c, B3[:, 0:384], rB3, 384, gq[:], rgq, junk[b][:, 0:384], rjunk[b], ss[b][:, 2:3],
                            rss[b], ss[b][:, 3:4], rrs[b][1], cqn[b][:], rcqn[b])
            if LIM < 9.1:
                return
            transposes_to(P, c, tpp, [cqn[b][:, k * 128:(k + 1) * 128] for k in range(3)], [rcqn[b]],
                          cqT[b][:], rcqT[b], copy_eng="dve")
            if LIM < 9.2:
                return
            for (q0, qw, h0, nh) in ((0, 480, 0, 5), (480, 288, 5, 3)):
                bk, rbk = mmp.next()
                for k in range(3):
                    P.add("pe", lambda e, k=k, bk=bk, q0=q0, qw=qw, b=b: e.matmul(
                        bk[:, 0:qw], lhsT=cqT[b][:, k * 128:(k + 1) * 128], rhs=Wuq[:, k, q0:q0 + qw],
                        start=(k == 0), stop=(k == 2)),
                          reads=[rcqT[b], rWuq[k]], writes=[rbk])
                if LIM < 9.3:
                    return
                bv = bk[:, 0:qw].rearrange("p (h d) -> p h d", h=nh)
                P.add("dve", lambda e, bv=bv, b=b, h0=h0, nh=nh: e.tensor_copy(
                    out=qbr[b][:, h0:h0 + nh, 0:64], in_=bv[:, :, 0:64]),
                      reads=[rbk], writes=[rqbr[b]])
                if LIM < 9.4:
                    return
                rope_ops(P, c, bv[:, :, 64:96], rbk, nh, 32, r32[b], rr32[b],
                         ta[b][:, 0:nh * 32].rearrange("p (h d) -> p h d", h=nh), rta[b],
                         tb[b][:, 0:nh * 32].rearrange("p (h d) -> p h d", h=nh), rtb[b],
                         qbr[b][:, h0:h0 + nh, 64:96], rqbr[b])
            if LIM < 9.5:
                return
            transposes_to(P, c, tpp, [qbr[b][:, h, :] for h in range(8)], [rqbr[b]], qbT[b][:], rqbT[b])
            if LIM < 9.6:
                return
            P.dma("sp", S.QB_T[:, :, cols].rearrange("h p t -> p h t"),
                  qbT[b][:].rearrange("p (h t) -> p h t", h=8), reads=[rqbT[b]], writes=[S.rQB[i]], key=f"qbT{b}")
            if LIM < 10:
                return
            rmsnorm_to_bf16(P, c, B4[:, 0:256], rB4, 256, gkv[:], rgkv, junk[b][:, 0:256], rjunk[b], ss[b][:, 4:5],
                            rss[b], ss[b][:, 5:6], rrs[b][2], ckn[b][:], rckn[b])
            transposes_to(P, c, tpp, [ckn[b][:, k * 128:(k + 1) * 128] for k in range(2)], [rckn[b]],
                          ckT[b][:], rckT[b], copy_eng="dve")
            P.add("pool", lambda e, b=b: e.tensor_copy(out=kbr[b][:, :, 64:96], in_=bc_mid(kpe[b][:], 8)),
                  reads=[rkpe[b]], writes=[rkbr[b]])
            for hf in range(2):
                bk, rbk = mmp.next()
                for k in range(2):
                    P.add("pe", lambda e, k=k, bk=bk, hf=hf, b=b: e.matmul(
                        bk[:, :], lhsT=ckT[b][:, k * 128:(k + 1) * 128], rhs=Wukv[:, k, hf * 512:(hf + 1) * 512],
                        start=(k == 0), stop=(k == 1)),
                          reads=[rckT[b], rWukv[k]], writes=[rbk])
                bv = bk[:, :].rearrange("p (h d) -> p h d", h=4)
                P.add("dve", lambda e, bv=bv, b=b, hf=hf: e.tensor_copy(
                    out=kbr[b][:, hf * 4:(hf + 1) * 4, 0:64], in_=bv[:, :, 0:64]),
                      reads=[rbk], writes=[rkbr[b]])
                P.add("dve", lambda e, bv=bv, b=b, hf=hf: e.tensor_copy(
                    out=vb1[b][:, hf * 4:(hf + 1) * 4, 64:128], in_=bv[:, :, 64:128]),
                      reads=[rbk], writes=[rvb1[b]])
            transposes_to(P, c, tpp, [kbr[b][:, h, :] for h in range(8)], [rkbr[b]], kbT[b][:], rkbT[b])
            P.dma("sp", S.KB_T[:, :, cols].rearrange("h p t -> p h t"),
                  kbT[b][:].rearrange("p (h t) -> p h t", h=8), reads=[rkbT[b]], writes=[S.rKB[i]], key=f"kbT{b}")
            P.dma("sp", S.VB1[rows, :, :], vb1[b][:], reads=[rvb1[b]], writes=[S.rVB[i]], key=f"vb1{b}")

        stage1a(0)
        for i in range(NT):
            if i + 1 < NT:
                stage1a(i + 1)
            stage2(i)
        return P.end_phase(ps)


NEG = -1.0e30
NBIS = 16


def normalize_out(P, c, O, rO, num_lo, rc, rrc, out_ap, out_res):
    den_lo = 64 - num_lo
    P.add("dve", lambda e: e.reciprocal(out=rc[den_lo:den_lo + 64, :], in_=O[den_lo:den_lo + 64, :]),
          reads=[rO], writes=[rrc])
    P.add("dve", lambda e: e.tensor_tensor(out=out_ap, in0=O[num_lo:num_lo + 64, :],
                                           in1=rc[den_lo:den_lo + 64, :], op=ALU.mult),
          reads=[rO, rrc], writes=[out_res])


def dsa_phase(nc, P, c, T, S):
    NT = T // 128
    TOPK = min(256, T // 4)
    QT0 = TOPK // 128
    with ExitStack() as ps:
        def sb(name, shape, dt):
            return ps.enter_context(nc.sbuf_tensor(name, shape, dt))

        def pm(name, shape, dt):
            return ps.enter_context(nc.psum_tensor(name, shape, dt))

        R = P.res

        def dbl(name, shape, dt):
            return [sb(f"{name}{i}", shape, dt) for i in range(2)], [R() for _ in range(2)]

        KA2 = sb("KA2", [128, T], BF16)
        KI2 = sb("KI2", [128, T], BF16)
        VAs = sb("VAs", [128, NT, 192], BF16)
        rKA2 = [R() for _ in range(NT)]
        rKI2 = [R() for _ in range(NT)]
        rVAs = [R() for _ in range(NT)]
        cneg = sb("cneg", [128, 128], F32)
        pow2 = sb("pow2", [128, NBIS], F32)
        rcn = R()
        qiT, rqiT = dbl("dqiT", [128, 1024], BF16)
        qaT, rqaT = dbl("dqaT", [128, 512], BF16)
        sg, rsg = dbl("dsg", [128, 16], F32)
        Rt = [sb(f"Rt{i}", [128, 512], BF16) for i in range(4)]
        Dg, rDg = dbl("Dg", [128, 16, 128], BF16)
        rRt = [R() for _ in range(4)]
        Isb, rIsb = dbl("Isb", [128, T], F32)
        cjunk = sb("cjunk", [128, T], BF16)
        rcj = R()
        st_, rst = dbl("dst", [128, 8 + NBIS], F32)
        maskq, rmq = dbl("maskq", [128, T], BF16)
        maskT, rmT = dbl("maskT", [128, NT, 128], BF16)
        PT = [sb(f"PT{i}", [128, 1024], BF16) for i in range(4)]
        rPT = [R() for _ in range(4)]
        rc, rrc = dbl("drc", [128, 512], F32)
        aT, raT = dbl("daT", [128, 512], BF16)
        Lt = [pm(f"dL{i}", [128, 512], F32) for i in range(4)]
        Lp = BankPool(Lt, [R() for _ in range(4)])
        At = [pm(f"dA{i}", [128, 512], F32) for i in range(2)]
        rAt = [R() for _ in range(2)]
        OE = pm("dOE", [128, 512], F32)
        OO = pm("dOO", [128, 512], F32)
        rOE, rOO = R(), R()

        P.add("pool", lambda e: e.memset(cneg[:], 0.0), writes=[rcn])
        P.add("pool", lambda e: e.affine_select(out=cneg[:], in_=cneg[:], pattern=[[-1, 128]],
                                                compare_op=ALU.is_ge, fill=NEG, base=0, channel_multiplier=1),
              reads=[rcn], writes=[rcn])
        for k in range(NBIS):
            P.add("pool", lambda e, k=k: e.memset(pow2[:, k:k + 1], 2.0 ** (-(k + 1))), writes=[rcn])
        CH = min(T, 1024)
        for ci in range(T // CH):
            cols = slice(ci * CH, (ci + 1) * CH)
            tl = range(ci * (CH // 128), (ci + 1) * (CH // 128))
            rk, rki, rv = R(), R(), R()
            P.dma("sp", KA2[:, cols], S.KA_T2[:, cols], reads=[S.rKA[i] for i in tl], writes=[rk], key=f"KA2_{ci}")
            P.dma("sp", KI2[:, cols], S.KI_T2[:, cols], reads=[S.rKI[i] for i in tl], writes=[rki], key=f"KI2_{ci}")
            P.dma("sp", VAs[:, ci * (CH // 128):(ci + 1) * (CH // 128), :],
                  S.VA1[cols, :].rearrange("(n p) c -> p n c", p=128), reads=[S.rVA[i] for i in tl], writes=[rv],
                  key=f"VAs_{ci}")
            for i in tl:
                rKA2[i], rKI2[i], rVAs[i] = rk, rki, rv

        cnt = {"ri": 0, "pti": 0, "ai": 0}

        def stage_A(qt):
            b = qt % 2
            SL = (qt + 1) * 128
            P.dma("sp", qiT[b][:], S.QI_T[qt], reads=[S.rQI[qt]], writes=[rqiT[b]], key=f"dqiT{b}")
            P.dma("sp", sg[b][:], S.SG[qt * 128:(qt + 1) * 128, :], reads=[S.rSG[qt]], writes=[rsg[b]], key=f"dsg{b}")
            P.add("dve", lambda e: e.tensor_tensor(out=Dg[b][:], in0=bc_mid(c.ident[:], 16), in1=bc_last(sg[b][:], 128),
                                                   op=ALU.mult),
                  reads=[c.rident, rsg[b]], writes=[rDg[b]])
            steps = []
            for sbk, s0 in enumerate(range(0, SL, 512)):
                for h in range(16):
                    steps.append((sbk, s0, h))

            def lmm(sbk, s0, h):
                sw = min(512, SL - s0)
                kres = [rKI2[j] for j in range(s0 // 128, (s0 + sw) // 128)]
                hp, par = h // 2, h % 2
                pl = par * 64
                L, rL = Lp.next()
                P.add("pe", lambda e: e.matmul(L[:, 0:sw], lhsT=qiT[b][pl:pl + 64, hp * 128:(hp + 1) * 128],
                                               rhs=KI2[pl:pl + 64, s0:s0 + sw], start=True, stop=True),
                      reads=[rqiT[b]] + kres, writes=[rL])
                r = cnt['ri'] % 4
                cnt['ri'] += 1
                P.add("act", lambda e: e.activation(out=Rt[r][:, 0:sw], in_=L[:, 0:sw], func=AF.Relu),
                      reads=[rL], writes=[rRt[r]])
                return r

            def acc(sbk, s0, h, r):
                sw = min(512, SL - s0)
                A, rA = At[(cnt['ai'] + sbk) % 2], rAt[(cnt['ai'] + sbk) % 2]
                P.add("pe", lambda e: e.matmul(A[:, 0:sw], lhsT=Dg[b][:, h, :], rhs=Rt[r][:, 0:sw], start=(h == 0),
                                               stop=(h == 15)),
                      reads=[rDg[b], rRt[r]], writes=[rA])
                if h == 15:
                    P.add("act", lambda e: e.activation(out=Isb[b][:, s0:s0 + sw], in_=A[:, 0:sw], func=AF.Copy),
                          reads=[rA], writes=[rIsb[b]])

            npair = len(steps) // 2
            pend = {0: (lmm(*steps[0]), lmm(*steps[1]))}
            for pi in range(npair):
                if pi + 1 < npair:
                    pend[pi + 1] = (lmm(*steps[2 * pi + 2]), lmm(*steps[2 * pi + 3]))
                r0, r1 = pend.pop(pi)
                acc(*steps[2 * pi], r0)
                acc(*steps[2 * pi + 1], r1)
            cnt['ai'] += len(range(0, SL, 512))

        def stage_B(qt):
            b = qt % 2
            SL = (qt + 1) * 128
            P.dma("sp", qaT[b][:], S.QA_T[qt], reads=[S.rQA[qt]], writes=[rqaT[b]], key=f"dqaT{b}")
            S_ = st_[b]
            if qt >= QT0:
                P.add("dve", lambda e, b=b, SL=SL, S_=S_: e.tensor_reduce(out=S_[:, 0:1], in_=Isb[b][:, 0:SL], axis=AX.X,
                                                                        op=ALU.min),
                      reads=[rIsb[b]], writes=[rst[b]])
                P.add("dve", lambda e, b=b, SL=SL, S_=S_: e.tensor_reduce(out=S_[:, 1:2], in_=Isb[b][:, 0:SL], axis=AX.X,
                                                                        op=ALU.max),
                      reads=[rIsb[b]], writes=[rst[b]])
            P.add("pool", lambda e, b=b, qt=qt: e.tensor_tensor(out=Isb[b][:, qt * 128:(qt + 1) * 128],
                                                              in0=Isb[b][:, qt * 128:(qt + 1) * 128], in1=cneg[:],
                                                              op=ALU.add),
                  reads=[rIsb[b], rcn], writes=[rIsb[b]])
            if qt >= QT0:
                P.add("dve", lambda e, S_=S_: e.tensor_tensor(out=S_[:, 2:3], in0=S_[:, 1:2], in1=S_[:, 0:1],
                                                             op=ALU.subtract),
                      reads=[rst[b]], writes=[rst[b]])
                P.add("dve", lambda e, S_=S_: e.tensor_scalar(out=S_[:, 8:8 + NBIS], in0=pow2[:], scalar1=S_[:, 2:3],
                                                             scalar2=None, op0=ALU.mult),
                      reads=[rst[b], rcn], writes=[rst[b]])
                P.add("dve", lambda e, S_=S_: e.tensor_copy(out=S_[:, 3:4], in_=S_[:, 0:1]),
                      reads=[rst[b]], writes=[rst[b]])
                for k in range(NBIS):
                    P.add("dve", lambda e, S_=S_, k=k: e.tensor_tensor(out=S_[:, 4:5], in0=S_[:, 3:4],
                                                                      in1=S_[:, 8 + k:9 + k], op=ALU.add),
                          reads=[rst[b]], writes=[rst[b]])
                    P.add("dve", lambda e, S_=S_, b=b, SL=SL: e.tensor_scalar(
                        out=cjunk[:, 0:SL], in0=Isb[b][:, 0:SL], scalar1=S_[:, 4:5], scalar2=None,
                        op0=ALU.is_ge, op1=ALU.add, accum_out=S_[:, 5:6]),
                          reads=[rst[b], rIsb[b]], writes=[rst[b], rcj])
                    P.add("dve", lambda e, S_=S_, k=k: e.tensor_scalar(
                        out=S_[:, 6:7], in0=S_[:, 5:6], scalar1=float(TOPK), scalar2=S_[:, 8 + k:9 + k],
                        op0=ALU.is_ge, op1=ALU.mult),
                          reads=[rst[b]], writes=[rst[b]])
                    P.add("dve", lambda e, S_=S_: e.tensor_tensor(out=S_[:, 3:4], in0=S_[:, 3:4], in1=S_[:, 6:7],
                                                                 op=ALU.add),
                          reads=[rst[b]], writes=[rst[b]])
            else:
                P.add("dve", lambda e, S_=S_: e.memset(S_[:, 3:4], -1.0e29), writes=[rst[b]])
            P.add("dve", lambda e, S_=S_, b=b, SL=SL: e.tensor_scalar(
                out=maskq[b][:, 0:SL], in0=Isb[b][:, 0:SL], scalar1=S_[:, 3:4], scalar2=None, op0=ALU.is_ge),
                  reads=[rst[b], rIsb[b]], writes=[rmq[b]])

        def stage_C(qt):
            b = qt % 2
            SL = (qt + 1) * 128
            for s8 in range(0, qt + 1, 8):
                n8 = min(8, qt + 1 - s8)
                L, rL = Lp.next()
                Lb = L[:, :].bitcast(BF16)
                for j in range(n8):
                    P.add("pe", lambda e, Lb=Lb, j=j, s8=s8, b=b: e.transpose(
                        out=Lb[:, j * 128:(j + 1) * 128], in_=maskq[b][:, (s8 + j) * 128:(s8 + j + 1) * 128],
                        identity=c.ident[:]),
                          reads=[rmq[b], c.rident], writes=[rL])
                P.add("act", lambda e, Lb=Lb, s8=s8, n8=n8, b=b: e.activation(
                    out=maskT[b][:, s8:s8 + n8, :].rearrange("p n q -> p (n q)"), in_=Lb[:, 0:n8 * 128],
                    func=AF.Copy),
                      reads=[rL], writes=[rmT[b]])
            def qk(st):
                sc = slice(st * 128, (st + 1) * 128)
                LE, rLE = Lp.next()
                LO, rLO = Lp.next()
                P.add("pe", lambda e: e.matmul(LE[:, :], lhsT=KA2[0:64, sc], rhs=qaT[b][0:64, :], start=True, stop=True),
                      reads=[rKA2[st], rqaT[b]], writes=[rLE])
                P.add("pe", lambda e: e.matmul(LO[:, :], lhsT=KA2[64:128, sc], rhs=qaT[b][64:128, :], start=True,
                                               stop=True),
                      reads=[rKA2[st], rqaT[b]], writes=[rLO])
                p = cnt["pti"] % len(PT)
                cnt["pti"] += 1
                P.add("act", lambda e: e.activation(out=PT[p][:, 0:512], in_=LE[:, :], func=AF.Exp, scale=0.125),
                      reads=[rLE], writes=[rPT[p]])
                P.add("act", lambda e: e.activation(out=PT[p][:, 512:1024], in_=LO[:, :], func=AF.Exp, scale=0.125),
                      reads=[rLO], writes=[rPT[p]])
                P.add("pool", lambda e: e.tensor_tensor(
                    out=PT[p][:].rearrange("p (a q) -> p a q", a=8), in0=PT[p][:].rearrange("p (a q) -> p a q", a=8),
                    in1=bc_mid(maskT[b][:, st, :], 8), op=ALU.mult),
                      reads=[rPT[p], rmT[b]], writes=[rPT[p]])
                return p

            def pv(st, p):
                P.add("pe", lambda e: e.matmul(OE[:, :], lhsT=VAs[:, st, 64:192], rhs=PT[p][:, 0:512],
                                               start=(st == 0), stop=(st == qt)),
                      reads=[rVAs[st], rPT[p]], writes=[rOE])
                P.add("pe", lambda e: e.matmul(OO[:, :], lhsT=VAs[:, st, 0:128], rhs=PT[p][:, 512:1024],
                                               start=(st == 0), stop=(st == qt)),
                      reads=[rVAs[st], rPT[p]], writes=[rOO])

            pend = {0: qk(0)}
            for st in range(qt + 1):
                if st + 1 <= qt:
                    pend[st + 1] = qk(st + 1)
                pv(st, pend.pop(st))
            normalize_out(P, c, OE, rOE, 0, rc[b], rrc[b], aT[b][0:64, :], raT[b])
            normalize_out(P, c, OO, rOO, 64, rc[b], rrc[b], aT[b][64:128, :], raT[b])
            P.dma("sp", S.ATT_T[0:4, :, qt * 128:(qt + 1) * 128].rearrange("j p t -> p j t"),
                  aT[b][:].rearrange("p (j t) -> p j t", j=4), reads=[raT[b]], writes=[S.rATa[qt]], key=f"daT{b}")

        for step in range(NT + 2):
            if step < NT:
                stage_A(step)
            if 0 <= step - 1 < NT:
                stage_B(step - 1)
            if 0 <= step - 2 < NT:
                stage_C(step - 2)
        return P.end_phase(ps)


SCALE_B = 96.0 ** -0.5


def mla_phase(nc, P, c, T, S):
    NT = T // 128
    NQB = T // 512
    with ExitStack() as ps:
        def sb(name, shape, dt):
            return ps.enter_context(nc.sbuf_tensor(name, shape, dt))

        def pm(name, shape, dt):
            return ps.enter_context(nc.psum_tensor(name, shape, dt))

        R = P.res

        def dbl(name, shape, dt):
            return [sb(f"{name}{i}", shape, dt) for i in range(2)], [R() for _ in range(2)]

        KB = [sb(f"mKB{i}", [128, T], BF16) for i in range(2)]
        VB = [sb(f"mVB{i}", [128, NT, 192], BF16) for i in range(2)]
        rKB = [[R() for _ in range(NQB)] for _ in range(2)]
        rVB = [[R() for _ in range(NQB)] for _ in range(2)]
        Cm = sb("Cm", [128, 4, 512], BF16)
        rCm = R()
        QT, rQT = dbl("mQT", [128, 512], BF16)
        PT = [sb(f"mPT{i}", [128, 512], BF16) for i in range(4)]
        rPT = [R() for _ in range(4)]
        rc, rrc = dbl("mrc", [128, 512], F32)
        aT, raT = dbl("maT", [128, 512], BF16)
        Lt = [pm(f"mL{i}", [128, 512], F32) for i in range(4)]
        Lp = BankPool(Lt, [R() for _ in range(4)])
        Ot = [pm(f"mO{i}", [128, 512], F32) for i in range(2)]
        rOt = [R() for _ in range(2)]

        P.add("pool", lambda e: e.memset(Cm[:], 1.0), writes=[rCm])
        P.add("pool", lambda e: e.affine_select(out=Cm[:], in_=Cm[:], pattern=[[-128, 4], [1, 512]],
                                                compare_op=ALU.is_ge, fill=0.0, base=0, channel_multiplier=-1),
              reads=[rCm], writes=[rCm])
        CH = min(T, 1024)
        NCH = T // CH

        def load_head(h):
            hb_ = h % 2
            for ci in range(NCH):
                cs = slice(ci * CH, (ci + 1) * CH)
                tl = range(ci * (CH // 128), (ci + 1) * (CH // 128))
                P.dma("sp", KB[hb_][:, cs], S.KB_T[h, :, cs], reads=[S.rKB[i] for i in tl],
                      writes=[rKB[hb_][ci]], key=f"mKB{hb_}_{ci}")
                P.dma("sp", VB[hb_][:, ci * (CH // 128):(ci + 1) * (CH // 128), :],
                      S.VB1[cs, h, :].rearrange("(n p) c -> p n c", p=128),
                      reads=[S.rVB[i] for i in tl], writes=[rVB[hb_][ci]], key=f"mVB{hb_}_{ci}")

        groups = [(h, qb) for h in range(8) for qb in range(NQB)]

        def load_q(g):
            h, qb = groups[g]
            bq = g % 2
            cs = slice(qb * 512, (qb + 1) * 512)
            P.dma("sp", QT[bq][:], S.QB_T[h, :, cs], reads=[S.rQB[i] for i in range(qb * 4, qb * 4 + 4)],
                  writes=[rQT[bq]], key=f"mQT{bq}")

        steps = [(g, st) for g, (h, qb) in enumerate(groups) for st in range(4 * (qb + 1))]
        cnt = {"pti": 0}

        def qk(g, st):
            h, qb = groups[g]
            hb_, bq = h % 2, g % 2
            L, rL = Lp.next()
            P.add("pe", lambda e: e.matmul(L[:, :], lhsT=KB[hb_][:, st * 128:(st + 1) * 128], rhs=QT[bq][:, :],
                                           start=True, stop=True),
                  reads=[rKB[hb_][(st * 128) // CH], rQT[bq]], writes=[rL])
            p = cnt["pti"] % len(PT)
            cnt["pti"] += 1
            P.add("act", lambda e: e.activation(out=PT[p][:], in_=L[:, :], func=AF.Exp, scale=SCALE_B),
                  reads=[rL], writes=[rPT[p]])
            j = st - 4 * qb
            if j >= 0:
                P.add("pool", lambda e: e.tensor_tensor(out=PT[p][:], in0=PT[p][:], in1=Cm[:, j, :], op=ALU.mult),
                      reads=[rPT[p], rCm], writes=[rPT[p]])
            return p

        def pv(g, st, p):
            h, qb = groups[g]
            hb_, bq = h % 2, g % 2
            nst = 4 * (qb + 1)
            O, rO = Ot[g % 2], rOt[g % 2]
            vsl = slice(64, 192) if h % 2 == 0 else slice(0, 128)
            num_lo = 0 if h % 2 == 0 else 64
            P.add("pe", lambda e: e.matmul(O[:, :], lhsT=VB[hb_][:, st, vsl], rhs=PT[p][:], start=(st == 0),
                                           stop=(st == nst - 1)),
                  reads=[rVB[hb_][(st * 128) // CH], rPT[p]], writes=[rO])
            if st == nst - 1:
                cs = slice(qb * 512, (qb + 1) * 512)
                normalize_out(P, c, O, rO, num_lo, rc[bq], rrc[bq], aT[bq][num_lo:num_lo + 64, :], raT[bq])
                P.dma("sp", S.ATT_T[4 + h // 2, num_lo:num_lo + 64, cs], aT[bq][num_lo:num_lo + 64, :],
                      reads=[raT[bq]], writes=[S.rATb[h][qb]], key=f"maT{bq}")

        LOOK = 3
        load_head(0)
        load_q(0)
        issued = {}
        loaded_q = {0}
        loaded_h = {0}

        def ensure_loads(g):
            if g >= len(groups):
                return
            h = groups[g][0]
            if h not in loaded_h:
                loaded_h.add(h)
                load_head(h)
            if g not in loaded_q:
                loaded_q.add(g)
                load_q(g)

        for i in range(min(LOOK, len(steps))):
            ensure_loads(steps[i][0])
            issued[i] = qk(*steps[i])
        for i, (g, st) in enumerate(steps):
            if i + LOOK < len(steps):
                ensure_loads(steps[i + LOOK][0])
                issued[i + LOOK] = qk(*steps[i + LOOK])
            pv(g, st, issued.pop(i))
            hh = groups[g][0]
            if st == 0 and groups[g][1] == 0 and hh + 1 < 8 and (hh + 1) not in loaded_h:
                loaded_h.add(hh + 1)
                load_head(hh + 1)
        return P.end_phase(ps)


def wout_phase(nc, P, c, T, S):
    NT = T // 128
    with ExitStack() as ps:
        def sb(name, shape, dt):
            return ps.enter_context(nc.sbuf_tensor(name, shape, dt))

        def pm(name, shape, dt):
            return ps.enter_context(nc.psum_tensor(name, shape, dt))

        R = P.res

        def dbl(name, shape, dt):
            return [sb(f"{name}{i}", shape, dt) for i in range(2)], [R() for _ in range(2)]

        Wo = sb("Wo", [128, 8, D], BF16)
        rWo = [R() for _ in range(8)]
        xt = [sb(f"wxt{i}", [128, D], F32) for i in range(3)]
        rxt = [R() for _ in range(3)]
        at = [sb(f"wat{i}", [128, 8, 128], BF16) for i in range(3)]
        rat = [R() for _ in range(3)]
        mmt = [pm(f"wmm{i}", [128, 512], F32) for i in range(4)]
        mmp = BankPool(mmt, [R() for _ in range(4)])
        stg, rstg = make_stage(nc, P, c, ps, "wo")
        load_weight_cast(P, c, Wo, [[r] for r in rWo], S.w_out, 8, [(0, 0, 512, 0), (512, 512, 512, 0)], stg, rstg)

        def loads(i):
            b = i % 3
            rows = slice(i * 128, (i + 1) * 128)
            P.dma("sp", xt[b][:], S.X1[rows, :], reads=[S.rX1[i]], writes=[rxt[b]], key=f"wxt{b}")
            P.dma("sp", at[b][:], S.ATT_T[:, :, rows].rearrange("c p t -> p c t"),
                  reads=[S.rATa[i]] + [S.rATb[h][i // 4] for h in range(8)], writes=[rat[b]], key=f"wat{b}")

        def compute(i):
            b = i % 3
            rows = slice(i * 128, (i + 1) * 128)
            for half in range(2):
                bk, rbk = mmp.next()
                for cc in range(8):
                    P.add("pe", lambda e, bk=bk, cc=cc, half=half: e.matmul(
                        bk[:, :], lhsT=at[b][:, cc, :], rhs=Wo[:, cc, half * 512:(half + 1) * 512],
                        start=(cc == 0), stop=(cc == 7)),
                          reads=[rat[b], rWo[cc]], writes=[rbk])
                P.add("dve", lambda e, bk=bk, half=half: e.tensor_tensor(
                    out=xt[b][:, half * 512:(half + 1) * 512], in0=bk[:, :], in1=xt[b][:, half * 512:(half + 1) * 512],
                    op=ALU.add),
                      reads=[rbk, rxt[b]], writes=[rxt[b]])
            P.dma("sp", S.X2[rows, :], xt[b][:], reads=[rxt[b]], writes=[S.rX2[i]], key=f"wxo{b}")

        for i0 in range(min(2, NT)):
            loads(i0)
        for i in range(NT):
            if i + 2 < NT:
                loads(i + 2)
            compute(i)
        return P.end_phase(ps)


IN_NAMES = ["x", "g_ffn1", "w1_gate", "w1_up", "w1_down", "g_mix", "w_in", "g_q_lat", "g_kv_lat", "w_uq", "w_ukv",
            "w_out", "g_ffn2", "w2_gate", "w2_up", "w2_down", "g_final"]
IN_SHAPES = {"g_ffn1": [D], "w1_gate": [D, DFF], "w1_up": [D, DFF], "w1_down": [DFF, D], "g_mix": [D],
             "w_in": [D, DPROJ], "g_q_lat": [384], "g_kv_lat": [256], "w_uq": [384, 768], "w_ukv": [256, 1024],
             "w_out": [D, D], "g_ffn2": [D], "w2_gate": [D, DFF], "w2_up": [D, DFF], "w2_down": [DFF, D],
             "g_final": [D]}


def build(T, debug=False, phases=("ffn1", "proj", "dsa", "mla", "wout", "ffn2")):
    NT = T // 128
    NQB = T // 512
    nc = bass.Bass("TRN2", target_bir_lowering=False)
    S = Ctx()
    x = nc.dram_tensor("x", [T, D], F32, kind="ExternalInput").ap()
    for n, shp in IN_SHAPES.items():
        setattr(S, n, nc.dram_tensor(n, list(shp), F32, kind="ExternalInput").ap())
    S.rope64 = nc.dram_tensor("rope64", [T, 128], F32, kind="ExternalInput").ap()
    S.rope32 = nc.dram_tensor("rope32", [T, 64], F32, kind="ExternalInput").ap()
    out = nc.dram_tensor("out", [T, D], F32, kind="ExternalOutput").ap()
    kind = "ExternalOutput" if debug else "Internal"

    def scr(name, shape, dt):
        return nc.dram_tensor(name, list(shape), dt, kind=kind).ap()

    S.X1 = scr("X1", [T, D], F32)
    S.X2 = scr("X2", [T, D], F32)
    S.QA_T = scr("QA_T", [NT, 128, 512], BF16)
    S.QI_T = scr("QI_T", [NT, 128, 1024], BF16)
    S.SG = scr("SG", [T, 16], F32)
    S.KA_T2 = scr("KA_T2", [128, T], BF16)
    S.KI_T2 = scr("KI_T2", [128, T], BF16)
    S.VA1 = scr("VA1", [T, 192], BF16)
    S.QB_T = scr("QB_T", [8, 128, T], BF16)
    S.KB_T = scr("KB_T", [8, 128, T], BF16)
    S.VB1 = scr("VB1", [T, 8, 192], BF16)
    S.ATT_T = scr("ATT_T", [8, 128, T], BF16)
    with ExitStack() as stack:
        P = Prog(nc, stack)
        c = Ctx()
        const_phase(nc, P, c, stack)
        rl = lambda: [P.res() for _ in range(NT)]
        rx = rl()
        rout = rl()
        S.rX1, S.rX2, S.rQA, S.rQI, S.rSG, S.rKA, S.rKI, S.rVA, S.rQB, S.rKB, S.rVB, S.rATa = [rl() for _ in range(12)]
        S.rATb = [[P.res() for _ in range(NQB)] for _ in range(8)]
        info = {}
        if "ffn1" in phases:
            info["ffn1"] = ffn_phase(nc, P, c, T, x, rx, S.X1, S.rX1, S.g_ffn1, S.w1_gate, S.w1_up, S.w1_down)
        if "proj" in phases:
            info["proj"] = proj_phase(nc, P, c, T, S)
        if "dsa" in phases:
            info["dsa"] = dsa_phase(nc, P, c, T, S)
        if "mla" in phases:
            info["mla"] = mla_phase(nc, P, c, T, S)
        if "wout" in phases:
            info["wout"] = wout_phase(nc, P, c, T, S)
        if "ffn2" in phases:
            info["ffn2"] = ffn_phase(nc, P, c, T, S.X2, S.rX2, out, rout, S.g_ffn2, S.w2_gate, S.w2_up, S.w2_down,
                                     final_g=S.g_final, tag="f2")
        nc._mk_info = (info, dict(P.cnt), max(P.dcnt.values()) if P.dcnt else 0)
    return nc


def rope_table(T, dim):
    pos = np.arange(T, dtype=np.float32)
    inv_freq = (np.float32(10000.0) ** (-np.arange(0, dim, 2, dtype=np.float32) / np.float32(dim))).astype(np.float32)
    ang = pos[:, None] * inv_freq[None, :]
    cs, sn = np.cos(ang).astype(np.float32), np.sin(ang).astype(np.float32)
    return np.concatenate([cs, cs, -sn, sn], axis=1).astype(np.float32)


_NC_CACHE = {}


def kernel(**inputs):
    x = np.ascontiguousarray(np.asarray(inputs["x"], dtype=np.float32))
    B, T, _ = x.shape
    if T not in _NC_CACHE:
        _NC_CACHE[T] = build(T)
    nc = _NC_CACHE[T]
    shared = {}
    for n, shp in IN_SHAPES.items():
        shared[n] = np.ascontiguousarray(np.asarray(inputs[n], dtype=np.float32).reshape(shp))
    shared["rope64"] = rope_table(T, 64)
    shared["rope32"] = rope_table(T, 32)
    in_maps = []
    for bi in range(B):
        m = dict(shared)
        m["x"] = x[bi]
        in_maps.append(m)
    res = run_bass_kernel_spmd(nc, in_maps, core_ids=list(range(B)))
    return np.stack([np.asarray(r["out"]) for r in res.results], axis=0).astype(np.float32)
```
